# Optimizing a Trainium2 kernel written in Bass

```python
import math
import jax
import jax.numpy as jnp
from jax import lax
import numpy as np

D_MODEL = 1024
BATCH = 16
SEQ = 2048
DEPTH = 2

PLE_DIM = 256
D_FF = 2816
N_BRANCH = 4
BRANCH_WIDTH = 256
CONV_WIDTH = 4
NORM_EPS = 1e-6

GDN_HEADS = 4
GDN_DK = 64
GDN_DV = 64
GDN_CHUNK = 64

MLSTM_HEADS = 4
MLSTM_DK = 64
MLSTM_DV = 64
MLSTM_CHUNK = 64

S5_GROUPS = 16
S5_GROUP_CH = 16
S5_STATE = 64

MOBA_HEADS = 4
MOBA_HEAD_DIM = 64
MOBA_BLOCK = 256
MOBA_TOPK = 3
MOBA_Q_SUB = 32
ROPE_THETA = 10000.0

IN_SPLITS = (
    2 * GDN_HEADS * GDN_DK + GDN_HEADS * GDN_DV,
    GDN_HEADS * GDN_DV,
    GDN_HEADS,
    GDN_HEADS,
    2 * MLSTM_HEADS * MLSTM_DK,
    MLSTM_HEADS * MLSTM_DV,
    MLSTM_HEADS * MLSTM_DV,
    MLSTM_HEADS,
    MLSTM_HEADS,
    S5_GROUPS * S5_GROUP_CH,
    3 * MOBA_HEADS * MOBA_HEAD_DIM,
)
N_IN = sum(IN_SPLITS)

kernel_name = 'hybrid_gated_parallel_mixer_block'


def split_cols(t, sizes):
    cuts = [int(c) for c in np.cumsum(sizes)[:-1]]
    return jnp.split(t, cuts, axis=-1)


def rms_norm(x, w):
    xf = x.astype(jnp.float32)
    y = xf * lax.rsqrt(jnp.mean(xf * xf, axis=-1, keepdims=True) + NORM_EPS)
    return (y * w.astype(jnp.float32)).astype(x.dtype)


def l2_normalize(x):
    return x * lax.rsqrt(jnp.sum(x * x, axis=-1, keepdims=True) + NORM_EPS)


def causal_conv(x, w):
    k_w, c = w.shape
    return lax.conv_general_dilated(
        x, w[:, None, :].astype(x.dtype), window_strides=(1,), padding=[(k_w - 1, 0)],
        dimension_numbers=('NWC', 'WIO', 'NWC'), feature_group_count=c)


def rope(x, positions):
    dh = x.shape[-1]
    inv_freq = ROPE_THETA ** (-jnp.arange(0, dh, 2, dtype=jnp.float32) / dh)
    ang = positions.astype(jnp.float32)[..., None] * inv_freq
    cos = jnp.cos(ang)[:, :, None, :]
    sin = jnp.sin(ang)[:, :, None, :]
    xf = x.astype(jnp.float32)
    x1, x2 = jnp.split(xf, 2, axis=-1)
    return jnp.concatenate([x1 * cos - x2 * sin, x2 * cos + x1 * sin], axis=-1).astype(x.dtype)


def swiglu(u, w_gu, w_down):
    gate, up = jnp.split(u @ w_gu, 2, axis=-1)
    return (jax.nn.silu(gate) * up) @ w_down


def to_chunks(t, chunk):
    b, s = t.shape[:2]
    t = t.reshape((b, s // chunk, chunk) + t.shape[2:])
    return jnp.moveaxis(t, 3, 1)


def gated_deltanet(qkv, z, b_pre, a_pre, conv_w, a_log, dt_bias, norm_w):
    bsz, s, _ = qkv.shape
    h, dk, dv, L = GDN_HEADS, GDN_DK, GDN_DV, GDN_CHUNK
    f32 = jnp.float32
    qkv = jax.nn.silu(causal_conv(qkv, conv_w)).astype(f32)
    q, k, v = split_cols(qkv, (h * dk, h * dk, h * dv))
    q = l2_normalize(q.reshape(bsz, s, h, dk)) * (dk ** -0.5)
    k = l2_normalize(k.reshape(bsz, s, h, dk))
    v = v.reshape(bsz, s, h, dv)
    beta = jax.nn.sigmoid(b_pre.astype(f32))
    g = -jnp.exp(a_log.astype(f32)) * jax.nn.softplus(a_pre.astype(f32) + dt_bias.astype(f32))
    q, k, v, beta, g = (to_chunks(t, L) for t in (q, k, v, beta, g))
    gc = jnp.cumsum(g, axis=-1)
    causal = jnp.tril(jnp.ones((L, L), dtype=bool))
    strict = jnp.tril(jnp.ones((L, L), dtype=bool), -1)
    decay = jnp.exp(jnp.where(causal, gc[..., :, None] - gc[..., None, :], -jnp.inf))
    k_beta = k * beta[..., None]
    m_low = jnp.where(strict, jnp.einsum('bhnid,bhnjd->bhnij', k_beta, k) * decay, 0.0)
    t_mat = jnp.eye(L, dtype=f32) + m_low
    u_vals = lax.linalg.triangular_solve(t_mat, v * beta[..., None], left_side=True, lower=True)
    w_keys = lax.linalg.triangular_solve(t_mat, k_beta * jnp.exp(gc)[..., None], left_side=True, lower=True)
    qk = jnp.einsum('bhnid,bhnjd->bhnij', q, k) * decay
    q_dec = q * jnp.exp(gc)[..., None]
    k_dec = k * jnp.exp(gc[..., -1:] - gc)[..., None]
    chunk_decay = jnp.exp(gc[..., -1])

    def step(state, inp):
        qk_c, q_c, k_c, u_c, w_c, d_c = inp
        v_new = u_c - jnp.einsum('bhld,bhde->bhle', w_c, state)
        out = jnp.einsum('bhld,bhde->bhle', q_c, state) + jnp.einsum('bhlm,bhme->bhle', qk_c, v_new)
        state = state * d_c[..., None, None] + jnp.einsum('bhld,bhle->bhde', k_c, v_new)
        return state, out

    xs = tuple(jnp.moveaxis(t, 2, 0) for t in (qk, q_dec, k_dec, u_vals, w_keys, chunk_decay))
    _, o = lax.scan(step, jnp.zeros((bsz, h, dk, dv), f32), xs)
    o = o.transpose(1, 0, 3, 2, 4).reshape(bsz, s, h, dv)
    o = rms_norm(o, norm_w) * jax.nn.silu(z.astype(f32).reshape(bsz, s, h, dv))
    return o.reshape(bsz, s, h * dv).astype(z.dtype)


def mlstm(qk, v, o_pre, i_pre, f_pre, conv_w, i_bias, f_bias, norm_w):
    bsz, s, _ = v.shape
    h, dk, dv, L = MLSTM_HEADS, MLSTM_DK, MLSTM_DV, MLSTM_CHUNK
    f32 = jnp.float32
    qk = jax.nn.silu(causal_conv(qk, conv_w)).astype(f32)
    q, k = jnp.split(qk, 2, axis=-1)
    q = q.reshape(bsz, s, h, dk)
    k = k.reshape(bsz, s, h, dk) * (dk ** -0.5)
    vv = v.astype(f32).reshape(bsz, s, h, dv)
    log_i = i_pre.astype(f32) + i_bias.astype(f32)
    log_f = jax.nn.log_sigmoid(f_pre.astype(f32) + f_bias.astype(f32))
    q, k, vv, log_i, log_f = (to_chunks(t, L) for t in (q, k, vv, log_i, log_f))
    b = jnp.cumsum(log_f, axis=-1)
    causal = jnp.tril(jnp.ones((L, L), dtype=bool))
    d_intra = jnp.where(causal, b[..., :, None] - b[..., None, :] + log_i[..., None, :], -jnp.inf)
    a_key = b[..., -1:] - b + log_i
    m_key = jnp.max(a_key, axis=-1)
    g_chunk = b[..., -1]

    def step(carry, inp):
        c_mem, n_mem, m_mem = carry
        k_c, v_c, a_c, mk_c, g_c = inp
        m_new = jnp.maximum(g_c + m_mem, mk_c)
        carry_scale = jnp.exp(g_c + m_mem - m_new)
        k_w = k_c * jnp.exp(a_c - m_new[..., None])[..., None]
        c_new = c_mem * carry_scale[..., None, None] + jnp.einsum('bhld,bhle->bhde', k_w, v_c)
        n_new = n_mem * carry_scale[..., None] + jnp.sum(k_w, axis=-2)
        return (c_new, n_new, m_new), (c_mem, n_mem, m_mem)

    xs = tuple(jnp.moveaxis(t, 2, 0) for t in (k, vv, a_key, m_key, g_chunk))
    init = (jnp.zeros((bsz, h, dk, dv), f32), jnp.zeros((bsz, h, dk), f32), jnp.zeros((bsz, h), f32))
    _, (c_prev, n_prev, m_prev) = lax.scan(step, init, xs)
    c_prev = jnp.moveaxis(c_prev, 0, 2)
    n_prev = jnp.moveaxis(n_prev, 0, 2)
    m_prev = jnp.moveaxis(m_prev, 0, 2)
    m_inter = b + m_prev[..., None]
    m_t = jnp.maximum(m_inter, jnp.max(d_intra, axis=-1))
    w_inter = jnp.exp(m_inter - m_t)
    s_qk = jnp.einsum('bhnld,bhnmd->bhnlm', q, k) * jnp.exp(d_intra - m_t[..., None])
    num = (w_inter[..., None] * jnp.einsum('bhnld,bhnde->bhnle', q, c_prev)
           + jnp.einsum('bhnlm,bhnme->bhnle', s_qk, vv))
    qn = w_inter * jnp.einsum('bhnld,bhnd->bhnl', q, n_prev) + jnp.sum(s_qk, axis=-1)
    h_tilde = num / jnp.maximum(jnp.abs(qn), jnp.exp(-m_t))[..., None]
    h_tilde = h_tilde.transpose(0, 2, 3, 1, 4).reshape(bsz, s, h, dv)
    h_norm = rms_norm(h_tilde, norm_w.reshape(h, dv)).reshape(bsz, s, h * dv)
    return (jax.nn.sigmoid(o_pre.astype(f32)) * h_norm).astype(v.dtype)


def s5_ssm(u, lam_re, lam_im, b_re, b_im, c_re, c_im, d_skip, log_dt, w_glu, b_glu):
    bsz, s, _ = u.shape
    G, N, P = S5_GROUPS, S5_GROUP_CH, S5_STATE
    f32 = jnp.float32
    uf = u.astype(f32).reshape(bsz, s, G, N)
    dt = jnp.exp(log_dt.astype(f32))[:, None]
    lr, li = lam_re.astype(f32), lam_im.astype(f32)
    mag = jnp.exp(lr * dt)
    a_re = mag * jnp.cos(li * dt)
    a_im = mag * jnp.sin(li * dt)
    den = lr * lr + li * li
    z_re = ((a_re - 1.0) * lr + a_im * li) / den
    z_im = (a_im * lr - (a_re - 1.0) * li) / den
    br, bi = b_re.astype(f32), b_im.astype(f32)
    bb_re = z_re[..., None] * br - z_im[..., None] * bi
    bb_im = z_re[..., None] * bi + z_im[..., None] * br
    bu_re = jnp.einsum('bsgn,gpn->bsgp', uf, bb_re)
    bu_im = jnp.einsum('bsgn,gpn->bsgp', uf, bb_im)
    a_re_s = jnp.broadcast_to(a_re, (1, s, G, P))
    a_im_s = jnp.broadcast_to(a_im, (1, s, G, P))

    def combine(left, right):
        ar1, ai1, br1, bi1 = left
        ar2, ai2, br2, bi2 = right
        return (ar2 * ar1 - ai2 * ai1, ar2 * ai1 + ai2 * ar1,
                ar2 * br1 - ai2 * bi1 + br2, ar2 * bi1 + ai2 * br1 + bi2)

    _, _, x_re, x_im = lax.associative_scan(combine, (a_re_s, a_im_s, bu_re, bu_im), axis=1)
    y = (jnp.einsum('gnp,bsgp->bsgn', c_re.astype(f32), x_re)
         - jnp.einsum('gnp,bsgp->bsgn', c_im.astype(f32), x_im)
         + d_skip.astype(f32).reshape(G, N) * uf)
    y = jax.nn.gelu(y.reshape(bsz, s, G * N))
    y = y * jax.nn.sigmoid(y @ w_glu.astype(f32) + b_glu.astype(f32))
    return y.astype(u.dtype)


def moba_attention(qkv, positions):
    bsz, s, _ = qkv.shape
    h, dh, blk, qs = MOBA_HEADS, MOBA_HEAD_DIM, MOBA_BLOCK, MOBA_Q_SUB
    f32 = jnp.float32
    q, k, v = jnp.split(qkv, 3, axis=-1)
    q = rope(q.reshape(bsz, s, h, dh), positions).transpose(0, 2, 1, 3)
    k = rope(k.reshape(bsz, s, h, dh), positions).transpose(0, 2, 1, 3)
    v = v.reshape(bsz, s, h, dh).transpose(0, 2, 1, 3)
    nb = -(-s // blk)
    pad = nb * blk - s
    k_blocks = jnp.pad(k, ((0, 0), (0, 0), (0, pad), (0, 0))).reshape(bsz, h, nb, blk, dh)
    v_blocks = jnp.pad(v, ((0, 0), (0, 0), (0, pad), (0, 0))).reshape(bsz, h, nb, blk, dh)
    k_mean = jnp.mean(k_blocks.astype(f32), axis=3)
    q_block = jnp.arange(s) // blk
    gate = jnp.einsum('bhsd,bhnd->bhsn', q.astype(f32), k_mean)
    past = jnp.arange(nb)[None, :] < q_block[:, None]
    gate = jnp.where(past, gate, -jnp.inf)
    n_sel = min(MOBA_TOPK, nb)
    _, sel = lax.top_k(gate, n_sel)
    sel_ok = sel < q_block[:, None]
    scale = dh ** -0.5
    bi = jnp.arange(bsz)[:, None, None, None]
    hi = jnp.arange(h)[None, :, None, None]

    def attend(sub):
        start = sub * qs
        q_s = lax.dynamic_slice_in_dim(q, start, qs, axis=2)
        sel_s = lax.dynamic_slice_in_dim(sel, start, qs, axis=2)
        ok_s = lax.dynamic_slice_in_dim(sel_ok, start, qs, axis=2)
        k_sel = k_blocks[bi, hi, sel_s]
        v_sel = v_blocks[bi, hi, sel_s]
        own = start // blk
        k_own = lax.dynamic_index_in_dim(k_blocks, own, axis=2, keepdims=False)
        v_own = lax.dynamic_index_in_dim(v_blocks, own, axis=2, keepdims=False)
        s_sel = jnp.einsum('bhqd,bhqnkd->bhqnk', q_s, k_sel).astype(f32) * scale
        s_sel = jnp.where(ok_s[..., None], s_sel, -jnp.inf)
        s_own = jnp.einsum('bhqd,bhkd->bhqk', q_s, k_own).astype(f32) * scale
        q_pos = start + jnp.arange(qs)
        k_pos = own * blk + jnp.arange(blk)
        s_own = jnp.where(k_pos[None, :] <= q_pos[:, None], s_own, -jnp.inf)
        scores = jnp.concatenate([s_sel.reshape(bsz, h, qs, n_sel * blk), s_own], axis=-1)
        probs = jax.nn.softmax(scores, axis=-1).astype(v.dtype)
        p_sel = probs[..., :n_sel * blk].reshape(bsz, h, qs, n_sel, blk)
        p_own = probs[..., n_sel * blk:]
        return (jnp.einsum('bhqnk,bhqnkd->bhqd', p_sel, v_sel)
                + jnp.einsum('bhqk,bhkd->bhqd', p_own, v_own))

    out = lax.map(attend, jnp.arange(s // qs))
    return out.transpose(1, 0, 3, 2, 4).reshape(bsz, s, h * dh)


def token_mixing(u, positions, w_in, gdn_conv, gdn_a_log, gdn_dt_bias, gdn_norm,
                 mlstm_conv, mlstm_i_bias, mlstm_f_bias, mlstm_norm,
                 s5_lambda_re, s5_lambda_im, s5_b_re, s5_b_im, s5_c_re, s5_c_im, s5_d, s5_log_dt,
                 s5_w_glu, s5_b_glu, w_gate, w_branch, w_out):
    (gdn_qkv, gdn_z, gdn_b, gdn_a, ml_qk, ml_v, ml_o, ml_i, ml_f, s5_u, moba_qkv) = split_cols(u @ w_in, IN_SPLITS)
    y_gdn = gated_deltanet(gdn_qkv, gdn_z, gdn_b, gdn_a, gdn_conv, gdn_a_log, gdn_dt_bias, gdn_norm)
    y_mlstm = mlstm(ml_qk, ml_v, ml_o, ml_i, ml_f, mlstm_conv, mlstm_i_bias, mlstm_f_bias, mlstm_norm)
    y_s5 = s5_ssm(s5_u, s5_lambda_re, s5_lambda_im, s5_b_re, s5_b_im, s5_c_re, s5_c_im, s5_d, s5_log_dt,
                  s5_w_glu, s5_b_glu)
    y_moba = moba_attention(moba_qkv, positions)
    ys = (y_gdn, y_mlstm, y_s5, y_moba)
    merged = jax.nn.sigmoid(u @ w_gate[0]) * (ys[0] @ w_branch[0])
    for n in range(1, N_BRANCH):
        merged = merged + jax.nn.sigmoid(u @ w_gate[n]) * (ys[n] @ w_branch[n])
    return merged @ w_out


def setup_inputs(seed: int = 0) -> dict:
    key = jax.random.key(seed)
    ks = iter(jax.random.split(key, 64))
    f32 = jnp.float32
    L, D = DEPTH, D_MODEL

    def nrm(shape, scale):
        return jax.random.normal(next(ks), shape, f32) * scale

    def gain(shape):
        return 1.0 + nrm(shape, 0.02)

    def log_uniform(shape, lo, hi):
        return jax.random.uniform(next(ks), shape, f32, minval=math.log(lo), maxval=math.log(hi))

    x = nrm((BATCH, SEQ, D), 1.0)
    p = nrm((DEPTH, BATCH, SEQ, PLE_DIM), 1.0)
    offset = jax.random.randint(next(ks), (BATCH, 1), 0, 4096, dtype=jnp.int32)
    positions = offset + jnp.arange(SEQ, dtype=jnp.int32)[None, :]
    G, N, P = S5_GROUPS, S5_GROUP_CH, S5_STATE
    gdn_dt = jnp.exp(log_uniform((L, GDN_HEADS), 1e-3, 1e-1))
    return {
        'x': x,
        'p': p,
        'positions': positions,
        'ffn1_norm': gain((L, D)),
        'ffn1_w_gu': nrm((L, D, 2 * D_FF), D ** -0.5),
        'ffn1_w_down': nrm((L, D_FF, D), D_FF ** -0.5),
        'mix_norm': gain((L, D)),
        'w_in': nrm((L, D, N_IN), D ** -0.5),
        'gdn_conv': nrm((L, CONV_WIDTH, IN_SPLITS[0]), CONV_WIDTH ** -0.5),
        'gdn_a_log': jnp.log(jax.random.uniform(next(ks), (L, GDN_HEADS), f32, minval=1.0, maxval=16.0)),
        'gdn_dt_bias': gdn_dt + jnp.log(-jnp.expm1(-gdn_dt)),
        'gdn_norm': gain((L, GDN_DV)),
        'mlstm_conv': nrm((L, CONV_WIDTH, IN_SPLITS[4]), CONV_WIDTH ** -0.5),
        'mlstm_i_bias': nrm((L, MLSTM_HEADS), 0.1),
        'mlstm_f_bias': jnp.linspace(3.0, 6.0, MLSTM_HEADS, dtype=f32)[None, :] + nrm((L, MLSTM_HEADS), 0.1),
        'mlstm_norm': gain((L, MLSTM_HEADS * MLSTM_DV)),
        's5_lambda_re': -0.5 + nrm((L, G, P), 0.01),
        's5_lambda_im': jnp.pi * jnp.arange(P, dtype=f32) + nrm((L, G, P), 0.01),
        's5_b_re': nrm((L, G, P, N), (2 * N) ** -0.5),
        's5_b_im': nrm((L, G, P, N), (2 * N) ** -0.5),
        's5_c_re': nrm((L, G, N, P), (2 * P) ** -0.5),
        's5_c_im': nrm((L, G, N, P), (2 * P) ** -0.5),
        's5_d': nrm((L, G * N), 0.5),
        's5_log_dt': log_uniform((L, G), 1e-3, 1e-1),
        's5_w_glu': nrm((L, G * N, G * N), (G * N) ** -0.5),
        's5_b_glu': nrm((L, G * N), 0.01),
        'w_gate': nrm((L, N_BRANCH, D, D), D ** -0.5),
        'w_branch': nrm((L, N_BRANCH, BRANCH_WIDTH, D), BRANCH_WIDTH ** -0.5),
        'w_out': nrm((L, D, D), D ** -0.5),
        'ffn2_norm': gain((L, D)),
        'ffn2_w_gu': nrm((L, D, 2 * D_FF), D ** -0.5),
        'ffn2_w_down': nrm((L, D_FF, D), D_FF ** -0.5),
        'ple_norm': gain((L, D)),
        'ple_w_proj': nrm((L, PLE_DIM, D), PLE_DIM ** -0.5),
        'ple_w_gate': nrm((L, D, D), D ** -0.5),
        'final_norm': gain((D,)),
    }


def reference(x, p, positions, ffn1_norm, ffn1_w_gu, ffn1_w_down, mix_norm, w_in,
              gdn_conv, gdn_a_log, gdn_dt_bias, gdn_norm,
              mlstm_conv, mlstm_i_bias, mlstm_f_bias, mlstm_norm,
              s5_lambda_re, s5_lambda_im, s5_b_re, s5_b_im, s5_c_re, s5_c_im, s5_d, s5_log_dt,
              s5_w_glu, s5_b_glu, w_gate, w_branch, w_out,
              ffn2_norm, ffn2_w_gu, ffn2_w_down, ple_norm, ple_w_proj, ple_w_gate, final_norm):
    h = x
    for i in range(DEPTH):
        h = h + 0.5 * swiglu(rms_norm(h, ffn1_norm[i]), ffn1_w_gu[i], ffn1_w_down[i])
        h = h + token_mixing(
            rms_norm(h, mix_norm[i]), positions, w_in[i],
            gdn_conv[i], gdn_a_log[i], gdn_dt_bias[i], gdn_norm[i],
            mlstm_conv[i], mlstm_i_bias[i], mlstm_f_bias[i], mlstm_norm[i],
            s5_lambda_re[i], s5_lambda_im[i], s5_b_re[i], s5_b_im[i], s5_c_re[i], s5_c_im[i],
            s5_d[i], s5_log_dt[i], s5_w_glu[i], s5_b_glu[i],
            w_gate[i], w_branch[i], w_out[i])
        h = h + 0.5 * swiglu(rms_norm(h, ffn2_norm[i]), ffn2_w_gu[i], ffn2_w_down[i])
        h = h + (p[i] @ ple_w_proj[i]) * jax.nn.sigmoid(rms_norm(h, ple_norm[i]) @ ple_w_gate[i])
    return rms_norm(h, final_norm)
```

```python
import numpy as np
from contextlib import ExitStack
import concourse.bass as bass
import concourse.mybir as mybir
from concourse.bass_utils import run_bass_kernel_spmd

F32 = mybir.dt.float32
BF16 = mybir.dt.bfloat16
I32 = mybir.dt.int32
AF = mybir.ActivationFunctionType
ALU = mybir.AluOpType
AX = mybir.AxisListType

ENGS = ["pe", "act", "dve", "pool", "sp"]
NT = 512
NL = 2
SEQ = 2048
PI = float(np.pi)


class Buf:
    __slots__ = ("name", "w", "r", "sem", "semcnt", "t")

    def __init__(self, name, t=None):
        self.name = name
        self.w = None
        self.r = {}
        self.sem = None
        self.semcnt = 0
        self.t = t


class Tmp:
    __slots__ = ("t", "bufs")

    def __init__(self, t, bufs):
        self.t = t
        self.bufs = bufs


def flat(lst):
    out = []
    for b in lst:
        if isinstance(b, Tmp):
            out.extend(b.bufs)
        else:
            out.append(b)
    return out


class Sched:
    def __init__(self, nc, stack):
        self.nc = nc
        self.stack = stack
        self.q = {e: [] for e in ENGS}
        self.cnt = {e: 0 for e in ENGS}
        self.sems = {}
        for e in ENGS:
            self.sems[e] = stack.enter_context(nc.semaphore("s_" + e))
        self.seen = {e: {} for e in ENGS}
        self.dmabufs = []

    def sb(self, name, shape, dt=F32):
        return Buf(name, self.stack.enter_context(self.nc.sbuf_tensor("sb_" + name, list(shape), dt)))

    def ps(self, name, shape, dt=F32):
        return Buf(name, self.stack.enter_context(self.nc.psum_tensor("ps_" + name, list(shape), dt)))

    def _waits(self, e, reads, writes):
        need = {}

        def add(k, v, src):
            if src == "pe" and e == "pe":
                return
            if v > need.get(k, 0):
                need[k] = v
        for b in reads:
            if b.w is not None:
                add(*b.w)
        for b in writes:
            if b.w is not None:
                add(*b.w)
            for k, (v, src) in b.r.items():
                add(k, v, src)
        out = []
        seen = self.seen[e]
        for k, v in need.items():
            if seen.get(k, 0) < v:
                seen[k] = v
                out.append((self.sems[k], v))
        return out

    def _record(self, dep, reads, writes):
        k, v, src = dep
        for b in reads:
            old = b.r.get(k)
            if old is None or old[0] < v:
                b.r[k] = (v, src)
        for b in writes:
            b.w = dep
            b.r = {}

    def op(self, e, fn, reads=(), writes=()):
        reads, writes = flat(reads), flat(writes)
        waits = self._waits(e, reads, writes)
        self.cnt[e] += 1
        n = self.cnt[e]
        sem = self.sems[e]

        def emit(engine, fn=fn, waits=waits, sem=sem):
            for s, v in waits:
                engine.wait_ge(s, v)
            fn(engine).then_inc(sem, 1)
        self.q[e].append(emit)
        self._record((e, n, e), reads, writes)

    def dma(self, qe, out, in_, reads, writes, sembuf):
        reads, writes = flat(reads), flat(writes)
        waits = self._waits(qe, reads, writes)
        if isinstance(sembuf, Tmp):
            sembuf = sembuf.bufs[0]
        if sembuf.sem is None:
            key = "d%d" % len(self.dmabufs)
            sembuf.sem = key
            self.sems[key] = self.stack.enter_context(self.nc.semaphore(key))
            self.dmabufs.append(sembuf)
        sembuf.semcnt += 16
        v = sembuf.semcnt
        sem = self.sems[sembuf.sem]

        def emit(engine, waits=waits, sem=sem, out=out, in_=in_):
            for s, vv in waits:
                engine.wait_ge(s, vv)
            engine.dma_start(out=out, in_=in_).then_inc(sem, 16)
        self.q[qe].append(emit)
        self._record((sembuf.sem, v, "dma"), reads, writes)

    def finish(self):
        waits = []
        for e in ENGS:
            if e != "sp" and self.cnt[e] > 0:
                waits.append((self.sems[e], self.cnt[e]))
        for b in self.dmabufs:
            waits.append((self.sems[b.sem], b.semcnt))

        def emit(engine, waits=waits):
            for s, v in waits:
                engine.wait_ge(s, v)
        self.q["sp"].append(emit)

    def emit_all(self):
        with self.nc.Block() as block:
            @block.tensor
            def _(eng):
                for f in self.q["pe"]:
                    f(eng)

            @block.scalar
            def _(eng):
                for f in self.q["act"]:
                    f(eng)

            @block.vector
            def _(eng):
                for f in self.q["dve"]:
                    f(eng)

            @block.gpsimd
            def _(eng):
                for f in self.q["pool"]:
                    f(eng)

            @block.sync
            def _(eng):
                for f in self.q["sp"]:
                    f(eng)


BIGW = {
    "wgu": (NL * 2 * 44 * 128, 8 * 128),
    "wdn": (NL * 2 * 8 * 128, 22 * 128),
    "win": (NL * 28 * 128, 8 * 128),
    "wgate": (NL * 8 * 4 * 128, 8 * 128),
    "wbr": (NL * 8 * 4 * 128, 2 * 128),
    "wout": (NL * 8 * 128, 8 * 128),
    "wplg": (NL * 8 * 128, 8 * 128),
    "wplp": (NL * 8 * 128, 2 * 128),
    "wglu": (NL * 2 * 128, 2 * 128),
}


class Kern:
    def __init__(self, nc, st, dbg=None):
        self.nc = nc
        self.dbg = dbg or {}
        self.S = Sched(nc, st)
        self.wi8 = 0
        self.wi22 = 0

    def mm(self, out, lhsT, rhs, start, stop, R, W):
        self.S.op("pe", lambda e: e.matmul(out, lhsT, rhs, start=start, stop=stop), R, W)

    def act(self, out, in_, func, R, W, **kw):
        self.S.op("act", lambda e: e.activation(out=out, in_=in_, func=func, **kw), R, W)

    def tt(self, out, a, b, op, R, W, eng="dve"):
        self.S.op(eng, lambda e: e.tensor_tensor(out=out, in0=a, in1=b, op=op), R, W)

    def ts(self, out, a, s1, op0, R, W, s2=None, op1=None, eng="dve"):
        if op1 is None:
            self.S.op(eng, lambda e: e.tensor_scalar(out=out, in0=a, scalar1=s1, scalar2=None, op0=op0), R, W)
        else:
            self.S.op(eng, lambda e: e.tensor_scalar(out=out, in0=a, scalar1=s1, scalar2=s2, op0=op0, op1=op1), R, W)

    def stt(self, out, a, s, b, op0, op1, R, W):
        self.S.op("dve", lambda e: e.scalar_tensor_tensor(out=out, in0=a, scalar=s, in1=b, op0=op0, op1=op1), R, W)

    def cp(self, out, in_, R, W, eng="pool"):
        if eng == "act":
            self.S.op("act", lambda e: e.copy(out=out, in_=in_), R, W)
        else:
            self.S.op(eng, lambda e: e.tensor_copy(out, in_), R, W)

    def loadw(self, name, tile, K):
        src = self.wb[name]
        ap = src.t[tile * 128:(tile + 1) * 128, :].rearrange("p (k c) -> p k c", c=128)
        if K > 8:
            b = self.w22[self.wi22 % len(self.w22)]
            self.wi22 += 1
        else:
            b = self.w8[self.wi8 % len(self.w8)]
            self.wi8 += 1
        self.S.dma("sp", b.t[:, 0:K, :], ap, [src], [b], b)
        return b

    def tmp(self, slot0, nslots, shape, dt=F32):
        ap = self.scr_t[:, slot0 * 512:(slot0 + nslots) * 512]
        if dt == BF16:
            ap = ap.bitcast(BF16)
        n = 1
        for d in shape[1:]:
            n *= d
        ap = ap[:, 0:n]
        if len(shape) == 3:
            ap = ap.rearrange("p (a b) -> p a b", b=shape[2])
        elif len(shape) == 4:
            ap = ap.rearrange("p (a b c) -> p a b c", b=shape[2], c=shape[3])
        return Tmp(ap, self.slots[slot0:slot0 + nslots])

    def declare(self):
        nc, S = self.nc, self.S
        D = {}

        def din(name, shape, dt=F32):
            D[name] = Buf(name, nc.dram_tensor(name, list(shape), dt, kind="ExternalInput").ap())
            return D[name]
        self.D = D
        din("xT", [2, 128, 8, SEQ])
        din("pT", [NL, 2, 128, 2, SEQ])
        din("gains", [128, 9, 8])
        din("consts", [128, 512])
        din("s5st", [NL, 128, 8, 3])
        din("s5rep", [NL, 3, 128, 8, 128])
        din("s5bexp", [NL, 2, 128, 8, 128])
        din("s5cexp", [NL, 2, 128, 8, 128])
        din("s5db", [NL, 128, 4])
        din("posr", [2, 128, SEQ], I32)
        din("consts2", [128, 512])
        din("wsmf", [128, NL, 8, 16])
        din("cwf", [128, NL, 40])
        din("mlrowf", [128, NL, 264])
        din("gdrowf", [128, NL, 72])
        din("gmaskf", [128, 256])
        din("pc", [128, 8])
        din("cmf", [128, 2, 256])
        for n, (r, c) in BIGW.items():
            din(n, [r, c])
        self.out = Buf("out", nc.dram_tensor("out", [2, 128, 8, SEQ], F32, kind="ExternalOutput").ap())
        self.wb = {}
        for n, (r, c) in BIGW.items():
            self.wb[n] = Buf(n + "_b", nc.dram_tensor(n + "_b", [r, c], BF16, kind="Internal").ap())
        self.s5f_d = Buf("s5f_d", nc.dram_tensor("s5f_d", [NL, 128, 3104], F32, kind="Internal").ap())
        self.s5b_d = Buf("s5b_d", nc.dram_tensor("s5b_d", [NL, 128, 4352], BF16, kind="Internal").ap())
        if "yin" in self.dbg:
            din("yin", [8, 128, 4, 2, NT], BF16)
        if "dumpy" in self.dbg:
            self.dumpy = Buf("dumpy", nc.dram_tensor("dumpy", [8, 128, 4, 2, NT], BF16, kind="ExternalOutput").ap())
        if "dump" in self.dbg:
            self.dump = Buf("dump", nc.dram_tensor("dump", self.dbg["dump"], F32, kind="ExternalOutput").ap())

        self.h = S.sb("h", [128, 8, NT], F32)
        self.u = S.sb("u", [128, 8, NT], BF16)
        NS = 24
        self.scr_t = S.sb("scr", [128, NS * 512], F32).t
        self.slots = [Buf("slot%d" % i) for i in range(NS)]
        self.actb = self.tmp(0, 11, [128, 22, NT], BF16)
        self.rstd = S.sb("rstd", [128, NT], F32)
        self.sg = [S.sb("sg%d" % i, [128, NT], F32) for i in range(2)]
        self.sg2 = [S.sb("sg2%d" % i, [128, NT], F32) for i in range(2)]
        self.macc = self.tmp(12, 1, [128, NT])
        self.ho = [self.tmp(13 + i, 1, [128, NT]) for i in range(2)]
        self.w8 = [S.sb("w8_%d" % i, [128, 8, 128], BF16) for i in range(6)]
        self.w22 = [S.sb("w22_%d" % i, [128, 22, 128], BF16) for i in range(2)]
        self.y = S.sb("y", [128, 4, 2, NT], BF16)
        self.pf = self.tmp(15, 2, [128, 2, NT])
        self.pb = self.tmp(17, 1, [128, 2, NT], BF16)
        self.gains = S.sb("gains", [128, 9, 8], F32)
        self.cst = S.sb("cst", [128, 8], F32)
        self.consts = S.sb("consts", [128, 512], F32)
        self.pc = S.sb("pc", [128, 8], F32)
        self.consts2 = S.sb("consts2", [128, 512], F32)
        self.wsm = S.sb("wsm", [128, NL, 8, 16], BF16)
        self.cwb = S.sb("cwb", [128, NL, 40], F32)
        self.mlrow = S.sb("mlrow", [128, NL, 264], F32)
        self.gdrow = S.sb("gdrow", [128, NL, 72], F32)
        self.mltail = [S.sb("mltail%d" % l, [128, 4, 3], F32) for l in range(NL)]
        self.gdtail = [S.sb("gdtail%d" % l, [128, 6, 3], F32) for l in range(NL)]
        self.mlC = [S.sb("mlC%d" % l, [128, 4, 66], F32) for l in range(NL)]
        self.mlm = [S.sb("mlm%d" % l, [128, 4], F32) for l in range(NL)]
        self.gdS = [S.sb("gdS%d" % l, [128, 4, 64], F32) for l in range(NL)]
        self.gmask = S.sb("gmask", [128, 256], F32)
        self.negones = S.sb("negones", [128, 64], F32)
        self.cm = S.sb("cm", [128, 2, 256], BF16)
        self.ident_bf = S.sb("ident_bf", [128, 128], BF16)
        self.kcache = [S.sb("kcache%d" % l, [128, 2, SEQ], BF16) for l in range(NL)]
        self.vcache = [S.sb("vcache%d" % l, [128, 16, 256], BF16) for l in range(NL)]
        self.kmean = [S.sb("kmean%d" % l, [128, 2, 8], F32) for l in range(NL)]
        self.s5f = S.sb("s5f", [128, 3104], F32)
        self.s5b = S.sb("s5b", [128, 4352], BF16)
        f = self.s5f.t
        self.s5F = {"CS": f[:, 0:2048].rearrange("p (a b c) -> p a b c", b=2, c=128),
                    "R": f[:, 2048:3072].rearrange("p (a b) -> p a b", b=128),
                    "E128": f[:, 3072:3088].rearrange("p (a b) -> p a b", b=2),
                    "rcol": f[:, 3088:3096], "dbg": f[:, 3096:3100]}
        b = self.s5b.t
        self.s5B = {"BT": b[:, 0:2048].rearrange("p (a b c) -> p a b c", b=2, c=128),
                    "CT": b[:, 2048:4096].rearrange("p (a b c) -> p a b c", b=2, c=128),
                    "Dg": b[:, 4096:4352].rearrange("p (a b) -> p a b", b=128)}
        self.s5state = [S.sb("s5st%d" % l, [128, 2, 8], F32) for l in range(NL)]
        self.ones_bf = S.sb("ones_bf", [128, 128], BF16)
        self.P = [S.ps("P%d" % i, [128, NT], F32) for i in range(8)]
        self.cstg = [self.tmp(12 + 4 * i, 4, [128, 4096], BF16) for i in range(2)]

    def setup(self):
        S = self.S
        S.op("pool", lambda e: e.memset(self.ones_bf.t[:], 1.0), [], [self.ones_bf])
        S.op("pool", lambda e: e.memset(self.cst.t[:, 0:1], 1e-6), [], [self.cst])
        S.op("pool", lambda e: e.memset(self.cst.t[:, 1:2], -PI), [], [self.cst])
        S.dma("sp", self.pc.t[:], self.D["pc"].t, [], [self.pc], self.pc)
        S.op("pool", lambda e: e.memset(self.cst.t[:, 3:4], 1.0), [], [self.cst])
        S.dma("sp", self.consts2.t[:], self.D["consts2"].t, [], [self.consts2], self.consts2)
        S.dma("sp", self.cwb.t[:], self.D["cwf"].t, [], [self.cwb], self.cwb)
        S.dma("sp", self.mlrow.t[:], self.D["mlrowf"].t, [], [self.mlrow], self.mlrow)
        S.dma("sp", self.gdrow.t[:], self.D["gdrowf"].t, [], [self.gdrow], self.gdrow)
        S.dma("sp", self.gmask.t[:], self.D["gmaskf"].t, [], [self.gmask], self.gmask)
        S.op("pool", lambda e: e.memset(self.negones.t[:], -1.0), [], [self.negones])
        wsf = self.tmp(13, 1, [128, NL, 8, 16])
        S.dma("sp", wsf.t, self.D["wsmf"].t, [], [wsf], wsf)
        self.cp(self.wsm.t[:], wsf.t, [wsf], [self.wsm], eng="dve")
        cmf = self.tmp(12, 1, [128, 2, 256])
        S.dma("sp", cmf.t, self.D["cmf"].t, [], [cmf], cmf)
        self.cp(self.cm.t[:], cmf.t, [cmf], [self.cm], eng="dve")
        for l in range(NL):
            S.op("pool", lambda e, l=l: e.memset(self.kmean[l].t[:], 0.0), [], [self.kmean[l]])
        S.dma("sp", self.gains.t[:], self.D["gains"].t, [], [self.gains], self.gains)
        S.dma("sp", self.consts.t[:], self.D["consts"].t, [], [self.consts], self.consts)
        self.cp(self.ident_bf.t[:], self.consts.t[:, 128:256], [self.consts], [self.ident_bf], eng="dve")
        order = ["wgu", "wdn", "win", "wgate", "wbr", "wout", "wplg", "wplp", "wglu"]
        i = 0
        for n in order:
            r, c = BIGW[n]
            nn = min(max(1, 4096 // c), r // 128)
            src, dst = self.D[n], self.wb[n]
            for r0 in range(0, r, nn * 128):
                stg = self.cstg[i % 2]
                i += 1
                S.dma("pool", stg.t[:, 0:nn * c].rearrange("p (n c) -> p n c", c=c),
                      src.t[r0:r0 + nn * 128, :].rearrange("(n p) c -> p n c", p=128), [], [stg], stg)
                S.dma("sp", dst.t[r0:r0 + nn * 128, :].rearrange("(n p) c -> p n c", p=128),
                      stg.t[:, 0:nn * c].rearrange("p (n c) -> p n c", c=c), [stg], [dst], dst)
        for l in range(NL):
            self.s5_setup(l)

    def norm(self, gcol):
        h, u, S = self.h, self.u, self.S
        pss, rstd = self.P[6], self.rstd
        self.act(u.t[:], h.t[:], AF.Square, [h], [u])
        for k in range(8):
            self.mm(pss.t[:], self.ones_bf.t[:], u.t[:, k, :], k == 0, k == 7, [u, self.ones_bf], [pss])
        self.act(rstd.t[:], pss.t[:], AF.Sqrt, [pss, self.cst], [rstd], scale=1.0 / 1024, bias=self.cst.t[:, 0:1])
        S.op("dve", lambda e: e.reciprocal(rstd.t[:], rstd.t[:]), [rstd], [rstd])
        for k in range(8):
            self.stt(u.t[:, k, :], h.t[:, k, :], self.gains.t[:, gcol, k:k + 1], rstd.t[:], ALU.mult, ALU.mult,
                     [h, rstd, self.gains], [u])

    def ffn(self, l, which):
        h, u, actb = self.h, self.u, self.actb
        self.norm(l * 4 + (0 if which == 0 else 2))
        base = (l * 2 + which) * 44
        for fc in range(22):
            wg = self.loadw("wgu", base + 2 * fc, 8)
            wu = self.loadw("wgu", base + 2 * fc + 1, 8)
            pg, pu = self.P[fc % 2], self.P[2 + fc % 2]
            sg = self.sg[fc % 2]
            for k in range(8):
                self.mm(pg.t[:], wg.t[:, k, :], u.t[:, k, :], k == 0, k == 7, [wg, u], [pg])
            for k in range(8):
                self.mm(pu.t[:], wu.t[:, k, :], u.t[:, k, :], k == 0, k == 7, [wu, u], [pu])
            self.act(sg.t[:], pg.t[:], AF.Silu, [pg], [sg])
            self.tt(actb.t[:, fc, :], sg.t[:], pu.t[:], ALU.mult, [sg, pu], [actb])
        base = (l * 2 + which) * 8
        for oc in range(8):
            wd = self.loadw("wdn", base + oc, 22)
            po = self.P[4 + oc % 2]
            for fc in range(22):
                self.mm(po.t[:], wd.t[:, fc, :], actb.t[:, fc, :], fc == 0, fc == 21, [wd, actb], [po])
            self.stt(h.t[:, oc, :], po.t[:], 0.5, h.t[:, oc, :], ALU.mult, ALU.add, [po, h], [h])

    def ple(self, l, s, t0):
        h, u, S = self.h, self.u, self.S
        self.norm(l * 4 + 3)
        S.dma("sp", self.pf.t[:], self.D["pT"].t[l, s, :, :, t0:t0 + NT], [], [self.pf], self.pf)
        self.cp(self.pb.t[:], self.pf.t[:], [self.pf], [self.pb], eng="pool")
        for oc in range(8):
            wg = self.loadw("wplg", l * 8 + oc, 8)
            wp = self.loadw("wplp", l * 8 + oc, 2)
            pa, pg = self.P[oc % 2], self.P[2 + oc % 2]
            sg, sg2 = self.sg[oc % 2], self.sg2[oc % 2]
            for k in range(2):
                self.mm(pa.t[:], wp.t[:, k, :], self.pb.t[:, k, :], k == 0, k == 1, [wp, self.pb], [pa])
            for k in range(8):
                self.mm(pg.t[:], wg.t[:, k, :], u.t[:, k, :], k == 0, k == 7, [wg, u], [pg])
            self.act(sg.t[:], pg.t[:], AF.Sigmoid, [pg], [sg])
            self.tt(sg2.t[:], sg.t[:], pa.t[:], ALU.mult, [sg, pa], [sg2])
            self.tt(h.t[:, oc, :], h.t[:, oc, :], sg2.t[:], ALU.add, [h, sg2], [h], eng="pool")

    def merge(self, l):
        h, u, y, actb = self.h, self.u, self.y, self.actb
        macc = self.macc
        for oc in range(8):
            for b in range(4):
                wg = self.loadw("wgate", (l * 8 + oc) * 4 + b, 8)
                wbr = self.loadw("wbr", (l * 8 + oc) * 4 + b, 2)
                pg, pbr = self.P[b % 2], self.P[2 + b % 2]
                sg, sg2 = self.sg[b % 2], self.sg2[b % 2]
                for k in range(8):
                    self.mm(pg.t[:], wg.t[:, k, :], u.t[:, k, :], k == 0, k == 7, [wg, u], [pg])
                for k in range(2):
                    self.mm(pbr.t[:], wbr.t[:, k, :], y.t[:, b, k, :], k == 0, k == 1, [wbr, y], [pbr])
                self.act(sg.t[:], pg.t[:], AF.Sigmoid, [pg], [sg])
                if b == 0:
                    self.tt(macc.t[:], sg.t[:], pbr.t[:], ALU.mult, [sg, pbr], [macc])
                else:
                    self.tt(sg2.t[:], sg.t[:], pbr.t[:], ALU.mult, [sg, pbr], [sg2])
                    if b < 3:
                        self.tt(macc.t[:], macc.t[:], sg2.t[:], ALU.add, [macc, sg2], [macc], eng="pool")
                    else:
                        self.tt(actb.t[:, oc, :], macc.t[:], sg2.t[:], ALU.add, [macc, sg2], [actb], eng="pool")
        for oc in range(8):
            wo = self.loadw("wout", l * 8 + oc, 8)
            po = self.P[4 + oc % 2]
            for k in range(8):
                self.mm(po.t[:], wo.t[:, k, :], actb.t[:, k, :], k == 0, k == 7, [wo, actb], [po])
            self.tt(h.t[:, oc, :], h.t[:, oc, :], po.t[:], ALU.add, [h, po], [h])

    def final(self, s, t0):
        h, S = self.h, self.S
        pss, rstd = self.P[6], self.rstd
        u = self.u
        self.act(u.t[:], h.t[:], AF.Square, [h], [u])
        for k in range(8):
            self.mm(pss.t[:], self.ones_bf.t[:], u.t[:, k, :], k == 0, k == 7, [u, self.ones_bf], [pss])
        self.act(rstd.t[:], pss.t[:], AF.Sqrt, [pss, self.cst], [rstd], scale=1.0 / 1024, bias=self.cst.t[:, 0:1])
        S.op("dve", lambda e: e.reciprocal(rstd.t[:], rstd.t[:]), [rstd], [rstd])
        for k in range(8):
            ho = self.ho[k % 2]
            self.stt(ho.t[:], h.t[:, k, :], self.gains.t[:, 8, k:k + 1], rstd.t[:], ALU.mult, ALU.mult,
                     [h, rstd, self.gains], [ho])
            S.dma("sp", self.out.t[s, :, k, t0:t0 + NT], ho.t[:], [ho], [self.out], ho)

    def dump_h(self, idx):
        S = self.S
        S.dma("sp", self.dump.t[idx], self.h.t[:], [self.h], [self.dump], self.h)


    def dd(self, name, src, R, shape, dt=F32):
        if name not in self.dbg.get("dd", ()):
            return
        b = Buf(name, self.nc.dram_tensor("dd_" + name, list(shape), dt, kind="ExternalOutput").ap())
        self.S.dma("sp", b.t, src, R, [b], b)

    def frac2pi(self, out, x, shift, tB, R, W):
        MAG = 12582912.0
        self.ts(out, x, 1.0 / (2 * PI), ALU.mult, R, W, s2=shift / (2 * PI), op1=ALU.add)
        self.ts(tB, out, MAG, ALU.add, R, W)
        self.ts(tB, tB, -MAG, ALU.add, R, W)
        self.tt(out, out, tB, ALU.subtract, R, W)

    def s5_disc(self, lr, li, ldt, T, TB, want_z):
        R = TB + [self.s5in]
        W = TB
        self.act(T[0], ldt, AF.Exp, R, W)
        self.tt(T[5], lr, T[0], ALU.mult, R, W)
        self.act(T[1], T[5], AF.Exp, R, W)
        self.tt(T[2], li, T[0], ALU.mult, R, W)
        if not want_z:
            self.frac2pi(T[5], T[2], 0.0, T[8], R, W)
            self.ts(T[2], T[5], 2 * PI, ALU.mult, R, W)
            return T[1], T[2], None, None
        self.frac2pi(T[5], T[2], 0.0, T[8], R, W)
        self.act(T[3], T[5], AF.Sin, R, W, scale=2 * PI)
        self.frac2pi(T[5], T[2], 0.5 * PI, T[8], R, W)
        self.act(T[4], T[5], AF.Sin, R, W, scale=2 * PI)
        self.tt(T[4], T[4], T[1], ALU.mult, R, W)
        self.tt(T[3], T[3], T[1], ALU.mult, R, W)
        self.ts(T[5], T[4], -1.0, ALU.add, R, W)
        self.tt(T[8], lr, lr, ALU.mult, R, W)
        self.tt(T[0], li, li, ALU.mult, R, W)
        self.tt(T[8], T[8], T[0], ALU.add, R, W)
        self.S.op("dve", lambda e: e.reciprocal(T[8], T[8]), R, W)
        self.tt(T[0], T[5], lr, ALU.mult, R, W)
        self.tt(T[6], T[3], li, ALU.mult, R, W)
        self.tt(T[6], T[6], T[0], ALU.add, R, W)
        self.tt(T[6], T[6], T[8], ALU.mult, R, W)
        self.tt(T[0], T[3], lr, ALU.mult, R, W)
        self.tt(T[7], T[5], li, ALU.mult, R, W)
        self.tt(T[7], T[0], T[7], ALU.subtract, R, W)
        self.tt(T[7], T[7], T[8], ALU.mult, R, W)
        return T[1], T[2], T[6], T[7]

    def s5_setup(self, l):
        S, D = self.S, self.D
        s5f, s5b, cst = self.s5f, self.s5b, self.cst
        F = self.s5F
        stt_ = self.tmp(12, 1, [128, 8, 3])
        self.s5in = stt_.bufs[0]
        S.dma("sp", stt_.t, D["s5st"].t[l], [], [stt_], stt_)
        T9 = self.tmp(13, 1, [128, 9, 8])
        T = [T9.t[:, i, :] for i in range(9)]
        mag, th, _, _ = self.s5_disc(stt_.t[:, :, 0], stt_.t[:, :, 1], stt_.t[:, :, 2], T, [T9.bufs[0]], False)
        R9 = [T9]
        X = self.tmp(14, 2, [128, 8, 128])
        Y = self.tmp(16, 2, [128, 8, 128])
        Z = self.tmp(18, 2, [128, 8, 128])
        jrow = self.consts.t[:, 0:128]
        for sc in range(8):
            self.ts(X.t[:, sc, :], jrow, th[:, sc:sc + 1], ALU.mult, R9 + [self.consts], [X])
        self.frac2pi(Y.t, X.t, 0.0, Z.t, [X, Y, Z], [Y, Z])
        self.act(F["CS"][:, :, 1, :], Y.t, AF.Sin, [Y], [s5f], scale=2 * PI)
        self.frac2pi(Y.t, X.t, 0.5 * PI, Z.t, [X, Y, Z], [Y, Z])
        self.act(F["CS"][:, :, 0, :], Y.t, AF.Sin, [Y], [s5f], scale=2 * PI)
        self.ts(T[3], th, 128.0, ALU.mult, R9, R9)
        self.frac2pi(T[4], T[3], 0.0, T[5], R9, R9)
        self.act(F["E128"][:, :, 1], T[4], AF.Sin, R9, [s5f], scale=2 * PI)
        self.frac2pi(T[4], T[3], 0.5 * PI, T[5], R9, R9)
        self.act(F["E128"][:, :, 0], T[4], AF.Sin, R9, [s5f], scale=2 * PI)
        self.cp(F["rcol"], mag, R9, [s5f], eng="dve")
        S.op("dve", lambda e: e.memset(F["R"], 0.0), [], [s5f])
        ones = self.consts.t[:, 384:511]
        for sc in range(8):
            self.ts(F["R"][:, sc, 1:128], ones, mag[:, sc:sc + 1], ALU.mult, R9 + [self.consts], [s5f])
        S.dma("sp", F["dbg"], D["s5db"].t[l], [], [s5f], s5f)
        for hh in range(2):
            prm = self.tmp(12, 3, [128, 3, 512])
            self.s5in = prm.bufs[0]
            S.dma("sp", prm.t.rearrange("p a (s c) -> p a s c", c=128),
                  D["s5rep"].t[l, :, :, 4 * hh:4 * hh + 4, :].rearrange("a p s c -> p a s c"), [], [prm], prm)
            TT_ = self.tmp(15, 9, [128, 9, 512])
            T = [TT_.t[:, i, :] for i in range(9)]
            TB = TT_.bufs + prm.bufs[1:]
            _, _, zr, zi = self.s5_disc(prm.t[:, 0, :], prm.t[:, 1, :], prm.t[:, 2, :], T, TB, True)
            bx = self.tmp(12, 2, [128, 2, 512])
            S.dma("sp", bx.t.rearrange("p a (s c) -> p a s c", c=128),
                  D["s5bexp"].t[l, :, :, 4 * hh:4 * hh + 4, :].rearrange("a p s c -> p a s c"), [], [bx], bx)
            RR = TB + bx.bufs
            self.tt(T[0], zr, bx.t[:, 0, :], ALU.mult, RR, TB)
            self.tt(T[1], zi, bx.t[:, 1, :], ALU.mult, RR, TB)
            self.tt(F32v(self, "BTre", hh), T[0], T[1], ALU.subtract, RR, [s5b])
            self.tt(T[0], zr, bx.t[:, 1, :], ALU.mult, RR, TB)
            self.tt(T[1], zi, bx.t[:, 0, :], ALU.mult, RR, TB)
            self.tt(F32v(self, "BTim", hh), T[0], T[1], ALU.add, RR, [s5b])
            cx = self.tmp(12, 2, [128, 2, 512])
            S.dma("sp", cx.t.rearrange("p a (s c) -> p a s c", c=128),
                  D["s5cexp"].t[l, :, :, 4 * hh:4 * hh + 4, :].rearrange("a p s c -> p a s c"), [], [cx], cx)
            self.cp(F32v(self, "CTre", hh), cx.t[:, 0, :], [cx], [s5b], eng="dve")
            self.ts(F32v(self, "CTim", hh), cx.t[:, 1, :], -1.0, ALU.mult, [cx], [s5b])
        ident = self.consts.t[:, 128:256]
        for kc in range(2):
            self.ts(self.s5B["Dg"][:, kc, :], ident, F["dbg"][:, kc:kc + 1], ALU.mult, [s5f, self.consts], [s5b])
        S.dma("sp", self.s5f_d.t[l], s5f.t[:], [s5f], [self.s5f_d], s5f)
        S.dma("sp", self.s5b_d.t[l], s5b.t[:], [s5b], [self.s5b_d], s5b)

    def s5_fwd(self, l, s, tb):
        S, u, y = self.S, self.u, self.y
        s5f, s5b = self.s5f, self.s5b
        F, B = self.s5F, self.s5B
        st = self.s5state[l]
        S.dma("sp", s5f.t[:], self.s5f_d.t[l], [self.s5f_d], [s5f], s5f)
        S.dma("sp", s5b.t[:], self.s5b_d.t[l], [self.s5b_d], [s5b], s5b)
        us5 = self.tmp(12, 1, [128, 2, NT], BF16)
        for kc in range(2):
            w = self.loadw("win", l * 28 + 16 + kc, 8)
            pp = self.P[kc]
            for k in range(8):
                self.mm(pp.t[:], w.t[:, k, :], u.t[:, k, :], k == 0, k == 7, [w, u], [pp])
            self.cp(us5.t[:, kc, :], pp.t[:], [pp], [us5], eng="act")
        A = self.tmp(13, 1, [128, 4, 128])
        Bt = self.tmp(14, 1, [128, 4, 128])
        bh = [self.tmp(15, 2, [128, 8, 128]), self.tmp(17, 2, [128, 8, 128])]
        xh = [self.tmp(19, 2, [128, 8, 128]), self.tmp(21, 2, [128, 8, 128])]
        xb = [self.tmp(23, 1, [128, 8, 128], BF16), self.tmp(0, 1, [128, 8, 128], BF16)]
        ini = self.tmp(1, 1, [128, 4, 8])
        ypre = [self.P[4], self.P[5]]
        CS = F["CS"]
        if tb == 0:
            S.op("dve", lambda e: e.memset(st.t[:], 0.0), [], [st])
        for sub in range(4):
            c0 = sub * 128
            for sc in range(8):
                for ri in range(2):
                    pp = self.P[2 * ri + sc // 4]
                    self.mm(pp.t[:, (sc % 4) * 128:(sc % 4 + 1) * 128], B["BT"][:, sc, ri, :],
                            us5.t[:, sc // 4, c0:c0 + 128], True, True, [s5b, us5], [pp])
            for hh in range(2):
                c = CS[:, 4 * hh:4 * hh + 4, 0, :]
                sn = CS[:, 4 * hh:4 * hh + 4, 1, :]
                pre = self.P[hh].t[:].rearrange("p (a b) -> p a b", b=128)
                pim = self.P[2 + hh].t[:].rearrange("p (a b) -> p a b", b=128)
                self.tt(A.t, pre, c, ALU.mult, [self.P[hh], s5f], [A])
                self.tt(Bt.t, pim, sn, ALU.mult, [self.P[2 + hh], s5f], [Bt])
                self.tt(bh[0].t[:, 4 * hh:4 * hh + 4, :], A.t, Bt.t, ALU.add, [A, Bt], [bh[0]])
                self.tt(A.t, pim, c, ALU.mult, [self.P[2 + hh], s5f], [A])
                self.tt(Bt.t, pre, sn, ALU.mult, [self.P[hh], s5f], [Bt])
                self.tt(bh[1].t[:, 4 * hh:4 * hh + 4, :], A.t, Bt.t, ALU.subtract, [A, Bt], [bh[1]])
            if not (tb == 0 and sub == 0):
                i0, i1, i2, i3 = (ini.t[:, i, :] for i in range(4))
                c1, s1 = F["E128"][:, :, 0], F["E128"][:, :, 1]
                self.tt(i0, c1, st.t[:, 0, :], ALU.mult, [s5f, st], [ini])
                self.tt(i1, s1, st.t[:, 1, :], ALU.mult, [s5f, st], [ini])
                self.tt(i0, i0, i1, ALU.subtract, [ini], [ini])
                self.tt(i2, c1, st.t[:, 1, :], ALU.mult, [s5f, st], [ini])
                self.tt(i3, s1, st.t[:, 0, :], ALU.mult, [s5f, st], [ini])
                self.tt(i2, i2, i3, ALU.add, [ini], [ini])
                self.tt(i0, i0, F["rcol"], ALU.mult, [ini, s5f], [ini])
                self.tt(i2, i2, F["rcol"], ALU.mult, [ini, s5f], [ini])
                self.tt(bh[0].t[:, :, 0], bh[0].t[:, :, 0], i0, ALU.add, [bh[0], ini], [bh[0]])
                self.tt(bh[1].t[:, :, 0], bh[1].t[:, :, 0], i2, ALU.add, [bh[1], ini], [bh[1]])
            Rf = F["R"].rearrange("p a b -> p (a b)")
            for ri in range(2):
                S.op("dve", lambda e, ri=ri: e.tensor_tensor_scan(
                    out=xh[ri].t.rearrange("p a b -> p (a b)"), data0=Rf,
                    data1=bh[ri].t.rearrange("p a b -> p (a b)"), initial=0.0, op0=ALU.mult, op1=ALU.add),
                    [bh[ri], s5f], [xh[ri]])
                self.cp(st.t[:, ri, :], xh[ri].t[:, :, 127], [xh[ri]], [st], eng="dve")
            for hh in range(2):
                c = CS[:, 4 * hh:4 * hh + 4, 0, :]
                sn = CS[:, 4 * hh:4 * hh + 4, 1, :]
                hs = slice(4 * hh, 4 * hh + 4)
                self.tt(A.t, xh[0].t[:, hs, :], c, ALU.mult, [xh[0], s5f], [A])
                self.tt(Bt.t, xh[1].t[:, hs, :], sn, ALU.mult, [xh[1], s5f], [Bt], eng="pool")
                self.tt(xb[0].t[:, hs, :], A.t, Bt.t, ALU.subtract, [A, Bt], [xb[0]])
                self.tt(A.t, xh[1].t[:, hs, :], c, ALU.mult, [xh[1], s5f], [A])
                self.tt(Bt.t, xh[0].t[:, hs, :], sn, ALU.mult, [xh[0], s5f], [Bt], eng="pool")
                self.tt(xb[1].t[:, hs, :], A.t, Bt.t, ALU.add, [A, Bt], [xb[1]])
            for kc in range(2):
                pp = ypre[kc]
                o = pp.t[:, c0:c0 + 128]
                n = 0
                for sc in range(4 * kc, 4 * kc + 4):
                    for ri in range(2):
                        self.mm(o, B["CT"][:, sc, ri, :], xb[ri].t[:, sc, :], n == 0, False, [s5b, xb[ri]], [pp])
                        n += 1
                self.mm(o, B["Dg"][:, kc, :], us5.t[:, kc, c0:c0 + 128], False, True, [s5b, us5], [pp])
        yg = self.tmp(13, 2, [128, 2, NT])
        t1 = self.tmp(15, 2, [128, 2, NT])
        ygb = self.tmp(17, 1, [128, 2, NT], BF16)
        for kc in range(2):
            self.cp(yg.t[:, kc, :], ypre[kc].t[:], [ypre[kc]], [yg], eng="act")
        self.act(t1.t, yg.t, AF.Square, [yg], [t1])
        self.ts(t1.t, t1.t, 0.044715, ALU.mult, [t1], [t1], s2=1.0, op1=ALU.add)
        self.tt(t1.t, t1.t, yg.t, ALU.mult, [t1, yg], [t1])
        self.act(t1.t, t1.t, AF.Sigmoid, [t1], [t1], scale=1.5957691216)
        self.tt(yg.t, yg.t, t1.t, ALU.mult, [t1, yg], [yg])
        self.cp(ygb.t, yg.t, [yg], [ygb], eng="pool")
        for oc in range(2):
            w = self.loadw("wglu", l * 2 + oc, 2)
            pp = self.P[6 + oc]
            for k in range(2):
                self.mm(pp.t[:], w.t[:, k, :], ygb.t[:, k, :], k == 0, k == 1, [w, ygb], [pp])
            self.act(t1.t[:, oc, :], pp.t[:], AF.Sigmoid, [pp, s5f], [t1], bias=F["dbg"][:, 2 + oc:3 + oc])
            self.tt(y.t[:, 2, oc, :], yg.t[:, oc, :], t1.t[:, oc, :], ALU.mult, [yg, t1], [y])


    def moba_fwd(self, l, s, tb):
        S, u, y, D = self.S, self.u, self.y, self.D
        t0 = tb * NT
        kc_, vc_, km = self.kcache[l], self.vcache[l], self.kmean[l]
        pc, consts = self.pc, self.consts
        cosT = self.tmp(0, 1, [128, NT])
        sinT = self.tmp(1, 1, [128, NT])
        posi = self.tmp(2, 1, [128, NT])
        ang = self.tmp(3, 1, [128, NT])
        fr = self.tmp(4, 1, [128, NT])
        fb = self.tmp(5, 1, [128, NT])
        qf = [self.tmp(6, 1, [128, NT]), self.tmp(7, 1, [128, NT])]
        kf = [self.tmp(8, 1, [128, NT]), self.tmp(9, 1, [128, NT])]
        t1 = self.tmp(10, 1, [128, NT])
        t2 = self.tmp(12, 1, [128, NT])
        qb_ = self.tmp(13, 1, [128, 2, NT], BF16)
        sm = self.tmp(14, 1, [128, 512])
        gm = sm.t[:, 0:32].rearrange("p (a b) -> p a b", b=8)
        mx = sm.t[:, 32:64].rearrange("p (a b) -> p a b", b=8)
        mnegb = sm.t[:, 64:128].bitcast(BF16).rearrange("p (a b) -> p a b", b=32)
        et = [self.tmp(15, 1, [128, 1024], BF16)]
        ets = [et[0].t[:, 0:512].rearrange("p (a b) -> p a b", b=256), et[0].t[:, 512:1024].rearrange("p (a b) -> p a b", b=256)]
        rden = self.tmp(16, 1, [128, 2, 256])
        S.dma("sp", posi.t.bitcast(I32), D["posr"].t[s, :, t0:t0 + NT], [], [posi], posi)
        self.cp(ang.t, posi.t.bitcast(I32), [posi], [ang], eng="dve")
        self.ts(ang.t, ang.t, pc.t[:, 0:1], ALU.mult, [ang, pc], [ang])
        self.frac2pi(fr.t, ang.t, 0.5 * PI, fb.t, [ang, fr, fb], [fr, fb])
        self.act(cosT.t, fr.t, AF.Sin, [fr], [cosT], scale=2 * PI)
        self.frac2pi(fr.t, ang.t, 0.0, fb.t, [ang, fr, fb], [fr, fb])
        self.act(sinT.t, fr.t, AF.Sin, [fr, pc], [sinT], scale=pc.t[:, 2:3])
        for c in range(2):
            for (dst, base) in ((qf[c], 18), (kf[c], 20)):
                w1 = self.loadw("win", l * 28 + base + c, 8)
                w2 = self.loadw("win", l * 28 + base + 6 + c, 8)
                p1, p2 = self.P[0], self.P[1]
                for k in range(8):
                    self.mm(p1.t[:], w1.t[:, k, :], u.t[:, k, :], k == 0, k == 7, [w1, u], [p1])
                for k in range(8):
                    self.mm(p2.t[:], w2.t[:, k, :], u.t[:, k, :], k == 0, k == 7, [w2, u], [p2])
                self.tt(t1.t, p1.t[:], cosT.t, ALU.mult, [p1, cosT], [t1])
                self.tt(t2.t, p2.t[:], sinT.t, ALU.mult, [p2, sinT], [t2])
                self.tt(dst.t, t1.t, t2.t, ALU.add, [t1, t2], [dst], eng="pool")
            self.cp(qb_.t[:, c, :], qf[c].t, [qf[c]], [qb_], eng="pool")
            self.cp(kc_.t[:, c, t0:t0 + NT], kf[c].t, [kf[c]], [kc_], eng="pool")
            S.op("dve", lambda e, c=c: e.tensor_reduce(out=km.t[:, c, 2 * tb:2 * tb + 2],
                                                      in_=kf[c].t.rearrange("p (a b) -> p a b", b=256),
                                                      axis=AX.X, op=ALU.add), [kf[c]], [km])
        self.ts(km.t[:, :, 2 * tb:2 * tb + 2], km.t[:, :, 2 * tb:2 * tb + 2], 1.0 / 256, ALU.mult, [km], [km])
        wv = [self.loadw("win", l * 28 + 22 + i, 8) for i in range(2)]
        for tt_ in range(4):
            pv = self.P[2 + tt_ % 2]
            for i in range(2):
                for k in range(8):
                    self.mm(pv.t[:, i * 128:(i + 1) * 128], u.t[:, k, tt_ * 128:(tt_ + 1) * 128], wv[i].t[:, k, :],
                            k == 0, k == 7, [wv[i], u], [pv])
            self.cp(vc_.t[:, tb * 4 + tt_, :], pv.t[:, 0:256], [pv], [vc_], eng="act")
        if l == 0 and tb == self.dbg.get("ddtb", 0):
            self.dd("cosT", cosT.t, [cosT], [128, NT])
            self.dd("sinT", sinT.t, [sinT], [128, NT])
            self.dd("qf0", qf[0].t, [qf[0]], [128, NT])
            self.dd("kf1", kf[1].t, [kf[1]], [128, NT])
            self.dd("km", km.t[:], [km], [128, 2, 8])
            self.dd("vc", vc_.t[:, tb * 4, :], [vc_], [128, 256], BF16)
        if tb > 0 or True:
            for qt in range(4):
                qblk = 2 * tb + qt // 2
                if qblk == 0:
                    continue
                pg = self.P[6]
                for h in range(4):
                    c, off = h // 2, (h % 2) * 64
                    self.mm(pg.t[:, h * 8:h * 8 + 8], qf[c].t[off:off + 64, qt * 128:(qt + 1) * 128],
                            km.t[off:off + 64, c, :], True, True, [qf[c], km], [pg])
                vm = consts.t[:, 256 + qblk * 8:256 + qblk * 8 + 8].unsqueeze(1).broadcast_to([128, 4, 8])
                self.tt(gm, pg.t[:, 0:32].rearrange("p (a b) -> p a b", b=8), vm, ALU.add, [pg, consts], [sm])
                for h in range(4):
                    S.op("dve", lambda e, h=h: e.max(out=mx[:, h, :], in_=gm[:, h, :]), [sm], [sm])
                for h in range(4):
                    self.ts(gm[:, h, :], gm[:, h, :], mx[:, h, 2:3], ALU.is_ge, [sm], [sm], s2=30000.0, op1=ALU.mult)
                self.ts(mnegb[:, qt, :], sm.t[:, 0:32], -30000.0, ALU.add, [sm], [sm])
                if l == 0 and tb == self.dbg.get("ddtb", 0) and qt == 3:
                    self.dd("sm", sm.t[:, 0:128], [sm], [128, 128])
        it = 0
        for c in range(2):
            for j in range(2):
                qblk = 2 * tb + j
                nkt = 2 * (qblk + 1)
                pacc, pden = self.P[2 + 2 * (it % 2)], self.P[3 + 2 * (it % 2)]
                it += 1
                qs = slice(j * 256, (j + 1) * 256)
                for kt in range(nkt):
                    n = kt // 2
                    ps_ = self.P[kt % 2]
                    e_ = ets[kt % 2]
                    for hh in range(2):
                        h, off = 2 * c + hh, hh * 64
                        o = ps_.t[:, hh * 256:(hh + 1) * 256]
                        self.mm(o, kc_.t[off:off + 64, c, kt * 128:(kt + 1) * 128], qb_.t[off:off + 64, c, qs],
                                True, False, [kc_, qb_], [ps_])
                        if n < qblk:
                            for q2 in range(2):
                                qt = 2 * j + q2
                                lh = mnegb[:, qt, h * 8 + n:h * 8 + n + 1].broadcast_to([128, 128])
                                self.mm(ps_.t[:, hh * 256 + q2 * 128:hh * 256 + (q2 + 1) * 128], lh, self.ident_bf.t[:],
                                        False, True, [sm, self.ident_bf], [ps_])
                        else:
                            self.mm(o, self.ident_bf.t[:], self.cm.t[:, kt % 2, :], False, True,
                                    [self.ident_bf, self.cm], [ps_])
                    if l == 0 and tb == self.dbg.get("ddtb", 0) and c == 0 and j == 0 and kt == 0 and "ps" in self.dbg.get("dd", ()):
                        dbgt = self.tmp(17, 1, [128, 512])
                        self.cp(dbgt.t, ps_.t[:], [ps_], [dbgt], eng="act")
                        self.dd("ps", dbgt.t, [dbgt], [128, 512])
                    self.act(e_, ps_.t[:].rearrange("p (a b) -> p a b", b=256), AF.Exp, [ps_], [et[0]], scale=0.125)
                    if l == 0 and tb == self.dbg.get("ddtb", 0) and c == 0 and j == 0 and kt == 0:
                        self.dd("et", et[0].t[:, 0:512], [et[0]], [128, 512], BF16)
                        self.dd("cm", self.cm.t[:], [self.cm], [128, 2, 256], BF16)
                    e2 = et[0].t[:, (kt % 2) * 512:(kt % 2 + 1) * 512]
                    self.mm(pacc.t[:], vc_.t[:, kt, c * 128:(c + 1) * 128], e2,
                            kt == 0, kt == nkt - 1, [vc_, et[0]], [pacc])
                    self.mm(pden.t[:], self.ones_bf.t[:], e2,
                            kt == 0, kt == nkt - 1, [self.ones_bf, et[0]], [pden])
                S.op("dve", lambda e, pden=pden: e.reciprocal(rden.t.rearrange("p a b -> p (a b)"), pden.t[:]), [pden], [rden])
                if l == 0 and tb == self.dbg.get("ddtb", 0):
                    self.dd("rden%d%d" % (c, j), rden.t, [rden], [128, 2, 256])
                for hh in range(2):
                    off = hh * 64
                    self.tt(y.t[off:off + 64, 3, c, qs], pacc.t[off:off + 64, hh * 256:(hh + 1) * 256],
                            rden.t[off:off + 64, hh, :], ALU.mult, [pacc, rden], [y])

    def tokproj_small(self, l):
        u, pp = self.u, self.P[7]
        for c in range(8):
            for k in range(8):
                self.mm(pp.t[0:64, c * 16:(c + 1) * 16], u.t[:, k, c * 64:(c + 1) * 64], self.wsm.t[:, l, k, :],
                        k == 0, k == 7, [u, self.wsm], [pp])
        self.sp = self.tmp(11, 1, [128, 512])
        self.cp(self.sp.t[0:64, 0:128], pp.t[0:64, 0:128], [pp], [self.sp], eng="act")
        return self.sp.t[0:64, 0:128].rearrange("p (c n) -> p c n", n=16)

    def conv_silu(self, l, tiles, nch, tail, cw, dst, tb):
        S, u = self.S, self.u
        xc = self.tmp(0, 7, [128, nch, NT + 3])
        acc = self.tmp(9, 1, [128, NT])
        if tb == 0:
            S.op("dve", lambda e: e.memset(tail.t[:], 0.0), [], [tail])
        self.cp(xc.t[:, :, 0:3], tail.t[:], [tail], [xc], eng="dve")
        for c in range(nch):
            w = self.loadw("win", l * 28 + tiles + c, 8)
            pp = self.P[c % 2]
            for k in range(8):
                self.mm(pp.t[:], w.t[:, k, :], u.t[:, k, :], k == 0, k == 7, [w, u], [pp])
            self.cp(xc.t[:, c, 3:NT + 3], pp.t[:], [pp], [xc], eng="act")
        self.cp(tail.t[:], xc.t[:, :, NT:NT + 3], [xc], [tail], eng="dve")
        for c in range(nch):
            self.ts(acc.t, xc.t[:, c, 0:NT], cw[:, c * 4:c * 4 + 1], ALU.mult, [xc, self.cwb], [acc])
            for j in range(1, 4):
                self.stt(acc.t, xc.t[:, c, j:NT + j], cw[:, c * 4 + j:c * 4 + j + 1], acc.t, ALU.mult, ALU.add,
                         [xc, acc, self.cwb], [acc])
            self.act(dst.t[:, c, :], acc.t, AF.Silu, [acc], [dst])

    def mlstm_fwd(self, l, s, tb, sp):
        S, u, y = self.S, self.u, self.y
        consts, c2, cst = self.consts, self.consts2, self.cst
        ident = consts.t[:, 128:256]
        ones = consts.t[:, 384:512]
        TRI = c2.t[0:64, 0:64]
        CMASK = c2.t[0:64, 64:128]
        rows = self.mlrow.t[0:64, l, :]
        Cx, mrep = self.mlC[l], self.mlm[l]
        qk = self.tmp(12, 4, [128, 4, NT])
        self.conv_silu(l, 8, 4, self.mltail[l], self.cwb.t[:, l, 24:40], qk, tb)
        self.ts(qk.t[:, 2:4, :], qk.t[:, 2:4, :], 0.125, ALU.mult, [qk], [qk])
        ms = self.dbg.get("mlstop", 99)
        if ms <= 1:
            return
        kz = self.tmp(4, 4, [128, 4, NT])
        S.op("pool", lambda e: e.memset(kz.t, 0.0), [], [kz])
        for h in range(4):
            pr, off = h // 2, (h % 2) * 64
            self.cp(kz.t[off:off + 64, h, :], qk.t[off:off + 64, 2 + pr, :], [qk], [kz], eng="pool")
        if tb == 0:
            S.op("dve", lambda e: e.memset(Cx.t[:], 0.0), [], [Cx])
            S.op("dve", lambda e: e.memset(mrep.t[:], 0.0), [], [mrep])
        A = self.tmp(16, 1, [128, 512])
        R_ = [A, self.sp]
        v3 = lambda lo: A.t[0:64, lo:lo + 32].rearrange("p (c h) -> p c h", h=4)
        li, lf, b_, ak, tx = v3(0), v3(32), v3(64), v3(96), v3(128)
        grep = A.t[:, 160:192].rearrange("p (c h) -> p c h", h=4)
        mkrep = A.t[:, 192:224].rearrange("p (c h) -> p c h", h=4)
        Mall = A.t[:, 224:260].rearrange("p (c h) -> p c h", h=4)
        scall = A.t[:, 260:292].rearrange("p (c h) -> p c h", h=4)
        kws = v3(292)
        mk32 = A.t[0:32, 324:325]
        dg32 = A.t[0:32, 328:360]
        ib = rows[:, 0:4].unsqueeze(1).broadcast_to([64, 8, 4])
        fb = rows[:, 4:8].unsqueeze(1).broadcast_to([64, 8, 4])
        self.tt(li, sp[:, :, 8:12], ib, ALU.add, R_ + [self.mlrow], [A])
        self.tt(tx, sp[:, :, 12:16], fb, ALU.add, R_ + [self.mlrow], [A])
        self.act(tx, tx, AF.Exp, [A], [A], scale=-1.0)
        self.act(tx, tx, AF.Ln, [A, cst], [A], bias=cst.t[0:64, 3:4])
        self.ts(lf, tx, -1.0, ALU.mult, [A], [A])
        p7 = self.P[7]
        lf2 = A.t[0:64, 32:64]
        self.mm(p7.t[0:64, 0:32], TRI, lf2, True, True, [A, c2], [p7])
        self.cp(A.t[0:64, 64:96], p7.t[0:64, 0:32], [p7], [A], eng="act")
        self.mm(p7.t[:, 32:64], ones[0:64, :], lf2, True, True, [A, consts], [p7])
        self.cp(A.t[:, 160:192], p7.t[:, 32:64], [p7], [A], eng="act")
        self.tt(ak, grep[0:64], b_, ALU.subtract, [A], [A])
        self.tt(ak, ak, li, ALU.add, [A], [A])
        self.mm(p7.t[0:32, 64:128], A.t[0:64, 96:128], ident[0:64, 0:64], True, True, [A, consts], [p7])
        S.op("dve", lambda e: e.tensor_reduce(out=mk32, in_=p7.t[0:32, 64:128], axis=AX.X, op=ALU.max), [p7], [A])
        self.ts(dg32, ident[0:32, 0:32], mk32, ALU.mult, [A, consts], [A])
        self.mm(p7.t[:, 128:160], ones[0:32, :], dg32, True, True, [A, consts], [p7])
        self.cp(A.t[:, 192:224], p7.t[:, 128:160], [p7], [A], eng="act")
        self.cp(Mall[:, 0, :], mrep.t[:], [mrep], [A], eng="dve")
        t4 = A.t[:, 364:368]
        for c in range(8):
            self.tt(t4, grep[:, c, :], Mall[:, c, :], ALU.add, [A], [A])
            self.tt(Mall[:, c + 1, :], t4, mkrep[:, c, :], ALU.max, [A], [A])
        self.cp(mrep.t[:], Mall[:, 8, :], [A], [mrep], eng="dve")
        self.tt(scall, grep, Mall[:, 0:8, :], ALU.add, [A], [A])
        self.tt(scall, scall, Mall[:, 1:9, :], ALU.subtract, [A], [A])
        self.act(scall, scall, AF.Exp, [A], [A])
        self.tt(kws, ak, Mall[0:64, 1:9, :], ALU.subtract, [A], [A])
        self.act(kws, kws, AF.Exp, [A], [A])
        if ms <= 2:
            return
        vx = self.tmp(17, 1, [128, 512])
        vext = vx.t[0:64, 0:264].rearrange("p (h e) -> p h e", e=66)
        S.op("dve", lambda e: e.memset(vx.t[0:64, 0:264], 1.0), [], [vx])
        osg = self.tmp(18, 1, [128, 512])
        Dm = self.tmp(19, 1, [128, 512])
        LR = self.tmp(20, 1, [128, 512])
        sq_ = self.tmp(21, 1, [128, 512])
        sT = self.tmp(22, 1, [128, 512])
        ne = self.tmp(23, 1, [128, 512])
        tq = self.tmp(0, 1, [128, 512])
        kt_ = self.tmp(1, 1, [128, 512])
        hh_ = self.tmp(2, 1, [128, 512])
        B = self.tmp(3, 1, [128, 512])
        v4 = lambda T, lo=0: T.t[0:64, lo:lo + 256].rearrange("p (h e) -> p h e", e=64)
        wv = [self.loadw("win", l * 28 + 12 + i, 8) for i in range(4)]
        for c in range(8):
            cs = slice(c * 64, (c + 1) * 64)
            p0 = self.P[0]
            for i in range(4):
                for k in range(8):
                    self.mm(p0.t[0:64, i * 128:(i + 1) * 128], u.t[:, k, cs], wv[i].t[:, k, :], k == 0, k == 7,
                            [u, wv[i]], [p0])
            self.cp(vext[:, :, 0:64], p0.t[0:64, 0:256].rearrange("p (h e) -> p h e", e=64), [p0], [vx], eng="act")
            self.act(osg.t[0:64, 0:256], p0.t[0:64, 256:512], AF.Sigmoid, [p0], [osg])
            if ms <= 3:
                continue
            lft = LR.t[0:64, 0:256].rearrange("p (h e) -> p h e", e=64)
            rm = LR.t[0:64, 256:512].rearrange("p (h e) -> p h e", e=64)
            tri_b = TRI.unsqueeze(1).broadcast_to([64, 4, 64])
            id_b = ident[0:64, 0:64].unsqueeze(1).broadcast_to([64, 4, 64])
            self.tt(lft, tri_b, lf[:, c, :].unsqueeze(2).broadcast_to([64, 4, 64]), ALU.mult, [A, c2], [LR])
            self.tt(rm, id_b, li[:, c, :].unsqueeze(2).broadcast_to([64, 4, 64]), ALU.mult, [A, consts], [LR])
            self.tt(rm, rm, lft, ALU.subtract, [LR], [LR])
            p1 = self.P[1]
            for h in range(4):
                o = p1.t[0:64, h * 64:(h + 1) * 64]
                self.mm(o, lft[:, h, :], ones[0:64, 0:64], True, False, [LR, consts], [p1])
                self.mm(o, ones[0:64, 0:64], rm[:, h, :], False, True, [LR, consts], [p1])
            dmv = v4(Dm)
            self.tt(dmv, p1.t[0:64, 0:256].rearrange("p (h e) -> p h e", e=64),
                    CMASK.unsqueeze(1).broadcast_to([64, 4, 64]), ALU.add, [p1, c2], [Dm])
            sm_ = B.t[0:64, 0:64]
            mloc, mint, mt, wint, e2, qn, den = (B.t[0:64, 4 * i:4 * i + 4] for i in range(7))
            S.op("dve", lambda e, dmv=dmv, mloc=mloc: e.tensor_reduce(out=mloc, in_=dmv, axis=AX.X, op=ALU.max), [Dm], [B])
            self.tt(mint, b_[:, c, :], Mall[0:64, c, :], ALU.add, [A], [B])
            self.tt(mt, mint, mloc, ALU.max, [B], [B])
            self.tt(wint, mint, mt, ALU.subtract, [B], [B])
            self.act(wint, wint, AF.Exp, [B], [B])
            self.act(e2, mt, AF.Exp, [B], [B], scale=-1.0)
            if ms <= 4:
                continue
            p2 = self.P[2]
            for h in range(4):
                o = p2.t[0:64, h * 64:(h + 1) * 64]
                self.mm(o, ones[0:64, 0:64], lft[:, h, :], True, False, [LR, consts], [p2])
                self.mm(o, rm[:, h, :], ones[0:64, 0:64], False, True, [LR, consts], [p2])
            etv = v4(sq_)
            self.tt(etv, p2.t[0:64, 0:256].rearrange("p (h e) -> p h e", e=64),
                    c2.t[0:64, 320:384].unsqueeze(1).broadcast_to([64, 4, 64]), ALU.add, [p2, c2], [sq_])
            self.act(etv, etv, AF.Exp, [sq_], [sq_])
            p3 = self.P[3]
            for h in range(4):
                self.mm(p3.t[0:64, h * 64:(h + 1) * 64], kz.t[:, h, cs], qk.t[:, h // 2, cs], True, True, [kz, qk], [p3])
            self.tt(v4(sT), p3.t[0:64, 0:256].rearrange("p (h e) -> p h e", e=64), etv, ALU.mult, [p3, sq_], [sT])
            if ms <= 5:
                continue
            stv = v4(sT)
            p4, p5 = self.P[4], self.P[5]
            for h in range(4):
                pr, off = h // 2, (h % 2) * 64
                self.mm(p4.t[0:64, h * 66:(h + 1) * 66], stv[:, h, :], vext[:, h, :], True, True, [sT, vx], [p4])
                self.mm(p5.t[0:64, h * 66:(h + 1) * 66], qk.t[:, pr, cs], Cx.t[:, h, :],
                        True, True, [qk, Cx], [p5])
            nev = ne.t[0:64, 0:264].rearrange("p (h e) -> p h e", e=66)
            tqv = tq.t[0:64, 0:264].rearrange("p (h e) -> p h e", e=66)
            self.tt(tqv, p5.t[0:64, 0:264].rearrange("p (h e) -> p h e", e=66),
                    wint.unsqueeze(2).broadcast_to([64, 4, 66]), ALU.mult, [p5, B], [tq])
            self.tt(nev, p4.t[0:64, 0:264].rearrange("p (h e) -> p h e", e=66),
                    e2.unsqueeze(2).broadcast_to([64, 4, 66]), ALU.mult, [p4, B], [ne])
            self.tt(nev, nev, tqv, ALU.add, [tq, ne], [ne])
            self.act(den, nev[:, :, 64], AF.Abs, [ne], [B])
            self.tt(den, den, e2, ALU.max, [B], [B])
            S.op("dve", lambda e, den=den: e.reciprocal(den, den), [B], [B])
            hv = v4(hh_)
            self.tt(hv, nev[:, :, 0:64], den.unsqueeze(2).broadcast_to([64, 4, 64]), ALU.mult, [ne, B], [hh_])
            if ms <= 6:
                continue
            h2 = v4(hh_, 256)
            ss = B.t[0:64, 32:36]
            self.tt(h2, hv, hv, ALU.mult, [hh_], [hh_])
            S.op("dve", lambda e, h2=h2, ss=ss: e.tensor_reduce(out=ss, in_=h2, axis=AX.X, op=ALU.add), [hh_], [B])
            self.act(ss, ss, AF.Sqrt, [B, cst], [B], scale=1.0 / 64, bias=cst.t[0:64, 0:1])
            S.op("dve", lambda e, ss=ss: e.reciprocal(ss, ss), [B], [B])
            self.tt(hv, hv, ss.unsqueeze(2).broadcast_to([64, 4, 64]), ALU.mult, [hh_, B], [hh_])
            self.tt(hh_.t[0:64, 0:256], hh_.t[0:64, 0:256], rows[:, 8:264], ALU.mult, [hh_, self.mlrow], [hh_])
            self.tt(hh_.t[0:64, 0:256], hh_.t[0:64, 0:256], osg.t[0:64, 0:256], ALU.mult, [hh_, osg], [hh_])
            for kc in range(2):
                self.mm(p3.t[:, 256 + kc * 64:256 + (kc + 1) * 64], hh_.t[0:64, kc * 128:(kc + 1) * 128],
                        ident[0:64, 0:64], True, True, [hh_, consts], [p3])
                self.cp(y.t[:, 1, kc, cs], p3.t[:, 256 + kc * 64:256 + (kc + 1) * 64], [p3], [y], eng="act")
            if ms <= 7:
                continue
            p6 = self.P[6]
            for pr in range(2):
                self.mm(p6.t[0:64, pr * 128:(pr + 1) * 128], qk.t[:, 2 + pr, cs], ident, True, True, [qk, consts], [p6])
            kwv = v4(kt_)
            self.tt(kwv, p6.t[0:64, 0:256].rearrange("p (h e) -> p h e", e=64),
                    kws[:, c, :].unsqueeze(2).broadcast_to([64, 4, 64]), ALU.mult, [p6, A], [kt_])
            p7b = self.P[7]
            for h in range(4):
                pr, off = h // 2, (h % 2) * 64
                o = p7b.t[:, h * 66:(h + 1) * 66]
                self.mm(o, kt_.t[0:64, pr * 128:(pr + 1) * 128], vext[:, h, :], True, True, [kt_, vx], [p7b])
                self.stt(Cx.t[off:off + 64, h, :], Cx.t[off:off + 64, h, :], scall[off:off + 64, c, h:h + 1],
                         p7b.t[off:off + 64, h * 66:(h + 1) * 66], ALU.mult, ALU.add, [Cx, A, p7b], [Cx])

    def gdn_fwd(self, l, s, tb, sp):
        S, u, y = self.S, self.u, self.y
        consts, c2, cst = self.consts, self.consts2, self.cst
        ident = consts.t[:, 128:256]
        id64 = ident[0:64, 0:64]
        ones = consts.t[:, 384:512]
        on64 = ones[0:64, 0:64]
        neg64 = self.negones.t[0:64, 0:64]
        TRI = c2.t[0:64, 0:64]
        SLADD = self.gmask.t[0:64, 0:64]
        SUADD = self.gmask.t[0:64, 64:128]
        CMT = c2.t[0:64, 320:384]
        BLK = self.gmask.t[:, 128:256]
        rows = self.gdrow.t[0:64, l, :]
        Sz = self.gdS[l]
        b3 = lambda ap: ap.unsqueeze(1).broadcast_to([64, 4, 64])
        v4 = lambda T, lo=0: T.t[0:64, lo:lo + 256].rearrange("p (h e) -> p h e", e=64)
        qkv = self.tmp(12, 6, [128, 6, NT])
        self.conv_silu(l, 0, 6, self.gdtail[l], self.cwb.t[:, l, 0:24], qkv, tb)
        if tb == 0:
            S.op("dve", lambda e: e.memset(Sz.t[:], 0.0), [], [Sz])
        sq = self.tmp(9, 1, [128, NT])
        rs = self.tmp(8, 1, [128, NT])
        for c4 in range(4):
            pp = self.P[c4 % 2]
            self.tt(sq.t, qkv.t[:, c4, :], qkv.t[:, c4, :], ALU.mult, [qkv], [sq])
            self.mm(pp.t[:], BLK, sq.t, True, True, [sq, self.gmask], [pp])
            self.act(rs.t, pp.t[:], AF.Sqrt, [pp, cst], [rs], bias=cst.t[:, 0:1])
            S.op("dve", lambda e: e.reciprocal(rs.t, rs.t), [rs], [rs])
            if c4 < 2:
                self.stt(qkv.t[:, c4, :], qkv.t[:, c4, :], 0.125, rs.t, ALU.mult, ALU.mult, [qkv, rs], [qkv])
            else:
                self.tt(qkv.t[:, c4, :], qkv.t[:, c4, :], rs.t, ALU.mult, [qkv, rs], [qkv])
        kz = self.tmp(4, 4, [128, 4, NT])
        S.op("pool", lambda e: e.memset(kz.t, 0.0), [], [kz])
        for h in range(4):
            pr, off = h // 2, (h % 2) * 64
            self.cp(kz.t[off:off + 64, h, :], qkv.t[off:off + 64, 2 + pr, :], [qkv], [kz], eng="pool")
        A = self.tmp(18, 1, [128, 512])
        v3 = lambda lo: A.t[0:64, lo:lo + 32].rearrange("p (c h) -> p c h", h=4)
        beta, g_, gc, egc, ekd, tx, bneg, begc = v3(0), v3(32), v3(64), v3(96), v3(128), v3(160), v3(192), v3(224)
        gLrep = A.t[:, 256:288].rearrange("p (c h) -> p c h", h=4)
        cdrep = A.t[:, 288:320].rearrange("p (c h) -> p c h", h=4)
        ea = A.t[0:64, 320:324]
        R_ = [A, self.sp]
        self.act(beta, sp[:, :, 0:4], AF.Sigmoid, R_, [A])
        self.tt(tx, sp[:, :, 4:8], rows[:, 4:8].unsqueeze(1).broadcast_to([64, 8, 4]), ALU.add, R_ + [self.gdrow], [A])
        self.act(tx, tx, AF.Exp, [A], [A])
        self.act(tx, tx, AF.Ln, [A, cst], [A], bias=cst.t[0:64, 3:4])
        self.act(ea, rows[:, 0:4], AF.Exp, [self.gdrow], [A])
        self.tt(g_, tx, ea.unsqueeze(1).broadcast_to([64, 8, 4]), ALU.mult, [A], [A])
        self.ts(g_, g_, -1.0, ALU.mult, [A], [A])
        p7 = self.P[7]
        g2 = A.t[0:64, 32:64]
        self.mm(p7.t[0:64, 0:32], TRI, g2, True, True, [A, c2], [p7])
        self.cp(A.t[0:64, 64:96], p7.t[0:64, 0:32], [p7], [A], eng="act")
        self.mm(p7.t[:, 32:64], ones[0:64, :], g2, True, True, [A, consts], [p7])
        self.cp(A.t[:, 256:288], p7.t[:, 32:64], [p7], [A], eng="act")
        self.act(egc, gc, AF.Exp, [A], [A])
        self.tt(ekd, gLrep[0:64], gc, ALU.subtract, [A], [A])
        self.act(ekd, ekd, AF.Exp, [A], [A])
        self.act(cdrep, gLrep, AF.Exp, [A], [A])
        self.ts(bneg, beta, -1.0, ALU.mult, [A], [A])
        self.tt(begc, beta, egc, ALU.mult, [A], [A])
        MM_ = self.tmp(19, 1, [128, 512])
        X = self.tmp(20, 1, [128, 512])
        DC = self.tmp(21, 1, [128, 512])
        QB = self.tmp(22, 1, [128, 512])
        GT = self.tmp(23, 1, [128, 512])
        VK = self.tmp(0, 1, [128, 512])
        XT = self.tmp(1, 1, [128, 512])
        VN = self.tmp(2, 1, [128, 512])
        ZS = self.tmp(3, 1, [128, 512])
        M2 = self.tmp(10, 1, [128, 512])
        wz = [self.loadw("win", l * 28 + 6 + i, 8) for i in range(2)]
        Xv = X.t[0:64, 0:512].rearrange("p (h e) -> p h e", e=128)
        for c in range(8):
            cs = slice(c * 64, (c + 1) * 64)
            p0 = self.P[0]
            for i in range(2):
                for k in range(8):
                    self.mm(p0.t[0:64, i * 128:(i + 1) * 128], u.t[:, k, cs], wz[i].t[:, k, :], k == 0, k == 7,
                            [u, wz[i]], [p0])
            self.act(ZS.t[0:64, 0:256], p0.t[0:64, 0:256], AF.Silu, [p0], [ZS])
            for pr in range(2):
                self.mm(p0.t[0:64, 256 + pr * 128:256 + (pr + 1) * 128], qkv.t[:, 4 + pr, cs], ident, True, True,
                        [qkv, consts], [p0])
            vtok = v4(VK)
            self.cp(vtok, p0.t[0:64, 256:512].rearrange("p (h e) -> p h e", e=64), [p0], [VK], eng="act")
            p1 = self.P[1]
            for pr in range(2):
                self.mm(p1.t[0:64, pr * 128:(pr + 1) * 128], qkv.t[:, 2 + pr, cs], ident, True, True, [qkv, consts], [p1])
            ktok = v4(VK, 256)
            self.cp(ktok, p1.t[0:64, 0:256].rearrange("p (h e) -> p h e", e=64), [p1], [VK], eng="act")
            for h in range(4):
                uo, wo = (64, 0) if h % 2 == 0 else (0, 64)
                self.ts(Xv[:, h, uo:uo + 64], vtok[:, h, :], beta[:, c, h:h + 1], ALU.mult, [VK, A], [X])
                self.ts(Xv[:, h, wo:wo + 64], ktok[:, h, :], begc[:, c, h:h + 1], ALU.mult, [VK, A], [X])
            kd = v4(XT, 256)
            self.tt(kd, ktok, ekd[:, c, :].unsqueeze(2).broadcast_to([64, 4, 64]), ALU.mult, [VK, A], [XT])
            gt = v4(GT)
            self.tt(gt, b3(TRI), g_[:, c, :].unsqueeze(2).broadcast_to([64, 4, 64]), ALU.mult, [A, c2], [GT])
            p2, p3 = self.P[2], self.P[3]
            for h in range(4):
                o = p2.t[0:64, h * 64:(h + 1) * 64]
                self.mm(o, gt[:, h, :], on64, True, False, [GT, consts], [p2])
                self.mm(o, neg64, gt[:, h, :], False, True, [GT, self.negones], [p2])
                o = p2.t[0:64, 256 + h * 64:256 + (h + 1) * 64]
                self.mm(o, on64, gt[:, h, :], True, False, [GT, consts], [p2])
                self.mm(o, gt[:, h, :], neg64, False, True, [GT, self.negones], [p2])
            dS, dT, dQ = v4(DC), v4(DC, 256), v4(QB)
            pD = p2.t[0:64, 0:256].rearrange("p (h e) -> p h e", e=64)
            pDT = p2.t[0:64, 256:512].rearrange("p (h e) -> p h e", e=64)
            self.tt(dS, pD, b3(SLADD), ALU.add, [p2, self.gmask], [DC])
            self.tt(dT, pDT, b3(SUADD), ALU.add, [p2, self.gmask], [DC])
            self.tt(dQ, pDT, b3(CMT), ALU.add, [p2, c2], [QB])
            self.act(DC.t[0:64, 0:512], DC.t[0:64, 0:512], AF.Exp, [DC], [DC])
            self.act(dQ, dQ, AF.Exp, [QB], [QB])
            dgb = v4(GT, 256)
            self.tt(dgb, b3(id64), bneg[:, c, :].unsqueeze(2).broadcast_to([64, 4, 64]), ALU.mult, [A, consts], [GT])
            for h in range(4):
                self.mm(p3.t[0:64, h * 64:(h + 1) * 64], qkv.t[:, 2 + h // 2, cs], kz.t[:, h, cs], True, True, [qkv, kz], [p3])
                self.mm(p3.t[0:64, 256 + h * 64:256 + (h + 1) * 64], on64, dgb[:, h, :], True, True, [GT, consts], [p3])
            pKK = p3.t[0:64, 0:256].rearrange("p (h e) -> p h e", e=64)
            pBf = p3.t[0:64, 256:512].rearrange("p (h e) -> p h e", e=64)
            Mk, MkT = v4(MM_), v4(MM_, 256)
            self.tt(Mk, pKK, dS, ALU.mult, [p3, DC], [MM_])
            self.tt(Mk, Mk, bneg[:, c, :].unsqueeze(2).broadcast_to([64, 4, 64]), ALU.mult, [MM_, A], [MM_])
            self.tt(MkT, pKK, dT, ALU.mult, [p3, DC], [MM_])
            self.tt(MkT, MkT, pBf, ALU.mult, [MM_, p3], [MM_])
            p4 = self.P[4]
            for h in range(4):
                self.mm(p4.t[0:64, h * 64:(h + 1) * 64], kz.t[:, h, cs], qkv.t[:, h // 2, cs], True, True, [qkv, kz], [p4])
            self.tt(dQ, p4.t[0:64, 0:256].rearrange("p (h e) -> p h e", e=64), dQ, ALU.mult, [p4, QB], [QB])
            cur, nxt = MM_, M2
            for step in range(6):
                cM, cMT = v4(cur), v4(cur, 256)
                p5 = self.P[5]
                for h in range(4):
                    self.mm(p5.t[0:64, h * 128:(h + 1) * 128], cMT[:, h, :], Xv[:, h, :], True, True, [cur, X], [p5])
                if step < 5:
                    p6 = self.P[6]
                    for h in range(4):
                        self.mm(p6.t[0:64, h * 64:(h + 1) * 64], cMT[:, h, :], cM[:, h, :], True, True, [cur], [p6])
                        self.mm(p6.t[0:64, 256 + h * 64:256 + (h + 1) * 64], cM[:, h, :], cMT[:, h, :], True, True, [cur], [p6])
                    self.cp(nxt.t[0:64, 0:512], p6.t[0:64, 0:512], [p6], [nxt], eng="act")
                self.tt(X.t[0:64, 0:512], X.t[0:64, 0:512], p5.t[0:64, 0:512], ALU.add, [X, p5], [X])
                cur, nxt = nxt, cur
            p5 = self.P[5]
            for h in range(4):
                self.mm(p5.t[:, h * 64:(h + 1) * 64], Xv[:, h, :], id64, True, True, [X, consts], [p5])
            xt = XT.t[:, 0:256].rearrange("p (h e) -> p h e", e=64)
            self.cp(XT.t[:, 0:256], p5.t[:, 0:256], [p5], [XT], eng="act")
            p6 = self.P[6]
            for h in range(4):
                self.mm(p6.t[0:64, h * 64:(h + 1) * 64], xt[:, h, :], Sz.t[:, h, :], True, True, [XT, Sz], [p6])
                self.mm(p6.t[0:64, 256 + h * 64:256 + (h + 1) * 64], qkv.t[:, h // 2, cs], Sz.t[:, h, :], True, True,
                        [qkv, Sz], [p6])
            vn = v4(VN)
            for h in range(4):
                uo = 64 if h % 2 == 0 else 0
                self.tt(vn[:, h, :], Xv[:, h, uo:uo + 64], p6.t[0:64, h * 64:(h + 1) * 64], ALU.subtract, [X, p6], [VN])
            oq = v4(VN, 256)
            self.tt(oq, p6.t[0:64, 256:512].rearrange("p (h e) -> p h e", e=64),
                    egc[:, c, :].unsqueeze(2).broadcast_to([64, 4, 64]), ALU.mult, [p6, A], [VN])
            p7 = self.P[7]
            for h in range(4):
                self.mm(p7.t[0:64, h * 64:(h + 1) * 64], dQ[:, h, :], vn[:, h, :], True, True, [QB, VN], [p7])
            self.tt(oq, oq, p7.t[0:64, 0:256].rearrange("p (h e) -> p h e", e=64), ALU.add, [VN, p7], [VN])
            p1 = self.P[1]
            for h in range(4):
                pr, off = h // 2, (h % 2) * 64
                self.mm(p1.t[:, h * 64:(h + 1) * 64], XT.t[0:64, 256 + pr * 128:256 + (pr + 1) * 128], vn[:, h, :],
                        True, True, [XT, VN], [p1])
                self.stt(Sz.t[off:off + 64, h, :], Sz.t[off:off + 64, h, :], cdrep[off:off + 64, c, h:h + 1],
                         p1.t[off:off + 64, h * 64:(h + 1) * 64], ALU.mult, ALU.add, [Sz, A, p1], [Sz])
            o2 = v4(ZS, 256)
            ss = A.t[0:64, 328:332]
            self.tt(o2, oq, oq, ALU.mult, [VN], [ZS])
            S.op("dve", lambda e, o2=o2, ss=ss: e.tensor_reduce(out=ss, in_=o2, axis=AX.X, op=ALU.add), [ZS], [A])
            self.act(ss, ss, AF.Sqrt, [A, cst], [A], scale=1.0 / 64, bias=cst.t[0:64, 0:1])
            S.op("dve", lambda e, ss=ss: e.reciprocal(ss, ss), [A], [A])
            self.tt(o2, oq, ss.unsqueeze(2).broadcast_to([64, 4, 64]), ALU.mult, [VN, A], [ZS])
            self.tt(o2, o2, b3(rows[:, 8:72]), ALU.mult, [ZS, self.gdrow], [ZS])
            self.tt(ZS.t[0:64, 256:512], ZS.t[0:64, 256:512], ZS.t[0:64, 0:256], ALU.mult, [ZS], [ZS])
            p0 = self.P[0]
            for kc in range(2):
                self.mm(p0.t[:, kc * 64:(kc + 1) * 64], ZS.t[0:64, 256 + kc * 128:256 + (kc + 1) * 128], id64, True, True,
                        [ZS, consts], [p0])
                self.cp(y.t[:, 0, kc, cs], p0.t[:, kc * 64:(kc + 1) * 64], [p0], [y], eng="act")

    def mixers(self, l, s, tb):
        only = self.dbg.get("only", "")
        sp = self.tokproj_small(l)
        if not only or "gdn" in only:
            self.gdn_fwd(l, s, tb, sp)
        if not only or "ml" in only:
            self.mlstm_fwd(l, s, tb, sp)
        if not only or "s5" in only:
            self.s5_fwd(l, s, tb)
        if not only or "moba" in only:
            self.moba_fwd(l, s, tb)

    def build(self):
        S = self.S
        self.declare()
        self.setup()
        units = self.dbg.get("units", [(s, tb) for s in range(2) for tb in range(SEQ // NT)])
        stop = self.dbg.get("stop", None)
        for (s, tb) in units:
            t0 = tb * NT
            S.dma("sp", self.h.t[:], self.D["xT"].t[s, :, :, t0:t0 + NT], [], [self.h], self.h)
            for l in range(NL):
                self.ffn(l, 0)
                if stop == "ffn1":
                    break
                self.norm(l * 4 + 1)
                if "yin" in self.dbg:
                    S.dma("sp", self.y.t[:], self.D["yin"].t[s * 4 + tb], [], [self.y], self.y)
                else:
                    self.mixers(l, s, tb)
                if "dumpy" in self.dbg and l == 0:
                    S.dma("sp", self.dumpy.t[s * 4 + tb], self.y.t[:], [self.y], [self.dumpy], self.y)
                if stop == "y":
                    break
                self.merge(l)
                self.ffn(l, 1)
                self.ple(l, s, t0)
            if stop is not None:
                self.dump_h(s * 4 + tb)
            self.final(s, t0)
        S.finish()
        S.emit_all()


def F32v(k, name, hh):
    ri = 0 if name.endswith("re") else 1
    t = k.s5B["BT" if name.startswith("BT") else "CT"]
    return t[:, 4 * hh:4 * hh + 4, ri, :]


def wt(w, K):
    Kd, N = w.shape
    assert Kd == K * 128 and N % 128 == 0
    a = w.reshape(K, 128, N // 128, 128).transpose(2, 1, 0, 3)
    return np.ascontiguousarray(a).reshape(N // 128 * 128, K * 128)


def fm(a, kc):
    T = a.shape[0]
    return np.ascontiguousarray(a.T.reshape(kc, 128, T).transpose(1, 0, 2))


def prep_shared(inp):
    f = lambda n: np.asarray(inp[n], dtype=np.float32)
    sh = {}
    wgu = []
    wdn = []
    for l in range(NL):
        for nm_gu, nm_d in (("ffn1_w_gu", "ffn1_w_down"), ("ffn2_w_gu", "ffn2_w_down")):
            w = f(nm_gu)[l]
            g = wt(w[:, :2816], 8).reshape(22, 128, 1024)
            u = wt(w[:, 2816:], 8).reshape(22, 128, 1024)
            wgu.append(np.stack([g, u], axis=1).reshape(44 * 128, 1024))
            wdn.append(wt(f(nm_d)[l], 22))
    sh["wgu"] = np.concatenate(wgu, 0)
    sh["wdn"] = np.concatenate(wdn, 0)
    swap = np.concatenate([(np.arange(64) + 32) % 64 + 64 * hh for hh in range(4)])
    cols = np.concatenate([np.arange(0, 1024), np.arange(1032, 2056), np.arange(2064, 3088),
                           2320 + swap, 2576 + swap])
    sh["win"] = np.concatenate([wt(f("w_in")[l][:, cols], 8) for l in range(NL)], 0)
    sh["wgate"] = np.concatenate([
        np.stack([wt(f("w_gate")[l, b], 8).reshape(8, 128, 1024) for b in range(4)], 1).reshape(8 * 4 * 128, 1024)
        for l in range(NL)], 0)
    sh["wbr"] = np.concatenate([
        np.stack([wt(f("w_branch")[l, b], 2).reshape(8, 128, 256) for b in range(4)], 1).reshape(8 * 4 * 128, 256)
        for l in range(NL)], 0)
    sh["wout"] = np.concatenate([wt(f("w_out")[l], 8) for l in range(NL)], 0)
    sh["wplg"] = np.concatenate([wt(f("ple_w_gate")[l], 8) for l in range(NL)], 0)
    sh["wplp"] = np.concatenate([wt(f("ple_w_proj")[l], 2) for l in range(NL)], 0)
    gains = np.zeros((9, 1024), np.float32)
    for l in range(NL):
        gains[l * 4 + 0] = f("ffn1_norm")[l]
        gains[l * 4 + 1] = f("mix_norm")[l]
        gains[l * 4 + 2] = f("ffn2_norm")[l]
        gains[l * 4 + 3] = f("ple_norm")[l]
    gains[8] = f("final_norm")
    sh["gains"] = np.ascontiguousarray(gains.reshape(9, 8, 128).transpose(2, 0, 1))
    sh["wglu"] = np.concatenate([wt(f("s5_w_glu")[l], 2) for l in range(NL)], 0)
    consts = np.zeros((128, 512), np.float32)
    consts[:, 0:128] = np.arange(128, dtype=np.float32)[None, :]
    consts[:, 128:256] = np.eye(128, dtype=np.float32)
    consts[:, 384:512] = 1.0
    for qb in range(8):
        for n in range(8):
            consts[:, 256 + qb * 8 + n] = 0.0 if n < qb else -1e9
    pc = np.zeros((128, 8), np.float32)
    invf = (10000.0 ** (-np.arange(0, 64, 2, dtype=np.float32) / 64)).astype(np.float32)
    for p_ in range(128):
        pc[p_, 0] = invf[p_ % 32]
        pc[p_, 1] = -1.0 if (p_ % 64) < 32 else 1.0
        pc[p_, 2] = pc[p_, 1] * 2 * np.pi
    sh["pc"] = pc
    c2 = np.zeros((128, 512), np.float32)
    ii = np.arange(64)
    c2[0:64, 0:64] = (ii[:, None] <= ii[None, :]).astype(np.float32)
    c2[0:64, 64:128] = np.where(ii[None, :] <= ii[:, None], 0.0, -60000.0)
    c2[0:64, 128:192] = (ii[None, :] < ii[:, None]).astype(np.float32)
    c2[0:64, 192:256] = (ii[None, :] <= ii[:, None]).astype(np.float32)
    c2[0:64, 256:320] = (ii[:, None] < ii[None, :]).astype(np.float32)
    c2[0:64, 320:384] = np.where(ii[:, None] <= ii[None, :], 0.0, -60000.0)
    sh["consts2"] = c2
    gmk = np.zeros((128, 256), np.float32)
    gmk[0:64, 0:64] = np.where(ii[None, :] < ii[:, None], 0.0, -60000.0)
    gmk[0:64, 64:128] = np.where(ii[:, None] < ii[None, :], 0.0, -60000.0)
    pp_ = np.arange(128)
    gmk[:, 128:256] = (pp_[:, None] // 64 == pp_[None, :] // 64).astype(np.float32)
    sh["gmaskf"] = gmk
    win = f("w_in")
    wsm = np.zeros((128, NL, 8, 16), np.float32)
    cwf = np.zeros((128, NL, 40), np.float32)
    mlrow = np.zeros((128, NL, 264), np.float32)
    gdrow = np.zeros((128, NL, 72), np.float32)
    for l in range(NL):
        small = np.concatenate([win[l][:, 1024:1032], win[l][:, 2056:2064]], 1)
        wsm[:, l] = small.reshape(8, 128, 16).transpose(1, 0, 2)
        gc = f("gdn_conv")[l]
        mc = f("mlstm_conv")[l]
        cwf[:, l, 0:24] = gc.T.reshape(6, 128, 4).transpose(1, 0, 2).reshape(128, 24)
        cwf[:, l, 24:40] = mc.T.reshape(4, 128, 4).transpose(1, 0, 2).reshape(128, 16)
        mlrow[:, l, 0:4] = f("mlstm_i_bias")[l][None, :]
        mlrow[:, l, 4:8] = f("mlstm_f_bias")[l][None, :]
        mlrow[:, l, 8:264] = f("mlstm_norm")[l][None, :]
        gdrow[:, l, 0:4] = f("gdn_a_log")[l][None, :]
        gdrow[:, l, 4:8] = f("gdn_dt_bias")[l][None, :]
        gdrow[:, l, 8:72] = f("gdn_norm")[l][None, :]
    sh["wsmf"], sh["cwf"], sh["mlrowf"], sh["gdrowf"] = wsm, cwf, mlrow, gdrow
    cmf = np.zeros((128, 2, 256), np.float32)
    for a in range(2):
        kl = a * 128 + np.arange(128)[:, None]
        cmf[:, a, :] = np.where(kl > np.arange(256)[None, :], -30000.0, 0.0)
    sh["cmf"] = cmf
    sh["consts"] = consts
    lre, lim, ldt = f("s5_lambda_re"), f("s5_lambda_im"), f("s5_log_dt")
    s5st = np.zeros((NL, 128, 8, 3), np.float32)
    s5rep = np.zeros((NL, 3, 128, 8, 128), np.float32)
    s5bexp = np.zeros((NL, 2, 128, 8, 128), np.float32)
    s5cexp = np.zeros((NL, 2, 128, 8, 128), np.float32)
    bre, bim, cre, cim = f("s5_b_re"), f("s5_b_im"), f("s5_c_re"), f("s5_c_im")
    for l in range(NL):
        for sc in range(8):
            for half in range(2):
                g = 2 * sc + half
                ps = slice(half * 64, half * 64 + 64)
                s5st[l, ps, sc, 0] = lre[l, g]
                s5st[l, ps, sc, 1] = lim[l, g]
                s5st[l, ps, sc, 2] = ldt[l, g]
                s5rep[l, 0, :, sc, ps] = lre[l, g][None, :]
                s5rep[l, 1, :, sc, ps] = lim[l, g][None, :]
                s5rep[l, 2, :, sc, ps] = ldt[l, g]
                r0 = (sc % 4) * 32 + half * 16
                s5bexp[l, 0, r0:r0 + 16, sc, ps] = bre[l, g].T
                s5bexp[l, 1, r0:r0 + 16, sc, ps] = bim[l, g].T
                s5cexp[l, 0, ps, sc, r0:r0 + 16] = cre[l, g].T
                s5cexp[l, 1, ps, sc, r0:r0 + 16] = cim[l, g].T
    sh["s5st"], sh["s5rep"], sh["s5bexp"], sh["s5cexp"] = s5st, s5rep, s5bexp, s5cexp
    s5db = np.zeros((NL, 128, 4), np.float32)
    for l in range(NL):
        s5db[l, :, 0:2] = f("s5_d")[l].reshape(2, 128).T
        s5db[l, :, 2:4] = f("s5_b_glu")[l].reshape(2, 128).T
    sh["s5db"] = s5db
    return sh


def prep_core(inp, c):
    x = np.asarray(inp["x"], dtype=np.float32)
    p = np.asarray(inp["p"], dtype=np.float32)
    m = {}
    m["xT"] = np.stack([fm(x[2 * c + s], 8) for s in range(2)], 0)
    m["pT"] = np.stack([np.stack([fm(p[l, 2 * c + s], 2) for s in range(2)], 0) for l in range(NL)], 0)
    pos = np.asarray(inp["positions"]).astype(np.int32)
    m["posr"] = np.ascontiguousarray(np.broadcast_to(pos[2 * c:2 * c + 2, None, :], (2, 128, SEQ)))
    return m


def build_nc(dbg=None):
    nc = bass.Bass("TRN2", target_bir_lowering=False)
    with ExitStack() as st:
        k = Kern(nc, st, dbg)
        k.build()
    return nc


def kernel(**inputs):
    sh = prep_shared(inputs)
    nc = build_nc()
    in_maps = []
    for c in range(8):
        m = dict(sh)
        m.update(prep_core(inputs, c))
        in_maps.append(m)
    res = run_bass_kernel_spmd(nc, in_maps, core_ids=list(range(8)))
    out = np.zeros((16, SEQ, 1024), np.float32)
    for c in range(8):
        o = res.results[c]["out"]
        for s in range(2):
            out[2 * c + s] = o[s].transpose(2, 1, 0).reshape(SEQ, 1024)
    return out
```

```python
import numpy as np
from contextlib import ExitStack
import concourse.bass as bass
import concourse.mybir as mybir
from concourse.bass_utils import run_bass_kernel_spmd

F32 = mybir.dt.float32
BF16 = mybir.dt.bfloat16
I32 = mybir.dt.int32
AF = mybir.ActivationFunctionType
ALU = mybir.AluOpType
AX = mybir.AxisListType

ENGS = ["pe", "act", "dve", "pool", "sp"]
NT = 512
NL = 2
SEQ = 2048
PI = float(np.pi)


class Buf:
    __slots__ = ("name", "w", "r", "sem", "semcnt", "t")

    def __init__(self, name, t=None):
        self.name = name
        self.w = None
        self.r = {}
        self.sem = None
        self.semcnt = 0
        self.t = t


class Tmp:
    __slots__ = ("t", "bufs")

    def __init__(self, t, bufs):
        self.t = t
        self.bufs = bufs


def flat(lst):
    out = []
    for b in lst:
        if isinstance(b, Tmp):
            out.extend(b.bufs)
        else:
            out.append(b)
    return out


class Sched:
    def __init__(self, nc, stack):
        self.nc = nc
        self.stack = stack
        self.q = {e: [] for e in ENGS}
        self.cnt = {e: 0 for e in ENGS}
        self.sems = {}
        for e in ENGS:
            self.sems[e] = stack.enter_context(nc.semaphore("s_" + e))
        self.seen = {e: {} for e in ENGS}
        self.dmabufs = []

    def sb(self, name, shape, dt=F32):
        return Buf(name, self.stack.enter_context(self.nc.sbuf_tensor("sb_" + name, list(shape), dt)))

    def ps(self, name, shape, dt=F32):
        return Buf(name, self.stack.enter_context(self.nc.psum_tensor("ps_" + name, list(shape), dt)))

    def _waits(self, e, reads, writes):
        need = {}

        def add(k, v, src):
            if src == "pe" and e == "pe":
                return
            if v > need.get(k, 0):
                need[k] = v
        for b in reads:
            if b.w is not None:
                add(*b.w)
        for b in writes:
            if b.w is not None:
                add(*b.w)
            for k, (v, src) in b.r.items():
                add(k, v, src)
        out = []
        seen = self.seen[e]
        for k, v in need.items():
            if seen.get(k, 0) < v:
                seen[k] = v
                out.append((self.sems[k], v))
        return out

    def _record(self, dep, reads, writes):
        k, v, src = dep
        for b in reads:
            old = b.r.get(k)
            if old is None or old[0] < v:
                b.r[k] = (v, src)
        for b in writes:
            b.w = dep
            b.r = {}

    def op(self, e, fn, reads=(), writes=()):
        reads, writes = flat(reads), flat(writes)
        waits = self._waits(e, reads, writes)
        self.cnt[e] += 1
        n = self.cnt[e]
        sem = self.sems[e]

        def emit(engine, fn=fn, waits=waits, sem=sem):
            for s, v in waits:
                engine.wait_ge(s, v)
            fn(engine).then_inc(sem, 1)
        self.q[e].append(emit)
        self._record((e, n, e), reads, writes)

    def dma(self, qe, out, in_, reads, writes, sembuf):
        reads, writes = flat(reads), flat(writes)
        waits = self._waits(qe, reads, writes)
        if isinstance(sembuf, Tmp):
            sembuf = sembuf.bufs[0]
        if sembuf.sem is None:
            key = "d%d" % len(self.dmabufs)
            sembuf.sem = key
            self.sems[key] = self.stack.enter_context(self.nc.semaphore(key))
            self.dmabufs.append(sembuf)
        sembuf.semcnt += 16
        v = sembuf.semcnt
        sem = self.sems[sembuf.sem]

        def emit(engine, waits=waits, sem=sem, out=out, in_=in_):
            for s, vv in waits:
                engine.wait_ge(s, vv)
            engine.dma_start(out=out, in_=in_).then_inc(sem, 16)
        self.q[qe].append(emit)
        self._record((sembuf.sem, v, "dma"), reads, writes)

    def finish(self):
        waits = []
        for e in ENGS:
            if e != "sp" and self.cnt[e] > 0:
                waits.append((self.sems[e], self.cnt[e]))
        for b in self.dmabufs:
            waits.append((self.sems[b.sem], b.semcnt))

        def emit(engine, waits=waits):
            for s, v in waits:
                engine.wait_ge(s, v)
        self.q["sp"].append(emit)

    def emit_all(self):
        with self.nc.Block() as block:
            @block.tensor
            def _(eng):
                for f in self.q["pe"]:
                    f(eng)

            @block.scalar
            def _(eng):
                for f in self.q["act"]:
                    f(eng)

            @block.vector
            def _(eng):
                for f in self.q["dve"]:
                    f(eng)

            @block.gpsimd
            def _(eng):
                for f in self.q["pool"]:
                    f(eng)

            @block.sync
            def _(eng):
                for f in self.q["sp"]:
                    f(eng)


BIGW = {
    "wgu": (NL * 2 * 44 * 128, 8 * 128),
    "wdn": (NL * 2 * 8 * 128, 22 * 128),
    "win": (NL * 28 * 128, 8 * 128),
    "wgate": (NL * 8 * 4 * 128, 8 * 128),
    "wbr": (NL * 8 * 4 * 128, 2 * 128),
    "wout": (NL * 8 * 128, 8 * 128),
    "wplg": (NL * 8 * 128, 8 * 128),
    "wplp": (NL * 8 * 128, 2 * 128),
    "wglu": (NL * 2 * 128, 2 * 128),
}


class Kern:
    def __init__(self, nc, st, dbg=None):
        self.nc = nc
        self.dbg = dbg or {}
        self.S = Sched(nc, st)
        self.wi8 = 0
        self.wi22 = 0
        self.wbg = {}

    def mm(self, out, lhsT, rhs, start, stop, R, W):
        self.S.op("pe", lambda e: e.matmul(out, lhsT, rhs, start=start, stop=stop), R, W)

    def act(self, out, in_, func, R, W, **kw):
        self.S.op("act", lambda e: e.activation(out=out, in_=in_, func=func, **kw), R, W)

    def tt(self, out, a, b, op, R, W, eng="dve"):
        self.S.op(eng, lambda e: e.tensor_tensor(out=out, in0=a, in1=b, op=op), R, W)

    def ts(self, out, a, s1, op0, R, W, s2=None, op1=None, eng="dve"):
        if op1 is None:
            self.S.op(eng, lambda e: e.tensor_scalar(out=out, in0=a, scalar1=s1, scalar2=None, op0=op0), R, W)
        else:
            self.S.op(eng, lambda e: e.tensor_scalar(out=out, in0=a, scalar1=s1, scalar2=s2, op0=op0, op1=op1), R, W)

    def stt(self, out, a, s, b, op0, op1, R, W):
        self.S.op("dve", lambda e: e.scalar_tensor_tensor(out=out, in0=a, scalar=s, in1=b, op0=op0, op1=op1), R, W)

    def cp(self, out, in_, R, W, eng="pool"):
        if eng == "act":
            self.S.op("act", lambda e: e.copy(out=out, in_=in_), R, W)
        else:
            self.S.op(eng, lambda e: e.tensor_copy(out, in_), R, W)

    def wgroup(self, name, l, which=0):
        per = {"wgu": 44, "wdn": 8, "win": 28, "wgate": 32, "wbr": 32, "wout": 8, "wplg": 8, "wplp": 8, "wglu": 2}[name]
        idx = (l * 2 + which) if name in ("wgu", "wdn") else l
        key = (name, idx)
        if key not in self.wbg:
            self.wbg[key] = Buf("%s_b%d" % (name, idx), self.wb[name].t)
        return idx * per * 128, (idx + 1) * per * 128, self.wbg[key]

    def convert(self, groups):
        S = self.S
        for g in groups:
            name = g[0]
            r0g, r1g, dstb = self.wgroup(*g)
            c = BIGW[name][1]
            nn = min(max(1, 4096 // c), (r1g - r0g) // 128)
            src = self.D[name]
            for r0 in range(r0g, r1g, nn * 128):
                stg = self.cstg[self.conv_i % 2]
                self.conv_i += 1
                S.dma("pool", stg.t[:, 0:nn * c].rearrange("p (n c) -> p n c", c=c),
                      src.t[r0:r0 + nn * 128, :].rearrange("(n p) c -> p n c", p=128), [], [stg], stg)
                if self.conv_pending is not None:
                    self.conv_pending()
                self.conv_pending = (lambda stg=stg, dstb=dstb, r0=r0, nn=nn, c=c: S.dma(
                    "pool", dstb.t[r0:r0 + nn * 128, :].rearrange("(n p) c -> p n c", p=128),
                    stg.t[:, 0:nn * c].rearrange("p (n c) -> p n c", c=c), [stg], [dstb], dstb))
        if self.conv_pending is not None:
            self.conv_pending()
            self.conv_pending = None

    def loadw(self, name, tile, K):
        per = {"wgu": 44, "wdn": 8, "win": 28, "wgate": 32, "wbr": 32, "wout": 8, "wplg": 8, "wplp": 8, "wglu": 2}[name]
        src = self.wbg[(name, tile // per)]
        ap = src.t[tile * 128:(tile + 1) * 128, :].rearrange("p (k c) -> p k c", c=128)
        if K > 8:
            b = self.w22[self.wi22 % len(self.w22)]
            self.wi22 += 1
        else:
            b = self.w8[self.wi8 % len(self.w8)]
            self.wi8 += 1
        self.S.dma("sp", b.t[:, 0:K, :], ap, [src], [b], b)
        return b

    def tmp(self, slot0, nslots, shape, dt=F32):
        ap = self.scr_t[:, slot0 * 512:(slot0 + nslots) * 512]
        if dt == BF16:
            ap = ap.bitcast(BF16)
        n = 1
        for d in shape[1:]:
            n *= d
        ap = ap[:, 0:n]
        if len(shape) == 3:
            ap = ap.rearrange("p (a b) -> p a b", b=shape[2])
        elif len(shape) == 4:
            ap = ap.rearrange("p (a b c) -> p a b c", b=shape[2], c=shape[3])
        return Tmp(ap, self.slots[slot0:slot0 + nslots])

    def declare(self):
        nc, S = self.nc, self.S
        D = {}

        def din(name, shape, dt=F32):
            D[name] = Buf(name, nc.dram_tensor(name, list(shape), dt, kind="ExternalInput").ap())
            return D[name]
        self.D = D
        din("xT", [2, 128, 8, SEQ])
        din("pT", [NL, 2, 128, 2, SEQ])
        din("gains", [128, 9, 8])
        din("consts", [128, 512])
        din("s5st", [NL, 128, 8, 3])
        din("s5rep", [NL, 3, 128, 8, 128])
        din("s5bexp", [NL, 2, 128, 8, 128])
        din("s5cexp", [NL, 2, 128, 8, 128])
        din("s5db", [NL, 128, 4])
        din("posr", [2, 128, SEQ], I32)
        din("consts2", [128, 512])
        din("wsmf", [128, NL, 8, 16])
        din("cwf", [128, NL, 40])
        din("mlrowf", [128, NL, 264])
        din("gdrowf", [128, NL, 72])
        din("gmaskf", [128, 256])
        din("pc", [128, 8])
        din("cmf", [128, 2, 256])
        for n, (r, c) in BIGW.items():
            din(n, [r, c])
        self.out = Buf("out", nc.dram_tensor("out", [2, 128, 8, SEQ], F32, kind="ExternalOutput").ap())
        self.wb = {}
        for n, (r, c) in BIGW.items():
            self.wb[n] = Buf(n + "_b", nc.dram_tensor(n + "_b", [r, c], BF16, kind="Internal").ap())
        self.s5f_d = Buf("s5f_d", nc.dram_tensor("s5f_d", [NL, 128, 3104], F32, kind="Internal").ap())
        self.s5b_d = Buf("s5b_d", nc.dram_tensor("s5b_d", [NL, 128, 4352], BF16, kind="Internal").ap())
        if "yin" in self.dbg:
            din("yin", [8, 128, 4, 2, NT], BF16)
        if "dumpy" in self.dbg:
            self.dumpy = Buf("dumpy", nc.dram_tensor("dumpy", [8, 128, 4, 2, NT], BF16, kind="ExternalOutput").ap())
        if "dump" in self.dbg:
            self.dump = Buf("dump", nc.dram_tensor("dump", self.dbg["dump"], F32, kind="ExternalOutput").ap())

        self.h = S.sb("h", [128, 8, NT], F32)
        self.u = S.sb("u", [128, 8, NT], BF16)
        NS = 24
        self.scr_t = S.sb("scr", [128, NS * 512], F32).t
        self.slots = [Buf("slot%d" % i) for i in range(NS)]
        self.actb = self.tmp(0, 11, [128, 22, NT], BF16)
        self.rstd = S.sb("rstd", [128, NT], F32)
        self.sg = [S.sb("sg%d" % i, [128, NT], F32) for i in range(2)]
        self.sg2 = [S.sb("sg2%d" % i, [128, NT], F32) for i in range(2)]
        self.macc = self.tmp(12, 1, [128, NT])
        self.ho = [self.tmp(13 + i, 1, [128, NT]) for i in range(2)]
        self.w8 = [S.sb("w8_%d" % i, [128, 8, 128], BF16) for i in range(6)]
        self.w22 = [S.sb("w22_%d" % i, [128, 22, 128], BF16) for i in range(2)]
        self.y = S.sb("y", [128, 4, 2, NT], BF16)
        self.pf = self.tmp(15, 2, [128, 2, NT])
        self.pb = self.tmp(17, 1, [128, 2, NT], BF16)
        self.gains = S.sb("gains", [128, 9, 8], F32)
        self.cst = S.sb("cst", [128, 8], F32)
        self.consts = S.sb("consts", [128, 512], F32)
        self.pc = S.sb("pc", [128, 8], F32)
        self.consts2 = S.sb("consts2", [128, 512], F32)
        self.wsm = S.sb("wsm", [128, NL, 8, 16], BF16)
        self.cwb = S.sb("cwb", [128, NL, 40], F32)
        self.mlrow = S.sb("mlrow", [128, NL, 264], F32)
        self.gdrow = S.sb("gdrow", [128, NL, 72], F32)
        self.mltail = [S.sb("mltail%d" % l, [128, 4, 3], F32) for l in range(NL)]
        self.gdtail = [S.sb("gdtail%d" % l, [128, 6, 3], F32) for l in range(NL)]
        self.mlC = [S.sb("mlC%d" % l, [128, 4, 66], F32) for l in range(NL)]
        self.mlm = [S.sb("mlm%d" % l, [128, 4], F32) for l in range(NL)]
        self.gdS = [S.sb("gdS%d" % l, [128, 4, 64], F32) for l in range(NL)]
        self.gmask = S.sb("gmask", [128, 256], F32)
        self.negones = S.sb("negones", [128, 64], F32)
        self.cm = S.sb("cm", [128, 2, 256], BF16)
        self.ident_bf = S.sb("ident_bf", [128, 128], BF16)
        self.kcache = [S.sb("kcache%d" % l, [128, 2, SEQ], BF16) for l in range(NL)]
        self.vcache = [S.sb("vcache%d" % l, [128, 16, 256], BF16) for l in range(NL)]
        self.kmean = [S.sb("kmean%d" % l, [128, 2, 8], F32) for l in range(NL)]
        self.s5f = S.sb("s5f", [128, 3104], F32)
        self.s5b = S.sb("s5b", [128, 4352], BF16)
        f = self.s5f.t
        self.s5F = {"CS": f[:, 0:2048].rearrange("p (a b c) -> p a b c", b=2, c=128),
                    "R": f[:, 2048:3072].rearrange("p (a b) -> p a b", b=128),
                    "E128": f[:, 3072:3088].rearrange("p (a b) -> p a b", b=2),
                    "rcol": f[:, 3088:3096], "dbg": f[:, 3096:3100]}
        b = self.s5b.t
        self.s5B = {"BT": b[:, 0:2048].rearrange("p (a b c) -> p a b c", b=2, c=128),
                    "CT": b[:, 2048:4096].rearrange("p (a b c) -> p a b c", b=2, c=128),
                    "Dg": b[:, 4096:4352].rearrange("p (a b) -> p a b", b=128)}
        self.s5state = [S.sb("s5st%d" % l, [128, 2, 8], F32) for l in range(NL)]
        self.ones_bf = S.sb("ones_bf", [128, 128], BF16)
        self.P = [S.ps("P%d" % i, [128, NT], F32) for i in range(8)]
        self.cstg = [S.sb("cstg%d" % i, [128, 4096], BF16) for i in range(2)]

    def setup(self):
        S = self.S
        S.op("pool", lambda e: e.memset(self.ones_bf.t[:], 1.0), [], [self.ones_bf])
        S.op("pool", lambda e: e.memset(self.cst.t[:, 0:1], 1e-6), [], [self.cst])
        S.op("pool", lambda e: e.memset(self.cst.t[:, 1:2], -PI), [], [self.cst])
        S.dma("sp", self.pc.t[:], self.D["pc"].t, [], [self.pc], self.pc)
        S.op("pool", lambda e: e.memset(self.cst.t[:, 3:4], 1.0), [], [self.cst])
        S.dma("sp", self.consts2.t[:], self.D["consts2"].t, [], [self.consts2], self.consts2)
        S.dma("sp", self.cwb.t[:], self.D["cwf"].t, [], [self.cwb], self.cwb)
        S.dma("sp", self.mlrow.t[:], self.D["mlrowf"].t, [], [self.mlrow], self.mlrow)
        S.dma("sp", self.gdrow.t[:], self.D["gdrowf"].t, [], [self.gdrow], self.gdrow)
        S.dma("sp", self.gmask.t[:], self.D["gmaskf"].t, [], [self.gmask], self.gmask)
        S.op("pool", lambda e: e.memset(self.negones.t[:], -1.0), [], [self.negones])
        wsf = self.tmp(13, 1, [128, NL, 8, 16])
        S.dma("sp", wsf.t, self.D["wsmf"].t, [], [wsf], wsf)
        self.cp(self.wsm.t[:], wsf.t, [wsf], [self.wsm], eng="dve")
        cmf = self.tmp(12, 1, [128, 2, 256])
        S.dma("sp", cmf.t, self.D["cmf"].t, [], [cmf], cmf)
        self.cp(self.cm.t[:], cmf.t, [cmf], [self.cm], eng="dve")
        for l in range(NL):
            S.op("pool", lambda e, l=l: e.memset(self.kmean[l].t[:], 0.0), [], [self.kmean[l]])
        S.dma("sp", self.gains.t[:], self.D["gains"].t, [], [self.gains], self.gains)
        S.dma("sp", self.consts.t[:], self.D["consts"].t, [], [self.consts], self.consts)
        self.cp(self.ident_bf.t[:], self.consts.t[:, 128:256], [self.consts], [self.ident_bf], eng="dve")
        self.conv_i = 0
        self.conv_pending = None
        self.convert([("wgu", 0, 0), ("wdn", 0, 0), ("win", 0), ("wgate", 0), ("wbr", 0), ("wout", 0), ("wglu", 0)])
        for l in range(NL):
            self.s5_setup(l)

    def norm(self, gcol):
        h, u, S = self.h, self.u, self.S
        pss, rstd = self.P[6], self.rstd
        self.act(u.t[:], h.t[:], AF.Square, [h], [u])
        for k in range(8):
            self.mm(pss.t[:], self.ones_bf.t[:], u.t[:, k, :], k == 0, k == 7, [u, self.ones_bf], [pss])
        self.act(rstd.t[:], pss.t[:], AF.Sqrt, [pss, self.cst], [rstd], scale=1.0 / 1024, bias=self.cst.t[:, 0:1])
        S.op("dve", lambda e: e.reciprocal(rstd.t[:], rstd.t[:]), [rstd], [rstd])
        for k in range(8):
            self.stt(u.t[:, k, :], h.t[:, k, :], self.gains.t[:, gcol, k:k + 1], rstd.t[:], ALU.mult, ALU.mult,
                     [h, rstd, self.gains], [u])

    def ffn(self, l, which):
        h, u, actb = self.h, self.u, self.actb
        self.norm(l * 4 + (0 if which == 0 else 2))
        base = (l * 2 + which) * 44
        for fc in range(22):
            wg = self.loadw("wgu", base + 2 * fc, 8)
            wu = self.loadw("wgu", base + 2 * fc + 1, 8)
            pg, pu = self.P[fc % 2], self.P[2 + fc % 2]
            sg = self.sg[fc % 2]
            for k in range(8):
                self.mm(pg.t[:], wg.t[:, k, :], u.t[:, k, :], k == 0, k == 7, [wg, u], [pg])
            for k in range(8):
                self.mm(pu.t[:], wu.t[:, k, :], u.t[:, k, :], k == 0, k == 7, [wu, u], [pu])
            self.act(sg.t[:], pg.t[:], AF.Silu, [pg], [sg])
            self.tt(actb.t[:, fc, :], sg.t[:], pu.t[:], ALU.mult, [sg, pu], [actb])
        base = (l * 2 + which) * 8
        for oc in range(8):
            wd = self.loadw("wdn", base + oc, 22)
            po = self.P[4 + oc % 2]
            for fc in range(22):
                self.mm(po.t[:], wd.t[:, fc, :], actb.t[:, fc, :], fc == 0, fc == 21, [wd, actb], [po])
            self.stt(h.t[:, oc, :], po.t[:], 0.5, h.t[:, oc, :], ALU.mult, ALU.add, [po, h], [h])

    def ple(self, l, s, t0):
        h, u, S = self.h, self.u, self.S
        self.norm(l * 4 + 3)
        S.dma("sp", self.pf.t[:], self.D["pT"].t[l, s, :, :, t0:t0 + NT], [], [self.pf], self.pf)
        self.cp(self.pb.t[:], self.pf.t[:], [self.pf], [self.pb], eng="pool")
        for oc in range(8):
            wg = self.loadw("wplg", l * 8 + oc, 8)
            wp = self.loadw("wplp", l * 8 + oc, 2)
            pa, pg = self.P[oc % 2], self.P[2 + oc % 2]
            sg, sg2 = self.sg[oc % 2], self.sg2[oc % 2]
            for k in range(2):
                self.mm(pa.t[:], wp.t[:, k, :], self.pb.t[:, k, :], k == 0, k == 1, [wp, self.pb], [pa])
            for k in range(8):
                self.mm(pg.t[:], wg.t[:, k, :], u.t[:, k, :], k == 0, k == 7, [wg, u], [pg])
            self.act(sg.t[:], pg.t[:], AF.Sigmoid, [pg], [sg])
            self.tt(sg2.t[:], sg.t[:], pa.t[:], ALU.mult, [sg, pa], [sg2])
            self.tt(h.t[:, oc, :], h.t[:, oc, :], sg2.t[:], ALU.add, [h, sg2], [h], eng="pool")

    def merge(self, l):
        h, u, y, actb = self.h, self.u, self.y, self.actb
        macc = self.macc
        for oc in range(8):
            for b in range(4):
                wg = self.loadw("wgate", (l * 8 + oc) * 4 + b, 8)
                wbr = self.loadw("wbr", (l * 8 + oc) * 4 + b, 2)
                pg, pbr = self.P[b % 2], self.P[2 + b % 2]
                sg, sg2 = self.sg[b % 2], self.sg2[b % 2]
                for k in range(8):
                    self.mm(pg.t[:], wg.t[:, k, :], u.t[:, k, :], k == 0, k == 7, [wg, u], [pg])
                for k in range(2):
                    self.mm(pbr.t[:], wbr.t[:, k, :], y.t[:, b, k, :], k == 0, k == 1, [wbr, y], [pbr])
                self.act(sg.t[:], pg.t[:], AF.Sigmoid, [pg], [sg])
                if b == 0:
                    self.tt(macc.t[:], sg.t[:], pbr.t[:], ALU.mult, [sg, pbr], [macc])
                else:
                    self.tt(sg2.t[:], sg.t[:], pbr.t[:], ALU.mult, [sg, pbr], [sg2])
                    if b < 3:
                        self.tt(macc.t[:], macc.t[:], sg2.t[:], ALU.add, [macc, sg2], [macc], eng="pool")
                    else:
                        self.tt(actb.t[:, oc, :], macc.t[:], sg2.t[:], ALU.add, [macc, sg2], [actb], eng="pool")
        for oc in range(8):
            wo = self.loadw("wout", l * 8 + oc, 8)
            po = self.P[4 + oc % 2]
            for k in range(8):
                self.mm(po.t[:], wo.t[:, k, :], actb.t[:, k, :], k == 0, k == 7, [wo, actb], [po])
            self.tt(h.t[:, oc, :], h.t[:, oc, :], po.t[:], ALU.add, [h, po], [h])

    def final(self, s, t0):
        h, S = self.h, self.S
        pss, rstd = self.P[6], self.rstd
        u = self.u
        self.act(u.t[:], h.t[:], AF.Square, [h], [u])
        for k in range(8):
            self.mm(pss.t[:], self.ones_bf.t[:], u.t[:, k, :], k == 0, k == 7, [u, self.ones_bf], [pss])
        self.act(rstd.t[:], pss.t[:], AF.Sqrt, [pss, self.cst], [rstd], scale=1.0 / 1024, bias=self.cst.t[:, 0:1])
        S.op("dve", lambda e: e.reciprocal(rstd.t[:], rstd.t[:]), [rstd], [rstd])
        for k in range(8):
            ho = self.ho[k % 2]
            self.stt(ho.t[:], h.t[:, k, :], self.gains.t[:, 8, k:k + 1], rstd.t[:], ALU.mult, ALU.mult,
                     [h, rstd, self.gains], [ho])
            S.dma("sp", self.out.t[s, :, k, t0:t0 + NT], ho.t[:], [ho], [self.out], ho)

    def dump_h(self, idx):
        S = self.S
        S.dma("sp", self.dump.t[idx], self.h.t[:], [self.h], [self.dump], self.h)


    def dd(self, name, src, R, shape, dt=F32):
        if name not in self.dbg.get("dd", ()):
            return
        b = Buf(name, self.nc.dram_tensor("dd_" + name, list(shape), dt, kind="ExternalOutput").ap())
        self.S.dma("sp", b.t, src, R, [b], b)

    def frac2pi(self, out, x, shift, tB, R, W):
        MAG = 12582912.0
        self.ts(out, x, 1.0 / (2 * PI), ALU.mult, R, W, s2=shift / (2 * PI), op1=ALU.add)
        self.ts(tB, out, MAG, ALU.add, R, W)
        self.ts(tB, tB, -MAG, ALU.add, R, W)
        self.tt(out, out, tB, ALU.subtract, R, W)

    def s5_disc(self, lr, li, ldt, T, TB, want_z):
        R = TB + [self.s5in]
        W = TB
        self.act(T[0], ldt, AF.Exp, R, W)
        self.tt(T[5], lr, T[0], ALU.mult, R, W)
        self.act(T[1], T[5], AF.Exp, R, W)
        self.tt(T[2], li, T[0], ALU.mult, R, W)
        if not want_z:
            self.frac2pi(T[5], T[2], 0.0, T[8], R, W)
            self.ts(T[2], T[5], 2 * PI, ALU.mult, R, W)
            return T[1], T[2], None, None
        self.frac2pi(T[5], T[2], 0.0, T[8], R, W)
        self.act(T[3], T[5], AF.Sin, R, W, scale=2 * PI)
        self.frac2pi(T[5], T[2], 0.5 * PI, T[8], R, W)
        self.act(T[4], T[5], AF.Sin, R, W, scale=2 * PI)
        self.tt(T[4], T[4], T[1], ALU.mult, R, W)
        self.tt(T[3], T[3], T[1], ALU.mult, R, W)
        self.ts(T[5], T[4], -1.0, ALU.add, R, W)
        self.tt(T[8], lr, lr, ALU.mult, R, W)
        self.tt(T[0], li, li, ALU.mult, R, W)
        self.tt(T[8], T[8], T[0], ALU.add, R, W)
        self.S.op("dve", lambda e: e.reciprocal(T[8], T[8]), R, W)
        self.tt(T[0], T[5], lr, ALU.mult, R, W)
        self.tt(T[6], T[3], li, ALU.mult, R, W)
        self.tt(T[6], T[6], T[0], ALU.add, R, W)
        self.tt(T[6], T[6], T[8], ALU.mult, R, W)
        self.tt(T[0], T[3], lr, ALU.mult, R, W)
        self.tt(T[7], T[5], li, ALU.mult, R, W)
        self.tt(T[7], T[0], T[7], ALU.subtract, R, W)
        self.tt(T[7], T[7], T[8], ALU.mult, R, W)
        return T[1], T[2], T[6], T[7]

    def s5_setup(self, l):
        S, D = self.S, self.D
        s5f, s5b, cst = self.s5f, self.s5b, self.cst
        F = self.s5F
        stt_ = self.tmp(12, 1, [128, 8, 3])
        self.s5in = stt_.bufs[0]
        S.dma("sp", stt_.t, D["s5st"].t[l], [], [stt_], stt_)
        T9 = self.tmp(13, 1, [128, 9, 8])
        T = [T9.t[:, i, :] for i in range(9)]
        mag, th, _, _ = self.s5_disc(stt_.t[:, :, 0], stt_.t[:, :, 1], stt_.t[:, :, 2], T, [T9.bufs[0]], False)
        R9 = [T9]
        X = self.tmp(14, 2, [128, 8, 128])
        Y = self.tmp(16, 2, [128, 8, 128])
        Z = self.tmp(18, 2, [128, 8, 128])
        jrow = self.consts.t[:, 0:128]
        for sc in range(8):
            self.ts(X.t[:, sc, :], jrow, th[:, sc:sc + 1], ALU.mult, R9 + [self.consts], [X])
        self.frac2pi(Y.t, X.t, 0.0, Z.t, [X, Y, Z], [Y, Z])
        self.act(F["CS"][:, :, 1, :], Y.t, AF.Sin, [Y], [s5f], scale=2 * PI)
        self.frac2pi(Y.t, X.t, 0.5 * PI, Z.t, [X, Y, Z], [Y, Z])
        self.act(F["CS"][:, :, 0, :], Y.t, AF.Sin, [Y], [s5f], scale=2 * PI)
        self.ts(T[3], th, 128.0, ALU.mult, R9, R9)
        self.frac2pi(T[4], T[3], 0.0, T[5], R9, R9)
        self.act(F["E128"][:, :, 1], T[4], AF.Sin, R9, [s5f], scale=2 * PI)
        self.frac2pi(T[4], T[3], 0.5 * PI, T[5], R9, R9)
        self.act(F["E128"][:, :, 0], T[4], AF.Sin, R9, [s5f], scale=2 * PI)
        self.cp(F["rcol"], mag, R9, [s5f], eng="dve")
        S.op("dve", lambda e: e.memset(F["R"], 0.0), [], [s5f])
        ones = self.consts.t[:, 384:511]
        for sc in range(8):
            self.ts(F["R"][:, sc, 1:128], ones, mag[:, sc:sc + 1], ALU.mult, R9 + [self.consts], [s5f])
        S.dma("sp", F["dbg"], D["s5db"].t[l], [], [s5f], s5f)
        for hh in range(2):
            prm = self.tmp(12, 3, [128, 3, 512])
            self.s5in = prm.bufs[0]
            S.dma("sp", prm.t.rearrange("p a (s c) -> p a s c", c=128),
                  D["s5rep"].t[l, :, :, 4 * hh:4 * hh + 4, :].rearrange("a p s c -> p a s c"), [], [prm], prm)
            TT_ = self.tmp(15, 9, [128, 9, 512])
            T = [TT_.t[:, i, :] for i in range(9)]
            TB = TT_.bufs + prm.bufs[1:]
            _, _, zr, zi = self.s5_disc(prm.t[:, 0, :], prm.t[:, 1, :], prm.t[:, 2, :], T, TB, True)
            bx = self.tmp(12, 2, [128, 2, 512])
            S.dma("sp", bx.t.rearrange("p a (s c) -> p a s c", c=128),
                  D["s5bexp"].t[l, :, :, 4 * hh:4 * hh + 4, :].rearrange("a p s c -> p a s c"), [], [bx], bx)
            RR = TB + bx.bufs
            self.tt(T[0], zr, bx.t[:, 0, :], ALU.mult, RR, TB)
            self.tt(T[1], zi, bx.t[:, 1, :], ALU.mult, RR, TB)
            self.tt(F32v(self, "BTre", hh), T[0], T[1], ALU.subtract, RR, [s5b])
            self.tt(T[0], zr, bx.t[:, 1, :], ALU.mult, RR, TB)
            self.tt(T[1], zi, bx.t[:, 0, :], ALU.mult, RR, TB)
            self.tt(F32v(self, "BTim", hh), T[0], T[1], ALU.add, RR, [s5b])
            cx = self.tmp(12, 2, [128, 2, 512])
            S.dma("sp", cx.t.rearrange("p a (s c) -> p a s c", c=128),
                  D["s5cexp"].t[l, :, :, 4 * hh:4 * hh + 4, :].rearrange("a p s c -> p a s c"), [], [cx], cx)
            self.cp(F32v(self, "CTre", hh), cx.t[:, 0, :], [cx], [s5b], eng="dve")
            self.ts(F32v(self, "CTim", hh), cx.t[:, 1, :], -1.0, ALU.mult, [cx], [s5b])
        ident = self.consts.t[:, 128:256]
        for kc in range(2):
            self.ts(self.s5B["Dg"][:, kc, :], ident, F["dbg"][:, kc:kc + 1], ALU.mult, [s5f, self.consts], [s5b])
        S.dma("sp", self.s5f_d.t[l], s5f.t[:], [s5f], [self.s5f_d], s5f)
        S.dma("sp", self.s5b_d.t[l], s5b.t[:], [s5b], [self.s5b_d], s5b)

    def s5_fwd(self, l, s, tb):
        S, u, y = self.S, self.u, self.y
        s5f, s5b = self.s5f, self.s5b
        F, B = self.s5F, self.s5B
        st = self.s5state[l]
        S.dma("sp", s5f.t[:], self.s5f_d.t[l], [self.s5f_d], [s5f], s5f)
        S.dma("sp", s5b.t[:], self.s5b_d.t[l], [self.s5b_d], [s5b], s5b)
        us5 = self.tmp(12, 1, [128, 2, NT], BF16)
        for kc in range(2):
            w = self.loadw("win", l * 28 + 16 + kc, 8)
            pp = self.P[kc]
            for k in range(8):
                self.mm(pp.t[:], w.t[:, k, :], u.t[:, k, :], k == 0, k == 7, [w, u], [pp])
            self.cp(us5.t[:, kc, :], pp.t[:], [pp], [us5], eng="act")
        A = self.tmp(13, 1, [128, 4, 128])
        Bt = self.tmp(14, 1, [128, 4, 128])
        bh = [self.tmp(15, 2, [128, 8, 128]), self.tmp(17, 2, [128, 8, 128])]
        xh = [self.tmp(19, 2, [128, 8, 128]), self.tmp(21, 2, [128, 8, 128])]
        xb = [self.tmp(23, 1, [128, 8, 128], BF16), self.tmp(0, 1, [128, 8, 128], BF16)]
        ini = self.tmp(1, 1, [128, 4, 8])
        A2 = self.tmp(2, 1, [128, 4, 128])
        B2 = self.tmp(3, 1, [128, 4, 128])
        ypre = [self.P[4], self.P[5]]
        CS = F["CS"]
        if tb == 0:
            S.op("dve", lambda e: e.memset(st.t[:], 0.0), [], [st])
        for sub in range(4):
            c0 = sub * 128
            for sc in range(8):
                for ri in range(2):
                    pp = self.P[2 * ri + sc // 4]
                    self.mm(pp.t[:, (sc % 4) * 128:(sc % 4 + 1) * 128], B["BT"][:, sc, ri, :],
                            us5.t[:, sc // 4, c0:c0 + 128], True, True, [s5b, us5], [pp])
            for hh in range(2):
                c = CS[:, 4 * hh:4 * hh + 4, 0, :]
                sn = CS[:, 4 * hh:4 * hh + 4, 1, :]
                pre = self.P[hh].t[:].rearrange("p (a b) -> p a b", b=128)
                pim = self.P[2 + hh].t[:].rearrange("p (a b) -> p a b", b=128)
                self.tt(A.t, pre, c, ALU.mult, [self.P[hh], s5f], [A])
                self.tt(Bt.t, pim, sn, ALU.mult, [self.P[2 + hh], s5f], [Bt])
                self.tt(bh[0].t[:, 4 * hh:4 * hh + 4, :], A.t, Bt.t, ALU.add, [A, Bt], [bh[0]])
                self.tt(A.t, pim, c, ALU.mult, [self.P[2 + hh], s5f], [A])
                self.tt(Bt.t, pre, sn, ALU.mult, [self.P[hh], s5f], [Bt])
                self.tt(bh[1].t[:, 4 * hh:4 * hh + 4, :], A.t, Bt.t, ALU.subtract, [A, Bt], [bh[1]])
            if not (tb == 0 and sub == 0):
                i0, i1, i2, i3 = (ini.t[:, i, :] for i in range(4))
                c1, s1 = F["E128"][:, :, 0], F["E128"][:, :, 1]
                self.tt(i0, c1, st.t[:, 0, :], ALU.mult, [s5f, st], [ini])
                self.tt(i1, s1, st.t[:, 1, :], ALU.mult, [s5f, st], [ini])
                self.tt(i0, i0, i1, ALU.subtract, [ini], [ini])
                self.tt(i2, c1, st.t[:, 1, :], ALU.mult, [s5f, st], [ini])
                self.tt(i3, s1, st.t[:, 0, :], ALU.mult, [s5f, st], [ini])
                self.tt(i2, i2, i3, ALU.add, [ini], [ini])
                self.tt(i0, i0, F["rcol"], ALU.mult, [ini, s5f], [ini])
                self.tt(i2, i2, F["rcol"], ALU.mult, [ini, s5f], [ini])
                self.tt(bh[0].t[:, :, 0], bh[0].t[:, :, 0], i0, ALU.add, [bh[0], ini], [bh[0]])
                self.tt(bh[1].t[:, :, 0], bh[1].t[:, :, 0], i2, ALU.add, [bh[1], ini], [bh[1]])
            Rf = F["R"].rearrange("p a b -> p (a b)")
            for ri in range(2):
                S.op("dve", lambda e, ri=ri: e.tensor_tensor_scan(
                    out=xh[ri].t.rearrange("p a b -> p (a b)"), data0=Rf,
                    data1=bh[ri].t.rearrange("p a b -> p (a b)"), initial=0.0, op0=ALU.mult, op1=ALU.add),
                    [bh[ri], s5f], [xh[ri]])
                self.cp(st.t[:, ri, :], xh[ri].t[:, :, 127], [xh[ri]], [st], eng="dve")
            for hh in range(2):
                c = CS[:, 4 * hh:4 * hh + 4, 0, :]
                sn = CS[:, 4 * hh:4 * hh + 4, 1, :]
                hs = slice(4 * hh, 4 * hh + 4)
                self.tt(A2.t, xh[0].t[:, hs, :], c, ALU.mult, [xh[0], s5f], [A2], eng="pool")
                self.tt(B2.t, xh[1].t[:, hs, :], sn, ALU.mult, [xh[1], s5f], [B2], eng="pool")
                self.tt(xb[0].t[:, hs, :], A2.t, B2.t, ALU.subtract, [A2, B2], [xb[0]], eng="pool")
                self.tt(A2.t, xh[1].t[:, hs, :], c, ALU.mult, [xh[1], s5f], [A2], eng="pool")
                self.tt(B2.t, xh[0].t[:, hs, :], sn, ALU.mult, [xh[0], s5f], [B2], eng="pool")
                self.tt(xb[1].t[:, hs, :], A2.t, B2.t, ALU.add, [A2, B2], [xb[1]], eng="pool")
            for kc in range(2):
                pp = ypre[kc]
                o = pp.t[:, c0:c0 + 128]
                n = 0
                for sc in range(4 * kc, 4 * kc + 4):
                    for ri in range(2):
                        self.mm(o, B["CT"][:, sc, ri, :], xb[ri].t[:, sc, :], n == 0, False, [s5b, xb[ri]], [pp])
                        n += 1
                self.mm(o, B["Dg"][:, kc, :], us5.t[:, kc, c0:c0 + 128], False, True, [s5b, us5], [pp])
        yg = self.tmp(13, 2, [128, 2, NT])
        t1 = self.tmp(15, 2, [128, 2, NT])
        ygb = self.tmp(17, 1, [128, 2, NT], BF16)
        for kc in range(2):
            self.cp(yg.t[:, kc, :], ypre[kc].t[:], [ypre[kc]], [yg], eng="act")
        self.act(t1.t, yg.t, AF.Square, [yg], [t1])
        self.ts(t1.t, t1.t, 0.044715, ALU.mult, [t1], [t1], s2=1.0, op1=ALU.add)
        self.tt(t1.t, t1.t, yg.t, ALU.mult, [t1, yg], [t1])
        self.act(t1.t, t1.t, AF.Sigmoid, [t1], [t1], scale=1.5957691216)
        self.tt(yg.t, yg.t, t1.t, ALU.mult, [t1, yg], [yg])
        self.cp(ygb.t, yg.t, [yg], [ygb], eng="pool")
        for oc in range(2):
            w = self.loadw("wglu", l * 2 + oc, 2)
            pp = self.P[6 + oc]
            for k in range(2):
                self.mm(pp.t[:], w.t[:, k, :], ygb.t[:, k, :], k == 0, k == 1, [w, ygb], [pp])
            self.act(t1.t[:, oc, :], pp.t[:], AF.Sigmoid, [pp, s5f], [t1], bias=F["dbg"][:, 2 + oc:3 + oc])
            self.tt(y.t[:, 2, oc, :], yg.t[:, oc, :], t1.t[:, oc, :], ALU.mult, [yg, t1], [y])


    def moba_fwd(self, l, s, tb):
        S, u, y, D = self.S, self.u, self.y, self.D
        t0 = tb * NT
        kc_, vc_, km = self.kcache[l], self.vcache[l], self.kmean[l]
        pc, consts = self.pc, self.consts
        cosT = self.tmp(0, 1, [128, NT])
        sinT = self.tmp(1, 1, [128, NT])
        posi = self.tmp(2, 1, [128, NT])
        ang = self.tmp(3, 1, [128, NT])
        fr = self.tmp(4, 1, [128, NT])
        fb = self.tmp(5, 1, [128, NT])
        qf = [self.tmp(6, 1, [128, NT]), self.tmp(7, 1, [128, NT])]
        kf = [self.tmp(8, 1, [128, NT]), self.tmp(9, 1, [128, NT])]
        t1 = self.tmp(10, 1, [128, NT])
        t2 = self.tmp(12, 1, [128, NT])
        qb_ = self.tmp(13, 1, [128, 2, NT], BF16)
        sm = self.tmp(14, 1, [128, 512])
        gm = sm.t[:, 0:32].rearrange("p (a b) -> p a b", b=8)
        mx = sm.t[:, 32:64].rearrange("p (a b) -> p a b", b=8)
        mnegb = sm.t[:, 64:128].bitcast(BF16).rearrange("p (a b) -> p a b", b=32)
        et = [self.tmp(15, 1, [128, 1024], BF16)]
        ets = [et[0].t[:, 0:512].rearrange("p (a b) -> p a b", b=256), et[0].t[:, 512:1024].rearrange("p (a b) -> p a b", b=256)]
        rden = self.tmp(16, 1, [128, 2, 256])
        S.dma("sp", posi.t.bitcast(I32), D["posr"].t[s, :, t0:t0 + NT], [], [posi], posi)
        self.cp(ang.t, posi.t.bitcast(I32), [posi], [ang], eng="dve")
        self.ts(ang.t, ang.t, pc.t[:, 0:1], ALU.mult, [ang, pc], [ang])
        self.frac2pi(fr.t, ang.t, 0.5 * PI, fb.t, [ang, fr, fb], [fr, fb])
        self.act(cosT.t, fr.t, AF.Sin, [fr], [cosT], scale=2 * PI)
        self.frac2pi(fr.t, ang.t, 0.0, fb.t, [ang, fr, fb], [fr, fb])
        self.act(sinT.t, fr.t, AF.Sin, [fr, pc], [sinT], scale=pc.t[:, 2:3])
        for c in range(2):
            for (dst, base) in ((qf[c], 18), (kf[c], 20)):
                w1 = self.loadw("win", l * 28 + base + c, 8)
                w2 = self.loadw("win", l * 28 + base + 6 + c, 8)
                p1, p2 = self.P[0], self.P[1]
                for k in range(8):
                    self.mm(p1.t[:], w1.t[:, k, :], u.t[:, k, :], k == 0, k == 7, [w1, u], [p1])
                for k in range(8):
                    self.mm(p2.t[:], w2.t[:, k, :], u.t[:, k, :], k == 0, k == 7, [w2, u], [p2])
                self.tt(t1.t, p1.t[:], cosT.t, ALU.mult, [p1, cosT], [t1])
                self.tt(t2.t, p2.t[:], sinT.t, ALU.mult, [p2, sinT], [t2])
                self.tt(dst.t, t1.t, t2.t, ALU.add, [t1, t2], [dst], eng="pool")
            self.cp(qb_.t[:, c, :], qf[c].t, [qf[c]], [qb_], eng="pool")
            self.cp(kc_.t[:, c, t0:t0 + NT], kf[c].t, [kf[c]], [kc_], eng="pool")
            S.op("dve", lambda e, c=c: e.tensor_reduce(out=km.t[:, c, 2 * tb:2 * tb + 2],
                                                      in_=kf[c].t.rearrange("p (a b) -> p a b", b=256),
                                                      axis=AX.X, op=ALU.add), [kf[c]], [km])
        self.ts(km.t[:, :, 2 * tb:2 * tb + 2], km.t[:, :, 2 * tb:2 * tb + 2], 1.0 / 256, ALU.mult, [km], [km])
        wv = [self.loadw("win", l * 28 + 22 + i, 8) for i in range(2)]
        for tt_ in range(4):
            pv = self.P[2 + tt_ % 2]
            for i in range(2):
                for k in range(8):
                    self.mm(pv.t[:, i * 128:(i + 1) * 128], u.t[:, k, tt_ * 128:(tt_ + 1) * 128], wv[i].t[:, k, :],
                            k == 0, k == 7, [wv[i], u], [pv])
            self.cp(vc_.t[:, tb * 4 + tt_, :], pv.t[:, 0:256], [pv], [vc_], eng="act")
        if l == 0 and tb == self.dbg.get("ddtb", 0):
            self.dd("cosT", cosT.t, [cosT], [128, NT])
            self.dd("sinT", sinT.t, [sinT], [128, NT])
            self.dd("qf0", qf[0].t, [qf[0]], [128, NT])
            self.dd("kf1", kf[1].t, [kf[1]], [128, NT])
            self.dd("km", km.t[:], [km], [128, 2, 8])
            self.dd("vc", vc_.t[:, tb * 4, :], [vc_], [128, 256], BF16)
        if tb > 0 or True:
            for qt in range(4):
                qblk = 2 * tb + qt // 2
                if qblk == 0:
                    continue
                pg = self.P[6]
                for h in range(4):
                    c, off = h // 2, (h % 2) * 64
                    self.mm(pg.t[:, h * 8:h * 8 + 8], qf[c].t[off:off + 64, qt * 128:(qt + 1) * 128],
                            km.t[off:off + 64, c, :], True, True, [qf[c], km], [pg])
                vm = consts.t[:, 256 + qblk * 8:256 + qblk * 8 + 8].unsqueeze(1).broadcast_to([128, 4, 8])
                self.tt(gm, pg.t[:, 0:32].rearrange("p (a b) -> p a b", b=8), vm, ALU.add, [pg, consts], [sm])
                for h in range(4):
                    S.op("dve", lambda e, h=h: e.max(out=mx[:, h, :], in_=gm[:, h, :]), [sm], [sm])
                for h in range(4):
                    self.ts(gm[:, h, :], gm[:, h, :], mx[:, h, 2:3], ALU.is_ge, [sm], [sm], s2=30000.0, op1=ALU.mult)
                self.ts(mnegb[:, qt, :], sm.t[:, 0:32], -30000.0, ALU.add, [sm], [sm])
                if l == 0 and tb == self.dbg.get("ddtb", 0) and qt == 3:
                    self.dd("sm", sm.t[:, 0:128], [sm], [128, 128])
        it = 0
        for c in range(2):
            for j in range(2):
                qblk = 2 * tb + j
                nkt = 2 * (qblk + 1)
                pacc, pden = self.P[2 + 2 * (it % 2)], self.P[3 + 2 * (it % 2)]
                it += 1
                qs = slice(j * 256, (j + 1) * 256)
                for kt in range(nkt):
                    n = kt // 2
                    ps_ = self.P[kt % 2]
                    e_ = ets[kt % 2]
                    for hh in range(2):
                        h, off = 2 * c + hh, hh * 64
                        o = ps_.t[:, hh * 256:(hh + 1) * 256]
                        self.mm(o, kc_.t[off:off + 64, c, kt * 128:(kt + 1) * 128], qb_.t[off:off + 64, c, qs],
                                True, False, [kc_, qb_], [ps_])
                        if n < qblk:
                            for q2 in range(2):
                                qt = 2 * j + q2
                                lh = mnegb[:, qt, h * 8 + n:h * 8 + n + 1].broadcast_to([128, 128])
                                self.mm(ps_.t[:, hh * 256 + q2 * 128:hh * 256 + (q2 + 1) * 128], lh, self.ident_bf.t[:],
                                        False, True, [sm, self.ident_bf], [ps_])
                        else:
                            self.mm(o, self.ident_bf.t[:], self.cm.t[:, kt % 2, :], False, True,
                                    [self.ident_bf, self.cm], [ps_])
                    if l == 0 and tb == self.dbg.get("ddtb", 0) and c == 0 and j == 0 and kt == 0 and "ps" in self.dbg.get("dd", ()):
                        dbgt = self.tmp(17, 1, [128, 512])
                        self.cp(dbgt.t, ps_.t[:], [ps_], [dbgt], eng="act")
                        self.dd("ps", dbgt.t, [dbgt], [128, 512])
                    self.act(e_, ps_.t[:].rearrange("p (a b) -> p a b", b=256), AF.Exp, [ps_], [et[0]], scale=0.125)
                    if l == 0 and tb == self.dbg.get("ddtb", 0) and c == 0 and j == 0 and kt == 0:
                        self.dd("et", et[0].t[:, 0:512], [et[0]], [128, 512], BF16)
                        self.dd("cm", self.cm.t[:], [self.cm], [128, 2, 256], BF16)
                    e2 = et[0].t[:, (kt % 2) * 512:(kt % 2 + 1) * 512]
                    self.mm(pacc.t[:], vc_.t[:, kt, c * 128:(c + 1) * 128], e2,
                            kt == 0, kt == nkt - 1, [vc_, et[0]], [pacc])
                    self.mm(pden.t[:], self.ones_bf.t[:], e2,
                            kt == 0, kt == nkt - 1, [self.ones_bf, et[0]], [pden])
                S.op("dve", lambda e, pden=pden: e.reciprocal(rden.t.rearrange("p a b -> p (a b)"), pden.t[:]), [pden], [rden])
                if l == 0 and tb == self.dbg.get("ddtb", 0):
                    self.dd("rden%d%d" % (c, j), rden.t, [rden], [128, 2, 256])
                for hh in range(2):
                    off = hh * 64
                    self.tt(y.t[off:off + 64, 3, c, qs], pacc.t[off:off + 64, hh * 256:(hh + 1) * 256],
                            rden.t[off:off + 64, hh, :], ALU.mult, [pacc, rden], [y])

    def tokproj_small(self, l):
        u, pp = self.u, self.P[7]
        for c in range(8):
            for k in range(8):
                self.mm(pp.t[0:64, c * 16:(c + 1) * 16], u.t[:, k, c * 64:(c + 1) * 64], self.wsm.t[:, l, k, :],
                        k == 0, k == 7, [u, self.wsm], [pp])
        self.sp = self.tmp(11, 1, [128, 512])
        self.cp(self.sp.t[0:64, 0:128], pp.t[0:64, 0:128], [pp], [self.sp], eng="act")
        return self.sp.t[0:64, 0:128].rearrange("p (c n) -> p c n", n=16)

    def conv_silu(self, l, tiles, nch, tail, cw, dst, tb):
        S, u = self.S, self.u
        xc = self.tmp(0, 7, [128, nch, NT + 3])
        acc = self.tmp(9, 1, [128, NT])
        if tb == 0:
            S.op("dve", lambda e: e.memset(tail.t[:], 0.0), [], [tail])
        self.cp(xc.t[:, :, 0:3], tail.t[:], [tail], [xc], eng="dve")
        for c in range(nch):
            w = self.loadw("win", l * 28 + tiles + c, 8)
            pp = self.P[c % 2]
            for k in range(8):
                self.mm(pp.t[:], w.t[:, k, :], u.t[:, k, :], k == 0, k == 7, [w, u], [pp])
            self.cp(xc.t[:, c, 3:NT + 3], pp.t[:], [pp], [xc], eng="act")
        self.cp(tail.t[:], xc.t[:, :, NT:NT + 3], [xc], [tail], eng="dve")
        for c in range(nch):
            self.ts(acc.t, xc.t[:, c, 0:NT], cw[:, c * 4:c * 4 + 1], ALU.mult, [xc, self.cwb], [acc])
            for j in range(1, 4):
                self.stt(acc.t, xc.t[:, c, j:NT + j], cw[:, c * 4 + j:c * 4 + j + 1], acc.t, ALU.mult, ALU.add,
                         [xc, acc, self.cwb], [acc])
            self.act(dst.t[:, c, :], acc.t, AF.Silu, [acc], [dst])

    def mlstm_fwd(self, l, s, tb, sp):
        S, u, y = self.S, self.u, self.y
        consts, c2, cst = self.consts, self.consts2, self.cst
        ident = consts.t[:, 128:256]
        ones = consts.t[:, 384:512]
        TRI = c2.t[0:64, 0:64]
        CMASK = c2.t[0:64, 64:128]
        rows = self.mlrow.t[0:64, l, :]
        Cx, mrep = self.mlC[l], self.mlm[l]
        qk = self.tmp(12, 4, [128, 4, NT])
        self.conv_silu(l, 8, 4, self.mltail[l], self.cwb.t[:, l, 24:40], qk, tb)
        self.ts(qk.t[:, 2:4, :], qk.t[:, 2:4, :], 0.125, ALU.mult, [qk], [qk])
        ms = self.dbg.get("mlstop", 99)
        if ms <= 1:
            return
        kz = self.tmp(4, 4, [128, 4, NT])
        S.op("pool", lambda e: e.memset(kz.t, 0.0), [], [kz])
        for h in range(4):
            pr, off = h // 2, (h % 2) * 64
            self.cp(kz.t[off:off + 64, h, :], qk.t[off:off + 64, 2 + pr, :], [qk], [kz], eng="pool")
        if tb == 0:
            S.op("dve", lambda e: e.memset(Cx.t[:], 0.0), [], [Cx])
            S.op("dve", lambda e: e.memset(mrep.t[:], 0.0), [], [mrep])
        A = self.tmp(16, 1, [128, 512])
        R_ = [A, self.sp]
        v3 = lambda lo: A.t[0:64, lo:lo + 32].rearrange("p (c h) -> p c h", h=4)
        li, lf, b_, ak, tx = v3(0), v3(32), v3(64), v3(96), v3(128)
        grep = A.t[:, 160:192].rearrange("p (c h) -> p c h", h=4)
        mkrep = A.t[:, 192:224].rearrange("p (c h) -> p c h", h=4)
        Mall = A.t[:, 224:260].rearrange("p (c h) -> p c h", h=4)
        scall = A.t[:, 260:292].rearrange("p (c h) -> p c h", h=4)
        kws = v3(292)
        mk32 = A.t[0:32, 324:325]
        dg32 = A.t[0:32, 328:360]
        ib = rows[:, 0:4].unsqueeze(1).broadcast_to([64, 8, 4])
        fb = rows[:, 4:8].unsqueeze(1).broadcast_to([64, 8, 4])
        self.tt(li, sp[:, :, 8:12], ib, ALU.add, R_ + [self.mlrow], [A])
        self.tt(tx, sp[:, :, 12:16], fb, ALU.add, R_ + [self.mlrow], [A])
        self.act(tx, tx, AF.Exp, [A], [A], scale=-1.0)
        self.act(tx, tx, AF.Ln, [A, cst], [A], bias=cst.t[0:64, 3:4])
        self.ts(lf, tx, -1.0, ALU.mult, [A], [A])
        p7 = self.P[7]
        lf2 = A.t[0:64, 32:64]
        self.mm(p7.t[0:64, 0:32], TRI, lf2, True, True, [A, c2], [p7])
        self.cp(A.t[0:64, 64:96], p7.t[0:64, 0:32], [p7], [A], eng="act")
        self.mm(p7.t[:, 32:64], ones[0:64, :], lf2, True, True, [A, consts], [p7])
        self.cp(A.t[:, 160:192], p7.t[:, 32:64], [p7], [A], eng="act")
        self.tt(ak, grep[0:64], b_, ALU.subtract, [A], [A])
        self.tt(ak, ak, li, ALU.add, [A], [A])
        self.mm(p7.t[0:32, 64:128], A.t[0:64, 96:128], ident[0:64, 0:64], True, True, [A, consts], [p7])
        S.op("dve", lambda e: e.tensor_reduce(out=mk32, in_=p7.t[0:32, 64:128], axis=AX.X, op=ALU.max), [p7], [A])
        self.ts(dg32, ident[0:32, 0:32], mk32, ALU.mult, [A, consts], [A])
        self.mm(p7.t[:, 128:160], ones[0:32, :], dg32, True, True, [A, consts], [p7])
        self.cp(A.t[:, 192:224], p7.t[:, 128:160], [p7], [A], eng="act")
        self.cp(Mall[:, 0, :], mrep.t[:], [mrep], [A], eng="dve")
        t4 = A.t[:, 364:368]
        for c in range(8):
            self.tt(t4, grep[:, c, :], Mall[:, c, :], ALU.add, [A], [A])
            self.tt(Mall[:, c + 1, :], t4, mkrep[:, c, :], ALU.max, [A], [A])
        self.cp(mrep.t[:], Mall[:, 8, :], [A], [mrep], eng="dve")
        self.tt(scall, grep, Mall[:, 0:8, :], ALU.add, [A], [A])
        self.tt(scall, scall, Mall[:, 1:9, :], ALU.subtract, [A], [A])
        self.act(scall, scall, AF.Exp, [A], [A])
        self.tt(kws, ak, Mall[0:64, 1:9, :], ALU.subtract, [A], [A])
        self.act(kws, kws, AF.Exp, [A], [A])
        if ms <= 2:
            return
        vx = self.tmp(17, 1, [128, 512])
        vext = vx.t[0:64, 0:264].rearrange("p (h e) -> p h e", e=66)
        S.op("dve", lambda e: e.memset(vx.t[0:64, 0:264], 1.0), [], [vx])
        osg = self.tmp(18, 1, [128, 512])
        Dm = self.tmp(19, 1, [128, 512])
        LR = self.tmp(20, 1, [128, 512])
        sq_ = self.tmp(21, 1, [128, 512])
        sT = self.tmp(22, 1, [128, 512])
        ne = self.tmp(23, 1, [128, 512])
        tq = self.tmp(0, 1, [128, 512])
        kt_ = self.tmp(1, 1, [128, 512])
        hh_ = self.tmp(2, 1, [128, 512])
        B = self.tmp(3, 1, [128, 512])
        v4 = lambda T, lo=0: T.t[0:64, lo:lo + 256].rearrange("p (h e) -> p h e", e=64)
        wv = [self.loadw("win", l * 28 + 12 + i, 8) for i in range(4)]
        for c in range(8):
            cs = slice(c * 64, (c + 1) * 64)
            p0 = self.P[0]
            for i in range(4):
                for k in range(8):
                    self.mm(p0.t[0:64, i * 128:(i + 1) * 128], u.t[:, k, cs], wv[i].t[:, k, :], k == 0, k == 7,
                            [u, wv[i]], [p0])
            self.cp(vext[:, :, 0:64], p0.t[0:64, 0:256].rearrange("p (h e) -> p h e", e=64), [p0], [vx], eng="act")
            self.act(osg.t[0:64, 0:256], p0.t[0:64, 256:512], AF.Sigmoid, [p0], [osg])
            if ms <= 3:
                continue
            lft = LR.t[0:64, 0:256].rearrange("p (h e) -> p h e", e=64)
            rm = LR.t[0:64, 256:512].rearrange("p (h e) -> p h e", e=64)
            tri_b = TRI.unsqueeze(1).broadcast_to([64, 4, 64])
            id_b = ident[0:64, 0:64].unsqueeze(1).broadcast_to([64, 4, 64])
            self.tt(lft, tri_b, lf[:, c, :].unsqueeze(2).broadcast_to([64, 4, 64]), ALU.mult, [A, c2], [LR])
            self.tt(rm, id_b, li[:, c, :].unsqueeze(2).broadcast_to([64, 4, 64]), ALU.mult, [A, consts], [LR])
            self.tt(rm, rm, lft, ALU.subtract, [LR], [LR])
            p1 = self.P[1]
            for h in range(4):
                o = p1.t[0:64, h * 64:(h + 1) * 64]
                self.mm(o, lft[:, h, :], ones[0:64, 0:64], True, False, [LR, consts], [p1])
                self.mm(o, ones[0:64, 0:64], rm[:, h, :], False, True, [LR, consts], [p1])
            dmv = v4(Dm)
            self.tt(dmv, p1.t[0:64, 0:256].rearrange("p (h e) -> p h e", e=64),
                    CMASK.unsqueeze(1).broadcast_to([64, 4, 64]), ALU.add, [p1, c2], [Dm])
            sm_ = B.t[0:64, 0:64]
            mloc, mint, mt, wint, e2, qn, den = (B.t[0:64, 4 * i:4 * i + 4] for i in range(7))
            S.op("dve", lambda e, dmv=dmv, mloc=mloc: e.tensor_reduce(out=mloc, in_=dmv, axis=AX.X, op=ALU.max), [Dm], [B])
            self.tt(mint, b_[:, c, :], Mall[0:64, c, :], ALU.add, [A], [B])
            self.tt(mt, mint, mloc, ALU.max, [B], [B])
            self.tt(wint, mint, mt, ALU.subtract, [B], [B])
            self.act(wint, wint, AF.Exp, [B], [B])
            self.act(e2, mt, AF.Exp, [B], [B], scale=-1.0)
            if ms <= 4:
                continue
            p2 = self.P[2]
            for h in range(4):
                o = p2.t[0:64, h * 64:(h + 1) * 64]
                self.mm(o, ones[0:64, 0:64], lft[:, h, :], True, False, [LR, consts], [p2])
                self.mm(o, rm[:, h, :], ones[0:64, 0:64], False, True, [LR, consts], [p2])
            etv = v4(sq_)
            self.tt(etv, p2.t[0:64, 0:256].rearrange("p (h e) -> p h e", e=64),
                    c2.t[0:64, 320:384].unsqueeze(1).broadcast_to([64, 4, 64]), ALU.add, [p2, c2], [sq_])
            self.act(etv, etv, AF.Exp, [sq_], [sq_])
            p3 = self.P[3]
            for h in range(4):
                self.mm(p3.t[0:64, h * 64:(h + 1) * 64], kz.t[:, h, cs], qk.t[:, h // 2, cs], True, True, [kz, qk], [p3])
            self.tt(v4(sT), p3.t[0:64, 0:256].rearrange("p (h e) -> p h e", e=64), etv, ALU.mult, [p3, sq_], [sT])
            if ms <= 5:
                continue
            stv = v4(sT)
            p4, p5 = self.P[4], self.P[5]
            for h in range(4):
                pr, off = h // 2, (h % 2) * 64
                self.mm(p4.t[0:64, h * 66:(h + 1) * 66], stv[:, h, :], vext[:, h, :], True, True, [sT, vx], [p4])
                self.mm(p5.t[0:64, h * 66:(h + 1) * 66], qk.t[:, pr, cs], Cx.t[:, h, :],
                        True, True, [qk, Cx], [p5])
            nev = ne.t[0:64, 0:264].rearrange("p (h e) -> p h e", e=66)
            tqv = tq.t[0:64, 0:264].rearrange("p (h e) -> p h e", e=66)
            self.tt(tqv, p5.t[0:64, 0:264].rearrange("p (h e) -> p h e", e=66),
                    wint.unsqueeze(2).broadcast_to([64, 4, 66]), ALU.mult, [p5, B], [tq])
            self.tt(nev, p4.t[0:64, 0:264].rearrange("p (h e) -> p h e", e=66),
                    e2.unsqueeze(2).broadcast_to([64, 4, 66]), ALU.mult, [p4, B], [ne])
            self.tt(nev, nev, tqv, ALU.add, [tq, ne], [ne])
            self.act(den, nev[:, :, 64], AF.Abs, [ne], [B])
            self.tt(den, den, e2, ALU.max, [B], [B])
            S.op("dve", lambda e, den=den: e.reciprocal(den, den), [B], [B])
            hv = v4(hh_)
            self.tt(hv, nev[:, :, 0:64], den.unsqueeze(2).broadcast_to([64, 4, 64]), ALU.mult, [ne, B], [hh_])
            if ms <= 6:
                continue
            h2 = v4(hh_, 256)
            ss = B.t[0:64, 32:36]
            self.tt(h2, hv, hv, ALU.mult, [hh_], [hh_])
            S.op("dve", lambda e, h2=h2, ss=ss: e.tensor_reduce(out=ss, in_=h2, axis=AX.X, op=ALU.add), [hh_], [B])
            self.act(ss, ss, AF.Sqrt, [B, cst], [B], scale=1.0 / 64, bias=cst.t[0:64, 0:1])
            S.op("dve", lambda e, ss=ss: e.reciprocal(ss, ss), [B], [B])
            self.tt(hv, hv, ss.unsqueeze(2).broadcast_to([64, 4, 64]), ALU.mult, [hh_, B], [hh_])
            self.tt(hh_.t[0:64, 0:256], hh_.t[0:64, 0:256], rows[:, 8:264], ALU.mult, [hh_, self.mlrow], [hh_])
            self.tt(hh_.t[0:64, 0:256], hh_.t[0:64, 0:256], osg.t[0:64, 0:256], ALU.mult, [hh_, osg], [hh_])
            for kc in range(2):
                self.mm(p3.t[:, 256 + kc * 64:256 + (kc + 1) * 64], hh_.t[0:64, kc * 128:(kc + 1) * 128],
                        ident[0:64, 0:64], True, True, [hh_, consts], [p3])
                self.cp(y.t[:, 1, kc, cs], p3.t[:, 256 + kc * 64:256 + (kc + 1) * 64], [p3], [y], eng="act")
            if ms <= 7:
                continue
            p6 = self.P[6]
            for pr in range(2):
                self.mm(p6.t[0:64, pr * 128:(pr + 1) * 128], qk.t[:, 2 + pr, cs], ident, True, True, [qk, consts], [p6])
            kwv = v4(kt_)
            self.tt(kwv, p6.t[0:64, 0:256].rearrange("p (h e) -> p h e", e=64),
                    kws[:, c, :].unsqueeze(2).broadcast_to([64, 4, 64]), ALU.mult, [p6, A], [kt_])
            p7b = self.P[7]
            for h in range(4):
                pr, off = h // 2, (h % 2) * 64
                o = p7b.t[:, h * 66:(h + 1) * 66]
                self.mm(o, kt_.t[0:64, pr * 128:(pr + 1) * 128], vext[:, h, :], True, True, [kt_, vx], [p7b])
                self.stt(Cx.t[off:off + 64, h, :], Cx.t[off:off + 64, h, :], scall[off:off + 64, c, h:h + 1],
                         p7b.t[off:off + 64, h * 66:(h + 1) * 66], ALU.mult, ALU.add, [Cx, A, p7b], [Cx])

    def gdn_fwd(self, l, s, tb, sp):
        S, u, y = self.S, self.u, self.y
        consts, c2, cst = self.consts, self.consts2, self.cst
        ident = consts.t[:, 128:256]
        id64 = ident[0:64, 0:64]
        ones = consts.t[:, 384:512]
        on64 = ones[0:64, 0:64]
        neg64 = self.negones.t[0:64, 0:64]
        TRI = c2.t[0:64, 0:64]
        SLADD = self.gmask.t[0:64, 0:64]
        SUADD = self.gmask.t[0:64, 64:128]
        CMT = c2.t[0:64, 320:384]
        BLK = self.gmask.t[:, 128:256]
        rows = self.gdrow.t[0:64, l, :]
        Sz = self.gdS[l]
        b3 = lambda ap: ap.unsqueeze(1).broadcast_to([64, 4, 64])
        v4 = lambda T, lo=0: T.t[0:64, lo:lo + 256].rearrange("p (h e) -> p h e", e=64)
        qkv = self.tmp(12, 6, [128, 6, NT])
        self.conv_silu(l, 0, 6, self.gdtail[l], self.cwb.t[:, l, 0:24], qkv, tb)
        if tb == 0:
            S.op("dve", lambda e: e.memset(Sz.t[:], 0.0), [], [Sz])
        sq = self.tmp(9, 1, [128, NT])
        rs = self.tmp(8, 1, [128, NT])
        for c4 in range(4):
            pp = self.P[c4 % 2]
            self.tt(sq.t, qkv.t[:, c4, :], qkv.t[:, c4, :], ALU.mult, [qkv], [sq])
            self.mm(pp.t[:], BLK, sq.t, True, True, [sq, self.gmask], [pp])
            self.act(rs.t, pp.t[:], AF.Sqrt, [pp, cst], [rs], bias=cst.t[:, 0:1])
            S.op("dve", lambda e: e.reciprocal(rs.t, rs.t), [rs], [rs])
            if c4 < 2:
                self.stt(qkv.t[:, c4, :], qkv.t[:, c4, :], 0.125, rs.t, ALU.mult, ALU.mult, [qkv, rs], [qkv])
            else:
                self.tt(qkv.t[:, c4, :], qkv.t[:, c4, :], rs.t, ALU.mult, [qkv, rs], [qkv])
        kz = self.tmp(4, 4, [128, 4, NT])
        S.op("pool", lambda e: e.memset(kz.t, 0.0), [], [kz])
        for h in range(4):
            pr, off = h // 2, (h % 2) * 64
            self.cp(kz.t[off:off + 64, h, :], qkv.t[off:off + 64, 2 + pr, :], [qkv], [kz], eng="pool")
        A = self.tmp(18, 1, [128, 512])
        v3 = lambda lo: A.t[0:64, lo:lo + 32].rearrange("p (c h) -> p c h", h=4)
        beta, g_, gc, egc, ekd, tx, bneg, begc = v3(0), v3(32), v3(64), v3(96), v3(128), v3(160), v3(192), v3(224)
        gLrep = A.t[:, 256:288].rearrange("p (c h) -> p c h", h=4)
        cdrep = A.t[:, 288:320].rearrange("p (c h) -> p c h", h=4)
        ea = A.t[0:64, 320:324]
        R_ = [A, self.sp]
        self.act(beta, sp[:, :, 0:4], AF.Sigmoid, R_, [A])
        self.tt(tx, sp[:, :, 4:8], rows[:, 4:8].unsqueeze(1).broadcast_to([64, 8, 4]), ALU.add, R_ + [self.gdrow], [A])
        self.act(tx, tx, AF.Exp, [A], [A])
        self.act(tx, tx, AF.Ln, [A, cst], [A], bias=cst.t[0:64, 3:4])
        self.act(ea, rows[:, 0:4], AF.Exp, [self.gdrow], [A])
        self.tt(g_, tx, ea.unsqueeze(1).broadcast_to([64, 8, 4]), ALU.mult, [A], [A])
        self.ts(g_, g_, -1.0, ALU.mult, [A], [A])
        p7 = self.P[7]
        g2 = A.t[0:64, 32:64]
        self.mm(p7.t[0:64, 0:32], TRI, g2, True, True, [A, c2], [p7])
        self.cp(A.t[0:64, 64:96], p7.t[0:64, 0:32], [p7], [A], eng="act")
        self.mm(p7.t[:, 32:64], ones[0:64, :], g2, True, True, [A, consts], [p7])
        self.cp(A.t[:, 256:288], p7.t[:, 32:64], [p7], [A], eng="act")
        self.act(egc, gc, AF.Exp, [A], [A])
        self.tt(ekd, gLrep[0:64], gc, ALU.subtract, [A], [A])
        self.act(ekd, ekd, AF.Exp, [A], [A])
        self.act(cdrep, gLrep, AF.Exp, [A], [A])
        self.ts(bneg, beta, -1.0, ALU.mult, [A], [A])
        self.tt(begc, beta, egc, ALU.mult, [A], [A])
        MM_ = self.tmp(19, 1, [128, 512])
        X = self.tmp(20, 1, [128, 512])
        DC = self.tmp(21, 1, [128, 512])
        QB = self.tmp(22, 1, [128, 512])
        GT = self.tmp(23, 1, [128, 512])
        VK = self.tmp(0, 1, [128, 512])
        XT = self.tmp(1, 1, [128, 512])
        VN = self.tmp(2, 1, [128, 512])
        ZS = self.tmp(3, 1, [128, 512])
        M2 = self.tmp(10, 1, [128, 512])
        wz = [self.loadw("win", l * 28 + 6 + i, 8) for i in range(2)]
        Xv = X.t[0:64, 0:512].rearrange("p (h e) -> p h e", e=128)
        for c in range(8):
            cs = slice(c * 64, (c + 1) * 64)
            p0 = self.P[0]
            for i in range(2):
                for k in range(8):
                    self.mm(p0.t[0:64, i * 128:(i + 1) * 128], u.t[:, k, cs], wz[i].t[:, k, :], k == 0, k == 7,
                            [u, wz[i]], [p0])
            self.act(ZS.t[0:64, 0:256], p0.t[0:64, 0:256], AF.Silu, [p0], [ZS])
            for pr in range(2):
                self.mm(p0.t[0:64, 256 + pr * 128:256 + (pr + 1) * 128], qkv.t[:, 4 + pr, cs], ident, True, True,
                        [qkv, consts], [p0])
            vtok = v4(VK)
            self.cp(vtok, p0.t[0:64, 256:512].rearrange("p (h e) -> p h e", e=64), [p0], [VK], eng="act")
            p1 = self.P[1]
            for pr in range(2):
                self.mm(p1.t[0:64, pr * 128:(pr + 1) * 128], qkv.t[:, 2 + pr, cs], ident, True, True, [qkv, consts], [p1])
            ktok = v4(VK, 256)
            self.cp(ktok, p1.t[0:64, 0:256].rearrange("p (h e) -> p h e", e=64), [p1], [VK], eng="act")
            for h in range(4):
                uo, wo = (64, 0) if h % 2 == 0 else (0, 64)
                self.ts(Xv[:, h, uo:uo + 64], vtok[:, h, :], beta[:, c, h:h + 1], ALU.mult, [VK, A], [X])
                self.ts(Xv[:, h, wo:wo + 64], ktok[:, h, :], begc[:, c, h:h + 1], ALU.mult, [VK, A], [X])
            kd = v4(XT, 256)
            self.tt(kd, ktok, ekd[:, c, :].unsqueeze(2).broadcast_to([64, 4, 64]), ALU.mult, [VK, A], [XT])
            gt = v4(GT)
            self.tt(gt, b3(TRI), g_[:, c, :].unsqueeze(2).broadcast_to([64, 4, 64]), ALU.mult, [A, c2], [GT])
            p2, p3 = self.P[2], self.P[3]
            for h in range(4):
                o = p2.t[0:64, h * 64:(h + 1) * 64]
                self.mm(o, gt[:, h, :], on64, True, False, [GT, consts], [p2])
                self.mm(o, neg64, gt[:, h, :], False, True, [GT, self.negones], [p2])
                o = p2.t[0:64, 256 + h * 64:256 + (h + 1) * 64]
                self.mm(o, on64, gt[:, h, :], True, False, [GT, consts], [p2])
                self.mm(o, gt[:, h, :], neg64, False, True, [GT, self.negones], [p2])
            dS, dT, dQ = v4(DC), v4(DC, 256), v4(QB)
            pD = p2.t[0:64, 0:256].rearrange("p (h e) -> p h e", e=64)
            pDT = p2.t[0:64, 256:512].rearrange("p (h e) -> p h e", e=64)
            self.tt(dS, pD, b3(SLADD), ALU.add, [p2, self.gmask], [DC])
            self.tt(dT, pDT, b3(SUADD), ALU.add, [p2, self.gmask], [DC])
            self.tt(dQ, pDT, b3(CMT), ALU.add, [p2, c2], [QB])
            self.act(DC.t[0:64, 0:512], DC.t[0:64, 0:512], AF.Exp, [DC], [DC])
            self.act(dQ, dQ, AF.Exp, [QB], [QB])
            dgb = v4(GT, 256)
            self.tt(dgb, b3(id64), bneg[:, c, :].unsqueeze(2).broadcast_to([64, 4, 64]), ALU.mult, [A, consts], [GT])
            for h in range(4):
                self.mm(p3.t[0:64, h * 64:(h + 1) * 64], qkv.t[:, 2 + h // 2, cs], kz.t[:, h, cs], True, True, [qkv, kz], [p3])
                self.mm(p3.t[0:64, 256 + h * 64:256 + (h + 1) * 64], on64, dgb[:, h, :], True, True, [GT, consts], [p3])
            pKK = p3.t[0:64, 0:256].rearrange("p (h e) -> p h e", e=64)
            pBf = p3.t[0:64, 256:512].rearrange("p (h e) -> p h e", e=64)
            Mk, MkT = v4(MM_), v4(MM_, 256)
            self.tt(Mk, pKK, dS, ALU.mult, [p3, DC], [MM_])
            self.tt(Mk, Mk, bneg[:, c, :].unsqueeze(2).broadcast_to([64, 4, 64]), ALU.mult, [MM_, A], [MM_])
            self.tt(MkT, pKK, dT, ALU.mult, [p3, DC], [MM_])
            self.tt(MkT, MkT, pBf, ALU.mult, [MM_, p3], [MM_])
            p4 = self.P[4]
            for h in range(4):
                self.mm(p4.t[0:64, h * 64:(h + 1) * 64], kz.t[:, h, cs], qkv.t[:, h // 2, cs], True, True, [qkv, kz], [p4])
            self.tt(dQ, p4.t[0:64, 0:256].rearrange("p (h e) -> p h e", e=64), dQ, ALU.mult, [p4, QB], [QB])
            cur, nxt = MM_, M2
            for step in range(6):
                cM, cMT = v4(cur), v4(cur, 256)
                p5 = self.P[5]
                for h in range(4):
                    self.mm(p5.t[0:64, h * 128:(h + 1) * 128], cMT[:, h, :], Xv[:, h, :], True, True, [cur, X], [p5])
                if step < 5:
                    p6 = self.P[6]
                    for h in range(4):
                        self.mm(p6.t[0:64, h * 64:(h + 1) * 64], cMT[:, h, :], cM[:, h, :], True, True, [cur], [p6])
                        self.mm(p6.t[0:64, 256 + h * 64:256 + (h + 1) * 64], cM[:, h, :], cMT[:, h, :], True, True, [cur], [p6])
                    self.cp(nxt.t[0:64, 0:512], p6.t[0:64, 0:512], [p6], [nxt], eng="act")
                self.tt(X.t[0:64, 0:512], X.t[0:64, 0:512], p5.t[0:64, 0:512], ALU.add, [X, p5], [X])
                cur, nxt = nxt, cur
            p5 = self.P[5]
            for h in range(4):
                self.mm(p5.t[:, h * 64:(h + 1) * 64], Xv[:, h, :], id64, True, True, [X, consts], [p5])
            xt = XT.t[:, 0:256].rearrange("p (h e) -> p h e", e=64)
            self.cp(XT.t[:, 0:256], p5.t[:, 0:256], [p5], [XT], eng="act")
            p6 = self.P[6]
            for h in range(4):
                self.mm(p6.t[0:64, h * 64:(h + 1) * 64], xt[:, h, :], Sz.t[:, h, :], True, True, [XT, Sz], [p6])
                self.mm(p6.t[0:64, 256 + h * 64:256 + (h + 1) * 64], qkv.t[:, h // 2, cs], Sz.t[:, h, :], True, True,
                        [qkv, Sz], [p6])
            vn = v4(VN)
            for h in range(4):
                uo = 64 if h % 2 == 0 else 0
                self.tt(vn[:, h, :], Xv[:, h, uo:uo + 64], p6.t[0:64, h * 64:(h + 1) * 64], ALU.subtract, [X, p6], [VN])
            oq = v4(VN, 256)
            self.tt(oq, p6.t[0:64, 256:512].rearrange("p (h e) -> p h e", e=64),
                    egc[:, c, :].unsqueeze(2).broadcast_to([64, 4, 64]), ALU.mult, [p6, A], [VN])
            p7 = self.P[7]
            for h in range(4):
                self.mm(p7.t[0:64, h * 64:(h + 1) * 64], dQ[:, h, :], vn[:, h, :], True, True, [QB, VN], [p7])
            self.tt(oq, oq, p7.t[0:64, 0:256].rearrange("p (h e) -> p h e", e=64), ALU.add, [VN, p7], [VN])
            p1 = self.P[1]
            for h in range(4):
                pr, off = h // 2, (h % 2) * 64
                self.mm(p1.t[:, h * 64:(h + 1) * 64], XT.t[0:64, 256 + pr * 128:256 + (pr + 1) * 128], vn[:, h, :],
                        True, True, [XT, VN], [p1])
                self.stt(Sz.t[off:off + 64, h, :], Sz.t[off:off + 64, h, :], cdrep[off:off + 64, c, h:h + 1],
                         p1.t[off:off + 64, h * 64:(h + 1) * 64], ALU.mult, ALU.add, [Sz, A, p1], [Sz])
            o2 = v4(ZS, 256)
            ss = A.t[0:64, 328:332]
            self.tt(o2, oq, oq, ALU.mult, [VN], [ZS])
            S.op("dve", lambda e, o2=o2, ss=ss: e.tensor_reduce(out=ss, in_=o2, axis=AX.X, op=ALU.add), [ZS], [A])
            self.act(ss, ss, AF.Sqrt, [A, cst], [A], scale=1.0 / 64, bias=cst.t[0:64, 0:1])
            S.op("dve", lambda e, ss=ss: e.reciprocal(ss, ss), [A], [A])
            self.tt(o2, oq, ss.unsqueeze(2).broadcast_to([64, 4, 64]), ALU.mult, [VN, A], [ZS])
            self.tt(o2, o2, b3(rows[:, 8:72]), ALU.mult, [ZS, self.gdrow], [ZS])
            self.tt(ZS.t[0:64, 256:512], ZS.t[0:64, 256:512], ZS.t[0:64, 0:256], ALU.mult, [ZS], [ZS])
            p0 = self.P[0]
            for kc in range(2):
                self.mm(p0.t[:, kc * 64:(kc + 1) * 64], ZS.t[0:64, 256 + kc * 128:256 + (kc + 1) * 128], id64, True, True,
                        [ZS, consts], [p0])
                self.cp(y.t[:, 0, kc, cs], p0.t[:, kc * 64:(kc + 1) * 64], [p0], [y], eng="act")

    def mixers(self, l, s, tb):
        only = self.dbg.get("only", "")
        sp = self.tokproj_small(l)
        if not only or "gdn" in only:
            self.gdn_fwd(l, s, tb, sp)
        if not only or "ml" in only:
            self.mlstm_fwd(l, s, tb, sp)
        if not only or "s5" in only:
            self.s5_fwd(l, s, tb)
        if not only or "moba" in only:
            self.moba_fwd(l, s, tb)

    def build(self):
        S = self.S
        self.declare()
        self.setup()
        units = self.dbg.get("units", [(s, tb) for s in range(2) for tb in range(SEQ // NT)])
        stop = self.dbg.get("stop", None)
        for (s, tb) in units:
            t0 = tb * NT
            S.dma("sp", self.h.t[:], self.D["xT"].t[s, :, :, t0:t0 + NT], [], [self.h], self.h)
            first = (s, tb) == units[0]
            for l in range(NL):
                self.ffn(l, 0)
                if stop == "ffn1":
                    break
                if first and l == 1:
                    self.convert([("wgu", 1, 1), ("wdn", 1, 1), ("wplg", 1), ("wplp", 1)])
                self.norm(l * 4 + 1)
                if "yin" in self.dbg:
                    S.dma("sp", self.y.t[:], self.D["yin"].t[s * 4 + tb], [], [self.y], self.y)
                else:
                    self.mixers(l, s, tb)
                if "dumpy" in self.dbg and l == 0:
                    S.dma("sp", self.dumpy.t[s * 4 + tb], self.y.t[:], [self.y], [self.dumpy], self.y)
                if stop == "y":
                    break
                if first and l == 0:
                    self.convert([("wgu", 0, 1), ("wdn", 0, 1), ("wplg", 0), ("wplp", 0)])
                self.merge(l)
                if first and l == 0:
                    self.convert([("wgu", 1, 0), ("wdn", 1, 0)])
                self.ffn(l, 1)
                if first and l == 0:
                    self.convert([("win", 1), ("wgate", 1), ("wbr", 1), ("wout", 1), ("wglu", 1)])
                self.ple(l, s, t0)
            if stop is not None:
                self.dump_h(s * 4 + tb)
            self.final(s, t0)
        S.finish()
        S.emit_all()


def F32v(k, name, hh):
    ri = 0 if name.endswith("re") else 1
    t = k.s5B["BT" if name.startswith("BT") else "CT"]
    return t[:, 4 * hh:4 * hh + 4, ri, :]


def wt(w, K):
    Kd, N = w.shape
    assert Kd == K * 128 and N % 128 == 0
    a = w.reshape(K, 128, N // 128, 128).transpose(2, 1, 0, 3)
    return np.ascontiguousarray(a).reshape(N // 128 * 128, K * 128)


def fm(a, kc):
    T = a.shape[0]
    return np.ascontiguousarray(a.T.reshape(kc, 128, T).transpose(1, 0, 2))


def prep_shared(inp):
    f = lambda n: np.asarray(inp[n], dtype=np.float32)
    sh = {}
    wgu = []
    wdn = []
    for l in range(NL):
        for nm_gu, nm_d in (("ffn1_w_gu", "ffn1_w_down"), ("ffn2_w_gu", "ffn2_w_down")):
            w = f(nm_gu)[l]
            g = wt(w[:, :2816], 8).reshape(22, 128, 1024)
            u = wt(w[:, 2816:], 8).reshape(22, 128, 1024)
            wgu.append(np.stack([g, u], axis=1).reshape(44 * 128, 1024))
            wdn.append(wt(f(nm_d)[l], 22))
    sh["wgu"] = np.concatenate(wgu, 0)
    sh["wdn"] = np.concatenate(wdn, 0)
    swap = np.concatenate([(np.arange(64) + 32) % 64 + 64 * hh for hh in range(4)])
    cols = np.concatenate([np.arange(0, 1024), np.arange(1032, 2056), np.arange(2064, 3088),
                           2320 + swap, 2576 + swap])
    sh["win"] = np.concatenate([wt(f("w_in")[l][:, cols], 8) for l in range(NL)], 0)
    sh["wgate"] = np.concatenate([
        np.stack([wt(f("w_gate")[l, b], 8).reshape(8, 128, 1024) for b in range(4)], 1).reshape(8 * 4 * 128, 1024)
        for l in range(NL)], 0)
    sh["wbr"] = np.concatenate([
        np.stack([wt(f("w_branch")[l, b], 2).reshape(8, 128, 256) for b in range(4)], 1).reshape(8 * 4 * 128, 256)
        for l in range(NL)], 0)
    sh["wout"] = np.concatenate([wt(f("w_out")[l], 8) for l in range(NL)], 0)
    sh["wplg"] = np.concatenate([wt(f("ple_w_gate")[l], 8) for l in range(NL)], 0)
    sh["wplp"] = np.concatenate([wt(f("ple_w_proj")[l], 2) for l in range(NL)], 0)
    gains = np.zeros((9, 1024), np.float32)
    for l in range(NL):
        gains[l * 4 + 0] = f("ffn1_norm")[l]
        gains[l * 4 + 1] = f("mix_norm")[l]
        gains[l * 4 + 2] = f("ffn2_norm")[l]
        gains[l * 4 + 3] = f("ple_norm")[l]
    gains[8] = f("final_norm")
    sh["gains"] = np.ascontiguousarray(gains.reshape(9, 8, 128).transpose(2, 0, 1))
    sh["wglu"] = np.concatenate([wt(f("s5_w_glu")[l], 2) for l in range(NL)], 0)
    consts = np.zeros((128, 512), np.float32)
    consts[:, 0:128] = np.arange(128, dtype=np.float32)[None, :]
    consts[:, 128:256] = np.eye(128, dtype=np.float32)
    consts[:, 384:512] = 1.0
    for qb in range(8):
        for n in range(8):
            consts[:, 256 + qb * 8 + n] = 0.0 if n < qb else -1e9
    pc = np.zeros((128, 8), np.float32)
    invf = (10000.0 ** (-np.arange(0, 64, 2, dtype=np.float32) / 64)).astype(np.float32)
    for p_ in range(128):
        pc[p_, 0] = invf[p_ % 32]
        pc[p_, 1] = -1.0 if (p_ % 64) < 32 else 1.0
        pc[p_, 2] = pc[p_, 1] * 2 * np.pi
    sh["pc"] = pc
    c2 = np.zeros((128, 512), np.float32)
    ii = np.arange(64)
    c2[0:64, 0:64] = (ii[:, None] <= ii[None, :]).astype(np.float32)
    c2[0:64, 64:128] = np.where(ii[None, :] <= ii[:, None], 0.0, -60000.0)
    c2[0:64, 128:192] = (ii[None, :] < ii[:, None]).astype(np.float32)
    c2[0:64, 192:256] = (ii[None, :] <= ii[:, None]).astype(np.float32)
    c2[0:64, 256:320] = (ii[:, None] < ii[None, :]).astype(np.float32)
    c2[0:64, 320:384] = np.where(ii[:, None] <= ii[None, :], 0.0, -60000.0)
    sh["consts2"] = c2
    gmk = np.zeros((128, 256), np.float32)
    gmk[0:64, 0:64] = np.where(ii[None, :] < ii[:, None], 0.0, -60000.0)
    gmk[0:64, 64:128] = np.where(ii[:, None] < ii[None, :], 0.0, -60000.0)
    pp_ = np.arange(128)
    gmk[:, 128:256] = (pp_[:, None] // 64 == pp_[None, :] // 64).astype(np.float32)
    sh["gmaskf"] = gmk
    win = f("w_in")
    wsm = np.zeros((128, NL, 8, 16), np.float32)
    cwf = np.zeros((128, NL, 40), np.float32)
    mlrow = np.zeros((128, NL, 264), np.float32)
    gdrow = np.zeros((128, NL, 72), np.float32)
    for l in range(NL):
        small = np.concatenate([win[l][:, 1024:1032], win[l][:, 2056:2064]], 1)
        wsm[:, l] = small.reshape(8, 128, 16).transpose(1, 0, 2)
        gc = f("gdn_conv")[l]
        mc = f("mlstm_conv")[l]
        cwf[:, l, 0:24] = gc.T.reshape(6, 128, 4).transpose(1, 0, 2).reshape(128, 24)
        cwf[:, l, 24:40] = mc.T.reshape(4, 128, 4).transpose(1, 0, 2).reshape(128, 16)
        mlrow[:, l, 0:4] = f("mlstm_i_bias")[l][None, :]
        mlrow[:, l, 4:8] = f("mlstm_f_bias")[l][None, :]
        mlrow[:, l, 8:264] = f("mlstm_norm")[l][None, :]
        gdrow[:, l, 0:4] = f("gdn_a_log")[l][None, :]
        gdrow[:, l, 4:8] = f("gdn_dt_bias")[l][None, :]
        gdrow[:, l, 8:72] = f("gdn_norm")[l][None, :]
    sh["wsmf"], sh["cwf"], sh["mlrowf"], sh["gdrowf"] = wsm, cwf, mlrow, gdrow
    cmf = np.zeros((128, 2, 256), np.float32)
    for a in range(2):
        kl = a * 128 + np.arange(128)[:, None]
        cmf[:, a, :] = np.where(kl > np.arange(256)[None, :], -30000.0, 0.0)
    sh["cmf"] = cmf
    sh["consts"] = consts
    lre, lim, ldt = f("s5_lambda_re"), f("s5_lambda_im"), f("s5_log_dt")
    s5st = np.zeros((NL, 128, 8, 3), np.float32)
    s5rep = np.zeros((NL, 3, 128, 8, 128), np.float32)
    s5bexp = np.zeros((NL, 2, 128, 8, 128), np.float32)
    s5cexp = np.zeros((NL, 2, 128, 8, 128), np.float32)
    bre, bim, cre, cim = f("s5_b_re"), f("s5_b_im"), f("s5_c_re"), f("s5_c_im")
    for l in range(NL):
        for sc in range(8):
            for half in range(2):
                g = 2 * sc + half
                ps = slice(half * 64, half * 64 + 64)
                s5st[l, ps, sc, 0] = lre[l, g]
                s5st[l, ps, sc, 1] = lim[l, g]
                s5st[l, ps, sc, 2] = ldt[l, g]
                s5rep[l, 0, :, sc, ps] = lre[l, g][None, :]
                s5rep[l, 1, :, sc, ps] = lim[l, g][None, :]
                s5rep[l, 2, :, sc, ps] = ldt[l, g]
                r0 = (sc % 4) * 32 + half * 16
                s5bexp[l, 0, r0:r0 + 16, sc, ps] = bre[l, g].T
                s5bexp[l, 1, r0:r0 + 16, sc, ps] = bim[l, g].T
                s5cexp[l, 0, ps, sc, r0:r0 + 16] = cre[l, g].T
                s5cexp[l, 1, ps, sc, r0:r0 + 16] = cim[l, g].T
    sh["s5st"], sh["s5rep"], sh["s5bexp"], sh["s5cexp"] = s5st, s5rep, s5bexp, s5cexp
    s5db = np.zeros((NL, 128, 4), np.float32)
    for l in range(NL):
        s5db[l, :, 0:2] = f("s5_d")[l].reshape(2, 128).T
        s5db[l, :, 2:4] = f("s5_b_glu")[l].reshape(2, 128).T
    sh["s5db"] = s5db
    return sh


def prep_core(inp, c):
    x = np.asarray(inp["x"], dtype=np.float32)
    p = np.asarray(inp["p"], dtype=np.float32)
    m = {}
    m["xT"] = np.stack([fm(x[2 * c + s], 8) for s in range(2)], 0)
    m["pT"] = np.stack([np.stack([fm(p[l, 2 * c + s], 2) for s in range(2)], 0) for l in range(NL)], 0)
    pos = np.asarray(inp["positions"]).astype(np.int32)
    m["posr"] = np.ascontiguousarray(np.broadcast_to(pos[2 * c:2 * c + 2, None, :], (2, 128, SEQ)))
    return m


def build_nc(dbg=None):
    nc = bass.Bass("TRN2", target_bir_lowering=False)
    with ExitStack() as st:
        k = Kern(nc, st, dbg)
        k.build()
    return nc


def kernel(**inputs):
    sh = prep_shared(inputs)
    nc = build_nc()
    in_maps = []
    for c in range(8):
        m = dict(sh)
        m.update(prep_core(inputs, c))
        in_maps.append(m)
    res = run_bass_kernel_spmd(nc, in_maps, core_ids=list(range(8)))
    out = np.zeros((16, SEQ, 1024), np.float32)
    for c in range(8):
        o = res.results[c]["out"]
        for s in range(2):
            out[2 * c + s] = o[s].transpose(2, 1, 0).reshape(SEQ, 1024)
    return out
```

```python
import numpy as np
from contextlib import ExitStack
import concourse.bass as bass
import concourse.mybir as mybir
from concourse.bass_utils import run_bass_kernel_spmd

F32 = mybir.dt.float32
BF16 = mybir.dt.bfloat16
I32 = mybir.dt.int32
AF = mybir.ActivationFunctionType
ALU = mybir.AluOpType
AX = mybir.AxisListType

ENGS = ["pe", "act", "dve", "pool", "sp"]
NT = 512
NL = 2
SEQ = 2048
PI = float(np.pi)


class Buf:
    __slots__ = ("name", "w", "r", "sem", "semcnt", "t")

    def __init__(self, name, t=None):
        self.name = name
        self.w = None
        self.r = {}
        self.sem = None
        self.semcnt = 0
        self.t = t


class Tmp:
    __slots__ = ("t", "bufs")

    def __init__(self, t, bufs):
        self.t = t
        self.bufs = bufs


def flat(lst):
    out = []
    for b in lst:
        if isinstance(b, Tmp):
            out.extend(b.bufs)
        else:
            out.append(b)
    return out


class Sched:
    def __init__(self, nc, stack):
        self.nc = nc
        self.stack = stack
        self.q = {e: [] for e in ENGS}
        self.cnt = {e: 0 for e in ENGS}
        self.sems = {}
        for e in ENGS:
            self.sems[e] = stack.enter_context(nc.semaphore("s_" + e))
        self.seen = {e: {} for e in ENGS}
        self.dmabufs = []

    def sb(self, name, shape, dt=F32):
        return Buf(name, self.stack.enter_context(self.nc.sbuf_tensor("sb_" + name, list(shape), dt)))

    def ps(self, name, shape, dt=F32):
        return Buf(name, self.stack.enter_context(self.nc.psum_tensor("ps_" + name, list(shape), dt)))

    def _waits(self, e, reads, writes):
        need = {}

        def add(k, v, src):
            if src == "pe" and e == "pe":
                return
            if v > need.get(k, 0):
                need[k] = v
        for b in reads:
            if b.w is not None:
                add(*b.w)
        for b in writes:
            if b.w is not None:
                add(*b.w)
            for k, (v, src) in b.r.items():
                add(k, v, src)
        out = []
        seen = self.seen[e]
        for k, v in need.items():
            if seen.get(k, 0) < v:
                seen[k] = v
                out.append((self.sems[k], v))
        return out

    def _record(self, dep, reads, writes):
        k, v, src = dep
        for b in reads:
            old = b.r.get(k)
            if old is None or old[0] < v:
                b.r[k] = (v, src)
        for b in writes:
            b.w = dep
            b.r = {}

    def op(self, e, fn, reads=(), writes=()):
        reads, writes = flat(reads), flat(writes)
        waits = self._waits(e, reads, writes)
        self.cnt[e] += 1
        n = self.cnt[e]
        sem = self.sems[e]

        def emit(engine, fn=fn, waits=waits, sem=sem):
            for s, v in waits:
                engine.wait_ge(s, v)
            fn(engine).then_inc(sem, 1)
        self.q[e].append(emit)
        self._record((e, n, e), reads, writes)

    def dma(self, qe, out, in_, reads, writes, sembuf):
        reads, writes = flat(reads), flat(writes)
        waits = self._waits(qe, reads, writes)
        if isinstance(sembuf, Tmp):
            sembuf = sembuf.bufs[0]
        if sembuf.sem is None:
            key = "d%d" % len(self.dmabufs)
            sembuf.sem = key
            self.sems[key] = self.stack.enter_context(self.nc.semaphore(key))
            self.dmabufs.append(sembuf)
        sembuf.semcnt += 16
        v = sembuf.semcnt
        sem = self.sems[sembuf.sem]

        def emit(engine, waits=waits, sem=sem, out=out, in_=in_):
            for s, vv in waits:
                engine.wait_ge(s, vv)
            engine.dma_start(out=out, in_=in_).then_inc(sem, 16)
        self.q[qe].append(emit)
        self._record((sembuf.sem, v, "dma"), reads, writes)

    def finish(self):
        waits = []
        for e in ENGS:
            if e != "sp" and self.cnt[e] > 0:
                waits.append((self.sems[e], self.cnt[e]))
        for b in self.dmabufs:
            waits.append((self.sems[b.sem], b.semcnt))

        def emit(engine, waits=waits):
            for s, v in waits:
                engine.wait_ge(s, v)
        self.q["sp"].append(emit)

    def emit_all(self):
        with self.nc.Block() as block:
            @block.tensor
            def _(eng):
                for f in self.q["pe"]:
                    f(eng)

            @block.scalar
            def _(eng):
                for f in self.q["act"]:
                    f(eng)

            @block.vector
            def _(eng):
                for f in self.q["dve"]:
                    f(eng)

            @block.gpsimd
            def _(eng):
                for f in self.q["pool"]:
                    f(eng)

            @block.sync
            def _(eng):
                for f in self.q["sp"]:
                    f(eng)


BIGW = {
    "wgu": (NL * 2 * 44 * 128, 8 * 128),
    "wdn": (NL * 2 * 8 * 128, 22 * 128),
    "win": (NL * 28 * 128, 8 * 128),
    "wgate": (NL * 8 * 4 * 128, 8 * 128),
    "wbr": (NL * 8 * 4 * 128, 2 * 128),
    "wout": (NL * 8 * 128, 8 * 128),
    "wplg": (NL * 8 * 128, 8 * 128),
    "wplp": (NL * 8 * 128, 2 * 128),
    "wglu": (NL * 2 * 128, 2 * 128),
}


class Kern:
    def __init__(self, nc, st, dbg=None):
        self.nc = nc
        self.dbg = dbg or {}
        self.S = Sched(nc, st)
        self.wi8 = 0
        self.wi22 = 0
        self.wbg = {}

    def mm(self, out, lhsT, rhs, start, stop, R, W):
        self.S.op("pe", lambda e: e.matmul(out, lhsT, rhs, start=start, stop=stop), R, W)

    def act(self, out, in_, func, R, W, **kw):
        self.S.op("act", lambda e: e.activation(out=out, in_=in_, func=func, **kw), R, W)

    def tt(self, out, a, b, op, R, W, eng="dve"):
        self.S.op(eng, lambda e: e.tensor_tensor(out=out, in0=a, in1=b, op=op), R, W)

    def ts(self, out, a, s1, op0, R, W, s2=None, op1=None, eng="dve"):
        if op1 is None:
            self.S.op(eng, lambda e: e.tensor_scalar(out=out, in0=a, scalar1=s1, scalar2=None, op0=op0), R, W)
        else:
            self.S.op(eng, lambda e: e.tensor_scalar(out=out, in0=a, scalar1=s1, scalar2=s2, op0=op0, op1=op1), R, W)

    def stt(self, out, a, s, b, op0, op1, R, W):
        self.S.op("dve", lambda e: e.scalar_tensor_tensor(out=out, in0=a, scalar=s, in1=b, op0=op0, op1=op1), R, W)

    def cp(self, out, in_, R, W, eng="pool"):
        if eng == "act":
            self.S.op("act", lambda e: e.copy(out=out, in_=in_), R, W)
        else:
            self.S.op(eng, lambda e: e.tensor_copy(out, in_), R, W)

    def wgroup(self, name, l, which=0):
        per = {"wgu": 44, "wdn": 8, "win": 28, "wgate": 32, "wbr": 32, "wout": 8, "wplg": 8, "wplp": 8, "wglu": 2}[name]
        idx = (l * 2 + which) if name in ("wgu", "wdn") else l
        key = (name, idx)
        if key not in self.wbg:
            self.wbg[key] = Buf("%s_b%d" % (name, idx), self.wb[name].t)
        return idx * per * 128, (idx + 1) * per * 128, self.wbg[key]

    def convert(self, groups):
        S = self.S
        for g in groups:
            name = g[0]
            r0g, r1g, dstb = self.wgroup(*g)
            c = BIGW[name][1]
            nn = min(max(1, 4096 // c), (r1g - r0g) // 128)
            src = self.D[name]
            for r0 in range(r0g, r1g, nn * 128):
                stg = self.cstg[self.conv_i % 2]
                self.conv_i += 1
                S.dma("pool", stg.t[:, 0:nn * c].rearrange("p (n c) -> p n c", c=c),
                      src.t[r0:r0 + nn * 128, :].rearrange("(n p) c -> p n c", p=128), [], [stg], stg)
                if self.conv_pending is not None:
                    self.conv_pending()
                self.conv_pending = (lambda stg=stg, dstb=dstb, r0=r0, nn=nn, c=c: S.dma(
                    "pool", dstb.t[r0:r0 + nn * 128, :].rearrange("(n p) c -> p n c", p=128),
                    stg.t[:, 0:nn * c].rearrange("p (n c) -> p n c", c=c), [stg], [dstb], dstb))
        if self.conv_pending is not None:
            self.conv_pending()
            self.conv_pending = None

    def loadw(self, name, tile, K):
        per = {"wgu": 44, "wdn": 8, "win": 28, "wgate": 32, "wbr": 32, "wout": 8, "wplg": 8, "wplp": 8, "wglu": 2}[name]
        src = self.wbg[(name, tile // per)]
        ap = src.t[tile * 128:(tile + 1) * 128, :].rearrange("p (k c) -> p k c", c=128)
        if K > 8:
            b = self.w22[self.wi22 % len(self.w22)]
            self.wi22 += 1
        else:
            b = self.w8[self.wi8 % len(self.w8)]
            self.wi8 += 1
        self.S.dma("sp", b.t[:, 0:K, :], ap, [src], [b], b)
        return b

    def tmp(self, slot0, nslots, shape, dt=F32):
        ap = self.scr_t[:, slot0 * 512:(slot0 + nslots) * 512]
        if dt == BF16:
            ap = ap.bitcast(BF16)
        n = 1
        for d in shape[1:]:
            n *= d
        ap = ap[:, 0:n]
        if len(shape) == 3:
            ap = ap.rearrange("p (a b) -> p a b", b=shape[2])
        elif len(shape) == 4:
            ap = ap.rearrange("p (a b c) -> p a b c", b=shape[2], c=shape[3])
        return Tmp(ap, self.slots[slot0:slot0 + nslots])

    def declare(self):
        nc, S = self.nc, self.S
        D = {}

        def din(name, shape, dt=F32):
            D[name] = Buf(name, nc.dram_tensor(name, list(shape), dt, kind="ExternalInput").ap())
            return D[name]
        self.D = D
        din("xT", [2, 128, 8, SEQ])
        din("pT", [NL, 2, 128, 2, SEQ])
        din("gains", [128, 9, 8])
        din("consts", [128, 512])
        din("s5st", [NL, 128, 8, 3])
        din("s5rep", [NL, 3, 128, 8, 128])
        din("s5bexp", [NL, 2, 128, 8, 128])
        din("s5cexp", [NL, 2, 128, 8, 128])
        din("s5db", [NL, 128, 4])
        din("posr", [2, 128, SEQ], I32)
        din("consts2", [128, 512])
        din("wsmf", [128, NL, 8, 16])
        din("cwf", [128, NL, 40])
        din("mlrowf", [128, NL, 264])
        din("gdrowf", [128, NL, 72])
        din("gmaskf", [128, 256])
        din("pc", [128, 8])
        din("cmf", [128, 2, 256])
        for n, (r, c) in BIGW.items():
            din(n, [r, c])
        self.out = Buf("out", nc.dram_tensor("out", [2, 128, 8, SEQ], F32, kind="ExternalOutput").ap())
        self.wb = {}
        for n, (r, c) in BIGW.items():
            self.wb[n] = Buf(n + "_b", nc.dram_tensor(n + "_b", [r, c], BF16, kind="Internal").ap())
        self.s5f_d = Buf("s5f_d", nc.dram_tensor("s5f_d", [NL, 128, 3104], F32, kind="Internal").ap())
        self.s5b_d = Buf("s5b_d", nc.dram_tensor("s5b_d", [NL, 128, 4352], BF16, kind="Internal").ap())
        if "yin" in self.dbg:
            din("yin", [8, 128, 4, 2, NT], BF16)
        if "dumpy" in self.dbg:
            self.dumpy = Buf("dumpy", nc.dram_tensor("dumpy", [8, 128, 4, 2, NT], BF16, kind="ExternalOutput").ap())
        if "dump" in self.dbg:
            self.dump = Buf("dump", nc.dram_tensor("dump", self.dbg["dump"], F32, kind="ExternalOutput").ap())

        self.h = S.sb("h", [128, 8, NT], F32)
        self.u = S.sb("u", [128, 8, NT], BF16)
        NS = 24
        self.scr_t = S.sb("scr", [128, NS * 512], F32).t
        self.slots = [Buf("slot%d" % i) for i in range(NS)]
        self.actb = self.tmp(0, 11, [128, 22, NT], BF16)
        self.rstd = S.sb("rstd", [128, NT], F32)
        self.sg = [S.sb("sg%d" % i, [128, NT], F32) for i in range(2)]
        self.sg2 = [S.sb("sg2%d" % i, [128, NT], F32) for i in range(2)]
        self.macc = self.tmp(12, 1, [128, NT])
        self.ho = [self.tmp(13 + i, 1, [128, NT]) for i in range(2)]
        self.w8 = [S.sb("w8_%d" % i, [128, 8, 128], BF16) for i in range(6)]
        self.w22 = [S.sb("w22_%d" % i, [128, 22, 128], BF16) for i in range(2)]
        self.y = S.sb("y", [128, 4, 2, NT], BF16)
        self.pf = self.tmp(15, 2, [128, 2, NT])
        self.pb = self.tmp(17, 1, [128, 2, NT], BF16)
        self.gains = S.sb("gains", [128, 9, 8], F32)
        self.cst = S.sb("cst", [128, 8], F32)
        self.consts = S.sb("consts", [128, 512], F32)
        self.pc = S.sb("pc", [128, 8], F32)
        self.consts2 = S.sb("consts2", [128, 512], F32)
        self.wsm = S.sb("wsm", [128, NL, 8, 16], BF16)
        self.cwb = S.sb("cwb", [128, NL, 40], F32)
        self.mlrow = S.sb("mlrow", [128, NL, 264], F32)
        self.gdrow = S.sb("gdrow", [128, NL, 72], F32)
        self.mltail = [S.sb("mltail%d" % l, [128, 4, 3], F32) for l in range(NL)]
        self.gdtail = [S.sb("gdtail%d" % l, [128, 6, 3], F32) for l in range(NL)]
        self.mlC = [S.sb("mlC%d" % l, [128, 4, 66], F32) for l in range(NL)]
        self.mlm = [S.sb("mlm%d" % l, [128, 4], F32) for l in range(NL)]
        self.gdS = [S.sb("gdS%d" % l, [128, 4, 64], F32) for l in range(NL)]
        self.gmask = S.sb("gmask", [128, 256], F32)
        self.negones = S.sb("negones", [128, 64], F32)
        self.cm = S.sb("cm", [128, 2, 256], BF16)
        self.ident_bf = S.sb("ident_bf", [128, 128], BF16)
        self.kcache = [S.sb("kcache%d" % l, [128, 2, SEQ], BF16) for l in range(NL)]
        self.vcache = [S.sb("vcache%d" % l, [128, 16, 256], BF16) for l in range(NL)]
        self.kmean = [S.sb("kmean%d" % l, [128, 2, 8], F32) for l in range(NL)]
        self.s5f = S.sb("s5f", [128, 3104], F32)
        self.s5b = S.sb("s5b", [128, 4352], BF16)
        f = self.s5f.t
        self.s5F = {"CS": f[:, 0:2048].rearrange("p (a b c) -> p a b c", b=2, c=128),
                    "R": f[:, 2048:3072].rearrange("p (a b) -> p a b", b=128),
                    "E128": f[:, 3072:3088].rearrange("p (a b) -> p a b", b=2),
                    "rcol": f[:, 3088:3096], "dbg": f[:, 3096:3100]}
        b = self.s5b.t
        self.s5B = {"BT": b[:, 0:2048].rearrange("p (a b c) -> p a b c", b=2, c=128),
                    "CT": b[:, 2048:4096].rearrange("p (a b c) -> p a b c", b=2, c=128),
                    "Dg": b[:, 4096:4352].rearrange("p (a b) -> p a b", b=128)}
        self.s5state = [S.sb("s5st%d" % l, [128, 2, 8], F32) for l in range(NL)]
        self.ones_bf = S.sb("ones_bf", [128, 128], BF16)
        self.P = [S.ps("P%d" % i, [128, NT], F32) for i in range(8)]
        self.cstg = [S.sb("cstg%d" % i, [128, 4096], BF16) for i in range(2)]

    def setup(self):
        S = self.S
        S.op("pool", lambda e: e.memset(self.ones_bf.t[:], 1.0), [], [self.ones_bf])
        S.op("pool", lambda e: e.memset(self.cst.t[:, 0:1], 1e-6), [], [self.cst])
        S.op("pool", lambda e: e.memset(self.cst.t[:, 1:2], -PI), [], [self.cst])
        S.dma("sp", self.pc.t[:], self.D["pc"].t, [], [self.pc], self.pc)
        S.op("pool", lambda e: e.memset(self.cst.t[:, 3:4], 1.0), [], [self.cst])
        S.dma("sp", self.consts2.t[:], self.D["consts2"].t, [], [self.consts2], self.consts2)
        S.dma("sp", self.cwb.t[:], self.D["cwf"].t, [], [self.cwb], self.cwb)
        S.dma("sp", self.mlrow.t[:], self.D["mlrowf"].t, [], [self.mlrow], self.mlrow)
        S.dma("sp", self.gdrow.t[:], self.D["gdrowf"].t, [], [self.gdrow], self.gdrow)
        S.dma("sp", self.gmask.t[:], self.D["gmaskf"].t, [], [self.gmask], self.gmask)
        S.op("pool", lambda e: e.memset(self.negones.t[:], -1.0), [], [self.negones])
        wsf = self.tmp(13, 1, [128, NL, 8, 16])
        S.dma("sp", wsf.t, self.D["wsmf"].t, [], [wsf], wsf)
        self.cp(self.wsm.t[:], wsf.t, [wsf], [self.wsm], eng="dve")
        cmf = self.tmp(12, 1, [128, 2, 256])
        S.dma("sp", cmf.t, self.D["cmf"].t, [], [cmf], cmf)
        self.cp(self.cm.t[:], cmf.t, [cmf], [self.cm], eng="dve")
        for l in range(NL):
            S.op("pool", lambda e, l=l: e.memset(self.kmean[l].t[:], 0.0), [], [self.kmean[l]])
        S.dma("sp", self.gains.t[:], self.D["gains"].t, [], [self.gains], self.gains)
        S.dma("sp", self.consts.t[:], self.D["consts"].t, [], [self.consts], self.consts)
        self.cp(self.ident_bf.t[:], self.consts.t[:, 128:256], [self.consts], [self.ident_bf], eng="dve")
        self.conv_i = 0
        self.conv_pending = None
        self.convert([("wgu", 0, 0), ("wdn", 0, 0), ("win", 0), ("wglu", 0), ("wgate", 0), ("wbr", 0), ("wout", 0),
                      ("wgu", 0, 1), ("wdn", 0, 1), ("wplg", 0), ("wplp", 0),
                      ("wgu", 1, 0), ("wdn", 1, 0), ("win", 1), ("wglu", 1), ("wgate", 1), ("wbr", 1), ("wout", 1),
                      ("wgu", 1, 1), ("wdn", 1, 1), ("wplg", 1), ("wplp", 1)])
        for l in range(NL):
            self.s5_setup(l)

    def norm(self, gcol):
        h, u, S = self.h, self.u, self.S
        pss, rstd = self.P[6], self.rstd
        self.act(u.t[:], h.t[:], AF.Square, [h], [u])
        for k in range(8):
            self.mm(pss.t[:], self.ones_bf.t[:], u.t[:, k, :], k == 0, k == 7, [u, self.ones_bf], [pss])
        self.act(rstd.t[:], pss.t[:], AF.Sqrt, [pss, self.cst], [rstd], scale=1.0 / 1024, bias=self.cst.t[:, 0:1])
        S.op("dve", lambda e: e.reciprocal(rstd.t[:], rstd.t[:]), [rstd], [rstd])
        for k in range(8):
            self.stt(u.t[:, k, :], h.t[:, k, :], self.gains.t[:, gcol, k:k + 1], rstd.t[:], ALU.mult, ALU.mult,
                     [h, rstd, self.gains], [u])

    def ffn(self, l, which):
        h, u, actb = self.h, self.u, self.actb
        self.norm(l * 4 + (0 if which == 0 else 2))
        base = (l * 2 + which) * 44
        for fc in range(22):
            wg = self.loadw("wgu", base + 2 * fc, 8)
            wu = self.loadw("wgu", base + 2 * fc + 1, 8)
            pg, pu = self.P[fc % 2], self.P[2 + fc % 2]
            sg = self.sg[fc % 2]
            for k in range(8):
                self.mm(pg.t[:], wg.t[:, k, :], u.t[:, k, :], k == 0, k == 7, [wg, u], [pg])
            for k in range(8):
                self.mm(pu.t[:], wu.t[:, k, :], u.t[:, k, :], k == 0, k == 7, [wu, u], [pu])
            self.act(sg.t[:], pg.t[:], AF.Silu, [pg], [sg])
            self.tt(actb.t[:, fc, :], sg.t[:], pu.t[:], ALU.mult, [sg, pu], [actb])
        base = (l * 2 + which) * 8
        for oc in range(8):
            wd = self.loadw("wdn", base + oc, 22)
            po = self.P[4 + oc % 2]
            for fc in range(22):
                self.mm(po.t[:], wd.t[:, fc, :], actb.t[:, fc, :], fc == 0, fc == 21, [wd, actb], [po])
            self.stt(h.t[:, oc, :], po.t[:], 0.5, h.t[:, oc, :], ALU.mult, ALU.add, [po, h], [h])

    def ple(self, l, s, t0):
        h, u, S = self.h, self.u, self.S
        self.norm(l * 4 + 3)
        S.dma("sp", self.pf.t[:], self.D["pT"].t[l, s, :, :, t0:t0 + NT], [], [self.pf], self.pf)
        self.cp(self.pb.t[:], self.pf.t[:], [self.pf], [self.pb], eng="pool")
        for oc in range(8):
            wg = self.loadw("wplg", l * 8 + oc, 8)
            wp = self.loadw("wplp", l * 8 + oc, 2)
            pa, pg = self.P[oc % 2], self.P[2 + oc % 2]
            sg, sg2 = self.sg[oc % 2], self.sg2[oc % 2]
            for k in range(2):
                self.mm(pa.t[:], wp.t[:, k, :], self.pb.t[:, k, :], k == 0, k == 1, [wp, self.pb], [pa])
            for k in range(8):
                self.mm(pg.t[:], wg.t[:, k, :], u.t[:, k, :], k == 0, k == 7, [wg, u], [pg])
            self.act(sg.t[:], pg.t[:], AF.Sigmoid, [pg], [sg])
            self.tt(sg2.t[:], sg.t[:], pa.t[:], ALU.mult, [sg, pa], [sg2])
            self.tt(h.t[:, oc, :], h.t[:, oc, :], sg2.t[:], ALU.add, [h, sg2], [h], eng="pool")

    def merge(self, l):
        h, u, y, actb = self.h, self.u, self.y, self.actb
        macc = self.macc
        for oc in range(8):
            for b in range(4):
                wg = self.loadw("wgate", (l * 8 + oc) * 4 + b, 8)
                wbr = self.loadw("wbr", (l * 8 + oc) * 4 + b, 2)
                pg, pbr = self.P[b % 2], self.P[2 + b % 2]
                sg, sg2 = self.sg[b % 2], self.sg2[b % 2]
                for k in range(8):
                    self.mm(pg.t[:], wg.t[:, k, :], u.t[:, k, :], k == 0, k == 7, [wg, u], [pg])
                for k in range(2):
                    self.mm(pbr.t[:], wbr.t[:, k, :], y.t[:, b, k, :], k == 0, k == 1, [wbr, y], [pbr])
                self.act(sg.t[:], pg.t[:], AF.Sigmoid, [pg], [sg])
                if b == 0:
                    self.tt(macc.t[:], sg.t[:], pbr.t[:], ALU.mult, [sg, pbr], [macc])
                else:
                    self.tt(sg2.t[:], sg.t[:], pbr.t[:], ALU.mult, [sg, pbr], [sg2])
                    if b < 3:
                        self.tt(macc.t[:], macc.t[:], sg2.t[:], ALU.add, [macc, sg2], [macc], eng="pool")
                    else:
                        self.tt(actb.t[:, oc, :], macc.t[:], sg2.t[:], ALU.add, [macc, sg2], [actb], eng="pool")
        for oc in range(8):
            wo = self.loadw("wout", l * 8 + oc, 8)
            po = self.P[4 + oc % 2]
            for k in range(8):
                self.mm(po.t[:], wo.t[:, k, :], actb.t[:, k, :], k == 0, k == 7, [wo, actb], [po])
            self.tt(h.t[:, oc, :], h.t[:, oc, :], po.t[:], ALU.add, [h, po], [h])

    def final(self, s, t0):
        h, S = self.h, self.S
        pss, rstd = self.P[6], self.rstd
        u = self.u
        self.act(u.t[:], h.t[:], AF.Square, [h], [u])
        for k in range(8):
            self.mm(pss.t[:], self.ones_bf.t[:], u.t[:, k, :], k == 0, k == 7, [u, self.ones_bf], [pss])
        self.act(rstd.t[:], pss.t[:], AF.Sqrt, [pss, self.cst], [rstd], scale=1.0 / 1024, bias=self.cst.t[:, 0:1])
        S.op("dve", lambda e: e.reciprocal(rstd.t[:], rstd.t[:]), [rstd], [rstd])
        for k in range(8):
            ho = self.ho[k % 2]
            self.stt(ho.t[:], h.t[:, k, :], self.gains.t[:, 8, k:k + 1], rstd.t[:], ALU.mult, ALU.mult,
                     [h, rstd, self.gains], [ho])
            S.dma("sp", self.out.t[s, :, k, t0:t0 + NT], ho.t[:], [ho], [self.out], ho)

    def dump_h(self, idx):
        S = self.S
        S.dma("sp", self.dump.t[idx], self.h.t[:], [self.h], [self.dump], self.h)


    def dd(self, name, src, R, shape, dt=F32):
        if name not in self.dbg.get("dd", ()):
            return
        b = Buf(name, self.nc.dram_tensor("dd_" + name, list(shape), dt, kind="ExternalOutput").ap())
        self.S.dma("sp", b.t, src, R, [b], b)

    def frac2pi(self, out, x, shift, tB, R, W):
        MAG = 12582912.0
        self.ts(out, x, 1.0 / (2 * PI), ALU.mult, R, W, s2=shift / (2 * PI), op1=ALU.add)
        self.ts(tB, out, MAG, ALU.add, R, W)
        self.ts(tB, tB, -MAG, ALU.add, R, W)
        self.tt(out, out, tB, ALU.subtract, R, W)

    def s5_disc(self, lr, li, ldt, T, TB, want_z):
        R = TB + [self.s5in]
        W = TB
        self.act(T[0], ldt, AF.Exp, R, W)
        self.tt(T[5], lr, T[0], ALU.mult, R, W)
        self.act(T[1], T[5], AF.Exp, R, W)
        self.tt(T[2], li, T[0], ALU.mult, R, W)
        if not want_z:
            self.frac2pi(T[5], T[2], 0.0, T[8], R, W)
            self.ts(T[2], T[5], 2 * PI, ALU.mult, R, W)
            return T[1], T[2], None, None
        self.frac2pi(T[5], T[2], 0.0, T[8], R, W)
        self.act(T[3], T[5], AF.Sin, R, W, scale=2 * PI)
        self.frac2pi(T[5], T[2], 0.5 * PI, T[8], R, W)
        self.act(T[4], T[5], AF.Sin, R, W, scale=2 * PI)
        self.tt(T[4], T[4], T[1], ALU.mult, R, W)
        self.tt(T[3], T[3], T[1], ALU.mult, R, W)
        self.ts(T[5], T[4], -1.0, ALU.add, R, W)
        self.tt(T[8], lr, lr, ALU.mult, R, W)
        self.tt(T[0], li, li, ALU.mult, R, W)
        self.tt(T[8], T[8], T[0], ALU.add, R, W)
        self.S.op("dve", lambda e: e.reciprocal(T[8], T[8]), R, W)
        self.tt(T[0], T[5], lr, ALU.mult, R, W)
        self.tt(T[6], T[3], li, ALU.mult, R, W)
        self.tt(T[6], T[6], T[0], ALU.add, R, W)
        self.tt(T[6], T[6], T[8], ALU.mult, R, W)
        self.tt(T[0], T[3], lr, ALU.mult, R, W)
        self.tt(T[7], T[5], li, ALU.mult, R, W)
        self.tt(T[7], T[0], T[7], ALU.subtract, R, W)
        self.tt(T[7], T[7], T[8], ALU.mult, R, W)
        return T[1], T[2], T[6], T[7]

    def s5_setup(self, l):
        S, D = self.S, self.D
        s5f, s5b, cst = self.s5f, self.s5b, self.cst
        F = self.s5F
        stt_ = self.tmp(12, 1, [128, 8, 3])
        self.s5in = stt_.bufs[0]
        S.dma("sp", stt_.t, D["s5st"].t[l], [], [stt_], stt_)
        T9 = self.tmp(13, 1, [128, 9, 8])
        T = [T9.t[:, i, :] for i in range(9)]
        mag, th, _, _ = self.s5_disc(stt_.t[:, :, 0], stt_.t[:, :, 1], stt_.t[:, :, 2], T, [T9.bufs[0]], False)
        R9 = [T9]
        X = self.tmp(14, 2, [128, 8, 128])
        Y = self.tmp(16, 2, [128, 8, 128])
        Z = self.tmp(18, 2, [128, 8, 128])
        jrow = self.consts.t[:, 0:128]
        for sc in range(8):
            self.ts(X.t[:, sc, :], jrow, th[:, sc:sc + 1], ALU.mult, R9 + [self.consts], [X])
        self.frac2pi(Y.t, X.t, 0.0, Z.t, [X, Y, Z], [Y, Z])
        self.act(F["CS"][:, :, 1, :], Y.t, AF.Sin, [Y], [s5f], scale=2 * PI)
        self.frac2pi(Y.t, X.t, 0.5 * PI, Z.t, [X, Y, Z], [Y, Z])
        self.act(F["CS"][:, :, 0, :], Y.t, AF.Sin, [Y], [s5f], scale=2 * PI)
        self.ts(T[3], th, 128.0, ALU.mult, R9, R9)
        self.frac2pi(T[4], T[3], 0.0, T[5], R9, R9)
        self.act(F["E128"][:, :, 1], T[4], AF.Sin, R9, [s5f], scale=2 * PI)
        self.frac2pi(T[4], T[3], 0.5 * PI, T[5], R9, R9)
        self.act(F["E128"][:, :, 0], T[4], AF.Sin, R9, [s5f], scale=2 * PI)
        self.cp(F["rcol"], mag, R9, [s5f], eng="dve")
        S.op("dve", lambda e: e.memset(F["R"], 0.0), [], [s5f])
        ones = self.consts.t[:, 384:511]
        for sc in range(8):
            self.ts(F["R"][:, sc, 1:128], ones, mag[:, sc:sc + 1], ALU.mult, R9 + [self.consts], [s5f])
        S.dma("sp", F["dbg"], D["s5db"].t[l], [], [s5f], s5f)
        for hh in range(2):
            prm = self.tmp(12, 3, [128, 3, 512])
            self.s5in = prm.bufs[0]
            S.dma("sp", prm.t.rearrange("p a (s c) -> p a s c", c=128),
                  D["s5rep"].t[l, :, :, 4 * hh:4 * hh + 4, :].rearrange("a p s c -> p a s c"), [], [prm], prm)
            TT_ = self.tmp(15, 9, [128, 9, 512])
            T = [TT_.t[:, i, :] for i in range(9)]
            TB = TT_.bufs + prm.bufs[1:]
            _, _, zr, zi = self.s5_disc(prm.t[:, 0, :], prm.t[:, 1, :], prm.t[:, 2, :], T, TB, True)
            bx = self.tmp(12, 2, [128, 2, 512])
            S.dma("sp", bx.t.rearrange("p a (s c) -> p a s c", c=128),
                  D["s5bexp"].t[l, :, :, 4 * hh:4 * hh + 4, :].rearrange("a p s c -> p a s c"), [], [bx], bx)
            RR = TB + bx.bufs
            self.tt(T[0], zr, bx.t[:, 0, :], ALU.mult, RR, TB)
            self.tt(T[1], zi, bx.t[:, 1, :], ALU.mult, RR, TB)
            self.tt(F32v(self, "BTre", hh), T[0], T[1], ALU.subtract, RR, [s5b])
            self.tt(T[0], zr, bx.t[:, 1, :], ALU.mult, RR, TB)
            self.tt(T[1], zi, bx.t[:, 0, :], ALU.mult, RR, TB)
            self.tt(F32v(self, "BTim", hh), T[0], T[1], ALU.add, RR, [s5b])
            cx = self.tmp(12, 2, [128, 2, 512])
            S.dma("sp", cx.t.rearrange("p a (s c) -> p a s c", c=128),
                  D["s5cexp"].t[l, :, :, 4 * hh:4 * hh + 4, :].rearrange("a p s c -> p a s c"), [], [cx], cx)
            self.cp(F32v(self, "CTre", hh), cx.t[:, 0, :], [cx], [s5b], eng="dve")
            self.ts(F32v(self, "CTim", hh), cx.t[:, 1, :], -1.0, ALU.mult, [cx], [s5b])
        ident = self.consts.t[:, 128:256]
        for kc in range(2):
            self.ts(self.s5B["Dg"][:, kc, :], ident, F["dbg"][:, kc:kc + 1], ALU.mult, [s5f, self.consts], [s5b])
        S.dma("sp", self.s5f_d.t[l], s5f.t[:], [s5f], [self.s5f_d], s5f)
        S.dma("sp", self.s5b_d.t[l], s5b.t[:], [s5b], [self.s5b_d], s5b)

    def s5_fwd(self, l, s, tb):
        S, u, y = self.S, self.u, self.y
        s5f, s5b = self.s5f, self.s5b
        F, B = self.s5F, self.s5B
        st = self.s5state[l]
        S.dma("sp", s5f.t[:], self.s5f_d.t[l], [self.s5f_d], [s5f], s5f)
        S.dma("sp", s5b.t[:], self.s5b_d.t[l], [self.s5b_d], [s5b], s5b)
        us5 = self.tmp(12, 1, [128, 2, NT], BF16)
        for kc in range(2):
            w = self.loadw("win", l * 28 + 16 + kc, 8)
            pp = self.P[kc]
            for k in range(8):
                self.mm(pp.t[:], w.t[:, k, :], u.t[:, k, :], k == 0, k == 7, [w, u], [pp])
            self.cp(us5.t[:, kc, :], pp.t[:], [pp], [us5], eng="act")
        A = self.tmp(13, 1, [128, 4, 128])
        Bt = self.tmp(14, 1, [128, 4, 128])
        bh = [self.tmp(15, 2, [128, 8, 128]), self.tmp(17, 2, [128, 8, 128])]
        xh = [self.tmp(19, 2, [128, 8, 128]), self.tmp(21, 2, [128, 8, 128])]
        xb = [self.tmp(23, 1, [128, 8, 128], BF16), self.tmp(0, 1, [128, 8, 128], BF16)]
        ini = self.tmp(1, 1, [128, 4, 8])
        A2 = self.tmp(2, 1, [128, 4, 128])
        B2 = self.tmp(3, 1, [128, 4, 128])
        ypre = [self.P[4], self.P[5]]
        CS = F["CS"]
        if tb == 0:
            S.op("dve", lambda e: e.memset(st.t[:], 0.0), [], [st])
        for sub in range(4):
            c0 = sub * 128
            for sc in range(8):
                for ri in range(2):
                    pp = self.P[2 * ri + sc // 4]
                    self.mm(pp.t[:, (sc % 4) * 128:(sc % 4 + 1) * 128], B["BT"][:, sc, ri, :],
                            us5.t[:, sc // 4, c0:c0 + 128], True, True, [s5b, us5], [pp])
            for hh in range(2):
                c = CS[:, 4 * hh:4 * hh + 4, 0, :]
                sn = CS[:, 4 * hh:4 * hh + 4, 1, :]
                pre = self.P[hh].t[:].rearrange("p (a b) -> p a b", b=128)
                pim = self.P[2 + hh].t[:].rearrange("p (a b) -> p a b", b=128)
                self.tt(A.t, pre, c, ALU.mult, [self.P[hh], s5f], [A])
                self.tt(Bt.t, pim, sn, ALU.mult, [self.P[2 + hh], s5f], [Bt])
                self.tt(bh[0].t[:, 4 * hh:4 * hh + 4, :], A.t, Bt.t, ALU.add, [A, Bt], [bh[0]])
                self.tt(A.t, pim, c, ALU.mult, [self.P[2 + hh], s5f], [A])
                self.tt(Bt.t, pre, sn, ALU.mult, [self.P[hh], s5f], [Bt])
                self.tt(bh[1].t[:, 4 * hh:4 * hh + 4, :], A.t, Bt.t, ALU.subtract, [A, Bt], [bh[1]])
            if not (tb == 0 and sub == 0):
                i0, i1, i2, i3 = (ini.t[:, i, :] for i in range(4))
                c1, s1 = F["E128"][:, :, 0], F["E128"][:, :, 1]
                self.tt(i0, c1, st.t[:, 0, :], ALU.mult, [s5f, st], [ini])
                self.tt(i1, s1, st.t[:, 1, :], ALU.mult, [s5f, st], [ini])
                self.tt(i0, i0, i1, ALU.subtract, [ini], [ini])
                self.tt(i2, c1, st.t[:, 1, :], ALU.mult, [s5f, st], [ini])
                self.tt(i3, s1, st.t[:, 0, :], ALU.mult, [s5f, st], [ini])
                self.tt(i2, i2, i3, ALU.add, [ini], [ini])
                self.tt(i0, i0, F["rcol"], ALU.mult, [ini, s5f], [ini])
                self.tt(i2, i2, F["rcol"], ALU.mult, [ini, s5f], [ini])
                self.tt(bh[0].t[:, :, 0], bh[0].t[:, :, 0], i0, ALU.add, [bh[0], ini], [bh[0]])
                self.tt(bh[1].t[:, :, 0], bh[1].t[:, :, 0], i2, ALU.add, [bh[1], ini], [bh[1]])
            Rf = F["R"].rearrange("p a b -> p (a b)")
            for ri in range(2):
                S.op("dve", lambda e, ri=ri: e.tensor_tensor_scan(
                    out=xh[ri].t.rearrange("p a b -> p (a b)"), data0=Rf,
                    data1=bh[ri].t.rearrange("p a b -> p (a b)"), initial=0.0, op0=ALU.mult, op1=ALU.add),
                    [bh[ri], s5f], [xh[ri]])
                self.cp(st.t[:, ri, :], xh[ri].t[:, :, 127], [xh[ri]], [st], eng="dve")
            for hh in range(2):
                c = CS[:, 4 * hh:4 * hh + 4, 0, :]
                sn = CS[:, 4 * hh:4 * hh + 4, 1, :]
                hs = slice(4 * hh, 4 * hh + 4)
                self.tt(A2.t, xh[0].t[:, hs, :], c, ALU.mult, [xh[0], s5f], [A2], eng="pool")
                self.tt(B2.t, xh[1].t[:, hs, :], sn, ALU.mult, [xh[1], s5f], [B2], eng="pool")
                self.tt(xb[0].t[:, hs, :], A2.t, B2.t, ALU.subtract, [A2, B2], [xb[0]], eng="pool")
                self.tt(A2.t, xh[1].t[:, hs, :], c, ALU.mult, [xh[1], s5f], [A2], eng="pool")
                self.tt(B2.t, xh[0].t[:, hs, :], sn, ALU.mult, [xh[0], s5f], [B2], eng="pool")
                self.tt(xb[1].t[:, hs, :], A2.t, B2.t, ALU.add, [A2, B2], [xb[1]], eng="pool")
            for kc in range(2):
                pp = ypre[kc]
                o = pp.t[:, c0:c0 + 128]
                n = 0
                for sc in range(4 * kc, 4 * kc + 4):
                    for ri in range(2):
                        self.mm(o, B["CT"][:, sc, ri, :], xb[ri].t[:, sc, :], n == 0, False, [s5b, xb[ri]], [pp])
                        n += 1
                self.mm(o, B["Dg"][:, kc, :], us5.t[:, kc, c0:c0 + 128], False, True, [s5b, us5], [pp])
        yg = self.tmp(13, 2, [128, 2, NT])
        t1 = self.tmp(15, 2, [128, 2, NT])
        ygb = self.tmp(17, 1, [128, 2, NT], BF16)
        for kc in range(2):
            self.cp(yg.t[:, kc, :], ypre[kc].t[:], [ypre[kc]], [yg], eng="act")
        self.act(t1.t, yg.t, AF.Square, [yg], [t1])
        self.ts(t1.t, t1.t, 0.044715, ALU.mult, [t1], [t1], s2=1.0, op1=ALU.add)
        self.tt(t1.t, t1.t, yg.t, ALU.mult, [t1, yg], [t1])
        self.act(t1.t, t1.t, AF.Sigmoid, [t1], [t1], scale=1.5957691216)
        self.tt(yg.t, yg.t, t1.t, ALU.mult, [t1, yg], [yg])
        self.cp(ygb.t, yg.t, [yg], [ygb], eng="pool")
        for oc in range(2):
            w = self.loadw("wglu", l * 2 + oc, 2)
            pp = self.P[6 + oc]
            for k in range(2):
                self.mm(pp.t[:], w.t[:, k, :], ygb.t[:, k, :], k == 0, k == 1, [w, ygb], [pp])
            self.act(t1.t[:, oc, :], pp.t[:], AF.Sigmoid, [pp, s5f], [t1], bias=F["dbg"][:, 2 + oc:3 + oc])
            self.tt(y.t[:, 2, oc, :], yg.t[:, oc, :], t1.t[:, oc, :], ALU.mult, [yg, t1], [y])


    def moba_fwd(self, l, s, tb):
        S, u, y, D = self.S, self.u, self.y, self.D
        t0 = tb * NT
        kc_, vc_, km = self.kcache[l], self.vcache[l], self.kmean[l]
        pc, consts = self.pc, self.consts
        cosT = self.tmp(0, 1, [128, NT])
        sinT = self.tmp(1, 1, [128, NT])
        posi = self.tmp(2, 1, [128, NT])
        ang = self.tmp(3, 1, [128, NT])
        fr = self.tmp(4, 1, [128, NT])
        fb = self.tmp(5, 1, [128, NT])
        qf = [self.tmp(6, 1, [128, NT]), self.tmp(7, 1, [128, NT])]
        kf = [self.tmp(8, 1, [128, NT]), self.tmp(9, 1, [128, NT])]
        t1 = self.tmp(10, 1, [128, NT])
        t2 = self.tmp(12, 1, [128, NT])
        qb_ = self.tmp(13, 1, [128, 2, NT], BF16)
        sm = self.tmp(14, 1, [128, 512])
        gm = sm.t[:, 0:32].rearrange("p (a b) -> p a b", b=8)
        mx = sm.t[:, 32:64].rearrange("p (a b) -> p a b", b=8)
        mnegb = sm.t[:, 64:128].bitcast(BF16).rearrange("p (a b) -> p a b", b=32)
        et = [self.tmp(15, 1, [128, 1024], BF16)]
        ets = [et[0].t[:, 0:512].rearrange("p (a b) -> p a b", b=256), et[0].t[:, 512:1024].rearrange("p (a b) -> p a b", b=256)]
        rden = self.tmp(16, 1, [128, 2, 256])
        S.dma("sp", posi.t.bitcast(I32), D["posr"].t[s, :, t0:t0 + NT], [], [posi], posi)
        self.cp(ang.t, posi.t.bitcast(I32), [posi], [ang], eng="dve")
        self.ts(ang.t, ang.t, pc.t[:, 0:1], ALU.mult, [ang, pc], [ang])
        self.frac2pi(fr.t, ang.t, 0.5 * PI, fb.t, [ang, fr, fb], [fr, fb])
        self.act(cosT.t, fr.t, AF.Sin, [fr], [cosT], scale=2 * PI)
        self.frac2pi(fr.t, ang.t, 0.0, fb.t, [ang, fr, fb], [fr, fb])
        self.act(sinT.t, fr.t, AF.Sin, [fr, pc], [sinT], scale=pc.t[:, 2:3])
        for c in range(2):
            for (dst, base) in ((qf[c], 18), (kf[c], 20)):
                w1 = self.loadw("win", l * 28 + base + c, 8)
                w2 = self.loadw("win", l * 28 + base + 6 + c, 8)
                p1, p2 = self.P[0], self.P[1]
                for k in range(8):
                    self.mm(p1.t[:], w1.t[:, k, :], u.t[:, k, :], k == 0, k == 7, [w1, u], [p1])
                for k in range(8):
                    self.mm(p2.t[:], w2.t[:, k, :], u.t[:, k, :], k == 0, k == 7, [w2, u], [p2])
                self.tt(t1.t, p1.t[:], cosT.t, ALU.mult, [p1, cosT], [t1])
                self.tt(t2.t, p2.t[:], sinT.t, ALU.mult, [p2, sinT], [t2])
                self.tt(dst.t, t1.t, t2.t, ALU.add, [t1, t2], [dst], eng="pool")
            self.cp(qb_.t[:, c, :], qf[c].t, [qf[c]], [qb_], eng="pool")
            self.cp(kc_.t[:, c, t0:t0 + NT], kf[c].t, [kf[c]], [kc_], eng="pool")
            S.op("dve", lambda e, c=c: e.tensor_reduce(out=km.t[:, c, 2 * tb:2 * tb + 2],
                                                      in_=kf[c].t.rearrange("p (a b) -> p a b", b=256),
                                                      axis=AX.X, op=ALU.add), [kf[c]], [km])
        self.ts(km.t[:, :, 2 * tb:2 * tb + 2], km.t[:, :, 2 * tb:2 * tb + 2], 1.0 / 256, ALU.mult, [km], [km])
        wv = [self.loadw("win", l * 28 + 22 + i, 8) for i in range(2)]
        for tt_ in range(4):
            pv = self.P[2 + tt_ % 2]
            for i in range(2):
                for k in range(8):
                    self.mm(pv.t[:, i * 128:(i + 1) * 128], u.t[:, k, tt_ * 128:(tt_ + 1) * 128], wv[i].t[:, k, :],
                            k == 0, k == 7, [wv[i], u], [pv])
            self.cp(vc_.t[:, tb * 4 + tt_, :], pv.t[:, 0:256], [pv], [vc_], eng="act")
        if l == 0 and tb == self.dbg.get("ddtb", 0):
            self.dd("cosT", cosT.t, [cosT], [128, NT])
            self.dd("sinT", sinT.t, [sinT], [128, NT])
            self.dd("qf0", qf[0].t, [qf[0]], [128, NT])
            self.dd("kf1", kf[1].t, [kf[1]], [128, NT])
            self.dd("km", km.t[:], [km], [128, 2, 8])
            self.dd("vc", vc_.t[:, tb * 4, :], [vc_], [128, 256], BF16)
        if tb > 0 or True:
            for qt in range(4):
                qblk = 2 * tb + qt // 2
                if qblk == 0:
                    continue
                pg = self.P[6]
                for h in range(4):
                    c, off = h // 2, (h % 2) * 64
                    self.mm(pg.t[:, h * 8:h * 8 + 8], qf[c].t[off:off + 64, qt * 128:(qt + 1) * 128],
                            km.t[off:off + 64, c, :], True, True, [qf[c], km], [pg])
                vm = consts.t[:, 256 + qblk * 8:256 + qblk * 8 + 8].unsqueeze(1).broadcast_to([128, 4, 8])
                self.tt(gm, pg.t[:, 0:32].rearrange("p (a b) -> p a b", b=8), vm, ALU.add, [pg, consts], [sm])
                for h in range(4):
                    S.op("dve", lambda e, h=h: e.max(out=mx[:, h, :], in_=gm[:, h, :]), [sm], [sm])
                for h in range(4):
                    self.ts(gm[:, h, :], gm[:, h, :], mx[:, h, 2:3], ALU.is_ge, [sm], [sm], s2=30000.0, op1=ALU.mult)
                self.ts(mnegb[:, qt, :], sm.t[:, 0:32], -30000.0, ALU.add, [sm], [sm])
                if l == 0 and tb == self.dbg.get("ddtb", 0) and qt == 3:
                    self.dd("sm", sm.t[:, 0:128], [sm], [128, 128])
        it = 0
        for c in range(2):
            for j in range(2):
                qblk = 2 * tb + j
                nkt = 2 * (qblk + 1)
                pacc, pden = self.P[2 + 2 * (it % 2)], self.P[3 + 2 * (it % 2)]
                it += 1
                qs = slice(j * 256, (j + 1) * 256)
                for kt in range(nkt):
                    n = kt // 2
                    ps_ = self.P[kt % 2]
                    e_ = ets[kt % 2]
                    for hh in range(2):
                        h, off = 2 * c + hh, hh * 64
                        o = ps_.t[:, hh * 256:(hh + 1) * 256]
                        self.mm(o, kc_.t[off:off + 64, c, kt * 128:(kt + 1) * 128], qb_.t[off:off + 64, c, qs],
                                True, False, [kc_, qb_], [ps_])
                        if n < qblk:
                            for q2 in range(2):
                                qt = 2 * j + q2
                                lh = mnegb[:, qt, h * 8 + n:h * 8 + n + 1].broadcast_to([128, 128])
                                self.mm(ps_.t[:, hh * 256 + q2 * 128:hh * 256 + (q2 + 1) * 128], lh, self.ident_bf.t[:],
                                        False, True, [sm, self.ident_bf], [ps_])
                        else:
                            self.mm(o, self.ident_bf.t[:], self.cm.t[:, kt % 2, :], False, True,
                                    [self.ident_bf, self.cm], [ps_])
                    if l == 0 and tb == self.dbg.get("ddtb", 0) and c == 0 and j == 0 and kt == 0 and "ps" in self.dbg.get("dd", ()):
                        dbgt = self.tmp(17, 1, [128, 512])
                        self.cp(dbgt.t, ps_.t[:], [ps_], [dbgt], eng="act")
                        self.dd("ps", dbgt.t, [dbgt], [128, 512])
                    self.act(e_, ps_.t[:].rearrange("p (a b) -> p a b", b=256), AF.Exp, [ps_], [et[0]], scale=0.125)
                    if l == 0 and tb == self.dbg.get("ddtb", 0) and c == 0 and j == 0 and kt == 0:
                        self.dd("et", et[0].t[:, 0:512], [et[0]], [128, 512], BF16)
                        self.dd("cm", self.cm.t[:], [self.cm], [128, 2, 256], BF16)
                    e2 = et[0].t[:, (kt % 2) * 512:(kt % 2 + 1) * 512]
                    self.mm(pacc.t[:], vc_.t[:, kt, c * 128:(c + 1) * 128], e2,
                            kt == 0, kt == nkt - 1, [vc_, et[0]], [pacc])
                    self.mm(pden.t[:], self.ones_bf.t[:], e2,
                            kt == 0, kt == nkt - 1, [self.ones_bf, et[0]], [pden])
                S.op("dve", lambda e, pden=pden: e.reciprocal(rden.t.rearrange("p a b -> p (a b)"), pden.t[:]), [pden], [rden])
                if l == 0 and tb == self.dbg.get("ddtb", 0):
                    self.dd("rden%d%d" % (c, j), rden.t, [rden], [128, 2, 256])
                for hh in range(2):
                    off = hh * 64
                    self.tt(y.t[off:off + 64, 3, c, qs], pacc.t[off:off + 64, hh * 256:(hh + 1) * 256],
                            rden.t[off:off + 64, hh, :], ALU.mult, [pacc, rden], [y])

    def tokproj_small(self, l):
        u, pp = self.u, self.P[7]
        for c in range(8):
            for k in range(8):
                self.mm(pp.t[0:64, c * 16:(c + 1) * 16], u.t[:, k, c * 64:(c + 1) * 64], self.wsm.t[:, l, k, :],
                        k == 0, k == 7, [u, self.wsm], [pp])
        self.sp = self.tmp(11, 1, [128, 512])
        self.cp(self.sp.t[0:64, 0:128], pp.t[0:64, 0:128], [pp], [self.sp], eng="act")
        return self.sp.t[0:64, 0:128].rearrange("p (c n) -> p c n", n=16)

    def conv_silu(self, l, tiles, nch, tail, cw, dst, tb):
        S, u = self.S, self.u
        xc = self.tmp(0, 7, [128, nch, NT + 3])
        acc = self.tmp(9, 1, [128, NT])
        if tb == 0:
            S.op("dve", lambda e: e.memset(tail.t[:], 0.0), [], [tail])
        self.cp(xc.t[:, :, 0:3], tail.t[:], [tail], [xc], eng="dve")
        for c in range(nch):
            w = self.loadw("win", l * 28 + tiles + c, 8)
            pp = self.P[c % 2]
            for k in range(8):
                self.mm(pp.t[:], w.t[:, k, :], u.t[:, k, :], k == 0, k == 7, [w, u], [pp])
            self.cp(xc.t[:, c, 3:NT + 3], pp.t[:], [pp], [xc], eng="act")
        self.cp(tail.t[:], xc.t[:, :, NT:NT + 3], [xc], [tail], eng="dve")
        for c in range(nch):
            self.ts(acc.t, xc.t[:, c, 0:NT], cw[:, c * 4:c * 4 + 1], ALU.mult, [xc, self.cwb], [acc])
            for j in range(1, 4):
                self.stt(acc.t, xc.t[:, c, j:NT + j], cw[:, c * 4 + j:c * 4 + j + 1], acc.t, ALU.mult, ALU.add,
                         [xc, acc, self.cwb], [acc])
            self.act(dst.t[:, c, :], acc.t, AF.Silu, [acc], [dst])

    def mlstm_fwd(self, l, s, tb, sp):
        S, u, y = self.S, self.u, self.y
        consts, c2, cst = self.consts, self.consts2, self.cst
        ident = consts.t[:, 128:256]
        ones = consts.t[:, 384:512]
        TRI = c2.t[0:64, 0:64]
        CMASK = c2.t[0:64, 64:128]
        rows = self.mlrow.t[0:64, l, :]
        Cx, mrep = self.mlC[l], self.mlm[l]
        qk = self.tmp(12, 4, [128, 4, NT])
        self.conv_silu(l, 8, 4, self.mltail[l], self.cwb.t[:, l, 24:40], qk, tb)
        self.ts(qk.t[:, 2:4, :], qk.t[:, 2:4, :], 0.125, ALU.mult, [qk], [qk])
        ms = self.dbg.get("mlstop", 99)
        if ms <= 1:
            return
        kz = self.tmp(4, 4, [128, 4, NT])
        S.op("dve", lambda e: e.memset(kz.t, 0.0), [], [kz])
        for h in range(4):
            pr, off = h // 2, (h % 2) * 64
            self.cp(kz.t[off:off + 64, h, :], qk.t[off:off + 64, 2 + pr, :], [qk], [kz], eng="dve")
        if tb == 0:
            S.op("dve", lambda e: e.memset(Cx.t[:], 0.0), [], [Cx])
            S.op("dve", lambda e: e.memset(mrep.t[:], 0.0), [], [mrep])
        A = self.tmp(16, 1, [128, 512])
        R_ = [A, self.sp]
        v3 = lambda lo: A.t[0:64, lo:lo + 32].rearrange("p (c h) -> p c h", h=4)
        li, lf, b_, ak, tx = v3(0), v3(32), v3(64), v3(96), v3(128)
        grep = A.t[:, 160:192].rearrange("p (c h) -> p c h", h=4)
        mkrep = A.t[:, 192:224].rearrange("p (c h) -> p c h", h=4)
        Mall = A.t[:, 224:260].rearrange("p (c h) -> p c h", h=4)
        scall = A.t[:, 260:292].rearrange("p (c h) -> p c h", h=4)
        kws = v3(292)
        mk32 = A.t[0:32, 324:325]
        dg32 = A.t[0:32, 328:360]
        ib = rows[:, 0:4].unsqueeze(1).broadcast_to([64, 8, 4])
        fb = rows[:, 4:8].unsqueeze(1).broadcast_to([64, 8, 4])
        self.tt(li, sp[:, :, 8:12], ib, ALU.add, R_ + [self.mlrow], [A])
        self.tt(tx, sp[:, :, 12:16], fb, ALU.add, R_ + [self.mlrow], [A])
        self.act(tx, tx, AF.Exp, [A], [A], scale=-1.0)
        self.act(tx, tx, AF.Ln, [A, cst], [A], bias=cst.t[0:64, 3:4])
        self.ts(lf, tx, -1.0, ALU.mult, [A], [A])
        p7 = self.P[7]
        lf2 = A.t[0:64, 32:64]
        self.mm(p7.t[0:64, 0:32], TRI, lf2, True, True, [A, c2], [p7])
        self.cp(A.t[0:64, 64:96], p7.t[0:64, 0:32], [p7], [A], eng="act")
        self.mm(p7.t[:, 32:64], ones[0:64, :], lf2, True, True, [A, consts], [p7])
        self.cp(A.t[:, 160:192], p7.t[:, 32:64], [p7], [A], eng="act")
        self.tt(ak, grep[0:64], b_, ALU.subtract, [A], [A])
        self.tt(ak, ak, li, ALU.add, [A], [A])
        self.mm(p7.t[0:32, 64:128], A.t[0:64, 96:128], ident[0:64, 0:64], True, True, [A, consts], [p7])
        S.op("dve", lambda e: e.tensor_reduce(out=mk32, in_=p7.t[0:32, 64:128], axis=AX.X, op=ALU.max), [p7], [A])
        self.ts(dg32, ident[0:32, 0:32], mk32, ALU.mult, [A, consts], [A])
        self.mm(p7.t[:, 128:160], ones[0:32, :], dg32, True, True, [A, consts], [p7])
        self.cp(A.t[:, 192:224], p7.t[:, 128:160], [p7], [A], eng="act")
        self.cp(Mall[:, 0, :], mrep.t[:], [mrep], [A], eng="dve")
        t4 = A.t[:, 364:368]
        for c in range(8):
            self.tt(t4, grep[:, c, :], Mall[:, c, :], ALU.add, [A], [A])
            self.tt(Mall[:, c + 1, :], t4, mkrep[:, c, :], ALU.max, [A], [A])
        self.cp(mrep.t[:], Mall[:, 8, :], [A], [mrep], eng="dve")
        self.tt(scall, grep, Mall[:, 0:8, :], ALU.add, [A], [A])
        self.tt(scall, scall, Mall[:, 1:9, :], ALU.subtract, [A], [A])
        self.act(scall, scall, AF.Exp, [A], [A])
        self.tt(kws, ak, Mall[0:64, 1:9, :], ALU.subtract, [A], [A])
        self.act(kws, kws, AF.Exp, [A], [A])
        if ms <= 2:
            return
        vx = self.tmp(17, 1, [128, 512])
        vext = vx.t[0:64, 0:264].rearrange("p (h e) -> p h e", e=66)
        S.op("dve", lambda e: e.memset(vx.t[0:64, 0:264], 1.0), [], [vx])
        osg = self.tmp(18, 1, [128, 512])
        Dm = self.tmp(19, 1, [128, 512])
        LR = self.tmp(20, 1, [128, 512])
        sq_ = self.tmp(21, 1, [128, 512])
        sT = self.tmp(22, 1, [128, 512])
        ne = self.tmp(23, 1, [128, 512])
        tq = self.tmp(0, 1, [128, 512])
        kt_ = self.tmp(1, 1, [128, 512])
        hh_ = self.tmp(2, 1, [128, 512])
        B = self.tmp(3, 1, [128, 512])
        v4 = lambda T, lo=0: T.t[0:64, lo:lo + 256].rearrange("p (h e) -> p h e", e=64)
        wv = [self.loadw("win", l * 28 + 12 + i, 8) for i in range(4)]
        for c in range(8):
            cs = slice(c * 64, (c + 1) * 64)
            p0 = self.P[0]
            for i in range(4):
                for k in range(8):
                    self.mm(p0.t[0:64, i * 128:(i + 1) * 128], u.t[:, k, cs], wv[i].t[:, k, :], k == 0, k == 7,
                            [u, wv[i]], [p0])
            self.cp(vext[:, :, 0:64], p0.t[0:64, 0:256].rearrange("p (h e) -> p h e", e=64), [p0], [vx], eng="act")
            self.act(osg.t[0:64, 0:256], p0.t[0:64, 256:512], AF.Sigmoid, [p0], [osg])
            if ms <= 3:
                continue
            lft = LR.t[0:64, 0:256].rearrange("p (h e) -> p h e", e=64)
            rm = LR.t[0:64, 256:512].rearrange("p (h e) -> p h e", e=64)
            tri_b = TRI.unsqueeze(1).broadcast_to([64, 4, 64])
            id_b = ident[0:64, 0:64].unsqueeze(1).broadcast_to([64, 4, 64])
            self.tt(lft, tri_b, lf[:, c, :].unsqueeze(2).broadcast_to([64, 4, 64]), ALU.mult, [A, c2], [LR])
            self.tt(rm, id_b, li[:, c, :].unsqueeze(2).broadcast_to([64, 4, 64]), ALU.mult, [A, consts], [LR])
            self.tt(rm, rm, lft, ALU.subtract, [LR], [LR])
            p1 = self.P[1]
            for h in range(4):
                o = p1.t[0:64, h * 64:(h + 1) * 64]
                self.mm(o, lft[:, h, :], ones[0:64, 0:64], True, False, [LR, consts], [p1])
                self.mm(o, ones[0:64, 0:64], rm[:, h, :], False, True, [LR, consts], [p1])
            dmv = v4(Dm)
            self.tt(dmv, p1.t[0:64, 0:256].rearrange("p (h e) -> p h e", e=64),
                    CMASK.unsqueeze(1).broadcast_to([64, 4, 64]), ALU.add, [p1, c2], [Dm])
            sm_ = B.t[0:64, 0:64]
            mloc, mint, mt, wint, e2, qn, den = (B.t[0:64, 4 * i:4 * i + 4] for i in range(7))
            S.op("dve", lambda e, dmv=dmv, mloc=mloc: e.tensor_reduce(out=mloc, in_=dmv, axis=AX.X, op=ALU.max), [Dm], [B])
            self.tt(mint, b_[:, c, :], Mall[0:64, c, :], ALU.add, [A], [B])
            self.tt(mt, mint, mloc, ALU.max, [B], [B])
            self.tt(wint, mint, mt, ALU.subtract, [B], [B])
            self.act(wint, wint, AF.Exp, [B], [B])
            self.act(e2, mt, AF.Exp, [B], [B], scale=-1.0)
            if ms <= 4:
                continue
            p2 = self.P[2]
            for h in range(4):
                o = p2.t[0:64, h * 64:(h + 1) * 64]
                self.mm(o, ones[0:64, 0:64], lft[:, h, :], True, False, [LR, consts], [p2])
                self.mm(o, rm[:, h, :], ones[0:64, 0:64], False, True, [LR, consts], [p2])
            etv = v4(sq_)
            self.tt(etv, p2.t[0:64, 0:256].rearrange("p (h e) -> p h e", e=64),
                    c2.t[0:64, 320:384].unsqueeze(1).broadcast_to([64, 4, 64]), ALU.add, [p2, c2], [sq_])
            self.act(etv, etv, AF.Exp, [sq_], [sq_])
            p3 = self.P[3]
            for h in range(4):
                self.mm(p3.t[0:64, h * 64:(h + 1) * 64], kz.t[:, h, cs], qk.t[:, h // 2, cs], True, True, [kz, qk], [p3])
            self.tt(v4(sT), p3.t[0:64, 0:256].rearrange("p (h e) -> p h e", e=64), etv, ALU.mult, [p3, sq_], [sT])
            if ms <= 5:
                continue
            stv = v4(sT)
            p4, p5 = self.P[4], self.P[5]
            for h in range(4):
                pr, off = h // 2, (h % 2) * 64
                self.mm(p4.t[0:64, h * 66:(h + 1) * 66], stv[:, h, :], vext[:, h, :], True, True, [sT, vx], [p4])
                self.mm(p5.t[0:64, h * 66:(h + 1) * 66], qk.t[:, pr, cs], Cx.t[:, h, :],
                        True, True, [qk, Cx], [p5])
            nev = ne.t[0:64, 0:264].rearrange("p (h e) -> p h e", e=66)
            tqv = tq.t[0:64, 0:264].rearrange("p (h e) -> p h e", e=66)
            self.tt(tqv, p5.t[0:64, 0:264].rearrange("p (h e) -> p h e", e=66),
                    wint.unsqueeze(2).broadcast_to([64, 4, 66]), ALU.mult, [p5, B], [tq])
            self.tt(nev, p4.t[0:64, 0:264].rearrange("p (h e) -> p h e", e=66),
                    e2.unsqueeze(2).broadcast_to([64, 4, 66]), ALU.mult, [p4, B], [ne])
            self.tt(nev, nev, tqv, ALU.add, [tq, ne], [ne])
            self.act(den, nev[:, :, 64], AF.Abs, [ne], [B])
            self.tt(den, den, e2, ALU.max, [B], [B])
            S.op("dve", lambda e, den=den: e.reciprocal(den, den), [B], [B])
            hv = v4(hh_)
            self.tt(hv, nev[:, :, 0:64], den.unsqueeze(2).broadcast_to([64, 4, 64]), ALU.mult, [ne, B], [hh_])
            if ms <= 6:
                continue
            h2 = v4(hh_, 256)
            ss = B.t[0:64, 32:36]
            self.tt(h2, hv, hv, ALU.mult, [hh_], [hh_])
            S.op("dve", lambda e, h2=h2, ss=ss: e.tensor_reduce(out=ss, in_=h2, axis=AX.X, op=ALU.add), [hh_], [B])
            self.act(ss, ss, AF.Sqrt, [B, cst], [B], scale=1.0 / 64, bias=cst.t[0:64, 0:1])
            S.op("dve", lambda e, ss=ss: e.reciprocal(ss, ss), [B], [B])
            self.tt(hv, hv, ss.unsqueeze(2).broadcast_to([64, 4, 64]), ALU.mult, [hh_, B], [hh_])
            self.tt(hh_.t[0:64, 0:256], hh_.t[0:64, 0:256], rows[:, 8:264], ALU.mult, [hh_, self.mlrow], [hh_])
            self.tt(hh_.t[0:64, 0:256], hh_.t[0:64, 0:256], osg.t[0:64, 0:256], ALU.mult, [hh_, osg], [hh_])
            for kc in range(2):
                self.mm(p3.t[:, 256 + kc * 64:256 + (kc + 1) * 64], hh_.t[0:64, kc * 128:(kc + 1) * 128],
                        ident[0:64, 0:64], True, True, [hh_, consts], [p3])
                self.cp(y.t[:, 1, kc, cs], p3.t[:, 256 + kc * 64:256 + (kc + 1) * 64], [p3], [y], eng="act")
            if ms <= 7:
                continue
            p6 = self.P[6]
            for pr in range(2):
                self.mm(p6.t[0:64, pr * 128:(pr + 1) * 128], qk.t[:, 2 + pr, cs], ident, True, True, [qk, consts], [p6])
            kwv = v4(kt_)
            self.tt(kwv, p6.t[0:64, 0:256].rearrange("p (h e) -> p h e", e=64),
                    kws[:, c, :].unsqueeze(2).broadcast_to([64, 4, 64]), ALU.mult, [p6, A], [kt_])
            p7b = self.P[7]
            for h in range(4):
                pr, off = h // 2, (h % 2) * 64
                o = p7b.t[:, h * 66:(h + 1) * 66]
                self.mm(o, kt_.t[0:64, pr * 128:(pr + 1) * 128], vext[:, h, :], True, True, [kt_, vx], [p7b])
                self.stt(Cx.t[off:off + 64, h, :], Cx.t[off:off + 64, h, :], scall[off:off + 64, c, h:h + 1],
                         p7b.t[off:off + 64, h * 66:(h + 1) * 66], ALU.mult, ALU.add, [Cx, A, p7b], [Cx])

    def gdn_fwd(self, l, s, tb, sp):
        S, u, y = self.S, self.u, self.y
        consts, c2, cst = self.consts, self.consts2, self.cst
        ident = consts.t[:, 128:256]
        id64 = ident[0:64, 0:64]
        ones = consts.t[:, 384:512]
        on64 = ones[0:64, 0:64]
        neg64 = self.negones.t[0:64, 0:64]
        TRI = c2.t[0:64, 0:64]
        SLADD = self.gmask.t[0:64, 0:64]
        SUADD = self.gmask.t[0:64, 64:128]
        CMT = c2.t[0:64, 320:384]
        BLK = self.gmask.t[:, 128:256]
        rows = self.gdrow.t[0:64, l, :]
        Sz = self.gdS[l]
        b3 = lambda ap: ap.unsqueeze(1).broadcast_to([64, 4, 64])
        v4 = lambda T, lo=0: T.t[0:64, lo:lo + 256].rearrange("p (h e) -> p h e", e=64)
        qkv = self.tmp(12, 6, [128, 6, NT])
        self.conv_silu(l, 0, 6, self.gdtail[l], self.cwb.t[:, l, 0:24], qkv, tb)
        if tb == 0:
            S.op("dve", lambda e: e.memset(Sz.t[:], 0.0), [], [Sz])
        sq = self.tmp(9, 1, [128, NT])
        rs = self.tmp(8, 1, [128, NT])
        for c4 in range(4):
            pp = self.P[c4 % 2]
            self.tt(sq.t, qkv.t[:, c4, :], qkv.t[:, c4, :], ALU.mult, [qkv], [sq])
            self.mm(pp.t[:], BLK, sq.t, True, True, [sq, self.gmask], [pp])
            self.act(rs.t, pp.t[:], AF.Sqrt, [pp, cst], [rs], bias=cst.t[:, 0:1])
            S.op("dve", lambda e: e.reciprocal(rs.t, rs.t), [rs], [rs])
            if c4 < 2:
                self.stt(qkv.t[:, c4, :], qkv.t[:, c4, :], 0.125, rs.t, ALU.mult, ALU.mult, [qkv, rs], [qkv])
            else:
                self.tt(qkv.t[:, c4, :], qkv.t[:, c4, :], rs.t, ALU.mult, [qkv, rs], [qkv])
        kz = self.tmp(4, 4, [128, 4, NT])
        S.op("dve", lambda e: e.memset(kz.t, 0.0), [], [kz])
        for h in range(4):
            pr, off = h // 2, (h % 2) * 64
            self.cp(kz.t[off:off + 64, h, :], qkv.t[off:off + 64, 2 + pr, :], [qkv], [kz], eng="dve")
        A = self.tmp(18, 1, [128, 512])
        v3 = lambda lo: A.t[0:64, lo:lo + 32].rearrange("p (c h) -> p c h", h=4)
        beta, g_, gc, egc, ekd, tx, bneg, begc = v3(0), v3(32), v3(64), v3(96), v3(128), v3(160), v3(192), v3(224)
        gLrep = A.t[:, 256:288].rearrange("p (c h) -> p c h", h=4)
        cdrep = A.t[:, 288:320].rearrange("p (c h) -> p c h", h=4)
        ea = A.t[0:64, 320:324]
        R_ = [A, self.sp]
        self.act(beta, sp[:, :, 0:4], AF.Sigmoid, R_, [A])
        self.tt(tx, sp[:, :, 4:8], rows[:, 4:8].unsqueeze(1).broadcast_to([64, 8, 4]), ALU.add, R_ + [self.gdrow], [A])
        self.act(tx, tx, AF.Exp, [A], [A])
        self.act(tx, tx, AF.Ln, [A, cst], [A], bias=cst.t[0:64, 3:4])
        self.act(ea, rows[:, 0:4], AF.Exp, [self.gdrow], [A])
        self.tt(g_, tx, ea.unsqueeze(1).broadcast_to([64, 8, 4]), ALU.mult, [A], [A])
        self.ts(g_, g_, -1.0, ALU.mult, [A], [A])
        p7 = self.P[7]
        g2 = A.t[0:64, 32:64]
        self.mm(p7.t[0:64, 0:32], TRI, g2, True, True, [A, c2], [p7])
        self.cp(A.t[0:64, 64:96], p7.t[0:64, 0:32], [p7], [A], eng="act")
        self.mm(p7.t[:, 32:64], ones[0:64, :], g2, True, True, [A, consts], [p7])
        self.cp(A.t[:, 256:288], p7.t[:, 32:64], [p7], [A], eng="act")
        self.act(egc, gc, AF.Exp, [A], [A])
        self.tt(ekd, gLrep[0:64], gc, ALU.subtract, [A], [A])
        self.act(ekd, ekd, AF.Exp, [A], [A])
        self.act(cdrep, gLrep, AF.Exp, [A], [A])
        self.ts(bneg, beta, -1.0, ALU.mult, [A], [A])
        self.tt(begc, beta, egc, ALU.mult, [A], [A])
        MM_ = self.tmp(19, 1, [128, 512], BF16)
        Xb = self.tmp(8, 1, [128, 512], BF16)
        Xbv = Xb.t[0:64, 0:512].rearrange("p (h e) -> p h e", e=128)
        X = self.tmp(20, 1, [128, 512])
        DC = self.tmp(21, 1, [128, 512])
        QB = self.tmp(22, 1, [128, 512])
        GT = self.tmp(23, 1, [128, 512])
        VK = self.tmp(0, 1, [128, 512])
        XT = self.tmp(1, 1, [128, 512])
        VN = self.tmp(2, 1, [128, 512])
        ZS = self.tmp(3, 1, [128, 512])
        M2 = self.tmp(10, 1, [128, 512], BF16)
        wz = [self.loadw("win", l * 28 + 6 + i, 8) for i in range(2)]
        Xv = X.t[0:64, 0:512].rearrange("p (h e) -> p h e", e=128)
        for c in range(8):
            cs = slice(c * 64, (c + 1) * 64)
            p0 = self.P[0]
            for i in range(2):
                for k in range(8):
                    self.mm(p0.t[0:64, i * 128:(i + 1) * 128], u.t[:, k, cs], wz[i].t[:, k, :], k == 0, k == 7,
                            [u, wz[i]], [p0])
            self.act(ZS.t[0:64, 0:256], p0.t[0:64, 0:256], AF.Silu, [p0], [ZS])
            for pr in range(2):
                self.mm(p0.t[0:64, 256 + pr * 128:256 + (pr + 1) * 128], qkv.t[:, 4 + pr, cs], ident, True, True,
                        [qkv, consts], [p0])
            vtok = v4(VK)
            self.cp(vtok, p0.t[0:64, 256:512].rearrange("p (h e) -> p h e", e=64), [p0], [VK], eng="act")
            p1 = self.P[1]
            for pr in range(2):
                self.mm(p1.t[0:64, pr * 128:(pr + 1) * 128], qkv.t[:, 2 + pr, cs], ident, True, True, [qkv, consts], [p1])
            ktok = v4(VK, 256)
            self.cp(ktok, p1.t[0:64, 0:256].rearrange("p (h e) -> p h e", e=64), [p1], [VK], eng="act")
            for h in range(4):
                uo, wo = (64, 0) if h % 2 == 0 else (0, 64)
                self.ts(Xv[:, h, uo:uo + 64], vtok[:, h, :], beta[:, c, h:h + 1], ALU.mult, [VK, A], [X])
                self.ts(Xv[:, h, wo:wo + 64], ktok[:, h, :], begc[:, c, h:h + 1], ALU.mult, [VK, A], [X])
            kd = v4(XT, 256)
            self.tt(kd, ktok, ekd[:, c, :].unsqueeze(2).broadcast_to([64, 4, 64]), ALU.mult, [VK, A], [XT])
            gt = v4(GT)
            self.tt(gt, b3(TRI), g_[:, c, :].unsqueeze(2).broadcast_to([64, 4, 64]), ALU.mult, [A, c2], [GT])
            p2, p3 = self.P[2], self.P[3]
            for h in range(4):
                o = p2.t[0:64, h * 64:(h + 1) * 64]
                self.mm(o, gt[:, h, :], on64, True, False, [GT, consts], [p2])
                self.mm(o, neg64, gt[:, h, :], False, True, [GT, self.negones], [p2])
                o = p2.t[0:64, 256 + h * 64:256 + (h + 1) * 64]
                self.mm(o, on64, gt[:, h, :], True, False, [GT, consts], [p2])
                self.mm(o, gt[:, h, :], neg64, False, True, [GT, self.negones], [p2])
            dS, dT, dQ = v4(DC), v4(DC, 256), v4(QB)
            pD = p2.t[0:64, 0:256].rearrange("p (h e) -> p h e", e=64)
            pDT = p2.t[0:64, 256:512].rearrange("p (h e) -> p h e", e=64)
            self.tt(dS, pD, b3(SLADD), ALU.add, [p2, self.gmask], [DC])
            self.tt(dT, pDT, b3(SUADD), ALU.add, [p2, self.gmask], [DC])
            self.tt(dQ, pDT, b3(CMT), ALU.add, [p2, c2], [QB])
            self.act(DC.t[0:64, 0:512], DC.t[0:64, 0:512], AF.Exp, [DC], [DC])
            self.act(dQ, dQ, AF.Exp, [QB], [QB])
            dgb = v4(GT, 256)
            self.tt(dgb, b3(id64), bneg[:, c, :].unsqueeze(2).broadcast_to([64, 4, 64]), ALU.mult, [A, consts], [GT])
            for h in range(4):
                self.mm(p3.t[0:64, h * 64:(h + 1) * 64], qkv.t[:, 2 + h // 2, cs], kz.t[:, h, cs], True, True, [qkv, kz], [p3])
                self.mm(p3.t[0:64, 256 + h * 64:256 + (h + 1) * 64], on64, dgb[:, h, :], True, True, [GT, consts], [p3])
            pKK = p3.t[0:64, 0:256].rearrange("p (h e) -> p h e", e=64)
            pBf = p3.t[0:64, 256:512].rearrange("p (h e) -> p h e", e=64)
            Mk, MkT = v4(MM_), v4(MM_, 256)
            self.tt(Mk, pKK, dS, ALU.mult, [p3, DC], [MM_])
            self.tt(Mk, Mk, bneg[:, c, :].unsqueeze(2).broadcast_to([64, 4, 64]), ALU.mult, [MM_, A], [MM_])
            self.tt(MkT, pKK, dT, ALU.mult, [p3, DC], [MM_])
            self.tt(MkT, MkT, pBf, ALU.mult, [MM_, p3], [MM_])
            p4 = self.P[4]
            for h in range(4):
                self.mm(p4.t[0:64, h * 64:(h + 1) * 64], kz.t[:, h, cs], qkv.t[:, h // 2, cs], True, True, [qkv, kz], [p4])
            self.tt(dQ, p4.t[0:64, 0:256].rearrange("p (h e) -> p h e", e=64), dQ, ALU.mult, [p4, QB], [QB])
            cur, nxt = MM_, M2
            for step in range(6):
                cM, cMT = v4(cur), v4(cur, 256)
                p5 = self.P[5]
                self.cp(Xb.t[0:64, 0:512], X.t[0:64, 0:512], [X], [Xb], eng="act")
                for h in range(4):
                    self.mm(p5.t[0:64, h * 128:(h + 1) * 128], cMT[:, h, :], Xbv[:, h, :], True, True, [cur, Xb], [p5])
                if step < 5:
                    p6 = self.P[6]
                    for h in range(4):
                        self.mm(p6.t[0:64, h * 64:(h + 1) * 64], cMT[:, h, :], cM[:, h, :], True, True, [cur], [p6])
                        self.mm(p6.t[0:64, 256 + h * 64:256 + (h + 1) * 64], cM[:, h, :], cMT[:, h, :], True, True, [cur], [p6])
                    self.cp(nxt.t[0:64, 0:512], p6.t[0:64, 0:512], [p6], [nxt], eng="act")
                self.tt(X.t[0:64, 0:512], X.t[0:64, 0:512], p5.t[0:64, 0:512], ALU.add, [X, p5], [X])
                cur, nxt = nxt, cur
            p5 = self.P[5]
            for h in range(4):
                self.mm(p5.t[:, h * 64:(h + 1) * 64], Xv[:, h, :], id64, True, True, [X, consts], [p5])
            xt = XT.t[:, 0:256].rearrange("p (h e) -> p h e", e=64)
            self.cp(XT.t[:, 0:256], p5.t[:, 0:256], [p5], [XT], eng="act")
            p6 = self.P[6]
            for h in range(4):
                self.mm(p6.t[0:64, h * 64:(h + 1) * 64], xt[:, h, :], Sz.t[:, h, :], True, True, [XT, Sz], [p6])
                self.mm(p6.t[0:64, 256 + h * 64:256 + (h + 1) * 64], qkv.t[:, h // 2, cs], Sz.t[:, h, :], True, True,
                        [qkv, Sz], [p6])
            vn = v4(VN)
            for h in range(4):
                uo = 64 if h % 2 == 0 else 0
                self.tt(vn[:, h, :], Xv[:, h, uo:uo + 64], p6.t[0:64, h * 64:(h + 1) * 64], ALU.subtract, [X, p6], [VN])
            oq = v4(VN, 256)
            self.tt(oq, p6.t[0:64, 256:512].rearrange("p (h e) -> p h e", e=64),
                    egc[:, c, :].unsqueeze(2).broadcast_to([64, 4, 64]), ALU.mult, [p6, A], [VN])
            p7 = self.P[7]
            for h in range(4):
                self.mm(p7.t[0:64, h * 64:(h + 1) * 64], dQ[:, h, :], vn[:, h, :], True, True, [QB, VN], [p7])
            self.tt(oq, oq, p7.t[0:64, 0:256].rearrange("p (h e) -> p h e", e=64), ALU.add, [VN, p7], [VN])
            p1 = self.P[1]
            for h in range(4):
                pr, off = h // 2, (h % 2) * 64
                self.mm(p1.t[:, h * 64:(h + 1) * 64], XT.t[0:64, 256 + pr * 128:256 + (pr + 1) * 128], vn[:, h, :],
                        True, True, [XT, VN], [p1])
                self.stt(Sz.t[off:off + 64, h, :], Sz.t[off:off + 64, h, :], cdrep[off:off + 64, c, h:h + 1],
                         p1.t[off:off + 64, h * 64:(h + 1) * 64], ALU.mult, ALU.add, [Sz, A, p1], [Sz])
            o2 = v4(ZS, 256)
            ss = A.t[0:64, 328:332]
            self.tt(o2, oq, oq, ALU.mult, [VN], [ZS])
            S.op("dve", lambda e, o2=o2, ss=ss: e.tensor_reduce(out=ss, in_=o2, axis=AX.X, op=ALU.add), [ZS], [A])
            self.act(ss, ss, AF.Sqrt, [A, cst], [A], scale=1.0 / 64, bias=cst.t[0:64, 0:1])
            S.op("dve", lambda e, ss=ss: e.reciprocal(ss, ss), [A], [A])
            self.tt(o2, oq, ss.unsqueeze(2).broadcast_to([64, 4, 64]), ALU.mult, [VN, A], [ZS])
            self.tt(o2, o2, b3(rows[:, 8:72]), ALU.mult, [ZS, self.gdrow], [ZS])
            self.tt(ZS.t[0:64, 256:512], ZS.t[0:64, 256:512], ZS.t[0:64, 0:256], ALU.mult, [ZS], [ZS])
            p0 = self.P[0]
            for kc in range(2):
                self.mm(p0.t[:, kc * 64:(kc + 1) * 64], ZS.t[0:64, 256 + kc * 128:256 + (kc + 1) * 128], id64, True, True,
                        [ZS, consts], [p0])
                self.cp(y.t[:, 0, kc, cs], p0.t[:, kc * 64:(kc + 1) * 64], [p0], [y], eng="act")

    def mixers(self, l, s, tb):
        only = self.dbg.get("only", "")
        sp = self.tokproj_small(l)
        if not only or "gdn" in only:
            self.gdn_fwd(l, s, tb, sp)
        if not only or "ml" in only:
            self.mlstm_fwd(l, s, tb, sp)
        if not only or "s5" in only:
            self.s5_fwd(l, s, tb)
        if not only or "moba" in only:
            self.moba_fwd(l, s, tb)

    def build(self):
        S = self.S
        self.declare()
        self.setup()
        units = self.dbg.get("units", [(s, tb) for s in range(2) for tb in range(SEQ // NT)])
        stop = self.dbg.get("stop", None)
        for (s, tb) in units:
            t0 = tb * NT
            S.dma("sp", self.h.t[:], self.D["xT"].t[s, :, :, t0:t0 + NT], [], [self.h], self.h)
            first = (s, tb) == units[0]
            for l in range(NL):
                self.ffn(l, 0)
                if stop == "ffn1":
                    break
                self.norm(l * 4 + 1)
                if "yin" in self.dbg:
                    S.dma("sp", self.y.t[:], self.D["yin"].t[s * 4 + tb], [], [self.y], self.y)
                else:
                    self.mixers(l, s, tb)
                if "dumpy" in self.dbg and l == 0:
                    S.dma("sp", self.dumpy.t[s * 4 + tb], self.y.t[:], [self.y], [self.dumpy], self.y)
                if stop == "y":
                    break
                self.merge(l)
                self.ffn(l, 1)
                self.ple(l, s, t0)
            if stop is not None:
                self.dump_h(s * 4 + tb)
            self.final(s, t0)
        S.finish()
        S.emit_all()


def F32v(k, name, hh):
    ri = 0 if name.endswith("re") else 1
    t = k.s5B["BT" if name.startswith("BT") else "CT"]
    return t[:, 4 * hh:4 * hh + 4, ri, :]


def wt(w, K):
    Kd, N = w.shape
    assert Kd == K * 128 and N % 128 == 0
    a = w.reshape(K, 128, N // 128, 128).transpose(2, 1, 0, 3)
    return np.ascontiguousarray(a).reshape(N // 128 * 128, K * 128)


def fm(a, kc):
    T = a.shape[0]
    return np.ascontiguousarray(a.T.reshape(kc, 128, T).transpose(1, 0, 2))


def prep_shared(inp):
    f = lambda n: np.asarray(inp[n], dtype=np.float32)
    sh = {}
    wgu = []
    wdn = []
    for l in range(NL):
        for nm_gu, nm_d in (("ffn1_w_gu", "ffn1_w_down"), ("ffn2_w_gu", "ffn2_w_down")):
            w = f(nm_gu)[l]
            g = wt(w[:, :2816], 8).reshape(22, 128, 1024)
            u = wt(w[:, 2816:], 8).reshape(22, 128, 1024)
            wgu.append(np.stack([g, u], axis=1).reshape(44 * 128, 1024))
            wdn.append(wt(f(nm_d)[l], 22))
    sh["wgu"] = np.concatenate(wgu, 0)
    sh["wdn"] = np.concatenate(wdn, 0)
    swap = np.concatenate([(np.arange(64) + 32) % 64 + 64 * hh for hh in range(4)])
    cols = np.concatenate([np.arange(0, 1024), np.arange(1032, 2056), np.arange(2064, 3088),
                           2320 + swap, 2576 + swap])
    sh["win"] = np.concatenate([wt(f("w_in")[l][:, cols], 8) for l in range(NL)], 0)
    sh["wgate"] = np.concatenate([
        np.stack([wt(f("w_gate")[l, b], 8).reshape(8, 128, 1024) for b in range(4)], 1).reshape(8 * 4 * 128, 1024)
        for l in range(NL)], 0)
    sh["wbr"] = np.concatenate([
        np.stack([wt(f("w_branch")[l, b], 2).reshape(8, 128, 256) for b in range(4)], 1).reshape(8 * 4 * 128, 256)
        for l in range(NL)], 0)
    sh["wout"] = np.concatenate([wt(f("w_out")[l], 8) for l in range(NL)], 0)
    sh["wplg"] = np.concatenate([wt(f("ple_w_gate")[l], 8) for l in range(NL)], 0)
    sh["wplp"] = np.concatenate([wt(f("ple_w_proj")[l], 2) for l in range(NL)], 0)
    gains = np.zeros((9, 1024), np.float32)
    for l in range(NL):
        gains[l * 4 + 0] = f("ffn1_norm")[l]
        gains[l * 4 + 1] = f("mix_norm")[l]
        gains[l * 4 + 2] = f("ffn2_norm")[l]
        gains[l * 4 + 3] = f("ple_norm")[l]
    gains[8] = f("final_norm")
    sh["gains"] = np.ascontiguousarray(gains.reshape(9, 8, 128).transpose(2, 0, 1))
    sh["wglu"] = np.concatenate([wt(f("s5_w_glu")[l], 2) for l in range(NL)], 0)
    consts = np.zeros((128, 512), np.float32)
    consts[:, 0:128] = np.arange(128, dtype=np.float32)[None, :]
    consts[:, 128:256] = np.eye(128, dtype=np.float32)
    consts[:, 384:512] = 1.0
    for qb in range(8):
        for n in range(8):
            consts[:, 256 + qb * 8 + n] = 0.0 if n < qb else -1e9
    pc = np.zeros((128, 8), np.float32)
    invf = (10000.0 ** (-np.arange(0, 64, 2, dtype=np.float32) / 64)).astype(np.float32)
    for p_ in range(128):
        pc[p_, 0] = invf[p_ % 32]
        pc[p_, 1] = -1.0 if (p_ % 64) < 32 else 1.0
        pc[p_, 2] = pc[p_, 1] * 2 * np.pi
    sh["pc"] = pc
    c2 = np.zeros((128, 512), np.float32)
    ii = np.arange(64)
    c2[0:64, 0:64] = (ii[:, None] <= ii[None, :]).astype(np.float32)
    c2[0:64, 64:128] = np.where(ii[None, :] <= ii[:, None], 0.0, -60000.0)
    c2[0:64, 128:192] = (ii[None, :] < ii[:, None]).astype(np.float32)
    c2[0:64, 192:256] = (ii[None, :] <= ii[:, None]).astype(np.float32)
    c2[0:64, 256:320] = (ii[:, None] < ii[None, :]).astype(np.float32)
    c2[0:64, 320:384] = np.where(ii[:, None] <= ii[None, :], 0.0, -60000.0)
    sh["consts2"] = c2
    gmk = np.zeros((128, 256), np.float32)
    gmk[0:64, 0:64] = np.where(ii[None, :] < ii[:, None], 0.0, -60000.0)
    gmk[0:64, 64:128] = np.where(ii[:, None] < ii[None, :], 0.0, -60000.0)
    pp_ = np.arange(128)
    gmk[:, 128:256] = (pp_[:, None] // 64 == pp_[None, :] // 64).astype(np.float32)
    sh["gmaskf"] = gmk
    win = f("w_in")
    wsm = np.zeros((128, NL, 8, 16), np.float32)
    cwf = np.zeros((128, NL, 40), np.float32)
    mlrow = np.zeros((128, NL, 264), np.float32)
    gdrow = np.zeros((128, NL, 72), np.float32)
    for l in range(NL):
        small = np.concatenate([win[l][:, 1024:1032], win[l][:, 2056:2064]], 1)
        wsm[:, l] = small.reshape(8, 128, 16).transpose(1, 0, 2)
        gc = f("gdn_conv")[l]
        mc = f("mlstm_conv")[l]
        cwf[:, l, 0:24] = gc.T.reshape(6, 128, 4).transpose(1, 0, 2).reshape(128, 24)
        cwf[:, l, 24:40] = mc.T.reshape(4, 128, 4).transpose(1, 0, 2).reshape(128, 16)
        mlrow[:, l, 0:4] = f("mlstm_i_bias")[l][None, :]
        mlrow[:, l, 4:8] = f("mlstm_f_bias")[l][None, :]
        mlrow[:, l, 8:264] = f("mlstm_norm")[l][None, :]
        gdrow[:, l, 0:4] = f("gdn_a_log")[l][None, :]
        gdrow[:, l, 4:8] = f("gdn_dt_bias")[l][None, :]
        gdrow[:, l, 8:72] = f("gdn_norm")[l][None, :]
    sh["wsmf"], sh["cwf"], sh["mlrowf"], sh["gdrowf"] = wsm, cwf, mlrow, gdrow
    cmf = np.zeros((128, 2, 256), np.float32)
    for a in range(2):
        kl = a * 128 + np.arange(128)[:, None]
        cmf[:, a, :] = np.where(kl > np.arange(256)[None, :], -30000.0, 0.0)
    sh["cmf"] = cmf
    sh["consts"] = consts
    lre, lim, ldt = f("s5_lambda_re"), f("s5_lambda_im"), f("s5_log_dt")
    s5st = np.zeros((NL, 128, 8, 3), np.float32)
    s5rep = np.zeros((NL, 3, 128, 8, 128), np.float32)
    s5bexp = np.zeros((NL, 2, 128, 8, 128), np.float32)
    s5cexp = np.zeros((NL, 2, 128, 8, 128), np.float32)
    bre, bim, cre, cim = f("s5_b_re"), f("s5_b_im"), f("s5_c_re"), f("s5_c_im")
    for l in range(NL):
        for sc in range(8):
            for half in range(2):
                g = 2 * sc + half
                ps = slice(half * 64, half * 64 + 64)
                s5st[l, ps, sc, 0] = lre[l, g]
                s5st[l, ps, sc, 1] = lim[l, g]
                s5st[l, ps, sc, 2] = ldt[l, g]
                s5rep[l, 0, :, sc, ps] = lre[l, g][None, :]
                s5rep[l, 1, :, sc, ps] = lim[l, g][None, :]
                s5rep[l, 2, :, sc, ps] = ldt[l, g]
                r0 = (sc % 4) * 32 + half * 16
                s5bexp[l, 0, r0:r0 + 16, sc, ps] = bre[l, g].T
                s5bexp[l, 1, r0:r0 + 16, sc, ps] = bim[l, g].T
                s5cexp[l, 0, ps, sc, r0:r0 + 16] = cre[l, g].T
                s5cexp[l, 1, ps, sc, r0:r0 + 16] = cim[l, g].T
    sh["s5st"], sh["s5rep"], sh["s5bexp"], sh["s5cexp"] = s5st, s5rep, s5bexp, s5cexp
    s5db = np.zeros((NL, 128, 4), np.float32)
    for l in range(NL):
        s5db[l, :, 0:2] = f("s5_d")[l].reshape(2, 128).T
        s5db[l, :, 2:4] = f("s5_b_glu")[l].reshape(2, 128).T
    sh["s5db"] = s5db
    return sh


def prep_core(inp, c):
    x = np.asarray(inp["x"], dtype=np.float32)
    p = np.asarray(inp["p"], dtype=np.float32)
    m = {}
    m["xT"] = np.stack([fm(x[2 * c + s], 8) for s in range(2)], 0)
    m["pT"] = np.stack([np.stack([fm(p[l, 2 * c + s], 2) for s in range(2)], 0) for l in range(NL)], 0)
    pos = np.asarray(inp["positions"]).astype(np.int32)
    m["posr"] = np.ascontiguousarray(np.broadcast_to(pos[2 * c:2 * c + 2, None, :], (2, 128, SEQ)))
    return m


def build_nc(dbg=None):
    nc = bass.Bass("TRN2", target_bir_lowering=False)
    with ExitStack() as st:
        k = Kern(nc, st, dbg)
        k.build()
    return nc


def kernel(**inputs):
    sh = prep_shared(inputs)
    nc = build_nc()
    in_maps = []
    for c in range(8):
        m = dict(sh)
        m.update(prep_core(inputs, c))
        in_maps.append(m)
    res = run_bass_kernel_spmd(nc, in_maps, core_ids=list(range(8)))
    out = np.zeros((16, SEQ, 1024), np.float32)
    for c in range(8):
        o = res.results[c]["out"]
        for s in range(2):
            out[2 * c + s] = o[s].transpose(2, 1, 0).reshape(SEQ, 1024)
    return out
```

```python
import numpy as np
from contextlib import ExitStack
import concourse.bass as bass
import concourse.mybir as mybir
from concourse.bass_utils import run_bass_kernel_spmd

F32 = mybir.dt.float32
BF16 = mybir.dt.bfloat16
I32 = mybir.dt.int32
AF = mybir.ActivationFunctionType
ALU = mybir.AluOpType
AX = mybir.AxisListType

ENGS = ["pe", "act", "dve", "pool", "sp"]
NT = 512
NL = 2
SEQ = 2048
PI = float(np.pi)


class Buf:
    __slots__ = ("name", "w", "r", "sem", "semcnt", "t")

    def __init__(self, name, t=None):
        self.name = name
        self.w = None
        self.r = {}
        self.sem = None
        self.semcnt = 0
        self.t = t


class Tmp:
    __slots__ = ("t", "bufs")

    def __init__(self, t, bufs):
        self.t = t
        self.bufs = bufs


def flat(lst):
    out = []
    for b in lst:
        if isinstance(b, Tmp):
            out.extend(b.bufs)
        else:
            out.append(b)
    return out


class Sched:
    def __init__(self, nc, stack):
        self.nc = nc
        self.stack = stack
        self.q = {e: [] for e in ENGS}
        self.cnt = {e: 0 for e in ENGS}
        self.sems = {}
        for e in ENGS:
            self.sems[e] = stack.enter_context(nc.semaphore("s_" + e))
        self.seen = {e: {} for e in ENGS}
        self.dmabufs = []

    def sb(self, name, shape, dt=F32):
        return Buf(name, self.stack.enter_context(self.nc.sbuf_tensor("sb_" + name, list(shape), dt)))

    def ps(self, name, shape, dt=F32):
        return Buf(name, self.stack.enter_context(self.nc.psum_tensor("ps_" + name, list(shape), dt)))

    def _waits(self, e, reads, writes):
        need = {}

        def add(k, v, src):
            if src == "pe" and e == "pe":
                return
            if v > need.get(k, 0):
                need[k] = v
        for b in reads:
            if b.w is not None:
                add(*b.w)
        for b in writes:
            if b.w is not None:
                add(*b.w)
            for k, (v, src) in b.r.items():
                add(k, v, src)
        out = []
        seen = self.seen[e]
        for k, v in need.items():
            if seen.get(k, 0) < v:
                seen[k] = v
                out.append((self.sems[k], v))
        return out

    def _record(self, dep, reads, writes):
        k, v, src = dep
        for b in reads:
            old = b.r.get(k)
            if old is None or old[0] < v:
                b.r[k] = (v, src)
        for b in writes:
            b.w = dep
            b.r = {}

    def op(self, e, fn, reads=(), writes=()):
        reads, writes = flat(reads), flat(writes)
        waits = self._waits(e, reads, writes)
        self.cnt[e] += 1
        n = self.cnt[e]
        sem = self.sems[e]

        def emit(engine, fn=fn, waits=waits, sem=sem):
            for s, v in waits:
                engine.wait_ge(s, v)
            fn(engine).then_inc(sem, 1)
        self.q[e].append(emit)
        self._record((e, n, e), reads, writes)

    def dma(self, qe, out, in_, reads, writes, sembuf):
        reads, writes = flat(reads), flat(writes)
        waits = self._waits(qe, reads, writes)
        if isinstance(sembuf, Tmp):
            sembuf = sembuf.bufs[0]
        if sembuf.sem is None:
            key = "d%d" % len(self.dmabufs)
            sembuf.sem = key
            self.sems[key] = self.stack.enter_context(self.nc.semaphore(key))
            self.dmabufs.append(sembuf)
        sembuf.semcnt += 16
        v = sembuf.semcnt
        sem = self.sems[sembuf.sem]

        def emit(engine, waits=waits, sem=sem, out=out, in_=in_):
            for s, vv in waits:
                engine.wait_ge(s, vv)
            engine.dma_start(out=out, in_=in_).then_inc(sem, 16)
        self.q[qe].append(emit)
        self._record((sembuf.sem, v, "dma"), reads, writes)

    def finish(self):
        waits = []
        for e in ENGS:
            if e != "sp" and self.cnt[e] > 0:
                waits.append((self.sems[e], self.cnt[e]))
        for b in self.dmabufs:
            waits.append((self.sems[b.sem], b.semcnt))

        def emit(engine, waits=waits):
            for s, v in waits:
                engine.wait_ge(s, v)
        self.q["sp"].append(emit)

    def emit_all(self):
        with self.nc.Block() as block:
            @block.tensor
            def _(eng):
                for f in self.q["pe"]:
                    f(eng)

            @block.scalar
            def _(eng):
                for f in self.q["act"]:
                    f(eng)

            @block.vector
            def _(eng):
                for f in self.q["dve"]:
                    f(eng)

            @block.gpsimd
            def _(eng):
                for f in self.q["pool"]:
                    f(eng)

            @block.sync
            def _(eng):
                for f in self.q["sp"]:
                    f(eng)


BIGW = {
    "wgu": (NL * 2 * 44 * 128, 8 * 128),
    "wdn": (NL * 2 * 8 * 128, 22 * 128),
    "win": (NL * 28 * 128, 8 * 128),
    "wgate": (NL * 8 * 4 * 128, 8 * 128),
    "wbr": (NL * 8 * 4 * 128, 2 * 128),
    "wout": (NL * 8 * 128, 8 * 128),
    "wplg": (NL * 8 * 128, 8 * 128),
    "wplp": (NL * 8 * 128, 2 * 128),
    "wglu": (NL * 2 * 128, 2 * 128),
}


class Kern:
    def __init__(self, nc, st, dbg=None):
        self.nc = nc
        self.dbg = dbg or {}
        self.S = Sched(nc, st)
        self.wi8 = 0
        self.wi22 = 0
        self.wbg = {}

    def mm(self, out, lhsT, rhs, start, stop, R, W):
        self.S.op("pe", lambda e: e.matmul(out, lhsT, rhs, start=start, stop=stop), R, W)

    def act(self, out, in_, func, R, W, **kw):
        self.S.op("act", lambda e: e.activation(out=out, in_=in_, func=func, **kw), R, W)

    def tt(self, out, a, b, op, R, W, eng="dve"):
        self.S.op(eng, lambda e: e.tensor_tensor(out=out, in0=a, in1=b, op=op), R, W)

    def ts(self, out, a, s1, op0, R, W, s2=None, op1=None, eng="dve"):
        if op1 is None:
            self.S.op(eng, lambda e: e.tensor_scalar(out=out, in0=a, scalar1=s1, scalar2=None, op0=op0), R, W)
        else:
            self.S.op(eng, lambda e: e.tensor_scalar(out=out, in0=a, scalar1=s1, scalar2=s2, op0=op0, op1=op1), R, W)

    def stt(self, out, a, s, b, op0, op1, R, W):
        self.S.op("dve", lambda e: e.scalar_tensor_tensor(out=out, in0=a, scalar=s, in1=b, op0=op0, op1=op1), R, W)

    def cp(self, out, in_, R, W, eng="pool"):
        if eng == "act":
            self.S.op("act", lambda e: e.copy(out=out, in_=in_), R, W)
        else:
            self.S.op(eng, lambda e: e.tensor_copy(out, in_), R, W)

    def wgroup(self, name, l, which=0):
        per = {"wgu": 44, "wdn": 8, "win": 28, "wgate": 32, "wbr": 32, "wout": 8, "wplg": 8, "wplp": 8, "wglu": 2}[name]
        idx = (l * 2 + which) if name in ("wgu", "wdn") else l
        key = (name, idx)
        if key not in self.wbg:
            self.wbg[key] = Buf("%s_b%d" % (name, idx), self.wb[name].t)
        return idx * per * 128, (idx + 1) * per * 128, self.wbg[key]

    def convert(self, groups):
        S = self.S
        for g in groups:
            name = g[0]
            r0g, r1g, dstb = self.wgroup(*g)
            c = BIGW[name][1]
            nn = min(max(1, 4096 // c), (r1g - r0g) // 128)
            src = self.D[name]
            for r0 in range(r0g, r1g, nn * 128):
                stg = self.cstg[self.conv_i % 2]
                self.conv_i += 1
                S.dma("pool", stg.t[:, 0:nn * c].rearrange("p (n c) -> p n c", c=c),
                      src.t[r0:r0 + nn * 128, :].rearrange("(n p) c -> p n c", p=128), [], [stg], stg)
                if self.conv_pending is not None:
                    self.conv_pending()
                self.conv_pending = (lambda stg=stg, dstb=dstb, r0=r0, nn=nn, c=c: S.dma(
                    "pool", dstb.t[r0:r0 + nn * 128, :].rearrange("(n p) c -> p n c", p=128),
                    stg.t[:, 0:nn * c].rearrange("p (n c) -> p n c", c=c), [stg], [dstb], dstb))
        if self.conv_pending is not None:
            self.conv_pending()
            self.conv_pending = None

    def loadw(self, name, tile, K):
        per = {"wgu": 44, "wdn": 8, "win": 28, "wgate": 32, "wbr": 32, "wout": 8, "wplg": 8, "wplp": 8, "wglu": 2}[name]
        src = self.wbg[(name, tile // per)]
        ap = src.t[tile * 128:(tile + 1) * 128, :].rearrange("p (k c) -> p k c", c=128)
        if K > 8:
            b = self.w22[self.wi22 % len(self.w22)]
            self.wi22 += 1
        else:
            b = self.w8[self.wi8 % len(self.w8)]
            self.wi8 += 1
        self.S.dma("sp", b.t[:, 0:K, :], ap, [src], [b], b)
        return b

    def tmp(self, slot0, nslots, shape, dt=F32):
        ap = self.scr_t[:, slot0 * 512:(slot0 + nslots) * 512]
        if dt == BF16:
            ap = ap.bitcast(BF16)
        n = 1
        for d in shape[1:]:
            n *= d
        ap = ap[:, 0:n]
        if len(shape) == 3:
            ap = ap.rearrange("p (a b) -> p a b", b=shape[2])
        elif len(shape) == 4:
            ap = ap.rearrange("p (a b c) -> p a b c", b=shape[2], c=shape[3])
        return Tmp(ap, self.slots[slot0:slot0 + nslots])

    def declare(self):
        nc, S = self.nc, self.S
        D = {}

        def din(name, shape, dt=F32):
            D[name] = Buf(name, nc.dram_tensor(name, list(shape), dt, kind="ExternalInput").ap())
            return D[name]
        self.D = D
        din("xT", [2, 128, 8, SEQ])
        din("pT", [NL, 2, 128, 2, SEQ])
        din("gains", [128, 9, 8])
        din("consts", [128, 512])
        din("s5st", [NL, 128, 8, 3])
        din("s5rep", [NL, 3, 128, 8, 128])
        din("s5bexp", [NL, 2, 128, 8, 128])
        din("s5cexp", [NL, 2, 128, 8, 128])
        din("s5db", [NL, 128, 4])
        din("posr", [2, 128, SEQ], I32)
        din("consts2", [128, 512])
        din("wsmf", [128, NL, 8, 16])
        din("cwf", [128, NL, 40])
        din("mlrowf", [128, NL, 264])
        din("gdrowf", [128, NL, 72])
        din("gmaskf", [128, 256])
        din("pc", [128, 8])
        din("cmf", [128, 2, 256])
        for n, (r, c) in BIGW.items():
            din(n, [r, c])
        self.out = Buf("out", nc.dram_tensor("out", [2, 128, 8, SEQ], F32, kind="ExternalOutput").ap())
        self.wb = {}
        for n, (r, c) in BIGW.items():
            self.wb[n] = Buf(n + "_b", nc.dram_tensor(n + "_b", [r, c], BF16, kind="Internal").ap())
        self.s5f_d = Buf("s5f_d", nc.dram_tensor("s5f_d", [NL, 128, 3104], F32, kind="Internal").ap())
        self.s5b_d = Buf("s5b_d", nc.dram_tensor("s5b_d", [NL, 128, 4352], BF16, kind="Internal").ap())
        if "yin" in self.dbg:
            din("yin", [8, 128, 4, 2, NT], BF16)
        if "dumpy" in self.dbg:
            self.dumpy = Buf("dumpy", nc.dram_tensor("dumpy", [8, 128, 4, 2, NT], BF16, kind="ExternalOutput").ap())
        if "dump" in self.dbg:
            self.dump = Buf("dump", nc.dram_tensor("dump", self.dbg["dump"], F32, kind="ExternalOutput").ap())

        self.h = S.sb("h", [128, 8, NT], F32)
        self.u = S.sb("u", [128, 8, NT], BF16)
        NS = 24
        self.scr_t = S.sb("scr", [128, NS * 512], F32).t
        self.slots = [Buf("slot%d" % i) for i in range(NS)]
        self.actb = self.tmp(0, 11, [128, 22, NT], BF16)
        self.rstd = S.sb("rstd", [128, NT], F32)
        self.sg = [S.sb("sg%d" % i, [128, NT], F32) for i in range(2)]
        self.sg2 = [S.sb("sg2%d" % i, [128, NT], F32) for i in range(2)]
        self.macc = self.tmp(12, 1, [128, NT])
        self.ho = [self.tmp(13 + i, 1, [128, NT]) for i in range(2)]
        self.w8 = [S.sb("w8_%d" % i, [128, 8, 128], BF16) for i in range(6)]
        self.w22 = [S.sb("w22_%d" % i, [128, 22, 128], BF16) for i in range(2)]
        self.y = S.sb("y", [128, 4, 2, NT], BF16)
        self.pf = self.tmp(15, 2, [128, 2, NT])
        self.pb = self.tmp(17, 1, [128, 2, NT], BF16)
        self.gains = S.sb("gains", [128, 9, 8], F32)
        self.cst = S.sb("cst", [128, 8], F32)
        self.consts = S.sb("consts", [128, 512], F32)
        self.pc = S.sb("pc", [128, 8], F32)
        self.consts2 = S.sb("consts2", [128, 512], F32)
        self.wsm = S.sb("wsm", [128, NL, 8, 16], BF16)
        self.cwb = S.sb("cwb", [128, NL, 40], F32)
        self.mlrow = S.sb("mlrow", [128, NL, 264], F32)
        self.gdrow = S.sb("gdrow", [128, NL, 72], F32)
        self.mltail = [S.sb("mltail%d" % l, [128, 4, 3], F32) for l in range(NL)]
        self.gdtail = [S.sb("gdtail%d" % l, [128, 6, 3], F32) for l in range(NL)]
        self.mlC = [S.sb("mlC%d" % l, [128, 4, 66], F32) for l in range(NL)]
        self.mlm = [S.sb("mlm%d" % l, [128, 4], F32) for l in range(NL)]
        self.gdS = [S.sb("gdS%d" % l, [128, 4, 64], F32) for l in range(NL)]
        self.gmask = S.sb("gmask", [128, 256], F32)
        self.negones = S.sb("negones", [128, 64], F32)
        self.cm = S.sb("cm", [128, 2, 256], BF16)
        self.ident_bf = S.sb("ident_bf", [128, 128], BF16)
        self.kcache = [S.sb("kcache%d" % l, [128, 2, SEQ], BF16) for l in range(NL)]
        self.vcache = [S.sb("vcache%d" % l, [128, 16, 256], BF16) for l in range(NL)]
        self.kmean = [S.sb("kmean%d" % l, [128, 2, 8], F32) for l in range(NL)]
        self.s5f = S.sb("s5f", [128, 3104], F32)
        self.s5b = S.sb("s5b", [128, 4352], BF16)
        f = self.s5f.t
        self.s5F = {"CS": f[:, 0:2048].rearrange("p (a b c) -> p a b c", b=2, c=128),
                    "R": f[:, 2048:3072].rearrange("p (a b) -> p a b", b=128),
                    "E128": f[:, 3072:3088].rearrange("p (a b) -> p a b", b=2),
                    "rcol": f[:, 3088:3096], "dbg": f[:, 3096:3100]}
        b = self.s5b.t
        self.s5B = {"BT": b[:, 0:2048].rearrange("p (a b c) -> p a b c", b=2, c=128),
                    "CT": b[:, 2048:4096].rearrange("p (a b c) -> p a b c", b=2, c=128),
                    "Dg": b[:, 4096:4352].rearrange("p (a b) -> p a b", b=128)}
        self.s5state = [S.sb("s5st%d" % l, [128, 2, 8], F32) for l in range(NL)]
        self.ones_bf = S.sb("ones_bf", [128, 128], BF16)
        self.P = [S.ps("P%d" % i, [128, NT], F32) for i in range(8)]
        self.cstg = [S.sb("cstg%d" % i, [128, 4096], BF16) for i in range(2)]

    def setup(self):
        S = self.S
        S.op("pool", lambda e: e.memset(self.ones_bf.t[:], 1.0), [], [self.ones_bf])
        S.op("pool", lambda e: e.memset(self.cst.t[:, 0:1], 1e-6), [], [self.cst])
        S.op("pool", lambda e: e.memset(self.cst.t[:, 1:2], -PI), [], [self.cst])
        S.dma("sp", self.pc.t[:], self.D["pc"].t, [], [self.pc], self.pc)
        S.op("pool", lambda e: e.memset(self.cst.t[:, 3:4], 1.0), [], [self.cst])
        S.dma("sp", self.consts2.t[:], self.D["consts2"].t, [], [self.consts2], self.consts2)
        S.dma("sp", self.cwb.t[:], self.D["cwf"].t, [], [self.cwb], self.cwb)
        S.dma("sp", self.mlrow.t[:], self.D["mlrowf"].t, [], [self.mlrow], self.mlrow)
        S.dma("sp", self.gdrow.t[:], self.D["gdrowf"].t, [], [self.gdrow], self.gdrow)
        S.dma("sp", self.gmask.t[:], self.D["gmaskf"].t, [], [self.gmask], self.gmask)
        S.op("pool", lambda e: e.memset(self.negones.t[:], -1.0), [], [self.negones])
        wsf = self.tmp(13, 1, [128, NL, 8, 16])
        S.dma("sp", wsf.t, self.D["wsmf"].t, [], [wsf], wsf)
        self.cp(self.wsm.t[:], wsf.t, [wsf], [self.wsm], eng="dve")
        cmf = self.tmp(12, 1, [128, 2, 256])
        S.dma("sp", cmf.t, self.D["cmf"].t, [], [cmf], cmf)
        self.cp(self.cm.t[:], cmf.t, [cmf], [self.cm], eng="dve")
        for l in range(NL):
            S.op("pool", lambda e, l=l: e.memset(self.kmean[l].t[:], 0.0), [], [self.kmean[l]])
        S.dma("sp", self.gains.t[:], self.D["gains"].t, [], [self.gains], self.gains)
        S.dma("sp", self.consts.t[:], self.D["consts"].t, [], [self.consts], self.consts)
        self.cp(self.ident_bf.t[:], self.consts.t[:, 128:256], [self.consts], [self.ident_bf], eng="dve")
        self.conv_i = 0
        self.conv_pending = None
        self.convert([("wgu", 0, 0), ("wdn", 0, 0), ("win", 0), ("wglu", 0), ("wgate", 0), ("wbr", 0), ("wout", 0),
                      ("wgu", 0, 1), ("wdn", 0, 1), ("wplg", 0), ("wplp", 0),
                      ("wgu", 1, 0), ("wdn", 1, 0), ("win", 1), ("wglu", 1), ("wgate", 1), ("wbr", 1), ("wout", 1),
                      ("wgu", 1, 1), ("wdn", 1, 1), ("wplg", 1), ("wplp", 1)])

    def norm(self, gcol):
        h, u, S = self.h, self.u, self.S
        pss, rstd = self.P[6], self.rstd
        self.act(u.t[:], h.t[:], AF.Square, [h], [u])
        for k in range(8):
            self.mm(pss.t[:], self.ones_bf.t[:], u.t[:, k, :], k == 0, k == 7, [u, self.ones_bf], [pss])
        self.act(rstd.t[:], pss.t[:], AF.Sqrt, [pss, self.cst], [rstd], scale=1.0 / 1024, bias=self.cst.t[:, 0:1])
        S.op("dve", lambda e: e.reciprocal(rstd.t[:], rstd.t[:]), [rstd], [rstd])
        for k in range(8):
            self.stt(u.t[:, k, :], h.t[:, k, :], self.gains.t[:, gcol, k:k + 1], rstd.t[:], ALU.mult, ALU.mult,
                     [h, rstd, self.gains], [u])

    def ffn(self, l, which):
        h, u, actb = self.h, self.u, self.actb
        self.norm(l * 4 + (0 if which == 0 else 2))
        base = (l * 2 + which) * 44
        for fc in range(22):
            wg = self.loadw("wgu", base + 2 * fc, 8)
            wu = self.loadw("wgu", base + 2 * fc + 1, 8)
            pg, pu = self.P[fc % 2], self.P[2 + fc % 2]
            sg = self.sg[fc % 2]
            for k in range(8):
                self.mm(pg.t[:], wg.t[:, k, :], u.t[:, k, :], k == 0, k == 7, [wg, u], [pg])
            for k in range(8):
                self.mm(pu.t[:], wu.t[:, k, :], u.t[:, k, :], k == 0, k == 7, [wu, u], [pu])
            self.act(sg.t[:], pg.t[:], AF.Silu, [pg], [sg])
            self.tt(actb.t[:, fc, :], sg.t[:], pu.t[:], ALU.mult, [sg, pu], [actb])
        base = (l * 2 + which) * 8
        for oc in range(8):
            wd = self.loadw("wdn", base + oc, 22)
            po = self.P[4 + oc % 2]
            for fc in range(22):
                self.mm(po.t[:], wd.t[:, fc, :], actb.t[:, fc, :], fc == 0, fc == 21, [wd, actb], [po])
            self.stt(h.t[:, oc, :], po.t[:], 0.5, h.t[:, oc, :], ALU.mult, ALU.add, [po, h], [h])

    def ple(self, l, s, t0):
        h, u, S = self.h, self.u, self.S
        self.norm(l * 4 + 3)
        S.dma("sp", self.pf.t[:], self.D["pT"].t[l, s, :, :, t0:t0 + NT], [], [self.pf], self.pf)
        self.cp(self.pb.t[:], self.pf.t[:], [self.pf], [self.pb], eng="pool")
        for oc in range(8):
            wg = self.loadw("wplg", l * 8 + oc, 8)
            wp = self.loadw("wplp", l * 8 + oc, 2)
            pa, pg = self.P[oc % 2], self.P[2 + oc % 2]
            sg, sg2 = self.sg[oc % 2], self.sg2[oc % 2]
            for k in range(2):
                self.mm(pa.t[:], wp.t[:, k, :], self.pb.t[:, k, :], k == 0, k == 1, [wp, self.pb], [pa])
            for k in range(8):
                self.mm(pg.t[:], wg.t[:, k, :], u.t[:, k, :], k == 0, k == 7, [wg, u], [pg])
            self.act(sg.t[:], pg.t[:], AF.Sigmoid, [pg], [sg])
            self.tt(sg2.t[:], sg.t[:], pa.t[:], ALU.mult, [sg, pa], [sg2])
            self.tt(h.t[:, oc, :], h.t[:, oc, :], sg2.t[:], ALU.add, [h, sg2], [h], eng="pool")

    def merge(self, l):
        h, u, y, actb = self.h, self.u, self.y, self.actb
        macc = self.macc
        for oc in range(8):
            for b in range(4):
                wg = self.loadw("wgate", (l * 8 + oc) * 4 + b, 8)
                wbr = self.loadw("wbr", (l * 8 + oc) * 4 + b, 2)
                pg, pbr = self.P[b % 2], self.P[2 + b % 2]
                sg, sg2 = self.sg[b % 2], self.sg2[b % 2]
                for k in range(8):
                    self.mm(pg.t[:], wg.t[:, k, :], u.t[:, k, :], k == 0, k == 7, [wg, u], [pg])
                for k in range(2):
                    self.mm(pbr.t[:], wbr.t[:, k, :], y.t[:, b, k, :], k == 0, k == 1, [wbr, y], [pbr])
                self.act(sg.t[:], pg.t[:], AF.Sigmoid, [pg], [sg])
                if b == 0:
                    self.tt(macc.t[:], sg.t[:], pbr.t[:], ALU.mult, [sg, pbr], [macc])
                else:
                    self.tt(sg2.t[:], sg.t[:], pbr.t[:], ALU.mult, [sg, pbr], [sg2])
                    if b < 3:
                        self.tt(macc.t[:], macc.t[:], sg2.t[:], ALU.add, [macc, sg2], [macc], eng="pool")
                    else:
                        self.tt(actb.t[:, oc, :], macc.t[:], sg2.t[:], ALU.add, [macc, sg2], [actb], eng="pool")
        for oc in range(8):
            wo = self.loadw("wout", l * 8 + oc, 8)
            po = self.P[4 + oc % 2]
            for k in range(8):
                self.mm(po.t[:], wo.t[:, k, :], actb.t[:, k, :], k == 0, k == 7, [wo, actb], [po])
            self.tt(h.t[:, oc, :], h.t[:, oc, :], po.t[:], ALU.add, [h, po], [h])

    def final(self, s, t0):
        h, S = self.h, self.S
        pss, rstd = self.P[6], self.rstd
        u = self.u
        self.act(u.t[:], h.t[:], AF.Square, [h], [u])
        for k in range(8):
            self.mm(pss.t[:], self.ones_bf.t[:], u.t[:, k, :], k == 0, k == 7, [u, self.ones_bf], [pss])
        self.act(rstd.t[:], pss.t[:], AF.Sqrt, [pss, self.cst], [rstd], scale=1.0 / 1024, bias=self.cst.t[:, 0:1])
        S.op("dve", lambda e: e.reciprocal(rstd.t[:], rstd.t[:]), [rstd], [rstd])
        for k in range(8):
            ho = self.ho[k % 2]
            self.stt(ho.t[:], h.t[:, k, :], self.gains.t[:, 8, k:k + 1], rstd.t[:], ALU.mult, ALU.mult,
                     [h, rstd, self.gains], [ho])
            S.dma("sp", self.out.t[s, :, k, t0:t0 + NT], ho.t[:], [ho], [self.out], ho)

    def dump_h(self, idx):
        S = self.S
        S.dma("sp", self.dump.t[idx], self.h.t[:], [self.h], [self.dump], self.h)


    def dd(self, name, src, R, shape, dt=F32):
        if name not in self.dbg.get("dd", ()):
            return
        b = Buf(name, self.nc.dram_tensor("dd_" + name, list(shape), dt, kind="ExternalOutput").ap())
        self.S.dma("sp", b.t, src, R, [b], b)

    def frac2pi(self, out, x, shift, tB, R, W):
        MAG = 12582912.0
        self.ts(out, x, 1.0 / (2 * PI), ALU.mult, R, W, s2=shift / (2 * PI), op1=ALU.add)
        self.ts(tB, out, MAG, ALU.add, R, W)
        self.ts(tB, tB, -MAG, ALU.add, R, W)
        self.tt(out, out, tB, ALU.subtract, R, W)

    def s5_disc(self, lr, li, ldt, T, TB, want_z):
        R = TB + [self.s5in]
        W = TB
        self.act(T[0], ldt, AF.Exp, R, W)
        self.tt(T[5], lr, T[0], ALU.mult, R, W)
        self.act(T[1], T[5], AF.Exp, R, W)
        self.tt(T[2], li, T[0], ALU.mult, R, W)
        if not want_z:
            self.frac2pi(T[5], T[2], 0.0, T[8], R, W)
            self.ts(T[2], T[5], 2 * PI, ALU.mult, R, W)
            return T[1], T[2], None, None
        self.frac2pi(T[5], T[2], 0.0, T[8], R, W)
        self.act(T[3], T[5], AF.Sin, R, W, scale=2 * PI)
        self.frac2pi(T[5], T[2], 0.5 * PI, T[8], R, W)
        self.act(T[4], T[5], AF.Sin, R, W, scale=2 * PI)
        self.tt(T[4], T[4], T[1], ALU.mult, R, W)
        self.tt(T[3], T[3], T[1], ALU.mult, R, W)
        self.ts(T[5], T[4], -1.0, ALU.add, R, W)
        self.tt(T[8], lr, lr, ALU.mult, R, W)
        self.tt(T[0], li, li, ALU.mult, R, W)
        self.tt(T[8], T[8], T[0], ALU.add, R, W)
        self.S.op("dve", lambda e: e.reciprocal(T[8], T[8]), R, W)
        self.tt(T[0], T[5], lr, ALU.mult, R, W)
        self.tt(T[6], T[3], li, ALU.mult, R, W)
        self.tt(T[6], T[6], T[0], ALU.add, R, W)
        self.tt(T[6], T[6], T[8], ALU.mult, R, W)
        self.tt(T[0], T[3], lr, ALU.mult, R, W)
        self.tt(T[7], T[5], li, ALU.mult, R, W)
        self.tt(T[7], T[0], T[7], ALU.subtract, R, W)
        self.tt(T[7], T[7], T[8], ALU.mult, R, W)
        return T[1], T[2], T[6], T[7]

    def s5_setup(self, l):
        S, D = self.S, self.D
        s5f, s5b, cst = self.s5f, self.s5b, self.cst
        F = self.s5F
        stt_ = self.tmp(12, 1, [128, 8, 3])
        self.s5in = stt_.bufs[0]
        S.dma("sp", stt_.t, D["s5st"].t[l], [], [stt_], stt_)
        T9 = self.tmp(13, 1, [128, 9, 8])
        T = [T9.t[:, i, :] for i in range(9)]
        mag, th, _, _ = self.s5_disc(stt_.t[:, :, 0], stt_.t[:, :, 1], stt_.t[:, :, 2], T, [T9.bufs[0]], False)
        R9 = [T9]
        X = self.tmp(14, 2, [128, 8, 128])
        Y = self.tmp(16, 2, [128, 8, 128])
        Z = self.tmp(18, 2, [128, 8, 128])
        jrow = self.consts.t[:, 0:128]
        for sc in range(8):
            self.ts(X.t[:, sc, :], jrow, th[:, sc:sc + 1], ALU.mult, R9 + [self.consts], [X])
        self.frac2pi(Y.t, X.t, 0.0, Z.t, [X, Y, Z], [Y, Z])
        self.act(F["CS"][:, :, 1, :], Y.t, AF.Sin, [Y], [s5f], scale=2 * PI)
        self.frac2pi(Y.t, X.t, 0.5 * PI, Z.t, [X, Y, Z], [Y, Z])
        self.act(F["CS"][:, :, 0, :], Y.t, AF.Sin, [Y], [s5f], scale=2 * PI)
        self.ts(T[3], th, 128.0, ALU.mult, R9, R9)
        self.frac2pi(T[4], T[3], 0.0, T[5], R9, R9)
        self.act(F["E128"][:, :, 1], T[4], AF.Sin, R9, [s5f], scale=2 * PI)
        self.frac2pi(T[4], T[3], 0.5 * PI, T[5], R9, R9)
        self.act(F["E128"][:, :, 0], T[4], AF.Sin, R9, [s5f], scale=2 * PI)
        self.cp(F["rcol"], mag, R9, [s5f], eng="dve")
        S.op("dve", lambda e: e.memset(F["R"], 0.0), [], [s5f])
        ones = self.consts.t[:, 384:511]
        for sc in range(8):
            self.ts(F["R"][:, sc, 1:128], ones, mag[:, sc:sc + 1], ALU.mult, R9 + [self.consts], [s5f])
        S.dma("sp", F["dbg"], D["s5db"].t[l], [], [s5f], s5f)
        for hh in range(2):
            prm = self.tmp(12, 3, [128, 3, 512])
            self.s5in = prm.bufs[0]
            S.dma("sp", prm.t.rearrange("p a (s c) -> p a s c", c=128),
                  D["s5rep"].t[l, :, :, 4 * hh:4 * hh + 4, :].rearrange("a p s c -> p a s c"), [], [prm], prm)
            TT_ = self.tmp(15, 9, [128, 9, 512])
            T = [TT_.t[:, i, :] for i in range(9)]
            TB = TT_.bufs + prm.bufs[1:]
            _, _, zr, zi = self.s5_disc(prm.t[:, 0, :], prm.t[:, 1, :], prm.t[:, 2, :], T, TB, True)
            bx = self.tmp(12, 2, [128, 2, 512])
            S.dma("sp", bx.t.rearrange("p a (s c) -> p a s c", c=128),
                  D["s5bexp"].t[l, :, :, 4 * hh:4 * hh + 4, :].rearrange("a p s c -> p a s c"), [], [bx], bx)
            RR = TB + bx.bufs
            self.tt(T[0], zr, bx.t[:, 0, :], ALU.mult, RR, TB)
            self.tt(T[1], zi, bx.t[:, 1, :], ALU.mult, RR, TB)
            self.tt(F32v(self, "BTre", hh), T[0], T[1], ALU.subtract, RR, [s5b])
            self.tt(T[0], zr, bx.t[:, 1, :], ALU.mult, RR, TB)
            self.tt(T[1], zi, bx.t[:, 0, :], ALU.mult, RR, TB)
            self.tt(F32v(self, "BTim", hh), T[0], T[1], ALU.add, RR, [s5b])
            cx = self.tmp(12, 2, [128, 2, 512])
            S.dma("sp", cx.t.rearrange("p a (s c) -> p a s c", c=128),
                  D["s5cexp"].t[l, :, :, 4 * hh:4 * hh + 4, :].rearrange("a p s c -> p a s c"), [], [cx], cx)
            self.cp(F32v(self, "CTre", hh), cx.t[:, 0, :], [cx], [s5b], eng="dve")
            self.ts(F32v(self, "CTim", hh), cx.t[:, 1, :], -1.0, ALU.mult, [cx], [s5b])
        ident = self.consts.t[:, 128:256]
        for kc in range(2):
            self.ts(self.s5B["Dg"][:, kc, :], ident, F["dbg"][:, kc:kc + 1], ALU.mult, [s5f, self.consts], [s5b])
        S.dma("sp", self.s5f_d.t[l], s5f.t[:], [s5f], [self.s5f_d], s5f)
        S.dma("sp", self.s5b_d.t[l], s5b.t[:], [s5b], [self.s5b_d], s5b)

    def s5_fwd(self, l, s, tb):
        S, u, y = self.S, self.u, self.y
        s5f, s5b = self.s5f, self.s5b
        F, B = self.s5F, self.s5B
        st = self.s5state[l]
        self.s5_calls = getattr(self, "s5_calls", 0) + 1
        S.dma("sp", s5f.t[:], self.s5f_d.t[l], [self.s5f_d], [s5f], s5f)
        S.dma("sp", s5b.t[:], self.s5b_d.t[l], [self.s5b_d], [s5b], s5b)
        us5 = self.tmp(12, 1, [128, 2, NT], BF16)
        for kc in range(2):
            w = self.loadw("win", l * 28 + 16 + kc, 8)
            pp = self.P[kc]
            for k in range(8):
                self.mm(pp.t[:], w.t[:, k, :], u.t[:, k, :], k == 0, k == 7, [w, u], [pp])
            self.cp(us5.t[:, kc, :], pp.t[:], [pp], [us5], eng="act")
        A = self.tmp(13, 1, [128, 4, 128])
        Bt = self.tmp(14, 1, [128, 4, 128])
        bh = [self.tmp(15, 2, [128, 8, 128]), self.tmp(17, 2, [128, 8, 128])]
        xh = [self.tmp(19, 2, [128, 8, 128]), self.tmp(21, 2, [128, 8, 128])]
        xb = [self.tmp(23, 1, [128, 8, 128], BF16), self.tmp(0, 1, [128, 8, 128], BF16)]
        ini = self.tmp(1, 1, [128, 4, 8])
        A2 = self.tmp(2, 1, [128, 4, 128])
        B2 = self.tmp(3, 1, [128, 4, 128])
        ypre = [self.P[4], self.P[5]]
        CS = F["CS"]
        if tb == 0:
            S.op("dve", lambda e: e.memset(st.t[:], 0.0), [], [st])
        for sub in range(4):
            c0 = sub * 128
            for sc in range(8):
                for ri in range(2):
                    pp = self.P[2 * ri + sc // 4]
                    self.mm(pp.t[:, (sc % 4) * 128:(sc % 4 + 1) * 128], B["BT"][:, sc, ri, :],
                            us5.t[:, sc // 4, c0:c0 + 128], True, True, [s5b, us5], [pp])
            for hh in range(2):
                c = CS[:, 4 * hh:4 * hh + 4, 0, :]
                sn = CS[:, 4 * hh:4 * hh + 4, 1, :]
                pre = self.P[hh].t[:].rearrange("p (a b) -> p a b", b=128)
                pim = self.P[2 + hh].t[:].rearrange("p (a b) -> p a b", b=128)
                self.tt(A.t, pre, c, ALU.mult, [self.P[hh], s5f], [A])
                self.tt(Bt.t, pim, sn, ALU.mult, [self.P[2 + hh], s5f], [Bt])
                self.tt(bh[0].t[:, 4 * hh:4 * hh + 4, :], A.t, Bt.t, ALU.add, [A, Bt], [bh[0]])
                self.tt(A.t, pim, c, ALU.mult, [self.P[2 + hh], s5f], [A])
                self.tt(Bt.t, pre, sn, ALU.mult, [self.P[hh], s5f], [Bt])
                self.tt(bh[1].t[:, 4 * hh:4 * hh + 4, :], A.t, Bt.t, ALU.subtract, [A, Bt], [bh[1]])
            if not (tb == 0 and sub == 0):
                i0, i1, i2, i3 = (ini.t[:, i, :] for i in range(4))
                c1, s1 = F["E128"][:, :, 0], F["E128"][:, :, 1]
                self.tt(i0, c1, st.t[:, 0, :], ALU.mult, [s5f, st], [ini])
                self.tt(i1, s1, st.t[:, 1, :], ALU.mult, [s5f, st], [ini])
                self.tt(i0, i0, i1, ALU.subtract, [ini], [ini])
                self.tt(i2, c1, st.t[:, 1, :], ALU.mult, [s5f, st], [ini])
                self.tt(i3, s1, st.t[:, 0, :], ALU.mult, [s5f, st], [ini])
                self.tt(i2, i2, i3, ALU.add, [ini], [ini])
                self.tt(i0, i0, F["rcol"], ALU.mult, [ini, s5f], [ini])
                self.tt(i2, i2, F["rcol"], ALU.mult, [ini, s5f], [ini])
                self.tt(bh[0].t[:, :, 0], bh[0].t[:, :, 0], i0, ALU.add, [bh[0], ini], [bh[0]])
                self.tt(bh[1].t[:, :, 0], bh[1].t[:, :, 0], i2, ALU.add, [bh[1], ini], [bh[1]])
            Rf = F["R"].rearrange("p a b -> p (a b)")
            for ri in range(2):
                S.op("dve", lambda e, ri=ri: e.tensor_tensor_scan(
                    out=xh[ri].t.rearrange("p a b -> p (a b)"), data0=Rf,
                    data1=bh[ri].t.rearrange("p a b -> p (a b)"), initial=0.0, op0=ALU.mult, op1=ALU.add),
                    [bh[ri], s5f], [xh[ri]])
                self.cp(st.t[:, ri, :], xh[ri].t[:, :, 127], [xh[ri]], [st], eng="dve")
            for hh in range(2):
                c = CS[:, 4 * hh:4 * hh + 4, 0, :]
                sn = CS[:, 4 * hh:4 * hh + 4, 1, :]
                hs = slice(4 * hh, 4 * hh + 4)
                re_ = "dve" if self.s5_calls == 1 else "pool"
                self.tt(A2.t, xh[0].t[:, hs, :], c, ALU.mult, [xh[0], s5f], [A2], eng=re_)
                self.tt(B2.t, xh[1].t[:, hs, :], sn, ALU.mult, [xh[1], s5f], [B2], eng=re_)
                self.tt(xb[0].t[:, hs, :], A2.t, B2.t, ALU.subtract, [A2, B2], [xb[0]], eng=re_)
                self.tt(A2.t, xh[1].t[:, hs, :], c, ALU.mult, [xh[1], s5f], [A2], eng=re_)
                self.tt(B2.t, xh[0].t[:, hs, :], sn, ALU.mult, [xh[0], s5f], [B2], eng=re_)
                self.tt(xb[1].t[:, hs, :], A2.t, B2.t, ALU.add, [A2, B2], [xb[1]], eng=re_)
            for kc in range(2):
                pp = ypre[kc]
                o = pp.t[:, c0:c0 + 128]
                n = 0
                for sc in range(4 * kc, 4 * kc + 4):
                    for ri in range(2):
                        self.mm(o, B["CT"][:, sc, ri, :], xb[ri].t[:, sc, :], n == 0, False, [s5b, xb[ri]], [pp])
                        n += 1
                self.mm(o, B["Dg"][:, kc, :], us5.t[:, kc, c0:c0 + 128], False, True, [s5b, us5], [pp])
        yg = self.tmp(13, 2, [128, 2, NT])
        t1 = self.tmp(15, 2, [128, 2, NT])
        ygb = self.tmp(17, 1, [128, 2, NT], BF16)
        for kc in range(2):
            self.cp(yg.t[:, kc, :], ypre[kc].t[:], [ypre[kc]], [yg], eng="act")
        self.act(t1.t, yg.t, AF.Square, [yg], [t1])
        self.ts(t1.t, t1.t, 0.044715, ALU.mult, [t1], [t1], s2=1.0, op1=ALU.add)
        self.tt(t1.t, t1.t, yg.t, ALU.mult, [t1, yg], [t1])
        self.act(t1.t, t1.t, AF.Sigmoid, [t1], [t1], scale=1.5957691216)
        self.tt(yg.t, yg.t, t1.t, ALU.mult, [t1, yg], [yg])
        self.cp(ygb.t, yg.t, [yg], [ygb], eng="dve" if self.s5_calls == 1 else "pool")
        for oc in range(2):
            w = self.loadw("wglu", l * 2 + oc, 2)
            pp = self.P[6 + oc]
            for k in range(2):
                self.mm(pp.t[:], w.t[:, k, :], ygb.t[:, k, :], k == 0, k == 1, [w, ygb], [pp])
            self.act(t1.t[:, oc, :], pp.t[:], AF.Sigmoid, [pp, s5f], [t1], bias=F["dbg"][:, 2 + oc:3 + oc])
            self.tt(y.t[:, 2, oc, :], yg.t[:, oc, :], t1.t[:, oc, :], ALU.mult, [yg, t1], [y])


    def moba_fwd(self, l, s, tb):
        S, u, y, D = self.S, self.u, self.y, self.D
        t0 = tb * NT
        kc_, vc_, km = self.kcache[l], self.vcache[l], self.kmean[l]
        pc, consts = self.pc, self.consts
        cosT = self.tmp(0, 1, [128, NT])
        sinT = self.tmp(1, 1, [128, NT])
        posi = self.tmp(2, 1, [128, NT])
        ang = self.tmp(3, 1, [128, NT])
        fr = self.tmp(4, 1, [128, NT])
        fb = self.tmp(5, 1, [128, NT])
        qf = [self.tmp(6, 1, [128, NT]), self.tmp(7, 1, [128, NT])]
        kf = [self.tmp(8, 1, [128, NT]), self.tmp(9, 1, [128, NT])]
        t1 = self.tmp(10, 1, [128, NT])
        t2 = self.tmp(12, 1, [128, NT])
        qb_ = self.tmp(13, 1, [128, 2, NT], BF16)
        sm = self.tmp(14, 1, [128, 512])
        gm = sm.t[:, 0:32].rearrange("p (a b) -> p a b", b=8)
        mx = sm.t[:, 32:64].rearrange("p (a b) -> p a b", b=8)
        mnegb = sm.t[:, 64:128].bitcast(BF16).rearrange("p (a b) -> p a b", b=32)
        et = [self.tmp(15, 1, [128, 1024], BF16)]
        ets = [et[0].t[:, 0:512].rearrange("p (a b) -> p a b", b=256), et[0].t[:, 512:1024].rearrange("p (a b) -> p a b", b=256)]
        rden = self.tmp(16, 1, [128, 2, 256])
        S.dma("sp", posi.t.bitcast(I32), D["posr"].t[s, :, t0:t0 + NT], [], [posi], posi)
        self.cp(ang.t, posi.t.bitcast(I32), [posi], [ang], eng="dve")
        self.ts(ang.t, ang.t, pc.t[:, 0:1], ALU.mult, [ang, pc], [ang])
        self.frac2pi(fr.t, ang.t, 0.5 * PI, fb.t, [ang, fr, fb], [fr, fb])
        self.act(cosT.t, fr.t, AF.Sin, [fr], [cosT], scale=2 * PI)
        self.frac2pi(fr.t, ang.t, 0.0, fb.t, [ang, fr, fb], [fr, fb])
        self.act(sinT.t, fr.t, AF.Sin, [fr, pc], [sinT], scale=pc.t[:, 2:3])
        for c in range(2):
            for (dst, base) in ((qf[c], 18), (kf[c], 20)):
                w1 = self.loadw("win", l * 28 + base + c, 8)
                w2 = self.loadw("win", l * 28 + base + 6 + c, 8)
                p1, p2 = self.P[0], self.P[1]
                for k in range(8):
                    self.mm(p1.t[:], w1.t[:, k, :], u.t[:, k, :], k == 0, k == 7, [w1, u], [p1])
                for k in range(8):
                    self.mm(p2.t[:], w2.t[:, k, :], u.t[:, k, :], k == 0, k == 7, [w2, u], [p2])
                self.tt(t1.t, p1.t[:], cosT.t, ALU.mult, [p1, cosT], [t1])
                self.tt(t2.t, p2.t[:], sinT.t, ALU.mult, [p2, sinT], [t2])
                self.tt(dst.t, t1.t, t2.t, ALU.add, [t1, t2], [dst], eng="pool")
            self.cp(qb_.t[:, c, :], qf[c].t, [qf[c]], [qb_], eng="pool")
            self.cp(kc_.t[:, c, t0:t0 + NT], kf[c].t, [kf[c]], [kc_], eng="pool")
            S.op("dve", lambda e, c=c: e.tensor_reduce(out=km.t[:, c, 2 * tb:2 * tb + 2],
                                                      in_=kf[c].t.rearrange("p (a b) -> p a b", b=256),
                                                      axis=AX.X, op=ALU.add), [kf[c]], [km])
        self.ts(km.t[:, :, 2 * tb:2 * tb + 2], km.t[:, :, 2 * tb:2 * tb + 2], 1.0 / 256, ALU.mult, [km], [km])
        wv = [self.loadw("win", l * 28 + 22 + i, 8) for i in range(2)]
        for tt_ in range(4):
            pv = self.P[2 + tt_ % 2]
            for i in range(2):
                for k in range(8):
                    self.mm(pv.t[:, i * 128:(i + 1) * 128], u.t[:, k, tt_ * 128:(tt_ + 1) * 128], wv[i].t[:, k, :],
                            k == 0, k == 7, [wv[i], u], [pv])
            self.cp(vc_.t[:, tb * 4 + tt_, :], pv.t[:, 0:256], [pv], [vc_], eng="act")
        if l == 0 and tb == self.dbg.get("ddtb", 0):
            self.dd("cosT", cosT.t, [cosT], [128, NT])
            self.dd("sinT", sinT.t, [sinT], [128, NT])
            self.dd("qf0", qf[0].t, [qf[0]], [128, NT])
            self.dd("kf1", kf[1].t, [kf[1]], [128, NT])
            self.dd("km", km.t[:], [km], [128, 2, 8])
            self.dd("vc", vc_.t[:, tb * 4, :], [vc_], [128, 256], BF16)
        if tb > 0 or True:
            for qt in range(4):
                qblk = 2 * tb + qt // 2
                if qblk == 0:
                    continue
                pg = self.P[6]
                for h in range(4):
                    c, off = h // 2, (h % 2) * 64
                    self.mm(pg.t[:, h * 8:h * 8 + 8], qf[c].t[off:off + 64, qt * 128:(qt + 1) * 128],
                            km.t[off:off + 64, c, :], True, True, [qf[c], km], [pg])
                vm = consts.t[:, 256 + qblk * 8:256 + qblk * 8 + 8].unsqueeze(1).broadcast_to([128, 4, 8])
                self.tt(gm, pg.t[:, 0:32].rearrange("p (a b) -> p a b", b=8), vm, ALU.add, [pg, consts], [sm])
                for h in range(4):
                    S.op("dve", lambda e, h=h: e.max(out=mx[:, h, :], in_=gm[:, h, :]), [sm], [sm])
                for h in range(4):
                    self.ts(gm[:, h, :], gm[:, h, :], mx[:, h, 2:3], ALU.is_ge, [sm], [sm], s2=30000.0, op1=ALU.mult)
                self.ts(mnegb[:, qt, :], sm.t[:, 0:32], -30000.0, ALU.add, [sm], [sm])
                if l == 0 and tb == self.dbg.get("ddtb", 0) and qt == 3:
                    self.dd("sm", sm.t[:, 0:128], [sm], [128, 128])
        it = 0
        for c in range(2):
            for j in range(2):
                qblk = 2 * tb + j
                nkt = 2 * (qblk + 1)
                pacc, pden = self.P[2 + 2 * (it % 2)], self.P[3 + 2 * (it % 2)]
                it += 1
                qs = slice(j * 256, (j + 1) * 256)
                for kt in range(nkt):
                    n = kt // 2
                    ps_ = self.P[kt % 2]
                    e_ = ets[kt % 2]
                    for hh in range(2):
                        h, off = 2 * c + hh, hh * 64
                        o = ps_.t[:, hh * 256:(hh + 1) * 256]
                        self.mm(o, kc_.t[off:off + 64, c, kt * 128:(kt + 1) * 128], qb_.t[off:off + 64, c, qs],
                                True, False, [kc_, qb_], [ps_])
                        if n < qblk:
                            for q2 in range(2):
                                qt = 2 * j + q2
                                lh = mnegb[:, qt, h * 8 + n:h * 8 + n + 1].broadcast_to([128, 128])
                                self.mm(ps_.t[:, hh * 256 + q2 * 128:hh * 256 + (q2 + 1) * 128], lh, self.ident_bf.t[:],
                                        False, True, [sm, self.ident_bf], [ps_])
                        else:
                            self.mm(o, self.ident_bf.t[:], self.cm.t[:, kt % 2, :], False, True,
                                    [self.ident_bf, self.cm], [ps_])
                    if l == 0 and tb == self.dbg.get("ddtb", 0) and c == 0 and j == 0 and kt == 0 and "ps" in self.dbg.get("dd", ()):
                        dbgt = self.tmp(17, 1, [128, 512])
                        self.cp(dbgt.t, ps_.t[:], [ps_], [dbgt], eng="act")
                        self.dd("ps", dbgt.t, [dbgt], [128, 512])
                    self.act(e_, ps_.t[:].rearrange("p (a b) -> p a b", b=256), AF.Exp, [ps_], [et[0]], scale=0.125)
                    if l == 0 and tb == self.dbg.get("ddtb", 0) and c == 0 and j == 0 and kt == 0:
                        self.dd("et", et[0].t[:, 0:512], [et[0]], [128, 512], BF16)
                        self.dd("cm", self.cm.t[:], [self.cm], [128, 2, 256], BF16)
                    e2 = et[0].t[:, (kt % 2) * 512:(kt % 2 + 1) * 512]
                    self.mm(pacc.t[:], vc_.t[:, kt, c * 128:(c + 1) * 128], e2,
                            kt == 0, kt == nkt - 1, [vc_, et[0]], [pacc])
                    self.mm(pden.t[:], self.ones_bf.t[:], e2,
                            kt == 0, kt == nkt - 1, [self.ones_bf, et[0]], [pden])
                S.op("dve", lambda e, pden=pden: e.reciprocal(rden.t.rearrange("p a b -> p (a b)"), pden.t[:]), [pden], [rden])
                if l == 0 and tb == self.dbg.get("ddtb", 0):
                    self.dd("rden%d%d" % (c, j), rden.t, [rden], [128, 2, 256])
                for hh in range(2):
                    off = hh * 64
                    self.tt(y.t[off:off + 64, 3, c, qs], pacc.t[off:off + 64, hh * 256:(hh + 1) * 256],
                            rden.t[off:off + 64, hh, :], ALU.mult, [pacc, rden], [y])

    def tokproj_small(self, l):
        u, pp = self.u, self.P[7]
        for c in range(8):
            for k in range(8):
                self.mm(pp.t[0:64, c * 16:(c + 1) * 16], u.t[:, k, c * 64:(c + 1) * 64], self.wsm.t[:, l, k, :],
                        k == 0, k == 7, [u, self.wsm], [pp])
        self.sp = self.tmp(11, 1, [128, 512])
        self.cp(self.sp.t[0:64, 0:128], pp.t[0:64, 0:128], [pp], [self.sp], eng="act")
        return self.sp.t[0:64, 0:128].rearrange("p (c n) -> p c n", n=16)

    def conv_silu(self, l, tiles, nch, tail, cw, dst, tb):
        S, u = self.S, self.u
        xc = self.tmp(0, 7, [128, nch, NT + 3])
        acc = self.tmp(9, 1, [128, NT])
        if tb == 0:
            S.op("dve", lambda e: e.memset(tail.t[:], 0.0), [], [tail])
        self.cp(xc.t[:, :, 0:3], tail.t[:], [tail], [xc], eng="dve")
        for c in range(nch):
            w = self.loadw("win", l * 28 + tiles + c, 8)
            pp = self.P[c % 2]
            for k in range(8):
                self.mm(pp.t[:], w.t[:, k, :], u.t[:, k, :], k == 0, k == 7, [w, u], [pp])
            self.cp(xc.t[:, c, 3:NT + 3], pp.t[:], [pp], [xc], eng="act")
        self.cp(tail.t[:], xc.t[:, :, NT:NT + 3], [xc], [tail], eng="dve")
        for c in range(nch):
            self.ts(acc.t, xc.t[:, c, 0:NT], cw[:, c * 4:c * 4 + 1], ALU.mult, [xc, self.cwb], [acc])
            for j in range(1, 4):
                self.stt(acc.t, xc.t[:, c, j:NT + j], cw[:, c * 4 + j:c * 4 + j + 1], acc.t, ALU.mult, ALU.add,
                         [xc, acc, self.cwb], [acc])
            self.act(dst.t[:, c, :], acc.t, AF.Silu, [acc], [dst])

    def mlstm_fwd(self, l, s, tb, sp):
        S, u, y = self.S, self.u, self.y
        consts, c2, cst = self.consts, self.consts2, self.cst
        ident = consts.t[:, 128:256]
        ones = consts.t[:, 384:512]
        TRI = c2.t[0:64, 0:64]
        CMASK = c2.t[0:64, 64:128]
        rows = self.mlrow.t[0:64, l, :]
        Cx, mrep = self.mlC[l], self.mlm[l]
        qk = self.tmp(12, 4, [128, 4, NT])
        self.conv_silu(l, 8, 4, self.mltail[l], self.cwb.t[:, l, 24:40], qk, tb)
        self.ts(qk.t[:, 2:4, :], qk.t[:, 2:4, :], 0.125, ALU.mult, [qk], [qk])
        ms = self.dbg.get("mlstop", 99)
        if ms <= 1:
            return
        kz = self.tmp(4, 4, [128, 4, NT])
        S.op("dve", lambda e: e.memset(kz.t, 0.0), [], [kz])
        for h in range(4):
            pr, off = h // 2, (h % 2) * 64
            self.cp(kz.t[off:off + 64, h, :], qk.t[off:off + 64, 2 + pr, :], [qk], [kz], eng="dve")
        if tb == 0:
            S.op("dve", lambda e: e.memset(Cx.t[:], 0.0), [], [Cx])
            S.op("dve", lambda e: e.memset(mrep.t[:], 0.0), [], [mrep])
        A = self.tmp(16, 1, [128, 512])
        R_ = [A, self.sp]
        v3 = lambda lo: A.t[0:64, lo:lo + 32].rearrange("p (c h) -> p c h", h=4)
        li, lf, b_, ak, tx = v3(0), v3(32), v3(64), v3(96), v3(128)
        grep = A.t[:, 160:192].rearrange("p (c h) -> p c h", h=4)
        mkrep = A.t[:, 192:224].rearrange("p (c h) -> p c h", h=4)
        Mall = A.t[:, 224:260].rearrange("p (c h) -> p c h", h=4)
        scall = A.t[:, 260:292].rearrange("p (c h) -> p c h", h=4)
        kws = v3(292)
        mk32 = A.t[0:32, 324:325]
        dg32 = A.t[0:32, 328:360]
        ib = rows[:, 0:4].unsqueeze(1).broadcast_to([64, 8, 4])
        fb = rows[:, 4:8].unsqueeze(1).broadcast_to([64, 8, 4])
        self.tt(li, sp[:, :, 8:12], ib, ALU.add, R_ + [self.mlrow], [A])
        self.tt(tx, sp[:, :, 12:16], fb, ALU.add, R_ + [self.mlrow], [A])
        self.act(tx, tx, AF.Exp, [A], [A], scale=-1.0)
        self.act(tx, tx, AF.Ln, [A, cst], [A], bias=cst.t[0:64, 3:4])
        self.ts(lf, tx, -1.0, ALU.mult, [A], [A])
        p7 = self.P[7]
        lf2 = A.t[0:64, 32:64]
        self.mm(p7.t[0:64, 0:32], TRI, lf2, True, True, [A, c2], [p7])
        self.cp(A.t[0:64, 64:96], p7.t[0:64, 0:32], [p7], [A], eng="act")
        self.mm(p7.t[:, 32:64], ones[0:64, :], lf2, True, True, [A, consts], [p7])
        self.cp(A.t[:, 160:192], p7.t[:, 32:64], [p7], [A], eng="act")
        self.tt(ak, grep[0:64], b_, ALU.subtract, [A], [A])
        self.tt(ak, ak, li, ALU.add, [A], [A])
        self.mm(p7.t[0:32, 64:128], A.t[0:64, 96:128], ident[0:64, 0:64], True, True, [A, consts], [p7])
        S.op("dve", lambda e: e.tensor_reduce(out=mk32, in_=p7.t[0:32, 64:128], axis=AX.X, op=ALU.max), [p7], [A])
        self.ts(dg32, ident[0:32, 0:32], mk32, ALU.mult, [A, consts], [A])
        self.mm(p7.t[:, 128:160], ones[0:32, :], dg32, True, True, [A, consts], [p7])
        self.cp(A.t[:, 192:224], p7.t[:, 128:160], [p7], [A], eng="act")
        self.cp(Mall[:, 0, :], mrep.t[:], [mrep], [A], eng="dve")
        t4 = A.t[:, 364:368]
        for c in range(8):
            self.tt(t4, grep[:, c, :], Mall[:, c, :], ALU.add, [A], [A])
            self.tt(Mall[:, c + 1, :], t4, mkrep[:, c, :], ALU.max, [A], [A])
        self.cp(mrep.t[:], Mall[:, 8, :], [A], [mrep], eng="dve")
        self.tt(scall, grep, Mall[:, 0:8, :], ALU.add, [A], [A])
        self.tt(scall, scall, Mall[:, 1:9, :], ALU.subtract, [A], [A])
        self.act(scall, scall, AF.Exp, [A], [A])
        self.tt(kws, ak, Mall[0:64, 1:9, :], ALU.subtract, [A], [A])
        self.act(kws, kws, AF.Exp, [A], [A])
        if ms <= 2:
            return
        vx = self.tmp(17, 1, [128, 512])
        vext = vx.t[0:64, 0:264].rearrange("p (h e) -> p h e", e=66)
        S.op("dve", lambda e: e.memset(vx.t[0:64, 0:264], 1.0), [], [vx])
        osg = self.tmp(18, 1, [128, 512])
        Dm = self.tmp(19, 1, [128, 512])
        LR = self.tmp(20, 1, [128, 512])
        sq_ = self.tmp(21, 1, [128, 512])
        sT = self.tmp(22, 1, [128, 512])
        ne = self.tmp(23, 1, [128, 512])
        tq = self.tmp(0, 1, [128, 512])
        kt_ = self.tmp(1, 1, [128, 512])
        hh_ = self.tmp(2, 1, [128, 512])
        B = self.tmp(3, 1, [128, 512])
        v4 = lambda T, lo=0: T.t[0:64, lo:lo + 256].rearrange("p (h e) -> p h e", e=64)
        wv = [self.loadw("win", l * 28 + 12 + i, 8) for i in range(4)]
        for c in range(8):
            cs = slice(c * 64, (c + 1) * 64)
            p0 = self.P[0]
            for i in range(4):
                for k in range(8):
                    self.mm(p0.t[0:64, i * 128:(i + 1) * 128], u.t[:, k, cs], wv[i].t[:, k, :], k == 0, k == 7,
                            [u, wv[i]], [p0])
            self.cp(vext[:, :, 0:64], p0.t[0:64, 0:256].rearrange("p (h e) -> p h e", e=64), [p0], [vx], eng="act")
            self.act(osg.t[0:64, 0:256], p0.t[0:64, 256:512], AF.Sigmoid, [p0], [osg])
            if ms <= 3:
                continue
            lft = LR.t[0:64, 0:256].rearrange("p (h e) -> p h e", e=64)
            rm = LR.t[0:64, 256:512].rearrange("p (h e) -> p h e", e=64)
            tri_b = TRI.unsqueeze(1).broadcast_to([64, 4, 64])
            id_b = ident[0:64, 0:64].unsqueeze(1).broadcast_to([64, 4, 64])
            self.tt(lft, tri_b, lf[:, c, :].unsqueeze(2).broadcast_to([64, 4, 64]), ALU.mult, [A, c2], [LR])
            self.tt(rm, id_b, li[:, c, :].unsqueeze(2).broadcast_to([64, 4, 64]), ALU.mult, [A, consts], [LR])
            self.tt(rm, rm, lft, ALU.subtract, [LR], [LR])
            p1 = self.P[1]
            for h in range(4):
                o = p1.t[0:64, h * 64:(h + 1) * 64]
                self.mm(o, lft[:, h, :], ones[0:64, 0:64], True, False, [LR, consts], [p1])
                self.mm(o, ones[0:64, 0:64], rm[:, h, :], False, True, [LR, consts], [p1])
            dmv = v4(Dm)
            self.tt(dmv, p1.t[0:64, 0:256].rearrange("p (h e) -> p h e", e=64),
                    CMASK.unsqueeze(1).broadcast_to([64, 4, 64]), ALU.add, [p1, c2], [Dm])
            sm_ = B.t[0:64, 0:64]
            mloc, mint, mt, wint, e2, qn, den = (B.t[0:64, 4 * i:4 * i + 4] for i in range(7))
            S.op("dve", lambda e, dmv=dmv, mloc=mloc: e.tensor_reduce(out=mloc, in_=dmv, axis=AX.X, op=ALU.max), [Dm], [B])
            self.tt(mint, b_[:, c, :], Mall[0:64, c, :], ALU.add, [A], [B])
            self.tt(mt, mint, mloc, ALU.max, [B], [B])
            self.tt(wint, mint, mt, ALU.subtract, [B], [B])
            self.act(wint, wint, AF.Exp, [B], [B])
            self.act(e2, mt, AF.Exp, [B], [B], scale=-1.0)
            if ms <= 4:
                continue
            p2 = self.P[2]
            for h in range(4):
                o = p2.t[0:64, h * 64:(h + 1) * 64]
                self.mm(o, ones[0:64, 0:64], lft[:, h, :], True, False, [LR, consts], [p2])
                self.mm(o, rm[:, h, :], ones[0:64, 0:64], False, True, [LR, consts], [p2])
            etv = v4(sq_)
            self.tt(etv, p2.t[0:64, 0:256].rearrange("p (h e) -> p h e", e=64),
                    c2.t[0:64, 320:384].unsqueeze(1).broadcast_to([64, 4, 64]), ALU.add, [p2, c2], [sq_])
            self.act(etv, etv, AF.Exp, [sq_], [sq_])
            p3 = self.P[3]
            for h in range(4):
                self.mm(p3.t[0:64, h * 64:(h + 1) * 64], kz.t[:, h, cs], qk.t[:, h // 2, cs], True, True, [kz, qk], [p3])
            self.tt(v4(sT), p3.t[0:64, 0:256].rearrange("p (h e) -> p h e", e=64), etv, ALU.mult, [p3, sq_], [sT])
            if ms <= 5:
                continue
            stv = v4(sT)
            p4, p5 = self.P[4], self.P[5]
            for h in range(4):
                pr, off = h // 2, (h % 2) * 64
                self.mm(p4.t[0:64, h * 66:(h + 1) * 66], stv[:, h, :], vext[:, h, :], True, True, [sT, vx], [p4])
                self.mm(p5.t[0:64, h * 66:(h + 1) * 66], qk.t[:, pr, cs], Cx.t[:, h, :],
                        True, True, [qk, Cx], [p5])
            nev = ne.t[0:64, 0:264].rearrange("p (h e) -> p h e", e=66)
            tqv = tq.t[0:64, 0:264].rearrange("p (h e) -> p h e", e=66)
            self.tt(tqv, p5.t[0:64, 0:264].rearrange("p (h e) -> p h e", e=66),
                    wint.unsqueeze(2).broadcast_to([64, 4, 66]), ALU.mult, [p5, B], [tq])
            self.tt(nev, p4.t[0:64, 0:264].rearrange("p (h e) -> p h e", e=66),
                    e2.unsqueeze(2).broadcast_to([64, 4, 66]), ALU.mult, [p4, B], [ne])
            self.tt(nev, nev, tqv, ALU.add, [tq, ne], [ne])
            self.act(den, nev[:, :, 64], AF.Abs, [ne], [B])
            self.tt(den, den, e2, ALU.max, [B], [B])
            S.op("dve", lambda e, den=den: e.reciprocal(den, den), [B], [B])
            hv = v4(hh_)
            self.tt(hv, nev[:, :, 0:64], den.unsqueeze(2).broadcast_to([64, 4, 64]), ALU.mult, [ne, B], [hh_])
            if ms <= 6:
                continue
            h2 = v4(hh_, 256)
            ss = B.t[0:64, 32:36]
            self.tt(h2, hv, hv, ALU.mult, [hh_], [hh_])
            S.op("dve", lambda e, h2=h2, ss=ss: e.tensor_reduce(out=ss, in_=h2, axis=AX.X, op=ALU.add), [hh_], [B])
            self.act(ss, ss, AF.Sqrt, [B, cst], [B], scale=1.0 / 64, bias=cst.t[0:64, 0:1])
            S.op("dve", lambda e, ss=ss: e.reciprocal(ss, ss), [B], [B])
            self.tt(hv, hv, ss.unsqueeze(2).broadcast_to([64, 4, 64]), ALU.mult, [hh_, B], [hh_])
            self.tt(hh_.t[0:64, 0:256], hh_.t[0:64, 0:256], rows[:, 8:264], ALU.mult, [hh_, self.mlrow], [hh_])
            self.tt(hh_.t[0:64, 0:256], hh_.t[0:64, 0:256], osg.t[0:64, 0:256], ALU.mult, [hh_, osg], [hh_])
            for kc in range(2):
                self.mm(p3.t[:, 256 + kc * 64:256 + (kc + 1) * 64], hh_.t[0:64, kc * 128:(kc + 1) * 128],
                        ident[0:64, 0:64], True, True, [hh_, consts], [p3])
                self.cp(y.t[:, 1, kc, cs], p3.t[:, 256 + kc * 64:256 + (kc + 1) * 64], [p3], [y], eng="act")
            if ms <= 7:
                continue
            p6 = self.P[6]
            for pr in range(2):
                self.mm(p6.t[0:64, pr * 128:(pr + 1) * 128], qk.t[:, 2 + pr, cs], ident, True, True, [qk, consts], [p6])
            kwv = v4(kt_)
            self.tt(kwv, p6.t[0:64, 0:256].rearrange("p (h e) -> p h e", e=64),
                    kws[:, c, :].unsqueeze(2).broadcast_to([64, 4, 64]), ALU.mult, [p6, A], [kt_])
            p7b = self.P[7]
            for h in range(4):
                pr, off = h // 2, (h % 2) * 64
                o = p7b.t[:, h * 66:(h + 1) * 66]
                self.mm(o, kt_.t[0:64, pr * 128:(pr + 1) * 128], vext[:, h, :], True, True, [kt_, vx], [p7b])
                self.stt(Cx.t[off:off + 64, h, :], Cx.t[off:off + 64, h, :], scall[off:off + 64, c, h:h + 1],
                         p7b.t[off:off + 64, h * 66:(h + 1) * 66], ALU.mult, ALU.add, [Cx, A, p7b], [Cx])

    def gdn_fwd(self, l, s, tb, sp):
        S, u, y = self.S, self.u, self.y
        consts, c2, cst = self.consts, self.consts2, self.cst
        ident = consts.t[:, 128:256]
        id64 = ident[0:64, 0:64]
        ones = consts.t[:, 384:512]
        on64 = ones[0:64, 0:64]
        neg64 = self.negones.t[0:64, 0:64]
        TRI = c2.t[0:64, 0:64]
        SLADD = self.gmask.t[0:64, 0:64]
        SUADD = self.gmask.t[0:64, 64:128]
        CMT = c2.t[0:64, 320:384]
        BLK = self.gmask.t[:, 128:256]
        rows = self.gdrow.t[0:64, l, :]
        Sz = self.gdS[l]
        b3 = lambda ap: ap.unsqueeze(1).broadcast_to([64, 4, 64])
        v4 = lambda T, lo=0: T.t[0:64, lo:lo + 256].rearrange("p (h e) -> p h e", e=64)
        qkv = self.tmp(12, 6, [128, 6, NT])
        self.conv_silu(l, 0, 6, self.gdtail[l], self.cwb.t[:, l, 0:24], qkv, tb)
        if tb == 0:
            S.op("dve", lambda e: e.memset(Sz.t[:], 0.0), [], [Sz])
        sq = self.tmp(9, 1, [128, NT])
        rs = self.tmp(8, 1, [128, NT])
        for c4 in range(4):
            pp = self.P[c4 % 2]
            self.tt(sq.t, qkv.t[:, c4, :], qkv.t[:, c4, :], ALU.mult, [qkv], [sq])
            self.mm(pp.t[:], BLK, sq.t, True, True, [sq, self.gmask], [pp])
            self.act(rs.t, pp.t[:], AF.Sqrt, [pp, cst], [rs], bias=cst.t[:, 0:1])
            S.op("dve", lambda e: e.reciprocal(rs.t, rs.t), [rs], [rs])
            if c4 < 2:
                self.stt(qkv.t[:, c4, :], qkv.t[:, c4, :], 0.125, rs.t, ALU.mult, ALU.mult, [qkv, rs], [qkv])
            else:
                self.tt(qkv.t[:, c4, :], qkv.t[:, c4, :], rs.t, ALU.mult, [qkv, rs], [qkv])
        kz = self.tmp(4, 4, [128, 4, NT])
        S.op("dve", lambda e: e.memset(kz.t, 0.0), [], [kz])
        for h in range(4):
            pr, off = h // 2, (h % 2) * 64
            self.cp(kz.t[off:off + 64, h, :], qkv.t[off:off + 64, 2 + pr, :], [qkv], [kz], eng="dve")
        A = self.tmp(18, 1, [128, 512])
        v3 = lambda lo: A.t[0:64, lo:lo + 32].rearrange("p (c h) -> p c h", h=4)
        beta, g_, gc, egc, ekd, tx, bneg, begc = v3(0), v3(32), v3(64), v3(96), v3(128), v3(160), v3(192), v3(224)
        gLrep = A.t[:, 256:288].rearrange("p (c h) -> p c h", h=4)
        cdrep = A.t[:, 288:320].rearrange("p (c h) -> p c h", h=4)
        ea = A.t[0:64, 320:324]
        R_ = [A, self.sp]
        self.act(beta, sp[:, :, 0:4], AF.Sigmoid, R_, [A])
        self.tt(tx, sp[:, :, 4:8], rows[:, 4:8].unsqueeze(1).broadcast_to([64, 8, 4]), ALU.add, R_ + [self.gdrow], [A])
        self.act(tx, tx, AF.Exp, [A], [A])
        self.act(tx, tx, AF.Ln, [A, cst], [A], bias=cst.t[0:64, 3:4])
        self.act(ea, rows[:, 0:4], AF.Exp, [self.gdrow], [A])
        self.tt(g_, tx, ea.unsqueeze(1).broadcast_to([64, 8, 4]), ALU.mult, [A], [A])
        self.ts(g_, g_, -1.0, ALU.mult, [A], [A])
        p7 = self.P[7]
        g2 = A.t[0:64, 32:64]
        self.mm(p7.t[0:64, 0:32], TRI, g2, True, True, [A, c2], [p7])
        self.cp(A.t[0:64, 64:96], p7.t[0:64, 0:32], [p7], [A], eng="act")
        self.mm(p7.t[:, 32:64], ones[0:64, :], g2, True, True, [A, consts], [p7])
        self.cp(A.t[:, 256:288], p7.t[:, 32:64], [p7], [A], eng="act")
        self.act(egc, gc, AF.Exp, [A], [A])
        self.tt(ekd, gLrep[0:64], gc, ALU.subtract, [A], [A])
        self.act(ekd, ekd, AF.Exp, [A], [A])
        self.act(cdrep, gLrep, AF.Exp, [A], [A])
        self.ts(bneg, beta, -1.0, ALU.mult, [A], [A])
        self.tt(begc, beta, egc, ALU.mult, [A], [A])
        MM_ = self.tmp(19, 1, [128, 512], BF16)
        Xb = self.tmp(8, 1, [128, 512], BF16)
        Xbv = Xb.t[0:64, 0:512].rearrange("p (h e) -> p h e", e=128)
        X = self.tmp(20, 1, [128, 512])
        DC = self.tmp(21, 1, [128, 512])
        QB = self.tmp(22, 1, [128, 512])
        GT = self.tmp(23, 1, [128, 512])
        VK = self.tmp(0, 1, [128, 512])
        XT = self.tmp(1, 1, [128, 512])
        VN = self.tmp(2, 1, [128, 512])
        ZS = self.tmp(3, 1, [128, 512])
        M2 = self.tmp(10, 1, [128, 512], BF16)
        wz = [self.loadw("win", l * 28 + 6 + i, 8) for i in range(2)]
        Xv = X.t[0:64, 0:512].rearrange("p (h e) -> p h e", e=128)
        for c in range(8):
            cs = slice(c * 64, (c + 1) * 64)
            p0 = self.P[0]
            for i in range(2):
                for k in range(8):
                    self.mm(p0.t[0:64, i * 128:(i + 1) * 128], u.t[:, k, cs], wz[i].t[:, k, :], k == 0, k == 7,
                            [u, wz[i]], [p0])
            self.act(ZS.t[0:64, 0:256], p0.t[0:64, 0:256], AF.Silu, [p0], [ZS])
            for pr in range(2):
                self.mm(p0.t[0:64, 256 + pr * 128:256 + (pr + 1) * 128], qkv.t[:, 4 + pr, cs], ident, True, True,
                        [qkv, consts], [p0])
            vtok = v4(VK)
            self.cp(vtok, p0.t[0:64, 256:512].rearrange("p (h e) -> p h e", e=64), [p0], [VK], eng="act")
            p1 = self.P[1]
            for pr in range(2):
                self.mm(p1.t[0:64, pr * 128:(pr + 1) * 128], qkv.t[:, 2 + pr, cs], ident, True, True, [qkv, consts], [p1])
            ktok = v4(VK, 256)
            self.cp(ktok, p1.t[0:64, 0:256].rearrange("p (h e) -> p h e", e=64), [p1], [VK], eng="act")
            for h in range(4):
                uo, wo = (64, 0) if h % 2 == 0 else (0, 64)
                self.ts(Xv[:, h, uo:uo + 64], vtok[:, h, :], beta[:, c, h:h + 1], ALU.mult, [VK, A], [X])
                self.ts(Xv[:, h, wo:wo + 64], ktok[:, h, :], begc[:, c, h:h + 1], ALU.mult, [VK, A], [X])
            kd = v4(XT, 256)
            self.tt(kd, ktok, ekd[:, c, :].unsqueeze(2).broadcast_to([64, 4, 64]), ALU.mult, [VK, A], [XT])
            gt = v4(GT)
            self.tt(gt, b3(TRI), g_[:, c, :].unsqueeze(2).broadcast_to([64, 4, 64]), ALU.mult, [A, c2], [GT])
            p2, p3 = self.P[2], self.P[3]
            for h in range(4):
                o = p2.t[0:64, h * 64:(h + 1) * 64]
                self.mm(o, gt[:, h, :], on64, True, False, [GT, consts], [p2])
                self.mm(o, neg64, gt[:, h, :], False, True, [GT, self.negones], [p2])
                o = p2.t[0:64, 256 + h * 64:256 + (h + 1) * 64]
                self.mm(o, on64, gt[:, h, :], True, False, [GT, consts], [p2])
                self.mm(o, gt[:, h, :], neg64, False, True, [GT, self.negones], [p2])
            dS, dT, dQ = v4(DC), v4(DC, 256), v4(QB)
            pD = p2.t[0:64, 0:256].rearrange("p (h e) -> p h e", e=64)
            pDT = p2.t[0:64, 256:512].rearrange("p (h e) -> p h e", e=64)
            self.tt(dS, pD, b3(SLADD), ALU.add, [p2, self.gmask], [DC])
            self.tt(dT, pDT, b3(SUADD), ALU.add, [p2, self.gmask], [DC])
            self.tt(dQ, pDT, b3(CMT), ALU.add, [p2, c2], [QB])
            self.act(DC.t[0:64, 0:512], DC.t[0:64, 0:512], AF.Exp, [DC], [DC])
            self.act(dQ, dQ, AF.Exp, [QB], [QB])
            dgb = v4(GT, 256)
            self.tt(dgb, b3(id64), bneg[:, c, :].unsqueeze(2).broadcast_to([64, 4, 64]), ALU.mult, [A, consts], [GT])
            for h in range(4):
                self.mm(p3.t[0:64, h * 64:(h + 1) * 64], qkv.t[:, 2 + h // 2, cs], kz.t[:, h, cs], True, True, [qkv, kz], [p3])
                self.mm(p3.t[0:64, 256 + h * 64:256 + (h + 1) * 64], on64, dgb[:, h, :], True, True, [GT, consts], [p3])
            pKK = p3.t[0:64, 0:256].rearrange("p (h e) -> p h e", e=64)
            pBf = p3.t[0:64, 256:512].rearrange("p (h e) -> p h e", e=64)
            Mk, MkT = v4(MM_), v4(MM_, 256)
            self.tt(Mk, pKK, dS, ALU.mult, [p3, DC], [MM_])
            self.tt(Mk, Mk, bneg[:, c, :].unsqueeze(2).broadcast_to([64, 4, 64]), ALU.mult, [MM_, A], [MM_])
            self.tt(MkT, pKK, dT, ALU.mult, [p3, DC], [MM_])
            self.tt(MkT, MkT, pBf, ALU.mult, [MM_, p3], [MM_])
            p4 = self.P[4]
            for h in range(4):
                self.mm(p4.t[0:64, h * 64:(h + 1) * 64], kz.t[:, h, cs], qkv.t[:, h // 2, cs], True, True, [qkv, kz], [p4])
            self.tt(dQ, p4.t[0:64, 0:256].rearrange("p (h e) -> p h e", e=64), dQ, ALU.mult, [p4, QB], [QB])
            cur, nxt = MM_, M2
            for step in range(6):
                cM, cMT = v4(cur), v4(cur, 256)
                p5 = self.P[5]
                self.cp(Xb.t[0:64, 0:512], X.t[0:64, 0:512], [X], [Xb], eng="act")
                for h in range(4):
                    self.mm(p5.t[0:64, h * 128:(h + 1) * 128], cMT[:, h, :], Xbv[:, h, :], True, True, [cur, Xb], [p5])
                if step < 5:
                    p6 = self.P[6]
                    for h in range(4):
                        self.mm(p6.t[0:64, h * 64:(h + 1) * 64], cMT[:, h, :], cM[:, h, :], True, True, [cur], [p6])
                        self.mm(p6.t[0:64, 256 + h * 64:256 + (h + 1) * 64], cM[:, h, :], cMT[:, h, :], True, True, [cur], [p6])
                    self.cp(nxt.t[0:64, 0:512], p6.t[0:64, 0:512], [p6], [nxt], eng="act")
                self.tt(X.t[0:64, 0:512], X.t[0:64, 0:512], p5.t[0:64, 0:512], ALU.add, [X, p5], [X])
                cur, nxt = nxt, cur
            p5 = self.P[5]
            for h in range(4):
                self.mm(p5.t[:, h * 64:(h + 1) * 64], Xv[:, h, :], id64, True, True, [X, consts], [p5])
            xt = XT.t[:, 0:256].rearrange("p (h e) -> p h e", e=64)
            self.cp(XT.t[:, 0:256], p5.t[:, 0:256], [p5], [XT], eng="act")
            p6 = self.P[6]
            for h in range(4):
                self.mm(p6.t[0:64, h * 64:(h + 1) * 64], xt[:, h, :], Sz.t[:, h, :], True, True, [XT, Sz], [p6])
                self.mm(p6.t[0:64, 256 + h * 64:256 + (h + 1) * 64], qkv.t[:, h // 2, cs], Sz.t[:, h, :], True, True,
                        [qkv, Sz], [p6])
            vn = v4(VN)
            for h in range(4):
                uo = 64 if h % 2 == 0 else 0
                self.tt(vn[:, h, :], Xv[:, h, uo:uo + 64], p6.t[0:64, h * 64:(h + 1) * 64], ALU.subtract, [X, p6], [VN])
            oq = v4(VN, 256)
            self.tt(oq, p6.t[0:64, 256:512].rearrange("p (h e) -> p h e", e=64),
                    egc[:, c, :].unsqueeze(2).broadcast_to([64, 4, 64]), ALU.mult, [p6, A], [VN])
            p7 = self.P[7]
            for h in range(4):
                self.mm(p7.t[0:64, h * 64:(h + 1) * 64], dQ[:, h, :], vn[:, h, :], True, True, [QB, VN], [p7])
            self.tt(oq, oq, p7.t[0:64, 0:256].rearrange("p (h e) -> p h e", e=64), ALU.add, [VN, p7], [VN])
            p1 = self.P[1]
            for h in range(4):
                pr, off = h // 2, (h % 2) * 64
                self.mm(p1.t[:, h * 64:(h + 1) * 64], XT.t[0:64, 256 + pr * 128:256 + (pr + 1) * 128], vn[:, h, :],
                        True, True, [XT, VN], [p1])
                self.stt(Sz.t[off:off + 64, h, :], Sz.t[off:off + 64, h, :], cdrep[off:off + 64, c, h:h + 1],
                         p1.t[off:off + 64, h * 64:(h + 1) * 64], ALU.mult, ALU.add, [Sz, A, p1], [Sz])
            o2 = v4(ZS, 256)
            ss = A.t[0:64, 328:332]
            self.tt(o2, oq, oq, ALU.mult, [VN], [ZS])
            S.op("dve", lambda e, o2=o2, ss=ss: e.tensor_reduce(out=ss, in_=o2, axis=AX.X, op=ALU.add), [ZS], [A])
            self.act(ss, ss, AF.Sqrt, [A, cst], [A], scale=1.0 / 64, bias=cst.t[0:64, 0:1])
            S.op("dve", lambda e, ss=ss: e.reciprocal(ss, ss), [A], [A])
            self.tt(o2, oq, ss.unsqueeze(2).broadcast_to([64, 4, 64]), ALU.mult, [VN, A], [ZS])
            self.tt(o2, o2, b3(rows[:, 8:72]), ALU.mult, [ZS, self.gdrow], [ZS])
            self.tt(ZS.t[0:64, 256:512], ZS.t[0:64, 256:512], ZS.t[0:64, 0:256], ALU.mult, [ZS], [ZS])
            p0 = self.P[0]
            for kc in range(2):
                self.mm(p0.t[:, kc * 64:(kc + 1) * 64], ZS.t[0:64, 256 + kc * 128:256 + (kc + 1) * 128], id64, True, True,
                        [ZS, consts], [p0])
                self.cp(y.t[:, 0, kc, cs], p0.t[:, kc * 64:(kc + 1) * 64], [p0], [y], eng="act")

    def mixers(self, l, s, tb):
        only = self.dbg.get("only", "")
        sp = self.tokproj_small(l)
        if not only or "gdn" in only:
            self.gdn_fwd(l, s, tb, sp)
        if not only or "ml" in only:
            self.mlstm_fwd(l, s, tb, sp)
        if not only or "s5" in only:
            self.s5_fwd(l, s, tb)
        if not only or "moba" in only:
            self.moba_fwd(l, s, tb)

    def build(self):
        S = self.S
        self.declare()
        self.setup()
        units = self.dbg.get("units", [(s, tb) for s in range(2) for tb in range(SEQ // NT)])
        stop = self.dbg.get("stop", None)
        for (s, tb) in units:
            t0 = tb * NT
            S.dma("sp", self.h.t[:], self.D["xT"].t[s, :, :, t0:t0 + NT], [], [self.h], self.h)
            first = (s, tb) == units[0]
            for l in range(NL):
                self.ffn(l, 0)
                if stop == "ffn1":
                    break
                if first and l == 0:
                    for l2 in range(NL):
                        self.s5_setup(l2)
                self.norm(l * 4 + 1)
                if "yin" in self.dbg:
                    S.dma("sp", self.y.t[:], self.D["yin"].t[s * 4 + tb], [], [self.y], self.y)
                else:
                    self.mixers(l, s, tb)
                if "dumpy" in self.dbg and l == 0:
                    S.dma("sp", self.dumpy.t[s * 4 + tb], self.y.t[:], [self.y], [self.dumpy], self.y)
                if stop == "y":
                    break
                self.merge(l)
                self.ffn(l, 1)
                self.ple(l, s, t0)
            if stop is not None:
                self.dump_h(s * 4 + tb)
            self.final(s, t0)
        S.finish()
        S.emit_all()


def F32v(k, name, hh):
    ri = 0 if name.endswith("re") else 1
    t = k.s5B["BT" if name.startswith("BT") else "CT"]
    return t[:, 4 * hh:4 * hh + 4, ri, :]


def wt(w, K):
    Kd, N = w.shape
    assert Kd == K * 128 and N % 128 == 0
    a = w.reshape(K, 128, N // 128, 128).transpose(2, 1, 0, 3)
    return np.ascontiguousarray(a).reshape(N // 128 * 128, K * 128)


def fm(a, kc):
    T = a.shape[0]
    return np.ascontiguousarray(a.T.reshape(kc, 128, T).transpose(1, 0, 2))


def prep_shared(inp):
    f = lambda n: np.asarray(inp[n], dtype=np.float32)
    sh = {}
    wgu = []
    wdn = []
    for l in range(NL):
        for nm_gu, nm_d in (("ffn1_w_gu", "ffn1_w_down"), ("ffn2_w_gu", "ffn2_w_down")):
            w = f(nm_gu)[l]
            g = wt(w[:, :2816], 8).reshape(22, 128, 1024)
            u = wt(w[:, 2816:], 8).reshape(22, 128, 1024)
            wgu.append(np.stack([g, u], axis=1).reshape(44 * 128, 1024))
            wdn.append(wt(f(nm_d)[l], 22))
    sh["wgu"] = np.concatenate(wgu, 0)
    sh["wdn"] = np.concatenate(wdn, 0)
    swap = np.concatenate([(np.arange(64) + 32) % 64 + 64 * hh for hh in range(4)])
    cols = np.concatenate([np.arange(0, 1024), np.arange(1032, 2056), np.arange(2064, 3088),
                           2320 + swap, 2576 + swap])
    sh["win"] = np.concatenate([wt(f("w_in")[l][:, cols], 8) for l in range(NL)], 0)
    sh["wgate"] = np.concatenate([
        np.stack([wt(f("w_gate")[l, b], 8).reshape(8, 128, 1024) for b in range(4)], 1).reshape(8 * 4 * 128, 1024)
        for l in range(NL)], 0)
    sh["wbr"] = np.concatenate([
        np.stack([wt(f("w_branch")[l, b], 2).reshape(8, 128, 256) for b in range(4)], 1).reshape(8 * 4 * 128, 256)
        for l in range(NL)], 0)
    sh["wout"] = np.concatenate([wt(f("w_out")[l], 8) for l in range(NL)], 0)
    sh["wplg"] = np.concatenate([wt(f("ple_w_gate")[l], 8) for l in range(NL)], 0)
    sh["wplp"] = np.concatenate([wt(f("ple_w_proj")[l], 2) for l in range(NL)], 0)
    gains = np.zeros((9, 1024), np.float32)
    for l in range(NL):
        gains[l * 4 + 0] = f("ffn1_norm")[l]
        gains[l * 4 + 1] = f("mix_norm")[l]
        gains[l * 4 + 2] = f("ffn2_norm")[l]
        gains[l * 4 + 3] = f("ple_norm")[l]
    gains[8] = f("final_norm")
    sh["gains"] = np.ascontiguousarray(gains.reshape(9, 8, 128).transpose(2, 0, 1))
    sh["wglu"] = np.concatenate([wt(f("s5_w_glu")[l], 2) for l in range(NL)], 0)
    consts = np.zeros((128, 512), np.float32)
    consts[:, 0:128] = np.arange(128, dtype=np.float32)[None, :]
    consts[:, 128:256] = np.eye(128, dtype=np.float32)
    consts[:, 384:512] = 1.0
    for qb in range(8):
        for n in range(8):
            consts[:, 256 + qb * 8 + n] = 0.0 if n < qb else -1e9
    pc = np.zeros((128, 8), np.float32)
    invf = (10000.0 ** (-np.arange(0, 64, 2, dtype=np.float32) / 64)).astype(np.float32)
    for p_ in range(128):
        pc[p_, 0] = invf[p_ % 32]
        pc[p_, 1] = -1.0 if (p_ % 64) < 32 else 1.0
        pc[p_, 2] = pc[p_, 1] * 2 * np.pi
    sh["pc"] = pc
    c2 = np.zeros((128, 512), np.float32)
    ii = np.arange(64)
    c2[0:64, 0:64] = (ii[:, None] <= ii[None, :]).astype(np.float32)
    c2[0:64, 64:128] = np.where(ii[None, :] <= ii[:, None], 0.0, -60000.0)
    c2[0:64, 128:192] = (ii[None, :] < ii[:, None]).astype(np.float32)
    c2[0:64, 192:256] = (ii[None, :] <= ii[:, None]).astype(np.float32)
    c2[0:64, 256:320] = (ii[:, None] < ii[None, :]).astype(np.float32)
    c2[0:64, 320:384] = np.where(ii[:, None] <= ii[None, :], 0.0, -60000.0)
    sh["consts2"] = c2
    gmk = np.zeros((128, 256), np.float32)
    gmk[0:64, 0:64] = np.where(ii[None, :] < ii[:, None], 0.0, -60000.0)
    gmk[0:64, 64:128] = np.where(ii[:, None] < ii[None, :], 0.0, -60000.0)
    pp_ = np.arange(128)
    gmk[:, 128:256] = (pp_[:, None] // 64 == pp_[None, :] // 64).astype(np.float32)
    sh["gmaskf"] = gmk
    win = f("w_in")
    wsm = np.zeros((128, NL, 8, 16), np.float32)
    cwf = np.zeros((128, NL, 40), np.float32)
    mlrow = np.zeros((128, NL, 264), np.float32)
    gdrow = np.zeros((128, NL, 72), np.float32)
    for l in range(NL):
        small = np.concatenate([win[l][:, 1024:1032], win[l][:, 2056:2064]], 1)
        wsm[:, l] = small.reshape(8, 128, 16).transpose(1, 0, 2)
        gc = f("gdn_conv")[l]
        mc = f("mlstm_conv")[l]
        cwf[:, l, 0:24] = gc.T.reshape(6, 128, 4).transpose(1, 0, 2).reshape(128, 24)
        cwf[:, l, 24:40] = mc.T.reshape(4, 128, 4).transpose(1, 0, 2).reshape(128, 16)
        mlrow[:, l, 0:4] = f("mlstm_i_bias")[l][None, :]
        mlrow[:, l, 4:8] = f("mlstm_f_bias")[l][None, :]
        mlrow[:, l, 8:264] = f("mlstm_norm")[l][None, :]
        gdrow[:, l, 0:4] = f("gdn_a_log")[l][None, :]
        gdrow[:, l, 4:8] = f("gdn_dt_bias")[l][None, :]
        gdrow[:, l, 8:72] = f("gdn_norm")[l][None, :]
    sh["wsmf"], sh["cwf"], sh["mlrowf"], sh["gdrowf"] = wsm, cwf, mlrow, gdrow
    cmf = np.zeros((128, 2, 256), np.float32)
    for a in range(2):
        kl = a * 128 + np.arange(128)[:, None]
        cmf[:, a, :] = np.where(kl > np.arange(256)[None, :], -30000.0, 0.0)
    sh["cmf"] = cmf
    sh["consts"] = consts
    lre, lim, ldt = f("s5_lambda_re"), f("s5_lambda_im"), f("s5_log_dt")
    s5st = np.zeros((NL, 128, 8, 3), np.float32)
    s5rep = np.zeros((NL, 3, 128, 8, 128), np.float32)
    s5bexp = np.zeros((NL, 2, 128, 8, 128), np.float32)
    s5cexp = np.zeros((NL, 2, 128, 8, 128), np.float32)
    bre, bim, cre, cim = f("s5_b_re"), f("s5_b_im"), f("s5_c_re"), f("s5_c_im")
    for l in range(NL):
        for sc in range(8):
            for half in range(2):
                g = 2 * sc + half
                ps = slice(half * 64, half * 64 + 64)
                s5st[l, ps, sc, 0] = lre[l, g]
                s5st[l, ps, sc, 1] = lim[l, g]
                s5st[l, ps, sc, 2] = ldt[l, g]
                s5rep[l, 0, :, sc, ps] = lre[l, g][None, :]
                s5rep[l, 1, :, sc, ps] = lim[l, g][None, :]
                s5rep[l, 2, :, sc, ps] = ldt[l, g]
                r0 = (sc % 4) * 32 + half * 16
                s5bexp[l, 0, r0:r0 + 16, sc, ps] = bre[l, g].T
                s5bexp[l, 1, r0:r0 + 16, sc, ps] = bim[l, g].T
                s5cexp[l, 0, ps, sc, r0:r0 + 16] = cre[l, g].T
                s5cexp[l, 1, ps, sc, r0:r0 + 16] = cim[l, g].T
    sh["s5st"], sh["s5rep"], sh["s5bexp"], sh["s5cexp"] = s5st, s5rep, s5bexp, s5cexp
    s5db = np.zeros((NL, 128, 4), np.float32)
    for l in range(NL):
        s5db[l, :, 0:2] = f("s5_d")[l].reshape(2, 128).T
        s5db[l, :, 2:4] = f("s5_b_glu")[l].reshape(2, 128).T
    sh["s5db"] = s5db
    return sh


def prep_core(inp, c):
    x = np.asarray(inp["x"], dtype=np.float32)
    p = np.asarray(inp["p"], dtype=np.float32)
    m = {}
    m["xT"] = np.stack([fm(x[2 * c + s], 8) for s in range(2)], 0)
    m["pT"] = np.stack([np.stack([fm(p[l, 2 * c + s], 2) for s in range(2)], 0) for l in range(NL)], 0)
    pos = np.asarray(inp["positions"]).astype(np.int32)
    m["posr"] = np.ascontiguousarray(np.broadcast_to(pos[2 * c:2 * c + 2, None, :], (2, 128, SEQ)))
    return m


def build_nc(dbg=None):
    nc = bass.Bass("TRN2", target_bir_lowering=False)
    with ExitStack() as st:
        k = Kern(nc, st, dbg)
        k.build()
    return nc


def kernel(**inputs):
    sh = prep_shared(inputs)
    nc = build_nc()
    in_maps = []
    for c in range(8):
        m = dict(sh)
        m.update(prep_core(inputs, c))
        in_maps.append(m)
    res = run_bass_kernel_spmd(nc, in_maps, core_ids=list(range(8)))
    out = np.zeros((16, SEQ, 1024), np.float32)
    for c in range(8):
        o = res.results[c]["out"]
        for s in range(2):
            out[2 * c + s] = o[s].transpose(2, 1, 0).reshape(SEQ, 1024)
    return out
```

```python
import numpy as np
from contextlib import ExitStack
import concourse.bass as bass
import concourse.mybir as mybir
from concourse.bass_utils import run_bass_kernel_spmd

F32 = mybir.dt.float32
BF16 = mybir.dt.bfloat16
I32 = mybir.dt.int32
AF = mybir.ActivationFunctionType
ALU = mybir.AluOpType
AX = mybir.AxisListType

ENGS = ["pe", "act", "dve", "pool", "sp"]
NT = 512
NL = 2
SEQ = 2048
PI = float(np.pi)


class Buf:
    __slots__ = ("name", "w", "r", "sem", "semcnt", "t")

    def __init__(self, name, t=None):
        self.name = name
        self.w = None
        self.r = {}
        self.sem = None
        self.semcnt = 0
        self.t = t


class Tmp:
    __slots__ = ("t", "bufs")

    def __init__(self, t, bufs):
        self.t = t
        self.bufs = bufs


def flat(lst):
    out = []
    for b in lst:
        if isinstance(b, Tmp):
            out.extend(b.bufs)
        else:
            out.append(b)
    return out


class Sched:
    def __init__(self, nc, stack):
        self.nc = nc
        self.stack = stack
        self.q = {e: [] for e in ENGS}
        self.cnt = {e: 0 for e in ENGS}
        self.sems = {}
        for e in ENGS:
            self.sems[e] = stack.enter_context(nc.semaphore("s_" + e))
        self.seen = {e: {} for e in ENGS}
        self.dmabufs = []
        self.rec = None

    def sb(self, name, shape, dt=F32):
        return Buf(name, self.stack.enter_context(self.nc.sbuf_tensor("sb_" + name, list(shape), dt)))

    def ps(self, name, shape, dt=F32):
        return Buf(name, self.stack.enter_context(self.nc.psum_tensor("ps_" + name, list(shape), dt)))

    def _waits(self, e, reads, writes):
        need = {}

        def add(k, v, src):
            if src == "pe" and e == "pe":
                return
            if v > need.get(k, 0):
                need[k] = v
        for b in reads:
            if b.w is not None:
                add(*b.w)
        for b in writes:
            if b.w is not None:
                add(*b.w)
            for k, (v, src) in b.r.items():
                add(k, v, src)
        out = []
        seen = self.seen[e]
        for k, v in need.items():
            if seen.get(k, 0) < v:
                seen[k] = v
                out.append((self.sems[k], v))
        return out

    def _record(self, dep, reads, writes):
        k, v, src = dep
        for b in reads:
            old = b.r.get(k)
            if old is None or old[0] < v:
                b.r[k] = (v, src)
        for b in writes:
            b.w = dep
            b.r = {}

    def replay(self, items):
        for it in items:
            if it[0] == "op":
                self.op(*it[1:])
            else:
                self.dma(*it[1:])

    def op(self, e, fn, reads=(), writes=()):
        if self.rec is not None:
            self.rec.append(("op", e, fn, reads, writes))
            return
        reads, writes = flat(reads), flat(writes)
        waits = self._waits(e, reads, writes)
        self.cnt[e] += 1
        n = self.cnt[e]
        sem = self.sems[e]

        def emit(engine, fn=fn, waits=waits, sem=sem):
            for s, v in waits:
                engine.wait_ge(s, v)
            fn(engine).then_inc(sem, 1)
        self.q[e].append(emit)
        self._record((e, n, e), reads, writes)

    def dma(self, qe, out, in_, reads, writes, sembuf):
        if self.rec is not None:
            self.rec.append(("dma", qe, out, in_, reads, writes, sembuf))
            return
        reads, writes = flat(reads), flat(writes)
        waits = self._waits(qe, reads, writes)
        if isinstance(sembuf, Tmp):
            sembuf = sembuf.bufs[0]
        if sembuf.sem is None:
            key = "d%d" % len(self.dmabufs)
            sembuf.sem = key
            self.sems[key] = self.stack.enter_context(self.nc.semaphore(key))
            self.dmabufs.append(sembuf)
        sembuf.semcnt += 16
        v = sembuf.semcnt
        sem = self.sems[sembuf.sem]

        def emit(engine, waits=waits, sem=sem, out=out, in_=in_):
            for s, vv in waits:
                engine.wait_ge(s, vv)
            engine.dma_start(out=out, in_=in_).then_inc(sem, 16)
        self.q[qe].append(emit)
        self._record((sembuf.sem, v, "dma"), reads, writes)

    def finish(self):
        waits = []
        for e in ENGS:
            if e != "sp" and self.cnt[e] > 0:
                waits.append((self.sems[e], self.cnt[e]))
        for b in self.dmabufs:
            waits.append((self.sems[b.sem], b.semcnt))

        def emit(engine, waits=waits):
            for s, v in waits:
                engine.wait_ge(s, v)
        self.q["sp"].append(emit)

    def emit_all(self):
        with self.nc.Block() as block:
            @block.tensor
            def _(eng):
                for f in self.q["pe"]:
                    f(eng)

            @block.scalar
            def _(eng):
                for f in self.q["act"]:
                    f(eng)

            @block.vector
            def _(eng):
                for f in self.q["dve"]:
                    f(eng)

            @block.gpsimd
            def _(eng):
                for f in self.q["pool"]:
                    f(eng)

            @block.sync
            def _(eng):
                for f in self.q["sp"]:
                    f(eng)


BIGW = {
    "wgu": (NL * 2 * 44 * 128, 8 * 128),
    "wdn": (NL * 2 * 8 * 128, 22 * 128),
    "win": (NL * 28 * 128, 8 * 128),
    "wgate": (NL * 8 * 4 * 128, 8 * 128),
    "wbr": (NL * 8 * 4 * 128, 2 * 128),
    "wout": (NL * 8 * 128, 8 * 128),
    "wplg": (NL * 8 * 128, 8 * 128),
    "wplp": (NL * 8 * 128, 2 * 128),
    "wglu": (NL * 2 * 128, 2 * 128),
}


class Kern:
    def __init__(self, nc, st, dbg=None):
        self.nc = nc
        self.dbg = dbg or {}
        self.S = Sched(nc, st)
        self.wi8 = 0
        self.wi22 = 0
        self.wbg = {}

    def mm(self, out, lhsT, rhs, start, stop, R, W):
        self.S.op("pe", lambda e: e.matmul(out, lhsT, rhs, start=start, stop=stop), R, W)

    def act(self, out, in_, func, R, W, **kw):
        self.S.op("act", lambda e: e.activation(out=out, in_=in_, func=func, **kw), R, W)

    def tt(self, out, a, b, op, R, W, eng="dve"):
        self.S.op(eng, lambda e: e.tensor_tensor(out=out, in0=a, in1=b, op=op), R, W)

    def ts(self, out, a, s1, op0, R, W, s2=None, op1=None, eng="dve"):
        if op1 is None:
            self.S.op(eng, lambda e: e.tensor_scalar(out=out, in0=a, scalar1=s1, scalar2=None, op0=op0), R, W)
        else:
            self.S.op(eng, lambda e: e.tensor_scalar(out=out, in0=a, scalar1=s1, scalar2=s2, op0=op0, op1=op1), R, W)

    def stt(self, out, a, s, b, op0, op1, R, W):
        self.S.op("dve", lambda e: e.scalar_tensor_tensor(out=out, in0=a, scalar=s, in1=b, op0=op0, op1=op1), R, W)

    def cp(self, out, in_, R, W, eng="pool"):
        if eng == "act":
            self.S.op("act", lambda e: e.copy(out=out, in_=in_), R, W)
        else:
            self.S.op(eng, lambda e: e.tensor_copy(out, in_), R, W)

    def wgroup(self, name, l, which=0):
        per = {"wgu": 44, "wdn": 8, "win": 28, "wgate": 32, "wbr": 32, "wout": 8, "wplg": 8, "wplp": 8, "wglu": 2}[name]
        idx = (l * 2 + which) if name in ("wgu", "wdn") else l
        key = (name, idx)
        if key not in self.wbg:
            self.wbg[key] = Buf("%s_b%d" % (name, idx), self.wb[name].t)
        return idx * per * 128, (idx + 1) * per * 128, self.wbg[key]

    def convert(self, groups):
        S = self.S
        for g in groups:
            name = g[0]
            r0g, r1g, dstb = self.wgroup(*g)
            c = BIGW[name][1]
            nn = min(max(1, 4096 // c), (r1g - r0g) // 128)
            src = self.D[name]
            for r0 in range(r0g, r1g, nn * 128):
                stg = self.cstg[self.conv_i % 2]
                self.conv_i += 1
                S.dma("pool", stg.t[:, 0:nn * c].rearrange("p (n c) -> p n c", c=c),
                      src.t[r0:r0 + nn * 128, :].rearrange("(n p) c -> p n c", p=128), [], [stg], stg)
                if self.conv_pending is not None:
                    self.conv_pending()
                self.conv_pending = (lambda stg=stg, dstb=dstb, r0=r0, nn=nn, c=c: S.dma(
                    "pool", dstb.t[r0:r0 + nn * 128, :].rearrange("(n p) c -> p n c", p=128),
                    stg.t[:, 0:nn * c].rearrange("p (n c) -> p n c", c=c), [stg], [dstb], dstb))
        if self.conv_pending is not None:
            self.conv_pending()
            self.conv_pending = None

    def loadw(self, name, tile, K):
        per = {"wgu": 44, "wdn": 8, "win": 28, "wgate": 32, "wbr": 32, "wout": 8, "wplg": 8, "wplp": 8, "wglu": 2}[name]
        src = self.wbg[(name, tile // per)]
        ap = src.t[tile * 128:(tile + 1) * 128, :].rearrange("p (k c) -> p k c", c=128)
        if K > 8:
            b = self.w22[self.wi22 % len(self.w22)]
            self.wi22 += 1
        else:
            b = self.w8[self.wi8 % len(self.w8)]
            self.wi8 += 1
        self.S.dma("sp", b.t[:, 0:K, :], ap, [src], [b], b)
        return b

    def tmp(self, slot0, nslots, shape, dt=F32):
        ap = self.scr_t[:, slot0 * 512:(slot0 + nslots) * 512]
        if dt == BF16:
            ap = ap.bitcast(BF16)
        n = 1
        for d in shape[1:]:
            n *= d
        ap = ap[:, 0:n]
        if len(shape) == 3:
            ap = ap.rearrange("p (a b) -> p a b", b=shape[2])
        elif len(shape) == 4:
            ap = ap.rearrange("p (a b c) -> p a b c", b=shape[2], c=shape[3])
        return Tmp(ap, self.slots[slot0:slot0 + nslots])

    def declare(self):
        nc, S = self.nc, self.S
        D = {}

        def din(name, shape, dt=F32):
            D[name] = Buf(name, nc.dram_tensor(name, list(shape), dt, kind="ExternalInput").ap())
            return D[name]
        self.D = D
        din("xT", [2, 128, 8, SEQ])
        din("pT", [NL, 2, 128, 2, SEQ])
        din("gains", [128, 9, 8])
        din("consts", [128, 512])
        din("s5st", [NL, 128, 8, 3])
        din("s5rep", [NL, 3, 128, 8, 128])
        din("s5bexp", [NL, 2, 128, 8, 128])
        din("s5cexp", [NL, 2, 128, 8, 128])
        din("s5db", [NL, 128, 4])
        din("posr", [2, 128, SEQ], I32)
        din("consts2", [128, 512])
        din("wsmf", [128, NL, 8, 16])
        din("cwf", [128, NL, 40])
        din("mlrowf", [128, NL, 264])
        din("gdrowf", [128, NL, 72])
        din("gmaskf", [128, 256])
        din("pc", [128, 8])
        din("cmf", [128, 2, 256])
        for n, (r, c) in BIGW.items():
            din(n, [r, c])
        self.out = Buf("out", nc.dram_tensor("out", [2, 128, 8, SEQ], F32, kind="ExternalOutput").ap())
        self.wb = {}
        for n, (r, c) in BIGW.items():
            self.wb[n] = Buf(n + "_b", nc.dram_tensor(n + "_b", [r, c], BF16, kind="Internal").ap())
        self.s5f_d = Buf("s5f_d", nc.dram_tensor("s5f_d", [NL, 128, 3104], F32, kind="Internal").ap())
        self.s5b_d = Buf("s5b_d", nc.dram_tensor("s5b_d", [NL, 128, 4352], BF16, kind="Internal").ap())
        if "yin" in self.dbg:
            din("yin", [8, 128, 4, 2, NT], BF16)
        if "dumpy" in self.dbg:
            self.dumpy = Buf("dumpy", nc.dram_tensor("dumpy", [8, 128, 4, 2, NT], BF16, kind="ExternalOutput").ap())
        if "dump" in self.dbg:
            self.dump = Buf("dump", nc.dram_tensor("dump", self.dbg["dump"], F32, kind="ExternalOutput").ap())

        self.h = S.sb("h", [128, 8, NT], F32)
        self.u = S.sb("u", [128, 8, NT], BF16)
        NS = 24
        self.scr_t = S.sb("scr", [128, NS * 512], F32).t
        self.slots = [Buf("slot%d" % i) for i in range(NS)]
        self.actb = self.tmp(0, 11, [128, 22, NT], BF16)
        self.xs = [Tmp(S.sb("xs%d" % i, [128, 512], F32).t, [Buf("xslot%d" % i)]) for i in range(4)]
        self.rstd = S.sb("rstd", [128, NT], F32)
        self.sg = [S.sb("sg%d" % i, [128, NT], F32) for i in range(2)]
        self.sg2 = [S.sb("sg2%d" % i, [128, NT], F32) for i in range(2)]
        self.macc = self.tmp(12, 1, [128, NT])
        self.ho = [self.tmp(13 + i, 1, [128, NT]) for i in range(2)]
        self.w8 = [S.sb("w8_%d" % i, [128, 8, 128], BF16) for i in range(6)]
        self.w22 = [S.sb("w22_%d" % i, [128, 22, 128], BF16) for i in range(2)]
        self.y = S.sb("y", [128, 4, 2, NT], BF16)
        self.pf = self.tmp(15, 2, [128, 2, NT])
        self.pb = self.tmp(17, 1, [128, 2, NT], BF16)
        self.gains = S.sb("gains", [128, 9, 8], F32)
        self.cst = S.sb("cst", [128, 8], F32)
        self.consts = S.sb("consts", [128, 512], F32)
        self.pc = S.sb("pc", [128, 8], F32)
        self.consts2 = S.sb("consts2", [128, 512], F32)
        self.wsm = S.sb("wsm", [128, NL, 8, 16], BF16)
        self.cwb = S.sb("cwb", [128, NL, 40], F32)
        self.mlrow = S.sb("mlrow", [128, NL, 264], F32)
        self.gdrow = S.sb("gdrow", [128, NL, 72], F32)
        self.mltail = [S.sb("mltail%d" % l, [128, 4, 3], F32) for l in range(NL)]
        self.gdtail = [S.sb("gdtail%d" % l, [128, 6, 3], F32) for l in range(NL)]
        self.mlC = [S.sb("mlC%d" % l, [128, 4, 66], F32) for l in range(NL)]
        self.mlm = [S.sb("mlm%d" % l, [128, 4], F32) for l in range(NL)]
        self.gdS = [S.sb("gdS%d" % l, [128, 4, 64], F32) for l in range(NL)]
        self.gmask = S.sb("gmask", [128, 256], F32)
        self.negones = S.sb("negones", [128, 64], F32)
        self.cm = S.sb("cm", [128, 2, 256], BF16)
        self.ident_bf = S.sb("ident_bf", [128, 128], BF16)
        self.kcache = [S.sb("kcache%d" % l, [128, 2, SEQ], BF16) for l in range(NL)]
        self.vcache = [S.sb("vcache%d" % l, [128, 16, 256], BF16) for l in range(NL)]
        self.kmean = [S.sb("kmean%d" % l, [128, 2, 8], F32) for l in range(NL)]
        self.s5f = S.sb("s5f", [128, 3104], F32)
        self.s5b = S.sb("s5b", [128, 4352], BF16)
        f = self.s5f.t
        self.s5F = {"CS": f[:, 0:2048].rearrange("p (a b c) -> p a b c", b=2, c=128),
                    "R": f[:, 2048:3072].rearrange("p (a b) -> p a b", b=128),
                    "E128": f[:, 3072:3088].rearrange("p (a b) -> p a b", b=2),
                    "rcol": f[:, 3088:3096], "dbg": f[:, 3096:3100]}
        b = self.s5b.t
        self.s5B = {"BT": b[:, 0:2048].rearrange("p (a b c) -> p a b c", b=2, c=128),
                    "CT": b[:, 2048:4096].rearrange("p (a b c) -> p a b c", b=2, c=128),
                    "Dg": b[:, 4096:4352].rearrange("p (a b) -> p a b", b=128)}
        self.s5state = [S.sb("s5st%d" % l, [128, 2, 8], F32) for l in range(NL)]
        self.ones_bf = S.sb("ones_bf", [128, 128], BF16)
        self.P = [S.ps("P%d" % i, [128, NT], F32) for i in range(8)]
        self.cstg = [S.sb("cstg%d" % i, [128, 4096], BF16) for i in range(2)]

    def setup(self):
        S = self.S
        S.op("pool", lambda e: e.memset(self.ones_bf.t[:], 1.0), [], [self.ones_bf])
        S.op("pool", lambda e: e.memset(self.cst.t[:, 0:1], 1e-6), [], [self.cst])
        S.op("pool", lambda e: e.memset(self.cst.t[:, 1:2], -PI), [], [self.cst])
        S.dma("sp", self.pc.t[:], self.D["pc"].t, [], [self.pc], self.pc)
        S.op("pool", lambda e: e.memset(self.cst.t[:, 3:4], 1.0), [], [self.cst])
        S.dma("sp", self.consts2.t[:], self.D["consts2"].t, [], [self.consts2], self.consts2)
        S.dma("sp", self.cwb.t[:], self.D["cwf"].t, [], [self.cwb], self.cwb)
        S.dma("sp", self.mlrow.t[:], self.D["mlrowf"].t, [], [self.mlrow], self.mlrow)
        S.dma("sp", self.gdrow.t[:], self.D["gdrowf"].t, [], [self.gdrow], self.gdrow)
        S.dma("sp", self.gmask.t[:], self.D["gmaskf"].t, [], [self.gmask], self.gmask)
        S.op("pool", lambda e: e.memset(self.negones.t[:], -1.0), [], [self.negones])
        wsf = self.tmp(13, 1, [128, NL, 8, 16])
        S.dma("sp", wsf.t, self.D["wsmf"].t, [], [wsf], wsf)
        self.cp(self.wsm.t[:], wsf.t, [wsf], [self.wsm], eng="dve")
        cmf = self.tmp(12, 1, [128, 2, 256])
        S.dma("sp", cmf.t, self.D["cmf"].t, [], [cmf], cmf)
        self.cp(self.cm.t[:], cmf.t, [cmf], [self.cm], eng="dve")
        for l in range(NL):
            S.op("pool", lambda e, l=l: e.memset(self.kmean[l].t[:], 0.0), [], [self.kmean[l]])
        S.dma("sp", self.gains.t[:], self.D["gains"].t, [], [self.gains], self.gains)
        S.dma("sp", self.consts.t[:], self.D["consts"].t, [], [self.consts], self.consts)
        self.cp(self.ident_bf.t[:], self.consts.t[:, 128:256], [self.consts], [self.ident_bf], eng="dve")
        self.conv_i = 0
        self.conv_pending = None
        self.convert([("wgu", 0, 0), ("wdn", 0, 0), ("win", 0), ("wglu", 0), ("wgate", 0), ("wbr", 0), ("wout", 0),
                      ("wgu", 0, 1), ("wdn", 0, 1), ("wplg", 0), ("wplp", 0),
                      ("wgu", 1, 0), ("wdn", 1, 0), ("win", 1), ("wglu", 1), ("wgate", 1), ("wbr", 1), ("wout", 1),
                      ("wgu", 1, 1), ("wdn", 1, 1), ("wplg", 1), ("wplp", 1)])

    def norm(self, gcol):
        h, u, S = self.h, self.u, self.S
        pss, rstd = self.P[6], self.rstd
        self.act(u.t[:], h.t[:], AF.Square, [h], [u])
        for k in range(8):
            self.mm(pss.t[:], self.ones_bf.t[:], u.t[:, k, :], k == 0, k == 7, [u, self.ones_bf], [pss])
        self.act(rstd.t[:], pss.t[:], AF.Sqrt, [pss, self.cst], [rstd], scale=1.0 / 1024, bias=self.cst.t[:, 0:1])
        S.op("dve", lambda e: e.reciprocal(rstd.t[:], rstd.t[:]), [rstd], [rstd])
        for k in range(8):
            self.stt(u.t[:, k, :], h.t[:, k, :], self.gains.t[:, gcol, k:k + 1], rstd.t[:], ALU.mult, ALU.mult,
                     [h, rstd, self.gains], [u])

    def ffn(self, l, which):
        h, u, actb = self.h, self.u, self.actb
        self.norm(l * 4 + (0 if which == 0 else 2))
        base = (l * 2 + which) * 44
        for fc in range(22):
            wg = self.loadw("wgu", base + 2 * fc, 8)
            wu = self.loadw("wgu", base + 2 * fc + 1, 8)
            pg, pu = self.P[fc % 2], self.P[2 + fc % 2]
            sg = self.sg[fc % 2]
            for k in range(8):
                self.mm(pg.t[:], wg.t[:, k, :], u.t[:, k, :], k == 0, k == 7, [wg, u], [pg])
            for k in range(8):
                self.mm(pu.t[:], wu.t[:, k, :], u.t[:, k, :], k == 0, k == 7, [wu, u], [pu])
            self.act(sg.t[:], pg.t[:], AF.Silu, [pg], [sg])
            self.tt(actb.t[:, fc, :], sg.t[:], pu.t[:], ALU.mult, [sg, pu], [actb])
        base = (l * 2 + which) * 8
        for oc in range(8):
            wd = self.loadw("wdn", base + oc, 22)
            po = self.P[4 + oc % 2]
            for fc in range(22):
                self.mm(po.t[:], wd.t[:, fc, :], actb.t[:, fc, :], fc == 0, fc == 21, [wd, actb], [po])
            self.stt(h.t[:, oc, :], po.t[:], 0.5, h.t[:, oc, :], ALU.mult, ALU.add, [po, h], [h])

    def ple(self, l, s, t0):
        h, u, S = self.h, self.u, self.S
        self.norm(l * 4 + 3)
        S.dma("sp", self.pf.t[:], self.D["pT"].t[l, s, :, :, t0:t0 + NT], [], [self.pf], self.pf)
        self.cp(self.pb.t[:], self.pf.t[:], [self.pf], [self.pb], eng="pool")
        for oc in range(8):
            wg = self.loadw("wplg", l * 8 + oc, 8)
            wp = self.loadw("wplp", l * 8 + oc, 2)
            pa, pg = self.P[oc % 2], self.P[2 + oc % 2]
            sg, sg2 = self.sg[oc % 2], self.sg2[oc % 2]
            for k in range(2):
                self.mm(pa.t[:], wp.t[:, k, :], self.pb.t[:, k, :], k == 0, k == 1, [wp, self.pb], [pa])
            for k in range(8):
                self.mm(pg.t[:], wg.t[:, k, :], u.t[:, k, :], k == 0, k == 7, [wg, u], [pg])
            self.act(sg.t[:], pg.t[:], AF.Sigmoid, [pg], [sg])
            self.tt(sg2.t[:], sg.t[:], pa.t[:], ALU.mult, [sg, pa], [sg2])
            self.tt(h.t[:, oc, :], h.t[:, oc, :], sg2.t[:], ALU.add, [h, sg2], [h], eng="pool")

    def merge(self, l):
        h, u, y, actb = self.h, self.u, self.y, self.actb
        macc = self.macc
        for oc in range(8):
            for b in range(4):
                wg = self.loadw("wgate", (l * 8 + oc) * 4 + b, 8)
                wbr = self.loadw("wbr", (l * 8 + oc) * 4 + b, 2)
                pg, pbr = self.P[b % 2], self.P[2 + b % 2]
                sg, sg2 = self.sg[b % 2], self.sg2[b % 2]
                for k in range(8):
                    self.mm(pg.t[:], wg.t[:, k, :], u.t[:, k, :], k == 0, k == 7, [wg, u], [pg])
                for k in range(2):
                    self.mm(pbr.t[:], wbr.t[:, k, :], y.t[:, b, k, :], k == 0, k == 1, [wbr, y], [pbr])
                self.act(sg.t[:], pg.t[:], AF.Sigmoid, [pg], [sg])
                if b == 0:
                    self.tt(macc.t[:], sg.t[:], pbr.t[:], ALU.mult, [sg, pbr], [macc])
                else:
                    self.tt(sg2.t[:], sg.t[:], pbr.t[:], ALU.mult, [sg, pbr], [sg2])
                    if b < 3:
                        self.tt(macc.t[:], macc.t[:], sg2.t[:], ALU.add, [macc, sg2], [macc], eng="pool")
                    else:
                        self.tt(actb.t[:, oc, :], macc.t[:], sg2.t[:], ALU.add, [macc, sg2], [actb], eng="pool")
        for oc in range(8):
            wo = self.loadw("wout", l * 8 + oc, 8)
            po = self.P[4 + oc % 2]
            for k in range(8):
                self.mm(po.t[:], wo.t[:, k, :], actb.t[:, k, :], k == 0, k == 7, [wo, actb], [po])
            self.tt(h.t[:, oc, :], h.t[:, oc, :], po.t[:], ALU.add, [h, po], [h])

    def final(self, s, t0):
        h, S = self.h, self.S
        pss, rstd = self.P[6], self.rstd
        u = self.u
        self.act(u.t[:], h.t[:], AF.Square, [h], [u])
        for k in range(8):
            self.mm(pss.t[:], self.ones_bf.t[:], u.t[:, k, :], k == 0, k == 7, [u, self.ones_bf], [pss])
        self.act(rstd.t[:], pss.t[:], AF.Sqrt, [pss, self.cst], [rstd], scale=1.0 / 1024, bias=self.cst.t[:, 0:1])
        S.op("dve", lambda e: e.reciprocal(rstd.t[:], rstd.t[:]), [rstd], [rstd])
        for k in range(8):
            ho = self.ho[k % 2]
            self.stt(ho.t[:], h.t[:, k, :], self.gains.t[:, 8, k:k + 1], rstd.t[:], ALU.mult, ALU.mult,
                     [h, rstd, self.gains], [ho])
            S.dma("sp", self.out.t[s, :, k, t0:t0 + NT], ho.t[:], [ho], [self.out], ho)

    def dump_h(self, idx):
        S = self.S
        S.dma("sp", self.dump.t[idx], self.h.t[:], [self.h], [self.dump], self.h)


    def dd(self, name, src, R, shape, dt=F32):
        if name not in self.dbg.get("dd", ()):
            return
        b = Buf(name, self.nc.dram_tensor("dd_" + name, list(shape), dt, kind="ExternalOutput").ap())
        self.S.dma("sp", b.t, src, R, [b], b)

    def frac2pi(self, out, x, shift, tB, R, W):
        MAG = 12582912.0
        self.ts(out, x, 1.0 / (2 * PI), ALU.mult, R, W, s2=shift / (2 * PI), op1=ALU.add)
        self.ts(tB, out, MAG, ALU.add, R, W)
        self.ts(tB, tB, -MAG, ALU.add, R, W)
        self.tt(out, out, tB, ALU.subtract, R, W)

    def s5_disc(self, lr, li, ldt, T, TB, want_z):
        R = TB + [self.s5in]
        W = TB
        self.act(T[0], ldt, AF.Exp, R, W)
        self.tt(T[5], lr, T[0], ALU.mult, R, W)
        self.act(T[1], T[5], AF.Exp, R, W)
        self.tt(T[2], li, T[0], ALU.mult, R, W)
        if not want_z:
            self.frac2pi(T[5], T[2], 0.0, T[8], R, W)
            self.ts(T[2], T[5], 2 * PI, ALU.mult, R, W)
            return T[1], T[2], None, None
        self.frac2pi(T[5], T[2], 0.0, T[8], R, W)
        self.act(T[3], T[5], AF.Sin, R, W, scale=2 * PI)
        self.frac2pi(T[5], T[2], 0.5 * PI, T[8], R, W)
        self.act(T[4], T[5], AF.Sin, R, W, scale=2 * PI)
        self.tt(T[4], T[4], T[1], ALU.mult, R, W)
        self.tt(T[3], T[3], T[1], ALU.mult, R, W)
        self.ts(T[5], T[4], -1.0, ALU.add, R, W)
        self.tt(T[8], lr, lr, ALU.mult, R, W)
        self.tt(T[0], li, li, ALU.mult, R, W)
        self.tt(T[8], T[8], T[0], ALU.add, R, W)
        self.S.op("dve", lambda e: e.reciprocal(T[8], T[8]), R, W)
        self.tt(T[0], T[5], lr, ALU.mult, R, W)
        self.tt(T[6], T[3], li, ALU.mult, R, W)
        self.tt(T[6], T[6], T[0], ALU.add, R, W)
        self.tt(T[6], T[6], T[8], ALU.mult, R, W)
        self.tt(T[0], T[3], lr, ALU.mult, R, W)
        self.tt(T[7], T[5], li, ALU.mult, R, W)
        self.tt(T[7], T[0], T[7], ALU.subtract, R, W)
        self.tt(T[7], T[7], T[8], ALU.mult, R, W)
        return T[1], T[2], T[6], T[7]

    def s5_setup(self, l):
        S, D = self.S, self.D
        s5f, s5b, cst = self.s5f, self.s5b, self.cst
        F = self.s5F
        stt_ = self.tmp(12, 1, [128, 8, 3])
        self.s5in = stt_.bufs[0]
        S.dma("sp", stt_.t, D["s5st"].t[l], [], [stt_], stt_)
        T9 = self.tmp(13, 1, [128, 9, 8])
        T = [T9.t[:, i, :] for i in range(9)]
        mag, th, _, _ = self.s5_disc(stt_.t[:, :, 0], stt_.t[:, :, 1], stt_.t[:, :, 2], T, [T9.bufs[0]], False)
        R9 = [T9]
        X = self.tmp(14, 2, [128, 8, 128])
        Y = self.tmp(16, 2, [128, 8, 128])
        Z = self.tmp(18, 2, [128, 8, 128])
        jrow = self.consts.t[:, 0:128]
        for sc in range(8):
            self.ts(X.t[:, sc, :], jrow, th[:, sc:sc + 1], ALU.mult, R9 + [self.consts], [X])
        self.frac2pi(Y.t, X.t, 0.0, Z.t, [X, Y, Z], [Y, Z])
        self.act(F["CS"][:, :, 1, :], Y.t, AF.Sin, [Y], [s5f], scale=2 * PI)
        self.frac2pi(Y.t, X.t, 0.5 * PI, Z.t, [X, Y, Z], [Y, Z])
        self.act(F["CS"][:, :, 0, :], Y.t, AF.Sin, [Y], [s5f], scale=2 * PI)
        self.ts(T[3], th, 128.0, ALU.mult, R9, R9)
        self.frac2pi(T[4], T[3], 0.0, T[5], R9, R9)
        self.act(F["E128"][:, :, 1], T[4], AF.Sin, R9, [s5f], scale=2 * PI)
        self.frac2pi(T[4], T[3], 0.5 * PI, T[5], R9, R9)
        self.act(F["E128"][:, :, 0], T[4], AF.Sin, R9, [s5f], scale=2 * PI)
        self.cp(F["rcol"], mag, R9, [s5f], eng="dve")
        S.op("dve", lambda e: e.memset(F["R"], 0.0), [], [s5f])
        ones = self.consts.t[:, 384:511]
        for sc in range(8):
            self.ts(F["R"][:, sc, 1:128], ones, mag[:, sc:sc + 1], ALU.mult, R9 + [self.consts], [s5f])
        S.dma("sp", F["dbg"], D["s5db"].t[l], [], [s5f], s5f)
        for hh in range(2):
            prm = self.tmp(12, 3, [128, 3, 512])
            self.s5in = prm.bufs[0]
            S.dma("sp", prm.t.rearrange("p a (s c) -> p a s c", c=128),
                  D["s5rep"].t[l, :, :, 4 * hh:4 * hh + 4, :].rearrange("a p s c -> p a s c"), [], [prm], prm)
            TT_ = self.tmp(15, 9, [128, 9, 512])
            T = [TT_.t[:, i, :] for i in range(9)]
            TB = TT_.bufs + prm.bufs[1:]
            _, _, zr, zi = self.s5_disc(prm.t[:, 0, :], prm.t[:, 1, :], prm.t[:, 2, :], T, TB, True)
            bx = self.tmp(12, 2, [128, 2, 512])
            S.dma("sp", bx.t.rearrange("p a (s c) -> p a s c", c=128),
                  D["s5bexp"].t[l, :, :, 4 * hh:4 * hh + 4, :].rearrange("a p s c -> p a s c"), [], [bx], bx)
            RR = TB + bx.bufs
            self.tt(T[0], zr, bx.t[:, 0, :], ALU.mult, RR, TB)
            self.tt(T[1], zi, bx.t[:, 1, :], ALU.mult, RR, TB)
            self.tt(F32v(self, "BTre", hh), T[0], T[1], ALU.subtract, RR, [s5b])
            self.tt(T[0], zr, bx.t[:, 1, :], ALU.mult, RR, TB)
            self.tt(T[1], zi, bx.t[:, 0, :], ALU.mult, RR, TB)
            self.tt(F32v(self, "BTim", hh), T[0], T[1], ALU.add, RR, [s5b])
            cx = self.tmp(12, 2, [128, 2, 512])
            S.dma("sp", cx.t.rearrange("p a (s c) -> p a s c", c=128),
                  D["s5cexp"].t[l, :, :, 4 * hh:4 * hh + 4, :].rearrange("a p s c -> p a s c"), [], [cx], cx)
            self.cp(F32v(self, "CTre", hh), cx.t[:, 0, :], [cx], [s5b], eng="dve")
            self.ts(F32v(self, "CTim", hh), cx.t[:, 1, :], -1.0, ALU.mult, [cx], [s5b])
        ident = self.consts.t[:, 128:256]
        for kc in range(2):
            self.ts(self.s5B["Dg"][:, kc, :], ident, F["dbg"][:, kc:kc + 1], ALU.mult, [s5f, self.consts], [s5b])
        S.dma("sp", self.s5f_d.t[l], s5f.t[:], [s5f], [self.s5f_d], s5f)
        S.dma("sp", self.s5b_d.t[l], s5b.t[:], [s5b], [self.s5b_d], s5b)

    def s5_fwd(self, l, s, tb):
        S, u, y = self.S, self.u, self.y
        s5f, s5b = self.s5f, self.s5b
        F, B = self.s5F, self.s5B
        st = self.s5state[l]
        self.s5_calls = getattr(self, "s5_calls", 0) + 1
        S.dma("sp", s5f.t[:], self.s5f_d.t[l], [self.s5f_d], [s5f], s5f)
        S.dma("sp", s5b.t[:], self.s5b_d.t[l], [self.s5b_d], [s5b], s5b)
        us5 = self.tmp(12, 1, [128, 2, NT], BF16)
        for kc in range(2):
            w = self.loadw("win", l * 28 + 16 + kc, 8)
            pp = self.P[kc]
            for k in range(8):
                self.mm(pp.t[:], w.t[:, k, :], u.t[:, k, :], k == 0, k == 7, [w, u], [pp])
            self.cp(us5.t[:, kc, :], pp.t[:], [pp], [us5], eng="act")
        A = self.tmp(13, 1, [128, 4, 128])
        Bt = self.tmp(14, 1, [128, 4, 128])
        bh = [self.tmp(15, 2, [128, 8, 128]), self.tmp(17, 2, [128, 8, 128])]
        xh = [self.tmp(19, 2, [128, 8, 128]), self.tmp(21, 2, [128, 8, 128])]
        xb = [self.tmp(23, 1, [128, 8, 128], BF16), self.tmp(0, 1, [128, 8, 128], BF16)]
        ini = self.tmp(1, 1, [128, 4, 8])
        A2 = self.tmp(2, 1, [128, 4, 128])
        B2 = self.tmp(3, 1, [128, 4, 128])
        ypre = [self.P[4], self.P[5]]
        CS = F["CS"]
        if tb == 0:
            S.op("dve", lambda e: e.memset(st.t[:], 0.0), [], [st])
        for sub in range(4):
            c0 = sub * 128
            for sc in range(8):
                for ri in range(2):
                    pp = self.P[2 * ri + sc // 4]
                    self.mm(pp.t[:, (sc % 4) * 128:(sc % 4 + 1) * 128], B["BT"][:, sc, ri, :],
                            us5.t[:, sc // 4, c0:c0 + 128], True, True, [s5b, us5], [pp])
            for hh in range(2):
                c = CS[:, 4 * hh:4 * hh + 4, 0, :]
                sn = CS[:, 4 * hh:4 * hh + 4, 1, :]
                pre = self.P[hh].t[:].rearrange("p (a b) -> p a b", b=128)
                pim = self.P[2 + hh].t[:].rearrange("p (a b) -> p a b", b=128)
                self.tt(A.t, pre, c, ALU.mult, [self.P[hh], s5f], [A])
                self.tt(Bt.t, pim, sn, ALU.mult, [self.P[2 + hh], s5f], [Bt])
                self.tt(bh[0].t[:, 4 * hh:4 * hh + 4, :], A.t, Bt.t, ALU.add, [A, Bt], [bh[0]])
                self.tt(A.t, pim, c, ALU.mult, [self.P[2 + hh], s5f], [A])
                self.tt(Bt.t, pre, sn, ALU.mult, [self.P[hh], s5f], [Bt])
                self.tt(bh[1].t[:, 4 * hh:4 * hh + 4, :], A.t, Bt.t, ALU.subtract, [A, Bt], [bh[1]])
            if not (tb == 0 and sub == 0):
                i0, i1, i2, i3 = (ini.t[:, i, :] for i in range(4))
                c1, s1 = F["E128"][:, :, 0], F["E128"][:, :, 1]
                self.tt(i0, c1, st.t[:, 0, :], ALU.mult, [s5f, st], [ini])
                self.tt(i1, s1, st.t[:, 1, :], ALU.mult, [s5f, st], [ini])
                self.tt(i0, i0, i1, ALU.subtract, [ini], [ini])
                self.tt(i2, c1, st.t[:, 1, :], ALU.mult, [s5f, st], [ini])
                self.tt(i3, s1, st.t[:, 0, :], ALU.mult, [s5f, st], [ini])
                self.tt(i2, i2, i3, ALU.add, [ini], [ini])
                self.tt(i0, i0, F["rcol"], ALU.mult, [ini, s5f], [ini])
                self.tt(i2, i2, F["rcol"], ALU.mult, [ini, s5f], [ini])
                self.tt(bh[0].t[:, :, 0], bh[0].t[:, :, 0], i0, ALU.add, [bh[0], ini], [bh[0]])
                self.tt(bh[1].t[:, :, 0], bh[1].t[:, :, 0], i2, ALU.add, [bh[1], ini], [bh[1]])
            Rf = F["R"].rearrange("p a b -> p (a b)")
            for ri in range(2):
                S.op("dve", lambda e, ri=ri: e.tensor_tensor_scan(
                    out=xh[ri].t.rearrange("p a b -> p (a b)"), data0=Rf,
                    data1=bh[ri].t.rearrange("p a b -> p (a b)"), initial=0.0, op0=ALU.mult, op1=ALU.add),
                    [bh[ri], s5f], [xh[ri]])
                self.cp(st.t[:, ri, :], xh[ri].t[:, :, 127], [xh[ri]], [st], eng="dve")
            for hh in range(2):
                c = CS[:, 4 * hh:4 * hh + 4, 0, :]
                sn = CS[:, 4 * hh:4 * hh + 4, 1, :]
                hs = slice(4 * hh, 4 * hh + 4)
                re_ = "dve" if self.s5_calls == 1 else "pool"
                self.tt(A2.t, xh[0].t[:, hs, :], c, ALU.mult, [xh[0], s5f], [A2], eng=re_)
                self.tt(B2.t, xh[1].t[:, hs, :], sn, ALU.mult, [xh[1], s5f], [B2], eng=re_)
                self.tt(xb[0].t[:, hs, :], A2.t, B2.t, ALU.subtract, [A2, B2], [xb[0]], eng=re_)
                self.tt(A2.t, xh[1].t[:, hs, :], c, ALU.mult, [xh[1], s5f], [A2], eng=re_)
                self.tt(B2.t, xh[0].t[:, hs, :], sn, ALU.mult, [xh[0], s5f], [B2], eng=re_)
                self.tt(xb[1].t[:, hs, :], A2.t, B2.t, ALU.add, [A2, B2], [xb[1]], eng=re_)
            for kc in range(2):
                pp = ypre[kc]
                o = pp.t[:, c0:c0 + 128]
                n = 0
                for sc in range(4 * kc, 4 * kc + 4):
                    for ri in range(2):
                        self.mm(o, B["CT"][:, sc, ri, :], xb[ri].t[:, sc, :], n == 0, False, [s5b, xb[ri]], [pp])
                        n += 1
                self.mm(o, B["Dg"][:, kc, :], us5.t[:, kc, c0:c0 + 128], False, True, [s5b, us5], [pp])
        yg = self.tmp(13, 2, [128, 2, NT])
        t1 = self.tmp(15, 2, [128, 2, NT])
        ygb = self.tmp(17, 1, [128, 2, NT], BF16)
        for kc in range(2):
            self.cp(yg.t[:, kc, :], ypre[kc].t[:], [ypre[kc]], [yg], eng="act")
        self.act(t1.t, yg.t, AF.Square, [yg], [t1])
        self.ts(t1.t, t1.t, 0.044715, ALU.mult, [t1], [t1], s2=1.0, op1=ALU.add)
        self.tt(t1.t, t1.t, yg.t, ALU.mult, [t1, yg], [t1])
        self.act(t1.t, t1.t, AF.Sigmoid, [t1], [t1], scale=1.5957691216)
        self.tt(yg.t, yg.t, t1.t, ALU.mult, [t1, yg], [yg])
        self.cp(ygb.t, yg.t, [yg], [ygb], eng="dve" if self.s5_calls == 1 else "pool")
        for oc in range(2):
            w = self.loadw("wglu", l * 2 + oc, 2)
            pp = self.P[6 + oc]
            for k in range(2):
                self.mm(pp.t[:], w.t[:, k, :], ygb.t[:, k, :], k == 0, k == 1, [w, ygb], [pp])
            self.act(t1.t[:, oc, :], pp.t[:], AF.Sigmoid, [pp, s5f], [t1], bias=F["dbg"][:, 2 + oc:3 + oc])
            self.tt(y.t[:, 2, oc, :], yg.t[:, oc, :], t1.t[:, oc, :], ALU.mult, [yg, t1], [y])


    def moba_fwd(self, l, s, tb):
        S, u, y, D = self.S, self.u, self.y, self.D
        t0 = tb * NT
        kc_, vc_, km = self.kcache[l], self.vcache[l], self.kmean[l]
        pc, consts = self.pc, self.consts
        cosT = self.tmp(0, 1, [128, NT])
        sinT = self.tmp(1, 1, [128, NT])
        posi = self.tmp(2, 1, [128, NT])
        ang = self.tmp(3, 1, [128, NT])
        fr = self.tmp(4, 1, [128, NT])
        fb = self.tmp(5, 1, [128, NT])
        qf = [self.tmp(6, 1, [128, NT]), self.tmp(7, 1, [128, NT])]
        kf = [self.tmp(8, 1, [128, NT]), self.tmp(9, 1, [128, NT])]
        t1 = self.tmp(10, 1, [128, NT])
        t2 = self.tmp(12, 1, [128, NT])
        qb_ = self.tmp(13, 1, [128, 2, NT], BF16)
        sm = self.tmp(14, 1, [128, 512])
        gm = sm.t[:, 0:32].rearrange("p (a b) -> p a b", b=8)
        mx = sm.t[:, 32:64].rearrange("p (a b) -> p a b", b=8)
        mnegb = sm.t[:, 64:128].bitcast(BF16).rearrange("p (a b) -> p a b", b=32)
        et = [self.tmp(15, 1, [128, 1024], BF16)]
        ets = [et[0].t[:, 0:512].rearrange("p (a b) -> p a b", b=256), et[0].t[:, 512:1024].rearrange("p (a b) -> p a b", b=256)]
        rden = self.tmp(16, 1, [128, 2, 256])
        S.dma("sp", posi.t.bitcast(I32), D["posr"].t[s, :, t0:t0 + NT], [], [posi], posi)
        self.cp(ang.t, posi.t.bitcast(I32), [posi], [ang], eng="dve")
        self.ts(ang.t, ang.t, pc.t[:, 0:1], ALU.mult, [ang, pc], [ang])
        self.frac2pi(fr.t, ang.t, 0.5 * PI, fb.t, [ang, fr, fb], [fr, fb])
        self.act(cosT.t, fr.t, AF.Sin, [fr], [cosT], scale=2 * PI)
        self.frac2pi(fr.t, ang.t, 0.0, fb.t, [ang, fr, fb], [fr, fb])
        self.act(sinT.t, fr.t, AF.Sin, [fr, pc], [sinT], scale=pc.t[:, 2:3])
        for c in range(2):
            for (dst, base) in ((qf[c], 18), (kf[c], 20)):
                w1 = self.loadw("win", l * 28 + base + c, 8)
                w2 = self.loadw("win", l * 28 + base + 6 + c, 8)
                p1, p2 = self.P[0], self.P[1]
                for k in range(8):
                    self.mm(p1.t[:], w1.t[:, k, :], u.t[:, k, :], k == 0, k == 7, [w1, u], [p1])
                for k in range(8):
                    self.mm(p2.t[:], w2.t[:, k, :], u.t[:, k, :], k == 0, k == 7, [w2, u], [p2])
                self.tt(t1.t, p1.t[:], cosT.t, ALU.mult, [p1, cosT], [t1])
                self.tt(t2.t, p2.t[:], sinT.t, ALU.mult, [p2, sinT], [t2])
                self.tt(dst.t, t1.t, t2.t, ALU.add, [t1, t2], [dst], eng="pool")
            self.cp(qb_.t[:, c, :], qf[c].t, [qf[c]], [qb_], eng="pool")
            self.cp(kc_.t[:, c, t0:t0 + NT], kf[c].t, [kf[c]], [kc_], eng="pool")
            S.op("dve", lambda e, c=c: e.tensor_reduce(out=km.t[:, c, 2 * tb:2 * tb + 2],
                                                      in_=kf[c].t.rearrange("p (a b) -> p a b", b=256),
                                                      axis=AX.X, op=ALU.add), [kf[c]], [km])
        self.ts(km.t[:, :, 2 * tb:2 * tb + 2], km.t[:, :, 2 * tb:2 * tb + 2], 1.0 / 256, ALU.mult, [km], [km])
        wv = [self.loadw("win", l * 28 + 22 + i, 8) for i in range(2)]
        for tt_ in range(4):
            pv = self.P[2 + tt_ % 2]
            for i in range(2):
                for k in range(8):
                    self.mm(pv.t[:, i * 128:(i + 1) * 128], u.t[:, k, tt_ * 128:(tt_ + 1) * 128], wv[i].t[:, k, :],
                            k == 0, k == 7, [wv[i], u], [pv])
            self.cp(vc_.t[:, tb * 4 + tt_, :], pv.t[:, 0:256], [pv], [vc_], eng="act")
        if l == 0 and tb == self.dbg.get("ddtb", 0):
            self.dd("cosT", cosT.t, [cosT], [128, NT])
            self.dd("sinT", sinT.t, [sinT], [128, NT])
            self.dd("qf0", qf[0].t, [qf[0]], [128, NT])
            self.dd("kf1", kf[1].t, [kf[1]], [128, NT])
            self.dd("km", km.t[:], [km], [128, 2, 8])
            self.dd("vc", vc_.t[:, tb * 4, :], [vc_], [128, 256], BF16)
        if tb > 0 or True:
            for qt in range(4):
                qblk = 2 * tb + qt // 2
                if qblk == 0:
                    continue
                pg = self.P[6]
                for h in range(4):
                    c, off = h // 2, (h % 2) * 64
                    self.mm(pg.t[:, h * 8:h * 8 + 8], qf[c].t[off:off + 64, qt * 128:(qt + 1) * 128],
                            km.t[off:off + 64, c, :], True, True, [qf[c], km], [pg])
                vm = consts.t[:, 256 + qblk * 8:256 + qblk * 8 + 8].unsqueeze(1).broadcast_to([128, 4, 8])
                self.tt(gm, pg.t[:, 0:32].rearrange("p (a b) -> p a b", b=8), vm, ALU.add, [pg, consts], [sm])
                for h in range(4):
                    S.op("dve", lambda e, h=h: e.max(out=mx[:, h, :], in_=gm[:, h, :]), [sm], [sm])
                for h in range(4):
                    self.ts(gm[:, h, :], gm[:, h, :], mx[:, h, 2:3], ALU.is_ge, [sm], [sm], s2=30000.0, op1=ALU.mult)
                self.ts(mnegb[:, qt, :], sm.t[:, 0:32], -30000.0, ALU.add, [sm], [sm])
                if l == 0 and tb == self.dbg.get("ddtb", 0) and qt == 3:
                    self.dd("sm", sm.t[:, 0:128], [sm], [128, 128])
        it = 0
        for c in range(2):
            for j in range(2):
                qblk = 2 * tb + j
                nkt = 2 * (qblk + 1)
                pacc, pden = self.P[2 + 2 * (it % 2)], self.P[3 + 2 * (it % 2)]
                it += 1
                qs = slice(j * 256, (j + 1) * 256)
                for kt in range(nkt):
                    n = kt // 2
                    ps_ = self.P[kt % 2]
                    e_ = ets[kt % 2]
                    for hh in range(2):
                        h, off = 2 * c + hh, hh * 64
                        o = ps_.t[:, hh * 256:(hh + 1) * 256]
                        self.mm(o, kc_.t[off:off + 64, c, kt * 128:(kt + 1) * 128], qb_.t[off:off + 64, c, qs],
                                True, False, [kc_, qb_], [ps_])
                        if n < qblk:
                            for q2 in range(2):
                                qt = 2 * j + q2
                                lh = mnegb[:, qt, h * 8 + n:h * 8 + n + 1].broadcast_to([128, 128])
                                self.mm(ps_.t[:, hh * 256 + q2 * 128:hh * 256 + (q2 + 1) * 128], lh, self.ident_bf.t[:],
                                        False, True, [sm, self.ident_bf], [ps_])
                        else:
                            self.mm(o, self.ident_bf.t[:], self.cm.t[:, kt % 2, :], False, True,
                                    [self.ident_bf, self.cm], [ps_])
                    if l == 0 and tb == self.dbg.get("ddtb", 0) and c == 0 and j == 0 and kt == 0 and "ps" in self.dbg.get("dd", ()):
                        dbgt = self.tmp(17, 1, [128, 512])
                        self.cp(dbgt.t, ps_.t[:], [ps_], [dbgt], eng="act")
                        self.dd("ps", dbgt.t, [dbgt], [128, 512])
                    self.act(e_, ps_.t[:].rearrange("p (a b) -> p a b", b=256), AF.Exp, [ps_], [et[0]], scale=0.125)
                    if l == 0 and tb == self.dbg.get("ddtb", 0) and c == 0 and j == 0 and kt == 0:
                        self.dd("et", et[0].t[:, 0:512], [et[0]], [128, 512], BF16)
                        self.dd("cm", self.cm.t[:], [self.cm], [128, 2, 256], BF16)
                    e2 = et[0].t[:, (kt % 2) * 512:(kt % 2 + 1) * 512]
                    self.mm(pacc.t[:], vc_.t[:, kt, c * 128:(c + 1) * 128], e2,
                            kt == 0, kt == nkt - 1, [vc_, et[0]], [pacc])
                    self.mm(pden.t[:], self.ones_bf.t[:], e2,
                            kt == 0, kt == nkt - 1, [self.ones_bf, et[0]], [pden])
                S.op("dve", lambda e, pden=pden: e.reciprocal(rden.t.rearrange("p a b -> p (a b)"), pden.t[:]), [pden], [rden])
                if l == 0 and tb == self.dbg.get("ddtb", 0):
                    self.dd("rden%d%d" % (c, j), rden.t, [rden], [128, 2, 256])
                for hh in range(2):
                    off = hh * 64
                    self.tt(y.t[off:off + 64, 3, c, qs], pacc.t[off:off + 64, hh * 256:(hh + 1) * 256],
                            rden.t[off:off + 64, hh, :], ALU.mult, [pacc, rden], [y])

    def tokproj_small(self, l):
        u, pp = self.u, self.P[7]
        for c in range(8):
            for k in range(8):
                self.mm(pp.t[0:64, c * 16:(c + 1) * 16], u.t[:, k, c * 64:(c + 1) * 64], self.wsm.t[:, l, k, :],
                        k == 0, k == 7, [u, self.wsm], [pp])
        self.sp = self.tmp(11, 1, [128, 512])
        self.cp(self.sp.t[0:64, 0:128], pp.t[0:64, 0:128], [pp], [self.sp], eng="act")
        return self.sp.t[0:64, 0:128].rearrange("p (c n) -> p c n", n=16)

    def conv_silu(self, l, tiles, nch, tail, cw, dst, tb):
        S, u = self.S, self.u
        xc = self.tmp(0, 7, [128, nch, NT + 3])
        acc = self.tmp(9, 1, [128, NT])
        if tb == 0:
            S.op("dve", lambda e: e.memset(tail.t[:], 0.0), [], [tail])
        self.cp(xc.t[:, :, 0:3], tail.t[:], [tail], [xc], eng="dve")
        for c in range(nch):
            w = self.loadw("win", l * 28 + tiles + c, 8)
            pp = self.P[c % 2]
            for k in range(8):
                self.mm(pp.t[:], w.t[:, k, :], u.t[:, k, :], k == 0, k == 7, [w, u], [pp])
            self.cp(xc.t[:, c, 3:NT + 3], pp.t[:], [pp], [xc], eng="act")
        self.cp(tail.t[:], xc.t[:, :, NT:NT + 3], [xc], [tail], eng="dve")
        for c in range(nch):
            self.ts(acc.t, xc.t[:, c, 0:NT], cw[:, c * 4:c * 4 + 1], ALU.mult, [xc, self.cwb], [acc])
            for j in range(1, 4):
                self.stt(acc.t, xc.t[:, c, j:NT + j], cw[:, c * 4 + j:c * 4 + j + 1], acc.t, ALU.mult, ALU.add,
                         [xc, acc, self.cwb], [acc])
            self.act(dst.t[:, c, :], acc.t, AF.Silu, [acc], [dst])

    def mlstm_fwd(self, l, s, tb, sp):
        S, u, y = self.S, self.u, self.y
        consts, c2, cst = self.consts, self.consts2, self.cst
        ident = consts.t[:, 128:256]
        ones = consts.t[:, 384:512]
        TRI = c2.t[0:64, 0:64]
        CMASK = c2.t[0:64, 64:128]
        rows = self.mlrow.t[0:64, l, :]
        Cx, mrep = self.mlC[l], self.mlm[l]
        qk = self.tmp(12, 4, [128, 4, NT])
        self.conv_silu(l, 8, 4, self.mltail[l], self.cwb.t[:, l, 24:40], qk, tb)
        self.ts(qk.t[:, 2:4, :], qk.t[:, 2:4, :], 0.125, ALU.mult, [qk], [qk])
        ms = self.dbg.get("mlstop", 99)
        if ms <= 1:
            return
        kz = self.tmp(4, 4, [128, 4, NT])
        S.op("dve", lambda e: e.memset(kz.t, 0.0), [], [kz])
        for h in range(4):
            pr, off = h // 2, (h % 2) * 64
            self.cp(kz.t[off:off + 64, h, :], qk.t[off:off + 64, 2 + pr, :], [qk], [kz], eng="dve")
        if tb == 0:
            S.op("dve", lambda e: e.memset(Cx.t[:], 0.0), [], [Cx])
            S.op("dve", lambda e: e.memset(mrep.t[:], 0.0), [], [mrep])
        A = self.tmp(16, 1, [128, 512])
        R_ = [A, self.sp]
        v3 = lambda lo: A.t[0:64, lo:lo + 32].rearrange("p (c h) -> p c h", h=4)
        li, lf, b_, ak, tx = v3(0), v3(32), v3(64), v3(96), v3(128)
        grep = A.t[:, 160:192].rearrange("p (c h) -> p c h", h=4)
        mkrep = A.t[:, 192:224].rearrange("p (c h) -> p c h", h=4)
        Mall = A.t[:, 224:260].rearrange("p (c h) -> p c h", h=4)
        scall = A.t[:, 260:292].rearrange("p (c h) -> p c h", h=4)
        kws = v3(292)
        mk32 = A.t[0:32, 324:325]
        dg32 = A.t[0:32, 328:360]
        ib = rows[:, 0:4].unsqueeze(1).broadcast_to([64, 8, 4])
        fb = rows[:, 4:8].unsqueeze(1).broadcast_to([64, 8, 4])
        self.tt(li, sp[:, :, 8:12], ib, ALU.add, R_ + [self.mlrow], [A])
        self.tt(tx, sp[:, :, 12:16], fb, ALU.add, R_ + [self.mlrow], [A])
        self.act(tx, tx, AF.Exp, [A], [A], scale=-1.0)
        self.act(tx, tx, AF.Ln, [A, cst], [A], bias=cst.t[0:64, 3:4])
        self.ts(lf, tx, -1.0, ALU.mult, [A], [A])
        p7 = self.P[7]
        lf2 = A.t[0:64, 32:64]
        self.mm(p7.t[0:64, 0:32], TRI, lf2, True, True, [A, c2], [p7])
        self.cp(A.t[0:64, 64:96], p7.t[0:64, 0:32], [p7], [A], eng="act")
        self.mm(p7.t[:, 32:64], ones[0:64, :], lf2, True, True, [A, consts], [p7])
        self.cp(A.t[:, 160:192], p7.t[:, 32:64], [p7], [A], eng="act")
        self.tt(ak, grep[0:64], b_, ALU.subtract, [A], [A])
        self.tt(ak, ak, li, ALU.add, [A], [A])
        self.mm(p7.t[0:32, 64:128], A.t[0:64, 96:128], ident[0:64, 0:64], True, True, [A, consts], [p7])
        S.op("dve", lambda e: e.tensor_reduce(out=mk32, in_=p7.t[0:32, 64:128], axis=AX.X, op=ALU.max), [p7], [A])
        self.ts(dg32, ident[0:32, 0:32], mk32, ALU.mult, [A, consts], [A])
        self.mm(p7.t[:, 128:160], ones[0:32, :], dg32, True, True, [A, consts], [p7])
        self.cp(A.t[:, 192:224], p7.t[:, 128:160], [p7], [A], eng="act")
        self.cp(Mall[:, 0, :], mrep.t[:], [mrep], [A], eng="dve")
        t4 = A.t[:, 364:368]
        for c in range(8):
            self.tt(t4, grep[:, c, :], Mall[:, c, :], ALU.add, [A], [A])
            self.tt(Mall[:, c + 1, :], t4, mkrep[:, c, :], ALU.max, [A], [A])
        self.cp(mrep.t[:], Mall[:, 8, :], [A], [mrep], eng="dve")
        self.tt(scall, grep, Mall[:, 0:8, :], ALU.add, [A], [A])
        self.tt(scall, scall, Mall[:, 1:9, :], ALU.subtract, [A], [A])
        self.act(scall, scall, AF.Exp, [A], [A])
        self.tt(kws, ak, Mall[0:64, 1:9, :], ALU.subtract, [A], [A])
        self.act(kws, kws, AF.Exp, [A], [A])
        if ms <= 2:
            return
        vx = self.tmp(17, 1, [128, 512])
        vext = vx.t[0:64, 0:264].rearrange("p (h e) -> p h e", e=66)
        S.op("dve", lambda e: e.memset(vx.t[0:64, 0:264], 1.0), [], [vx])
        osg = self.tmp(18, 1, [128, 512])
        Dm = self.tmp(19, 1, [128, 512])
        LR = self.tmp(20, 1, [128, 512])
        sq_ = self.tmp(21, 1, [128, 512])
        sT = self.tmp(22, 1, [128, 512])
        ne = self.tmp(23, 1, [128, 512])
        tq = self.tmp(0, 1, [128, 512])
        kt_ = self.tmp(1, 1, [128, 512])
        hh_ = self.tmp(2, 1, [128, 512])
        B = self.tmp(3, 1, [128, 512])
        v4 = lambda T, lo=0: T.t[0:64, lo:lo + 256].rearrange("p (h e) -> p h e", e=64)
        wv = [self.loadw("win", l * 28 + 12 + i, 8) for i in range(4)]
        for c in range(8):
            cs = slice(c * 64, (c + 1) * 64)
            p0 = self.P[0]
            for i in range(4):
                for k in range(8):
                    self.mm(p0.t[0:64, i * 128:(i + 1) * 128], u.t[:, k, cs], wv[i].t[:, k, :], k == 0, k == 7,
                            [u, wv[i]], [p0])
            self.cp(vext[:, :, 0:64], p0.t[0:64, 0:256].rearrange("p (h e) -> p h e", e=64), [p0], [vx], eng="act")
            self.act(osg.t[0:64, 0:256], p0.t[0:64, 256:512], AF.Sigmoid, [p0], [osg])
            if ms <= 3:
                continue
            lft = LR.t[0:64, 0:256].rearrange("p (h e) -> p h e", e=64)
            rm = LR.t[0:64, 256:512].rearrange("p (h e) -> p h e", e=64)
            tri_b = TRI.unsqueeze(1).broadcast_to([64, 4, 64])
            id_b = ident[0:64, 0:64].unsqueeze(1).broadcast_to([64, 4, 64])
            self.tt(lft, tri_b, lf[:, c, :].unsqueeze(2).broadcast_to([64, 4, 64]), ALU.mult, [A, c2], [LR])
            self.tt(rm, id_b, li[:, c, :].unsqueeze(2).broadcast_to([64, 4, 64]), ALU.mult, [A, consts], [LR])
            self.tt(rm, rm, lft, ALU.subtract, [LR], [LR])
            p1 = self.P[1]
            for h in range(4):
                o = p1.t[0:64, h * 64:(h + 1) * 64]
                self.mm(o, lft[:, h, :], ones[0:64, 0:64], True, False, [LR, consts], [p1])
                self.mm(o, ones[0:64, 0:64], rm[:, h, :], False, True, [LR, consts], [p1])
            dmv = v4(Dm)
            self.tt(dmv, p1.t[0:64, 0:256].rearrange("p (h e) -> p h e", e=64),
                    CMASK.unsqueeze(1).broadcast_to([64, 4, 64]), ALU.add, [p1, c2], [Dm])
            sm_ = B.t[0:64, 0:64]
            mloc, mint, mt, wint, e2, qn, den = (B.t[0:64, 4 * i:4 * i + 4] for i in range(7))
            S.op("dve", lambda e, dmv=dmv, mloc=mloc: e.tensor_reduce(out=mloc, in_=dmv, axis=AX.X, op=ALU.max), [Dm], [B])
            self.tt(mint, b_[:, c, :], Mall[0:64, c, :], ALU.add, [A], [B])
            self.tt(mt, mint, mloc, ALU.max, [B], [B])
            self.tt(wint, mint, mt, ALU.subtract, [B], [B])
            self.act(wint, wint, AF.Exp, [B], [B])
            self.act(e2, mt, AF.Exp, [B], [B], scale=-1.0)
            if ms <= 4:
                continue
            p2 = self.P[2]
            for h in range(4):
                o = p2.t[0:64, h * 64:(h + 1) * 64]
                self.mm(o, ones[0:64, 0:64], lft[:, h, :], True, False, [LR, consts], [p2])
                self.mm(o, rm[:, h, :], ones[0:64, 0:64], False, True, [LR, consts], [p2])
            etv = v4(sq_)
            self.tt(etv, p2.t[0:64, 0:256].rearrange("p (h e) -> p h e", e=64),
                    c2.t[0:64, 320:384].unsqueeze(1).broadcast_to([64, 4, 64]), ALU.add, [p2, c2], [sq_])
            self.act(etv, etv, AF.Exp, [sq_], [sq_])
            p3 = self.P[3]
            for h in range(4):
                self.mm(p3.t[0:64, h * 64:(h + 1) * 64], kz.t[:, h, cs], qk.t[:, h // 2, cs], True, True, [kz, qk], [p3])
            self.tt(v4(sT), p3.t[0:64, 0:256].rearrange("p (h e) -> p h e", e=64), etv, ALU.mult, [p3, sq_], [sT])
            if ms <= 5:
                continue
            stv = v4(sT)
            p4, p5 = self.P[4], self.P[5]
            for h in range(4):
                pr, off = h // 2, (h % 2) * 64
                self.mm(p4.t[0:64, h * 66:(h + 1) * 66], stv[:, h, :], vext[:, h, :], True, True, [sT, vx], [p4])
                self.mm(p5.t[0:64, h * 66:(h + 1) * 66], qk.t[:, pr, cs], Cx.t[:, h, :],
                        True, True, [qk, Cx], [p5])
            nev = ne.t[0:64, 0:264].rearrange("p (h e) -> p h e", e=66)
            tqv = tq.t[0:64, 0:264].rearrange("p (h e) -> p h e", e=66)
            self.tt(tqv, p5.t[0:64, 0:264].rearrange("p (h e) -> p h e", e=66),
                    wint.unsqueeze(2).broadcast_to([64, 4, 66]), ALU.mult, [p5, B], [tq])
            self.tt(nev, p4.t[0:64, 0:264].rearrange("p (h e) -> p h e", e=66),
                    e2.unsqueeze(2).broadcast_to([64, 4, 66]), ALU.mult, [p4, B], [ne])
            self.tt(nev, nev, tqv, ALU.add, [tq, ne], [ne])
            self.act(den, nev[:, :, 64], AF.Abs, [ne], [B])
            self.tt(den, den, e2, ALU.max, [B], [B])
            S.op("dve", lambda e, den=den: e.reciprocal(den, den), [B], [B])
            hv = v4(hh_)
            self.tt(hv, nev[:, :, 0:64], den.unsqueeze(2).broadcast_to([64, 4, 64]), ALU.mult, [ne, B], [hh_])
            if ms <= 6:
                continue
            h2 = v4(hh_, 256)
            ss = B.t[0:64, 32:36]
            self.tt(h2, hv, hv, ALU.mult, [hh_], [hh_])
            S.op("dve", lambda e, h2=h2, ss=ss: e.tensor_reduce(out=ss, in_=h2, axis=AX.X, op=ALU.add), [hh_], [B])
            self.act(ss, ss, AF.Sqrt, [B, cst], [B], scale=1.0 / 64, bias=cst.t[0:64, 0:1])
            S.op("dve", lambda e, ss=ss: e.reciprocal(ss, ss), [B], [B])
            self.tt(hv, hv, ss.unsqueeze(2).broadcast_to([64, 4, 64]), ALU.mult, [hh_, B], [hh_])
            self.tt(hh_.t[0:64, 0:256], hh_.t[0:64, 0:256], rows[:, 8:264], ALU.mult, [hh_, self.mlrow], [hh_])
            self.tt(hh_.t[0:64, 0:256], hh_.t[0:64, 0:256], osg.t[0:64, 0:256], ALU.mult, [hh_, osg], [hh_])
            for kc in range(2):
                self.mm(p3.t[:, 256 + kc * 64:256 + (kc + 1) * 64], hh_.t[0:64, kc * 128:(kc + 1) * 128],
                        ident[0:64, 0:64], True, True, [hh_, consts], [p3])
                self.cp(y.t[:, 1, kc, cs], p3.t[:, 256 + kc * 64:256 + (kc + 1) * 64], [p3], [y], eng="act")
            if ms <= 7:
                continue
            p6 = self.P[6]
            for pr in range(2):
                self.mm(p6.t[0:64, pr * 128:(pr + 1) * 128], qk.t[:, 2 + pr, cs], ident, True, True, [qk, consts], [p6])
            kwv = v4(kt_)
            self.tt(kwv, p6.t[0:64, 0:256].rearrange("p (h e) -> p h e", e=64),
                    kws[:, c, :].unsqueeze(2).broadcast_to([64, 4, 64]), ALU.mult, [p6, A], [kt_])
            p7b = self.P[7]
            for h in range(4):
                pr, off = h // 2, (h % 2) * 64
                o = p7b.t[:, h * 66:(h + 1) * 66]
                self.mm(o, kt_.t[0:64, pr * 128:(pr + 1) * 128], vext[:, h, :], True, True, [kt_, vx], [p7b])
                self.stt(Cx.t[off:off + 64, h, :], Cx.t[off:off + 64, h, :], scall[off:off + 64, c, h:h + 1],
                         p7b.t[off:off + 64, h * 66:(h + 1) * 66], ALU.mult, ALU.add, [Cx, A, p7b], [Cx])

    def gdn_fwd(self, l, s, tb, sp):
        S, u, y = self.S, self.u, self.y
        consts, c2, cst = self.consts, self.consts2, self.cst
        ident = consts.t[:, 128:256]
        id64 = ident[0:64, 0:64]
        ones = consts.t[:, 384:512]
        on64 = ones[0:64, 0:64]
        neg64 = self.negones.t[0:64, 0:64]
        TRI = c2.t[0:64, 0:64]
        SLADD = self.gmask.t[0:64, 0:64]
        SUADD = self.gmask.t[0:64, 64:128]
        CMT = c2.t[0:64, 320:384]
        BLK = self.gmask.t[:, 128:256]
        rows = self.gdrow.t[0:64, l, :]
        Sz = self.gdS[l]
        b3 = lambda ap: ap.unsqueeze(1).broadcast_to([64, 4, 64])
        v4 = lambda T, lo=0: T.t[0:64, lo:lo + 256].rearrange("p (h e) -> p h e", e=64)
        qkv = self.tmp(12, 6, [128, 6, NT])
        self.conv_silu(l, 0, 6, self.gdtail[l], self.cwb.t[:, l, 0:24], qkv, tb)
        if tb == 0:
            S.op("dve", lambda e: e.memset(Sz.t[:], 0.0), [], [Sz])
        sq = self.tmp(9, 1, [128, NT])
        rs = self.tmp(8, 1, [128, NT])
        for c4 in range(4):
            pp = self.P[c4 % 2]
            self.tt(sq.t, qkv.t[:, c4, :], qkv.t[:, c4, :], ALU.mult, [qkv], [sq])
            self.mm(pp.t[:], BLK, sq.t, True, True, [sq, self.gmask], [pp])
            self.act(rs.t, pp.t[:], AF.Sqrt, [pp, cst], [rs], bias=cst.t[:, 0:1])
            S.op("dve", lambda e: e.reciprocal(rs.t, rs.t), [rs], [rs])
            if c4 < 2:
                self.stt(qkv.t[:, c4, :], qkv.t[:, c4, :], 0.125, rs.t, ALU.mult, ALU.mult, [qkv, rs], [qkv])
            else:
                self.tt(qkv.t[:, c4, :], qkv.t[:, c4, :], rs.t, ALU.mult, [qkv, rs], [qkv])
        kz = self.tmp(4, 4, [128, 4, NT])
        S.op("dve", lambda e: e.memset(kz.t, 0.0), [], [kz])
        for h in range(4):
            pr, off = h // 2, (h % 2) * 64
            self.cp(kz.t[off:off + 64, h, :], qkv.t[off:off + 64, 2 + pr, :], [qkv], [kz], eng="dve")
        A = self.tmp(18, 1, [128, 512])
        v3 = lambda lo: A.t[0:64, lo:lo + 32].rearrange("p (c h) -> p c h", h=4)
        beta, g_, gc, egc, ekd, tx, bneg, begc = v3(0), v3(32), v3(64), v3(96), v3(128), v3(160), v3(192), v3(224)
        gLrep = A.t[:, 256:288].rearrange("p (c h) -> p c h", h=4)
        cdrep = A.t[:, 288:320].rearrange("p (c h) -> p c h", h=4)
        ea = A.t[0:64, 320:324]
        R_ = [A, self.sp]
        self.act(beta, sp[:, :, 0:4], AF.Sigmoid, R_, [A])
        self.tt(tx, sp[:, :, 4:8], rows[:, 4:8].unsqueeze(1).broadcast_to([64, 8, 4]), ALU.add, R_ + [self.gdrow], [A])
        self.act(tx, tx, AF.Exp, [A], [A])
        self.act(tx, tx, AF.Ln, [A, cst], [A], bias=cst.t[0:64, 3:4])
        self.act(ea, rows[:, 0:4], AF.Exp, [self.gdrow], [A])
        self.tt(g_, tx, ea.unsqueeze(1).broadcast_to([64, 8, 4]), ALU.mult, [A], [A])
        self.ts(g_, g_, -1.0, ALU.mult, [A], [A])
        p7 = self.P[7]
        g2 = A.t[0:64, 32:64]
        self.mm(p7.t[0:64, 0:32], TRI, g2, True, True, [A, c2], [p7])
        self.cp(A.t[0:64, 64:96], p7.t[0:64, 0:32], [p7], [A], eng="act")
        self.mm(p7.t[:, 32:64], ones[0:64, :], g2, True, True, [A, consts], [p7])
        self.cp(A.t[:, 256:288], p7.t[:, 32:64], [p7], [A], eng="act")
        self.act(egc, gc, AF.Exp, [A], [A])
        self.tt(ekd, gLrep[0:64], gc, ALU.subtract, [A], [A])
        self.act(ekd, ekd, AF.Exp, [A], [A])
        self.act(cdrep, gLrep, AF.Exp, [A], [A])
        self.ts(bneg, beta, -1.0, ALU.mult, [A], [A])
        self.tt(begc, beta, egc, ALU.mult, [A], [A])
        MM_ = self.tmp(19, 1, [128, 512], BF16)
        Xb = self.tmp(8, 1, [128, 512], BF16)
        Xbv = Xb.t[0:64, 0:512].rearrange("p (h e) -> p h e", e=128)
        X = self.tmp(20, 1, [128, 512])
        DC = self.tmp(21, 1, [128, 512])
        QB = self.tmp(22, 1, [128, 512])
        GT = self.tmp(23, 1, [128, 512])
        VK = self.tmp(0, 1, [128, 512])
        XT = self.tmp(1, 1, [128, 512])
        VN = self.tmp(2, 1, [128, 512])
        ZS = self.tmp(3, 1, [128, 512])
        M2 = self.tmp(10, 1, [128, 512], BF16)
        wz = [self.loadw("win", l * 28 + 6 + i, 8) for i in range(2)]
        Xs = [X, self.xs[0]]
        QBs = [QB, self.xs[1]]
        KZs = [self.xs[2], self.xs[3]]

        def prepN(c):
            cs = slice(c * 64, (c + 1) * 64)
            X, QB, KZ = Xs[c % 2], QBs[c % 2], KZs[c % 2]
            Xv = X.t[0:64, 0:512].rearrange("p (h e) -> p h e", e=128)
            p0 = self.P[0]
            for i in range(2):
                for k in range(8):
                    self.mm(p0.t[0:64, i * 128:(i + 1) * 128], u.t[:, k, cs], wz[i].t[:, k, :], k == 0, k == 7,
                            [u, wz[i]], [p0])
            self.act(KZ.t[0:64, 256:512], p0.t[0:64, 0:256], AF.Silu, [p0], [KZ])
            for pr in range(2):
                self.mm(p0.t[0:64, 256 + pr * 128:256 + (pr + 1) * 128], qkv.t[:, 4 + pr, cs], ident, True, True,
                        [qkv, consts], [p0])
            vtok = v4(VK)
            self.cp(vtok, p0.t[0:64, 256:512].rearrange("p (h e) -> p h e", e=64), [p0], [VK], eng="act")
            p1 = self.P[4]
            for pr in range(2):
                self.mm(p1.t[0:64, pr * 128:(pr + 1) * 128], qkv.t[:, 2 + pr, cs], ident, True, True, [qkv, consts], [p1])
            ktok = v4(VK, 256)
            self.cp(ktok, p1.t[0:64, 0:256].rearrange("p (h e) -> p h e", e=64), [p1], [VK], eng="act")
            for h in range(4):
                uo, wo = (64, 0) if h % 2 == 0 else (0, 64)
                self.ts(Xv[:, h, uo:uo + 64], vtok[:, h, :], beta[:, c, h:h + 1], ALU.mult, [VK, A], [X])
                self.ts(Xv[:, h, wo:wo + 64], ktok[:, h, :], begc[:, c, h:h + 1], ALU.mult, [VK, A], [X])
            kd = v4(KZ)
            self.tt(kd, ktok, ekd[:, c, :].unsqueeze(2).broadcast_to([64, 4, 64]), ALU.mult, [VK, A], [KZ])
            gt = v4(GT)
            self.tt(gt, b3(TRI), g_[:, c, :].unsqueeze(2).broadcast_to([64, 4, 64]), ALU.mult, [A, c2], [GT])
            p2, p3 = self.P[2], self.P[3]
            for h in range(4):
                o = p2.t[0:64, h * 64:(h + 1) * 64]
                self.mm(o, gt[:, h, :], on64, True, False, [GT, consts], [p2])
                self.mm(o, neg64, gt[:, h, :], False, True, [GT, self.negones], [p2])
                o = p2.t[0:64, 256 + h * 64:256 + (h + 1) * 64]
                self.mm(o, on64, gt[:, h, :], True, False, [GT, consts], [p2])
                self.mm(o, gt[:, h, :], neg64, False, True, [GT, self.negones], [p2])
            dS, dT, dQ = v4(DC), v4(DC, 256), v4(QB)
            pD = p2.t[0:64, 0:256].rearrange("p (h e) -> p h e", e=64)
            pDT = p2.t[0:64, 256:512].rearrange("p (h e) -> p h e", e=64)
            self.tt(dS, pD, b3(SLADD), ALU.add, [p2, self.gmask], [DC])
            self.tt(dT, pDT, b3(SUADD), ALU.add, [p2, self.gmask], [DC])
            self.tt(dQ, pDT, b3(CMT), ALU.add, [p2, c2], [QB])
            self.act(DC.t[0:64, 0:512], DC.t[0:64, 0:512], AF.Exp, [DC], [DC])
            self.act(dQ, dQ, AF.Exp, [QB], [QB])
            dgb = v4(GT, 256)
            self.tt(dgb, b3(id64), bneg[:, c, :].unsqueeze(2).broadcast_to([64, 4, 64]), ALU.mult, [A, consts], [GT])
            for h in range(4):
                self.mm(p3.t[0:64, h * 64:(h + 1) * 64], qkv.t[:, 2 + h // 2, cs], kz.t[:, h, cs], True, True, [qkv, kz], [p3])
                self.mm(p3.t[0:64, 256 + h * 64:256 + (h + 1) * 64], on64, dgb[:, h, :], True, True, [GT, consts], [p3])
            pKK = p3.t[0:64, 0:256].rearrange("p (h e) -> p h e", e=64)
            pBf = p3.t[0:64, 256:512].rearrange("p (h e) -> p h e", e=64)
            Mk, MkT = v4(MM_), v4(MM_, 256)
            self.tt(Mk, pKK, dS, ALU.mult, [p3, DC], [MM_])
            self.tt(Mk, Mk, bneg[:, c, :].unsqueeze(2).broadcast_to([64, 4, 64]), ALU.mult, [MM_, A], [MM_])
            self.tt(MkT, pKK, dT, ALU.mult, [p3, DC], [MM_])
            self.tt(MkT, MkT, pBf, ALU.mult, [MM_, p3], [MM_])
            p4 = self.P[4]
            for h in range(4):
                self.mm(p4.t[0:64, h * 64:(h + 1) * 64], kz.t[:, h, cs], qkv.t[:, h // 2, cs], True, True, [qkv, kz], [p4])
            self.tt(dQ, p4.t[0:64, 0:256].rearrange("p (h e) -> p h e", e=64), dQ, ALU.mult, [p4, QB], [QB])
            cur, nxt = MM_, M2
            for step in range(6):
                cM, cMT = v4(cur), v4(cur, 256)
                p5 = self.P[5]
                self.cp(Xb.t[0:64, 0:512], X.t[0:64, 0:512], [X], [Xb], eng="act")
                for h in range(4):
                    self.mm(p5.t[0:64, h * 128:(h + 1) * 128], cMT[:, h, :], Xbv[:, h, :], True, True, [cur, Xb], [p5])
                if step < 5:
                    p6 = self.P[6]
                    for h in range(4):
                        self.mm(p6.t[0:64, h * 64:(h + 1) * 64], cMT[:, h, :], cM[:, h, :], True, True, [cur], [p6])
                        self.mm(p6.t[0:64, 256 + h * 64:256 + (h + 1) * 64], cM[:, h, :], cMT[:, h, :], True, True, [cur], [p6])
                    self.cp(nxt.t[0:64, 0:512], p6.t[0:64, 0:512], [p6], [nxt], eng="act")
                self.tt(X.t[0:64, 0:512], X.t[0:64, 0:512], p5.t[0:64, 0:512], ALU.add, [X, p5], [X])
                cur, nxt = nxt, cur

        def rec(c):
            cs = slice(c * 64, (c + 1) * 64)
            X, QB, KZ = Xs[c % 2], QBs[c % 2], KZs[c % 2]
            Xv = X.t[0:64, 0:512].rearrange("p (h e) -> p h e", e=128)
            dQ = v4(QB)
            p5 = self.P[1]
            for h in range(4):
                self.mm(p5.t[:, h * 64:(h + 1) * 64], Xv[:, h, :], id64, True, True, [X, consts], [p5])
            xt = XT.t[:, 0:256].rearrange("p (h e) -> p h e", e=64)
            self.cp(XT.t[:, 0:256], p5.t[:, 0:256], [p5], [XT], eng="act")
            p6 = self.P[7]
            for h in range(4):
                self.mm(p6.t[0:64, h * 64:(h + 1) * 64], xt[:, h, :], Sz.t[:, h, :], True, True, [XT, Sz], [p6])
                self.mm(p6.t[0:64, 256 + h * 64:256 + (h + 1) * 64], qkv.t[:, h // 2, cs], Sz.t[:, h, :], True, True,
                        [qkv, Sz], [p6])
            vn = v4(VN)
            for h in range(4):
                uo = 64 if h % 2 == 0 else 0
                self.tt(vn[:, h, :], Xv[:, h, uo:uo + 64], p6.t[0:64, h * 64:(h + 1) * 64], ALU.subtract, [X, p6], [VN])
            oq = v4(VN, 256)
            self.tt(oq, p6.t[0:64, 256:512].rearrange("p (h e) -> p h e", e=64),
                    egc[:, c, :].unsqueeze(2).broadcast_to([64, 4, 64]), ALU.mult, [p6, A], [VN])
            p7 = self.P[7]
            for h in range(4):
                self.mm(p7.t[0:64, h * 64:(h + 1) * 64], dQ[:, h, :], vn[:, h, :], True, True, [QB, VN], [p7])
            self.tt(oq, oq, p7.t[0:64, 0:256].rearrange("p (h e) -> p h e", e=64), ALU.add, [VN, p7], [VN])
            p1 = self.P[1]
            for h in range(4):
                pr, off = h // 2, (h % 2) * 64
                self.mm(p1.t[:, 256 + h * 64:256 + (h + 1) * 64], KZ.t[0:64, pr * 128:(pr + 1) * 128], vn[:, h, :],
                        True, True, [KZ, VN], [p1])
                self.stt(Sz.t[off:off + 64, h, :], Sz.t[off:off + 64, h, :], cdrep[off:off + 64, c, h:h + 1],
                         p1.t[off:off + 64, 256 + h * 64:256 + (h + 1) * 64], ALU.mult, ALU.add, [Sz, A, p1], [Sz])
            o2 = v4(ZS, 256)
            ss = A.t[0:64, 328:332]
            self.tt(o2, oq, oq, ALU.mult, [VN], [ZS])
            S.op("dve", lambda e, o2=o2, ss=ss: e.tensor_reduce(out=ss, in_=o2, axis=AX.X, op=ALU.add), [ZS], [A])
            self.act(ss, ss, AF.Sqrt, [A, cst], [A], scale=1.0 / 64, bias=cst.t[0:64, 0:1])
            S.op("dve", lambda e, ss=ss: e.reciprocal(ss, ss), [A], [A])
            self.tt(o2, oq, ss.unsqueeze(2).broadcast_to([64, 4, 64]), ALU.mult, [VN, A], [ZS])
            self.tt(o2, o2, b3(rows[:, 8:72]), ALU.mult, [ZS, self.gdrow], [ZS])
            self.tt(ZS.t[0:64, 256:512], ZS.t[0:64, 256:512], KZ.t[0:64, 256:512], ALU.mult, [ZS, KZ], [ZS])
            p0 = self.P[7]
            for kc in range(2):
                self.mm(p0.t[:, 256 + kc * 64:256 + (kc + 1) * 64], ZS.t[0:64, 256 + kc * 128:256 + (kc + 1) * 128], id64, True, True,
                        [ZS, consts], [p0])
                self.cp(y.t[:, 0, kc, cs], p0.t[:, 256 + kc * 64:256 + (kc + 1) * 64], [p0], [y], eng="act")

        def record(fn, c):
            S.rec = []
            fn(c)
            items, S.rec = S.rec, None
            return items

        def merge2(a, b):
            out, i, j, na, nb = [], 0, 0, len(a), len(b)
            while i < na or j < nb:
                if j >= nb or (i < na and i * nb <= j * na):
                    out.append(a[i])
                    i += 1
                else:
                    out.append(b[j])
                    j += 1
            return out

        S.replay(record(prepN, 0))
        for c in range(8):
            B_ = record(rec, c)
            A_ = record(prepN, c + 1) if c < 7 else []
            S.replay(merge2(A_, B_))

    def mixers(self, l, s, tb):
        only = self.dbg.get("only", "")
        sp = self.tokproj_small(l)
        if not only or "gdn" in only:
            self.gdn_fwd(l, s, tb, sp)
        if not only or "ml" in only:
            self.mlstm_fwd(l, s, tb, sp)
        if not only or "s5" in only:
            self.s5_fwd(l, s, tb)
        if not only or "moba" in only:
            self.moba_fwd(l, s, tb)

    def build(self):
        S = self.S
        self.declare()
        self.setup()
        units = self.dbg.get("units", [(s, tb) for s in range(2) for tb in range(SEQ // NT)])
        stop = self.dbg.get("stop", None)
        for (s, tb) in units:
            t0 = tb * NT
            S.dma("sp", self.h.t[:], self.D["xT"].t[s, :, :, t0:t0 + NT], [], [self.h], self.h)
            first = (s, tb) == units[0]
            for l in range(NL):
                self.ffn(l, 0)
                if stop == "ffn1":
                    break
                if first and l == 0:
                    for l2 in range(NL):
                        self.s5_setup(l2)
                self.norm(l * 4 + 1)
                if "yin" in self.dbg:
                    S.dma("sp", self.y.t[:], self.D["yin"].t[s * 4 + tb], [], [self.y], self.y)
                else:
                    self.mixers(l, s, tb)
                if "dumpy" in self.dbg and l == 0:
                    S.dma("sp", self.dumpy.t[s * 4 + tb], self.y.t[:], [self.y], [self.dumpy], self.y)
                if stop == "y":
                    break
                self.merge(l)
                self.ffn(l, 1)
                self.ple(l, s, t0)
            if stop is not None:
                self.dump_h(s * 4 + tb)
            self.final(s, t0)
        S.finish()
        S.emit_all()


def F32v(k, name, hh):
    ri = 0 if name.endswith("re") else 1
    t = k.s5B["BT" if name.startswith("BT") else "CT"]
    return t[:, 4 * hh:4 * hh + 4, ri, :]


def wt(w, K):
    Kd, N = w.shape
    assert Kd == K * 128 and N % 128 == 0
    a = w.reshape(K, 128, N // 128, 128).transpose(2, 1, 0, 3)
    return np.ascontiguousarray(a).reshape(N // 128 * 128, K * 128)


def fm(a, kc):
    T = a.shape[0]
    return np.ascontiguousarray(a.T.reshape(kc, 128, T).transpose(1, 0, 2))


def prep_shared(inp):
    f = lambda n: np.asarray(inp[n], dtype=np.float32)
    sh = {}
    wgu = []
    wdn = []
    for l in range(NL):
        for nm_gu, nm_d in (("ffn1_w_gu", "ffn1_w_down"), ("ffn2_w_gu", "ffn2_w_down")):
            w = f(nm_gu)[l]
            g = wt(w[:, :2816], 8).reshape(22, 128, 1024)
            u = wt(w[:, 2816:], 8).reshape(22, 128, 1024)
            wgu.append(np.stack([g, u], axis=1).reshape(44 * 128, 1024))
            wdn.append(wt(f(nm_d)[l], 22))
    sh["wgu"] = np.concatenate(wgu, 0)
    sh["wdn"] = np.concatenate(wdn, 0)
    swap = np.concatenate([(np.arange(64) + 32) % 64 + 64 * hh for hh in range(4)])
    cols = np.concatenate([np.arange(0, 1024), np.arange(1032, 2056), np.arange(2064, 3088),
                           2320 + swap, 2576 + swap])
    sh["win"] = np.concatenate([wt(f("w_in")[l][:, cols], 8) for l in range(NL)], 0)
    sh["wgate"] = np.concatenate([
        np.stack([wt(f("w_gate")[l, b], 8).reshape(8, 128, 1024) for b in range(4)], 1).reshape(8 * 4 * 128, 1024)
        for l in range(NL)], 0)
    sh["wbr"] = np.concatenate([
        np.stack([wt(f("w_branch")[l, b], 2).reshape(8, 128, 256) for b in range(4)], 1).reshape(8 * 4 * 128, 256)
        for l in range(NL)], 0)
    sh["wout"] = np.concatenate([wt(f("w_out")[l], 8) for l in range(NL)], 0)
    sh["wplg"] = np.concatenate([wt(f("ple_w_gate")[l], 8) for l in range(NL)], 0)
    sh["wplp"] = np.concatenate([wt(f("ple_w_proj")[l], 2) for l in range(NL)], 0)
    gains = np.zeros((9, 1024), np.float32)
    for l in range(NL):
        gains[l * 4 + 0] = f("ffn1_norm")[l]
        gains[l * 4 + 1] = f("mix_norm")[l]
        gains[l * 4 + 2] = f("ffn2_norm")[l]
        gains[l * 4 + 3] = f("ple_norm")[l]
    gains[8] = f("final_norm")
    sh["gains"] = np.ascontiguousarray(gains.reshape(9, 8, 128).transpose(2, 0, 1))
    sh["wglu"] = np.concatenate([wt(f("s5_w_glu")[l], 2) for l in range(NL)], 0)
    consts = np.zeros((128, 512), np.float32)
    consts[:, 0:128] = np.arange(128, dtype=np.float32)[None, :]
    consts[:, 128:256] = np.eye(128, dtype=np.float32)
    consts[:, 384:512] = 1.0
    for qb in range(8):
        for n in range(8):
            consts[:, 256 + qb * 8 + n] = 0.0 if n < qb else -1e9
    pc = np.zeros((128, 8), np.float32)
    invf = (10000.0 ** (-np.arange(0, 64, 2, dtype=np.float32) / 64)).astype(np.float32)
    for p_ in range(128):
        pc[p_, 0] = invf[p_ % 32]
        pc[p_, 1] = -1.0 if (p_ % 64) < 32 else 1.0
        pc[p_, 2] = pc[p_, 1] * 2 * np.pi
    sh["pc"] = pc
    c2 = np.zeros((128, 512), np.float32)
    ii = np.arange(64)
    c2[0:64, 0:64] = (ii[:, None] <= ii[None, :]).astype(np.float32)
    c2[0:64, 64:128] = np.where(ii[None, :] <= ii[:, None], 0.0, -60000.0)
    c2[0:64, 128:192] = (ii[None, :] < ii[:, None]).astype(np.float32)
    c2[0:64, 192:256] = (ii[None, :] <= ii[:, None]).astype(np.float32)
    c2[0:64, 256:320] = (ii[:, None] < ii[None, :]).astype(np.float32)
    c2[0:64, 320:384] = np.where(ii[:, None] <= ii[None, :], 0.0, -60000.0)
    sh["consts2"] = c2
    gmk = np.zeros((128, 256), np.float32)
    gmk[0:64, 0:64] = np.where(ii[None, :] < ii[:, None], 0.0, -60000.0)
    gmk[0:64, 64:128] = np.where(ii[:, None] < ii[None, :], 0.0, -60000.0)
    pp_ = np.arange(128)
    gmk[:, 128:256] = (pp_[:, None] // 64 == pp_[None, :] // 64).astype(np.float32)
    sh["gmaskf"] = gmk
    win = f("w_in")
    wsm = np.zeros((128, NL, 8, 16), np.float32)
    cwf = np.zeros((128, NL, 40), np.float32)
    mlrow = np.zeros((128, NL, 264), np.float32)
    gdrow = np.zeros((128, NL, 72), np.float32)
    for l in range(NL):
        small = np.concatenate([win[l][:, 1024:1032], win[l][:, 2056:2064]], 1)
        wsm[:, l] = small.reshape(8, 128, 16).transpose(1, 0, 2)
        gc = f("gdn_conv")[l]
        mc = f("mlstm_conv")[l]
        cwf[:, l, 0:24] = gc.T.reshape(6, 128, 4).transpose(1, 0, 2).reshape(128, 24)
        cwf[:, l, 24:40] = mc.T.reshape(4, 128, 4).transpose(1, 0, 2).reshape(128, 16)
        mlrow[:, l, 0:4] = f("mlstm_i_bias")[l][None, :]
        mlrow[:, l, 4:8] = f("mlstm_f_bias")[l][None, :]
        mlrow[:, l, 8:264] = f("mlstm_norm")[l][None, :]
        gdrow[:, l, 0:4] = f("gdn_a_log")[l][None, :]
        gdrow[:, l, 4:8] = f("gdn_dt_bias")[l][None, :]
        gdrow[:, l, 8:72] = f("gdn_norm")[l][None, :]
    sh["wsmf"], sh["cwf"], sh["mlrowf"], sh["gdrowf"] = wsm, cwf, mlrow, gdrow
    cmf = np.zeros((128, 2, 256), np.float32)
    for a in range(2):
        kl = a * 128 + np.arange(128)[:, None]
        cmf[:, a, :] = np.where(kl > np.arange(256)[None, :], -30000.0, 0.0)
    sh["cmf"] = cmf
    sh["consts"] = consts
    lre, lim, ldt = f("s5_lambda_re"), f("s5_lambda_im"), f("s5_log_dt")
    s5st = np.zeros((NL, 128, 8, 3), np.float32)
    s5rep = np.zeros((NL, 3, 128, 8, 128), np.float32)
    s5bexp = np.zeros((NL, 2, 128, 8, 128), np.float32)
    s5cexp = np.zeros((NL, 2, 128, 8, 128), np.float32)
    bre, bim, cre, cim = f("s5_b_re"), f("s5_b_im"), f("s5_c_re"), f("s5_c_im")
    for l in range(NL):
        for sc in range(8):
            for half in range(2):
                g = 2 * sc + half
                ps = slice(half * 64, half * 64 + 64)
                s5st[l, ps, sc, 0] = lre[l, g]
                s5st[l, ps, sc, 1] = lim[l, g]
                s5st[l, ps, sc, 2] = ldt[l, g]
                s5rep[l, 0, :, sc, ps] = lre[l, g][None, :]
                s5rep[l, 1, :, sc, ps] = lim[l, g][None, :]
                s5rep[l, 2, :, sc, ps] = ldt[l, g]
                r0 = (sc % 4) * 32 + half * 16
                s5bexp[l, 0, r0:r0 + 16, sc, ps] = bre[l, g].T
                s5bexp[l, 1, r0:r0 + 16, sc, ps] = bim[l, g].T
                s5cexp[l, 0, ps, sc, r0:r0 + 16] = cre[l, g].T
                s5cexp[l, 1, ps, sc, r0:r0 + 16] = cim[l, g].T
    sh["s5st"], sh["s5rep"], sh["s5bexp"], sh["s5cexp"] = s5st, s5rep, s5bexp, s5cexp
    s5db = np.zeros((NL, 128, 4), np.float32)
    for l in range(NL):
        s5db[l, :, 0:2] = f("s5_d")[l].reshape(2, 128).T
        s5db[l, :, 2:4] = f("s5_b_glu")[l].reshape(2, 128).T
    sh["s5db"] = s5db
    return sh


def prep_core(inp, c):
    x = np.asarray(inp["x"], dtype=np.float32)
    p = np.asarray(inp["p"], dtype=np.float32)
    m = {}
    m["xT"] = np.stack([fm(x[2 * c + s], 8) for s in range(2)], 0)
    m["pT"] = np.stack([np.stack([fm(p[l, 2 * c + s], 2) for s in range(2)], 0) for l in range(NL)], 0)
    pos = np.asarray(inp["positions"]).astype(np.int32)
    m["posr"] = np.ascontiguousarray(np.broadcast_to(pos[2 * c:2 * c + 2, None, :], (2, 128, SEQ)))
    return m


def build_nc(dbg=None):
    nc = bass.Bass("TRN2", target_bir_lowering=False)
    with ExitStack() as st:
        k = Kern(nc, st, dbg)
        k.build()
    return nc


def kernel(**inputs):
    sh = prep_shared(inputs)
    nc = build_nc()
    in_maps = []
    for c in range(8):
        m = dict(sh)
        m.update(prep_core(inputs, c))
        in_maps.append(m)
    res = run_bass_kernel_spmd(nc, in_maps, core_ids=list(range(8)))
    out = np.zeros((16, SEQ, 1024), np.float32)
    for c in range(8):
        o = res.results[c]["out"]
        for s in range(2):
            out[2 * c + s] = o[s].transpose(2, 1, 0).reshape(SEQ, 1024)
    return out
```

```python
import numpy as np
from contextlib import ExitStack
import concourse.bass as bass
import concourse.mybir as mybir
from concourse.bass_utils import run_bass_kernel_spmd

F32 = mybir.dt.float32
BF16 = mybir.dt.bfloat16
I32 = mybir.dt.int32
AF = mybir.ActivationFunctionType
ALU = mybir.AluOpType
AX = mybir.AxisListType

ENGS = ["pe", "act", "dve", "pool", "sp"]
NT = 512
NL = 2
SEQ = 2048
PI = float(np.pi)


class Buf:
    __slots__ = ("name", "w", "r", "sem", "semcnt", "t")

    def __init__(self, name, t=None):
        self.name = name
        self.w = None
        self.r = {}
        self.sem = None
        self.semcnt = 0
        self.t = t


class Tmp:
    __slots__ = ("t", "bufs")

    def __init__(self, t, bufs):
        self.t = t
        self.bufs = bufs


def flat(lst):
    out = []
    for b in lst:
        if isinstance(b, Tmp):
            out.extend(b.bufs)
        else:
            out.append(b)
    return out


class Sched:
    def __init__(self, nc, stack):
        self.nc = nc
        self.stack = stack
        self.q = {e: [] for e in ENGS}
        self.cnt = {e: 0 for e in ENGS}
        self.sems = {}
        for e in ENGS:
            self.sems[e] = stack.enter_context(nc.semaphore("s_" + e))
        self.seen = {e: {} for e in ENGS}
        self.dmabufs = []
        self.rec = None

    def sb(self, name, shape, dt=F32):
        return Buf(name, self.stack.enter_context(self.nc.sbuf_tensor("sb_" + name, list(shape), dt)))

    def ps(self, name, shape, dt=F32):
        return Buf(name, self.stack.enter_context(self.nc.psum_tensor("ps_" + name, list(shape), dt)))

    def _waits(self, e, reads, writes):
        need = {}

        def add(k, v, src):
            if src == "pe" and e == "pe":
                return
            if v > need.get(k, 0):
                need[k] = v
        for b in reads:
            if b.w is not None:
                add(*b.w)
        for b in writes:
            if b.w is not None:
                add(*b.w)
            for k, (v, src) in b.r.items():
                add(k, v, src)
        out = []
        seen = self.seen[e]
        for k, v in need.items():
            if seen.get(k, 0) < v:
                seen[k] = v
                out.append((self.sems[k], v))
        return out

    def _record(self, dep, reads, writes):
        k, v, src = dep
        for b in reads:
            old = b.r.get(k)
            if old is None or old[0] < v:
                b.r[k] = (v, src)
        for b in writes:
            b.w = dep
            b.r = {}

    def replay(self, items):
        for it in items:
            if it[0] == "op":
                self.op(*it[1:])
            else:
                self.dma(*it[1:])

    def op(self, e, fn, reads=(), writes=()):
        if self.rec is not None:
            self.rec.append(("op", e, fn, reads, writes))
            return
        reads, writes = flat(reads), flat(writes)
        waits = self._waits(e, reads, writes)
        self.cnt[e] += 1
        n = self.cnt[e]
        sem = self.sems[e]

        def emit(engine, fn=fn, waits=waits, sem=sem):
            for s, v in waits:
                engine.wait_ge(s, v)
            fn(engine).then_inc(sem, 1)
        self.q[e].append(emit)
        self._record((e, n, e), reads, writes)

    def dma(self, qe, out, in_, reads, writes, sembuf):
        if self.rec is not None:
            self.rec.append(("dma", qe, out, in_, reads, writes, sembuf))
            return
        reads, writes = flat(reads), flat(writes)
        waits = self._waits(qe, reads, writes)
        if isinstance(sembuf, Tmp):
            sembuf = sembuf.bufs[0]
        if sembuf.sem is None:
            key = "d%d" % len(self.dmabufs)
            sembuf.sem = key
            self.sems[key] = self.stack.enter_context(self.nc.semaphore(key))
            self.dmabufs.append(sembuf)
        sembuf.semcnt += 16
        v = sembuf.semcnt
        sem = self.sems[sembuf.sem]

        def emit(engine, waits=waits, sem=sem, out=out, in_=in_):
            for s, vv in waits:
                engine.wait_ge(s, vv)
            engine.dma_start(out=out, in_=in_).then_inc(sem, 16)
        self.q[qe].append(emit)
        self._record((sembuf.sem, v, "dma"), reads, writes)

    def finish(self):
        waits = []
        for e in ENGS:
            if e != "sp" and self.cnt[e] > 0:
                waits.append((self.sems[e], self.cnt[e]))
        for b in self.dmabufs:
            waits.append((self.sems[b.sem], b.semcnt))

        def emit(engine, waits=waits):
            for s, v in waits:
                engine.wait_ge(s, v)
        self.q["sp"].append(emit)

    def emit_all(self):
        with self.nc.Block() as block:
            @block.tensor
            def _(eng):
                for f in self.q["pe"]:
                    f(eng)

            @block.scalar
            def _(eng):
                for f in self.q["act"]:
                    f(eng)

            @block.vector
            def _(eng):
                for f in self.q["dve"]:
                    f(eng)

            @block.gpsimd
            def _(eng):
                for f in self.q["pool"]:
                    f(eng)

            @block.sync
            def _(eng):
                for f in self.q["sp"]:
                    f(eng)


BIGW = {
    "wgu": (NL * 2 * 44 * 128, 8 * 128),
    "wdn": (NL * 2 * 8 * 128, 22 * 128),
    "win": (NL * 28 * 128, 8 * 128),
    "wgate": (NL * 8 * 4 * 128, 8 * 128),
    "wbr": (NL * 8 * 4 * 128, 2 * 128),
    "wout": (NL * 8 * 128, 8 * 128),
    "wplg": (NL * 8 * 128, 8 * 128),
    "wplp": (NL * 8 * 128, 2 * 128),
    "wglu": (NL * 2 * 128, 2 * 128),
}


class Kern:
    def __init__(self, nc, st, dbg=None):
        self.nc = nc
        self.dbg = dbg or {}
        self.S = Sched(nc, st)
        self.wi8 = 0
        self.wi22 = 0
        self.wbg = {}

    def record(self, fn, c):
        self.S.rec = []
        fn(c)
        items, self.S.rec = self.S.rec, None
        return items

    @staticmethod
    def merge2(a, b):
        out, i, j, na, nb = [], 0, 0, len(a), len(b)
        while i < na or j < nb:
            if j >= nb or (i < na and i * nb <= j * na):
                out.append(a[i])
                i += 1
            else:
                out.append(b[j])
                j += 1
        return out

    def mm(self, out, lhsT, rhs, start, stop, R, W):
        self.S.op("pe", lambda e: e.matmul(out, lhsT, rhs, start=start, stop=stop), R, W)

    def act(self, out, in_, func, R, W, **kw):
        self.S.op("act", lambda e: e.activation(out=out, in_=in_, func=func, **kw), R, W)

    def tt(self, out, a, b, op, R, W, eng="dve"):
        self.S.op(eng, lambda e: e.tensor_tensor(out=out, in0=a, in1=b, op=op), R, W)

    def ts(self, out, a, s1, op0, R, W, s2=None, op1=None, eng="dve"):
        if op1 is None:
            self.S.op(eng, lambda e: e.tensor_scalar(out=out, in0=a, scalar1=s1, scalar2=None, op0=op0), R, W)
        else:
            self.S.op(eng, lambda e: e.tensor_scalar(out=out, in0=a, scalar1=s1, scalar2=s2, op0=op0, op1=op1), R, W)

    def stt(self, out, a, s, b, op0, op1, R, W):
        self.S.op("dve", lambda e: e.scalar_tensor_tensor(out=out, in0=a, scalar=s, in1=b, op0=op0, op1=op1), R, W)

    def cp(self, out, in_, R, W, eng="pool"):
        if eng == "act":
            self.S.op("act", lambda e: e.copy(out=out, in_=in_), R, W)
        else:
            self.S.op(eng, lambda e: e.tensor_copy(out, in_), R, W)

    def wgroup(self, name, l, which=0):
        per = {"wgu": 44, "wdn": 8, "win": 28, "wgate": 32, "wbr": 32, "wout": 8, "wplg": 8, "wplp": 8, "wglu": 2}[name]
        idx = (l * 2 + which) if name in ("wgu", "wdn") else l
        key = (name, idx)
        if key not in self.wbg:
            self.wbg[key] = Buf("%s_b%d" % (name, idx), self.wb[name].t)
        return idx * per * 128, (idx + 1) * per * 128, self.wbg[key]

    def convert(self, groups):
        S = self.S
        for g in groups:
            name = g[0]
            r0g, r1g, dstb = self.wgroup(*g)
            c = BIGW[name][1]
            nn = min(max(1, 4096 // c), (r1g - r0g) // 128)
            src = self.D[name]
            for r0 in range(r0g, r1g, nn * 128):
                stg = self.cstg[self.conv_i % 2]
                self.conv_i += 1
                S.dma("pool", stg.t[:, 0:nn * c].rearrange("p (n c) -> p n c", c=c),
                      src.t[r0:r0 + nn * 128, :].rearrange("(n p) c -> p n c", p=128), [], [stg], stg)
                if self.conv_pending is not None:
                    self.conv_pending()
                self.conv_pending = (lambda stg=stg, dstb=dstb, r0=r0, nn=nn, c=c: S.dma(
                    "pool", dstb.t[r0:r0 + nn * 128, :].rearrange("(n p) c -> p n c", p=128),
                    stg.t[:, 0:nn * c].rearrange("p (n c) -> p n c", c=c), [stg], [dstb], dstb))
        if self.conv_pending is not None:
            self.conv_pending()
            self.conv_pending = None

    def loadw(self, name, tile, K):
        per = {"wgu": 44, "wdn": 8, "win": 28, "wgate": 32, "wbr": 32, "wout": 8, "wplg": 8, "wplp": 8, "wglu": 2}[name]
        src = self.wbg[(name, tile // per)]
        ap = src.t[tile * 128:(tile + 1) * 128, :].rearrange("p (k c) -> p k c", c=128)
        if K > 8:
            b = self.w22[self.wi22 % len(self.w22)]
            self.wi22 += 1
        else:
            b = self.w8[self.wi8 % len(self.w8)]
            self.wi8 += 1
        self.S.dma("sp", b.t[:, 0:K, :], ap, [src], [b], b)
        return b

    def tmp(self, slot0, nslots, shape, dt=F32):
        ap = self.scr_t[:, slot0 * 512:(slot0 + nslots) * 512]
        if dt == BF16:
            ap = ap.bitcast(BF16)
        n = 1
        for d in shape[1:]:
            n *= d
        ap = ap[:, 0:n]
        if len(shape) == 3:
            ap = ap.rearrange("p (a b) -> p a b", b=shape[2])
        elif len(shape) == 4:
            ap = ap.rearrange("p (a b c) -> p a b c", b=shape[2], c=shape[3])
        return Tmp(ap, self.slots[slot0:slot0 + nslots])

    def declare(self):
        nc, S = self.nc, self.S
        D = {}

        def din(name, shape, dt=F32):
            D[name] = Buf(name, nc.dram_tensor(name, list(shape), dt, kind="ExternalInput").ap())
            return D[name]
        self.D = D
        din("xT", [2, 128, 8, SEQ])
        din("pT", [NL, 2, 128, 2, SEQ])
        din("gains", [128, 9, 8])
        din("consts", [128, 512])
        din("s5st", [NL, 128, 8, 3])
        din("s5rep", [NL, 3, 128, 8, 128])
        din("s5bexp", [NL, 2, 128, 8, 128])
        din("s5cexp", [NL, 2, 128, 8, 128])
        din("s5db", [NL, 128, 4])
        din("posr", [2, 128, SEQ], I32)
        din("consts2", [128, 512])
        din("wsmf", [128, NL, 8, 16])
        din("cwf", [128, NL, 40])
        din("mlrowf", [128, NL, 264])
        din("gdrowf", [128, NL, 72])
        din("gmaskf", [128, 256])
        din("pc", [128, 8])
        din("cmf", [128, 2, 256])
        for n, (r, c) in BIGW.items():
            din(n, [r, c])
        self.out = Buf("out", nc.dram_tensor("out", [2, 128, 8, SEQ], F32, kind="ExternalOutput").ap())
        self.wb = {}
        for n, (r, c) in BIGW.items():
            self.wb[n] = Buf(n + "_b", nc.dram_tensor(n + "_b", [r, c], BF16, kind="Internal").ap())
        self.s5f_d = Buf("s5f_d", nc.dram_tensor("s5f_d", [NL, 128, 3104], F32, kind="Internal").ap())
        self.s5b_d = Buf("s5b_d", nc.dram_tensor("s5b_d", [NL, 128, 4352], BF16, kind="Internal").ap())
        if "yin" in self.dbg:
            din("yin", [8, 128, 4, 2, NT], BF16)
        if "dumpy" in self.dbg:
            self.dumpy = Buf("dumpy", nc.dram_tensor("dumpy", [8, 128, 4, 2, NT], BF16, kind="ExternalOutput").ap())
        if "dump" in self.dbg:
            self.dump = Buf("dump", nc.dram_tensor("dump", self.dbg["dump"], F32, kind="ExternalOutput").ap())

        self.h = S.sb("h", [128, 8, NT], F32)
        self.u = S.sb("u", [128, 8, NT], BF16)
        NS = 24
        self.scr_t = S.sb("scr", [128, NS * 512], F32).t
        self.slots = [Buf("slot%d" % i) for i in range(NS)]
        self.actb = self.tmp(0, 11, [128, 22, NT], BF16)
        self.xs = [Tmp(S.sb("xs%d" % i, [128, 512], F32).t, [Buf("xslot%d" % i)]) for i in range(4)]
        self.rstd = S.sb("rstd", [128, NT], F32)
        self.sg = [S.sb("sg%d" % i, [128, NT], F32) for i in range(2)]
        self.sg2 = [S.sb("sg2%d" % i, [128, NT], F32) for i in range(2)]
        self.macc = self.tmp(12, 1, [128, NT])
        self.ho = [self.tmp(13 + i, 1, [128, NT]) for i in range(2)]
        self.w8 = [S.sb("w8_%d" % i, [128, 8, 128], BF16) for i in range(6)]
        self.w22 = [S.sb("w22_%d" % i, [128, 22, 128], BF16) for i in range(2)]
        self.y = S.sb("y", [128, 4, 2, NT], BF16)
        self.pf = self.tmp(15, 2, [128, 2, NT])
        self.pb = self.tmp(17, 1, [128, 2, NT], BF16)
        self.gains = S.sb("gains", [128, 9, 8], F32)
        self.cst = S.sb("cst", [128, 8], F32)
        self.consts = S.sb("consts", [128, 512], F32)
        self.pc = S.sb("pc", [128, 8], F32)
        self.consts2 = S.sb("consts2", [128, 512], F32)
        self.wsm = S.sb("wsm", [128, NL, 8, 16], BF16)
        self.cwb = S.sb("cwb", [128, NL, 40], F32)
        self.mlrow = S.sb("mlrow", [128, NL, 264], F32)
        self.gdrow = S.sb("gdrow", [128, NL, 72], F32)
        self.mltail = [S.sb("mltail%d" % l, [128, 4, 3], F32) for l in range(NL)]
        self.gdtail = [S.sb("gdtail%d" % l, [128, 6, 3], F32) for l in range(NL)]
        self.mlC = [S.sb("mlC%d" % l, [128, 4, 66], F32) for l in range(NL)]
        self.mlm = [S.sb("mlm%d" % l, [128, 4], F32) for l in range(NL)]
        self.gdS = [S.sb("gdS%d" % l, [128, 4, 64], F32) for l in range(NL)]
        self.gmask = S.sb("gmask", [128, 256], F32)
        self.negones = S.sb("negones", [128, 64], F32)
        self.cm = S.sb("cm", [128, 2, 256], BF16)
        self.ident_bf = S.sb("ident_bf", [128, 128], BF16)
        self.kcache = [S.sb("kcache%d" % l, [128, 2, SEQ], BF16) for l in range(NL)]
        self.vcache = [S.sb("vcache%d" % l, [128, 16, 256], BF16) for l in range(NL)]
        self.kmean = [S.sb("kmean%d" % l, [128, 2, 8], F32) for l in range(NL)]
        self.s5f = S.sb("s5f", [128, 3104], F32)
        self.s5b = S.sb("s5b", [128, 4352], BF16)
        f = self.s5f.t
        self.s5F = {"CS": f[:, 0:2048].rearrange("p (a b c) -> p a b c", b=2, c=128),
                    "R": f[:, 2048:3072].rearrange("p (a b) -> p a b", b=128),
                    "E128": f[:, 3072:3088].rearrange("p (a b) -> p a b", b=2),
                    "rcol": f[:, 3088:3096], "dbg": f[:, 3096:3100]}
        b = self.s5b.t
        self.s5B = {"BT": b[:, 0:2048].rearrange("p (a b c) -> p a b c", b=2, c=128),
                    "CT": b[:, 2048:4096].rearrange("p (a b c) -> p a b c", b=2, c=128),
                    "Dg": b[:, 4096:4352].rearrange("p (a b) -> p a b", b=128)}
        self.s5state = [S.sb("s5st%d" % l, [128, 2, 8], F32) for l in range(NL)]
        self.ones_bf = S.sb("ones_bf", [128, 128], BF16)
        self.P = [S.ps("P%d" % i, [128, NT], F32) for i in range(8)]
        self.cstg = [S.sb("cstg%d" % i, [128, 4096], BF16) for i in range(2)]

    def setup(self):
        S = self.S
        S.op("pool", lambda e: e.memset(self.ones_bf.t[:], 1.0), [], [self.ones_bf])
        S.op("pool", lambda e: e.memset(self.cst.t[:, 0:1], 1e-6), [], [self.cst])
        S.op("pool", lambda e: e.memset(self.cst.t[:, 1:2], -PI), [], [self.cst])
        S.dma("sp", self.pc.t[:], self.D["pc"].t, [], [self.pc], self.pc)
        S.op("pool", lambda e: e.memset(self.cst.t[:, 3:4], 1.0), [], [self.cst])
        S.dma("sp", self.consts2.t[:], self.D["consts2"].t, [], [self.consts2], self.consts2)
        S.dma("sp", self.cwb.t[:], self.D["cwf"].t, [], [self.cwb], self.cwb)
        S.dma("sp", self.mlrow.t[:], self.D["mlrowf"].t, [], [self.mlrow], self.mlrow)
        S.dma("sp", self.gdrow.t[:], self.D["gdrowf"].t, [], [self.gdrow], self.gdrow)
        S.dma("sp", self.gmask.t[:], self.D["gmaskf"].t, [], [self.gmask], self.gmask)
        S.op("pool", lambda e: e.memset(self.negones.t[:], -1.0), [], [self.negones])
        wsf = self.tmp(13, 1, [128, NL, 8, 16])
        S.dma("sp", wsf.t, self.D["wsmf"].t, [], [wsf], wsf)
        self.cp(self.wsm.t[:], wsf.t, [wsf], [self.wsm], eng="dve")
        cmf = self.tmp(12, 1, [128, 2, 256])
        S.dma("sp", cmf.t, self.D["cmf"].t, [], [cmf], cmf)
        self.cp(self.cm.t[:], cmf.t, [cmf], [self.cm], eng="dve")
        for l in range(NL):
            S.op("pool", lambda e, l=l: e.memset(self.kmean[l].t[:], 0.0), [], [self.kmean[l]])
        S.dma("sp", self.gains.t[:], self.D["gains"].t, [], [self.gains], self.gains)
        S.dma("sp", self.consts.t[:], self.D["consts"].t, [], [self.consts], self.consts)
        self.cp(self.ident_bf.t[:], self.consts.t[:, 128:256], [self.consts], [self.ident_bf], eng="dve")
        self.conv_i = 0
        self.conv_pending = None
        self.convert([("wgu", 0, 0), ("wdn", 0, 0), ("win", 0), ("wglu", 0), ("wgate", 0), ("wbr", 0), ("wout", 0),
                      ("wgu", 0, 1), ("wdn", 0, 1), ("wplg", 0), ("wplp", 0),
                      ("wgu", 1, 0), ("wdn", 1, 0), ("win", 1), ("wglu", 1), ("wgate", 1), ("wbr", 1), ("wout", 1),
                      ("wgu", 1, 1), ("wdn", 1, 1), ("wplg", 1), ("wplp", 1)])

    def norm(self, gcol):
        h, u, S = self.h, self.u, self.S
        pss, rstd = self.P[6], self.rstd
        self.act(u.t[:], h.t[:], AF.Square, [h], [u])
        for k in range(8):
            self.mm(pss.t[:], self.ones_bf.t[:], u.t[:, k, :], k == 0, k == 7, [u, self.ones_bf], [pss])
        self.act(rstd.t[:], pss.t[:], AF.Sqrt, [pss, self.cst], [rstd], scale=1.0 / 1024, bias=self.cst.t[:, 0:1])
        S.op("dve", lambda e: e.reciprocal(rstd.t[:], rstd.t[:]), [rstd], [rstd])
        for k in range(8):
            self.stt(u.t[:, k, :], h.t[:, k, :], self.gains.t[:, gcol, k:k + 1], rstd.t[:], ALU.mult, ALU.mult,
                     [h, rstd, self.gains], [u])

    def ffn(self, l, which):
        h, u, actb = self.h, self.u, self.actb
        self.norm(l * 4 + (0 if which == 0 else 2))
        base = (l * 2 + which) * 44
        for fc in range(22):
            wg = self.loadw("wgu", base + 2 * fc, 8)
            wu = self.loadw("wgu", base + 2 * fc + 1, 8)
            pg, pu = self.P[fc % 2], self.P[2 + fc % 2]
            sg = self.sg[fc % 2]
            for k in range(8):
                self.mm(pg.t[:], wg.t[:, k, :], u.t[:, k, :], k == 0, k == 7, [wg, u], [pg])
            for k in range(8):
                self.mm(pu.t[:], wu.t[:, k, :], u.t[:, k, :], k == 0, k == 7, [wu, u], [pu])
            self.act(sg.t[:], pg.t[:], AF.Silu, [pg], [sg])
            self.tt(actb.t[:, fc, :], sg.t[:], pu.t[:], ALU.mult, [sg, pu], [actb])
        base = (l * 2 + which) * 8
        for oc in range(8):
            wd = self.loadw("wdn", base + oc, 22)
            po = self.P[4 + oc % 2]
            for fc in range(22):
                self.mm(po.t[:], wd.t[:, fc, :], actb.t[:, fc, :], fc == 0, fc == 21, [wd, actb], [po])
            self.stt(h.t[:, oc, :], po.t[:], 0.5, h.t[:, oc, :], ALU.mult, ALU.add, [po, h], [h])

    def ple(self, l, s, t0):
        h, u, S = self.h, self.u, self.S
        self.norm(l * 4 + 3)
        S.dma("sp", self.pf.t[:], self.D["pT"].t[l, s, :, :, t0:t0 + NT], [], [self.pf], self.pf)
        self.cp(self.pb.t[:], self.pf.t[:], [self.pf], [self.pb], eng="pool")
        for oc in range(8):
            wg = self.loadw("wplg", l * 8 + oc, 8)
            wp = self.loadw("wplp", l * 8 + oc, 2)
            pa, pg = self.P[oc % 2], self.P[2 + oc % 2]
            sg, sg2 = self.sg[oc % 2], self.sg2[oc % 2]
            for k in range(2):
                self.mm(pa.t[:], wp.t[:, k, :], self.pb.t[:, k, :], k == 0, k == 1, [wp, self.pb], [pa])
            for k in range(8):
                self.mm(pg.t[:], wg.t[:, k, :], u.t[:, k, :], k == 0, k == 7, [wg, u], [pg])
            self.act(sg.t[:], pg.t[:], AF.Sigmoid, [pg], [sg])
            self.tt(sg2.t[:], sg.t[:], pa.t[:], ALU.mult, [sg, pa], [sg2])
            self.tt(h.t[:, oc, :], h.t[:, oc, :], sg2.t[:], ALU.add, [h, sg2], [h], eng="pool")

    def merge(self, l):
        h, u, y, actb = self.h, self.u, self.y, self.actb
        macc = self.macc
        for oc in range(8):
            for b in range(4):
                wg = self.loadw("wgate", (l * 8 + oc) * 4 + b, 8)
                wbr = self.loadw("wbr", (l * 8 + oc) * 4 + b, 2)
                pg, pbr = self.P[b % 2], self.P[2 + b % 2]
                sg, sg2 = self.sg[b % 2], self.sg2[b % 2]
                for k in range(8):
                    self.mm(pg.t[:], wg.t[:, k, :], u.t[:, k, :], k == 0, k == 7, [wg, u], [pg])
                for k in range(2):
                    self.mm(pbr.t[:], wbr.t[:, k, :], y.t[:, b, k, :], k == 0, k == 1, [wbr, y], [pbr])
                self.act(sg.t[:], pg.t[:], AF.Sigmoid, [pg], [sg])
                if b == 0:
                    self.tt(macc.t[:], sg.t[:], pbr.t[:], ALU.mult, [sg, pbr], [macc])
                else:
                    self.tt(sg2.t[:], sg.t[:], pbr.t[:], ALU.mult, [sg, pbr], [sg2])
                    if b < 3:
                        self.tt(macc.t[:], macc.t[:], sg2.t[:], ALU.add, [macc, sg2], [macc], eng="pool")
                    else:
                        self.tt(actb.t[:, oc, :], macc.t[:], sg2.t[:], ALU.add, [macc, sg2], [actb], eng="pool")
        for oc in range(8):
            wo = self.loadw("wout", l * 8 + oc, 8)
            po = self.P[4 + oc % 2]
            for k in range(8):
                self.mm(po.t[:], wo.t[:, k, :], actb.t[:, k, :], k == 0, k == 7, [wo, actb], [po])
            self.tt(h.t[:, oc, :], h.t[:, oc, :], po.t[:], ALU.add, [h, po], [h])

    def final(self, s, t0):
        h, S = self.h, self.S
        pss, rstd = self.P[6], self.rstd
        u = self.u
        self.act(u.t[:], h.t[:], AF.Square, [h], [u])
        for k in range(8):
            self.mm(pss.t[:], self.ones_bf.t[:], u.t[:, k, :], k == 0, k == 7, [u, self.ones_bf], [pss])
        self.act(rstd.t[:], pss.t[:], AF.Sqrt, [pss, self.cst], [rstd], scale=1.0 / 1024, bias=self.cst.t[:, 0:1])
        S.op("dve", lambda e: e.reciprocal(rstd.t[:], rstd.t[:]), [rstd], [rstd])
        for k in range(8):
            ho = self.ho[k % 2]
            self.stt(ho.t[:], h.t[:, k, :], self.gains.t[:, 8, k:k + 1], rstd.t[:], ALU.mult, ALU.mult,
                     [h, rstd, self.gains], [ho])
            S.dma("sp", self.out.t[s, :, k, t0:t0 + NT], ho.t[:], [ho], [self.out], ho)

    def dump_h(self, idx):
        S = self.S
        S.dma("sp", self.dump.t[idx], self.h.t[:], [self.h], [self.dump], self.h)


    def dd(self, name, src, R, shape, dt=F32):
        if name not in self.dbg.get("dd", ()):
            return
        b = Buf(name, self.nc.dram_tensor("dd_" + name, list(shape), dt, kind="ExternalOutput").ap())
        self.S.dma("sp", b.t, src, R, [b], b)

    def frac2pi(self, out, x, shift, tB, R, W):
        MAG = 12582912.0
        self.ts(out, x, 1.0 / (2 * PI), ALU.mult, R, W, s2=shift / (2 * PI), op1=ALU.add)
        self.ts(tB, out, MAG, ALU.add, R, W)
        self.ts(tB, tB, -MAG, ALU.add, R, W)
        self.tt(out, out, tB, ALU.subtract, R, W)

    def s5_disc(self, lr, li, ldt, T, TB, want_z):
        R = TB + [self.s5in]
        W = TB
        self.act(T[0], ldt, AF.Exp, R, W)
        self.tt(T[5], lr, T[0], ALU.mult, R, W)
        self.act(T[1], T[5], AF.Exp, R, W)
        self.tt(T[2], li, T[0], ALU.mult, R, W)
        if not want_z:
            self.frac2pi(T[5], T[2], 0.0, T[8], R, W)
            self.ts(T[2], T[5], 2 * PI, ALU.mult, R, W)
            return T[1], T[2], None, None
        self.frac2pi(T[5], T[2], 0.0, T[8], R, W)
        self.act(T[3], T[5], AF.Sin, R, W, scale=2 * PI)
        self.frac2pi(T[5], T[2], 0.5 * PI, T[8], R, W)
        self.act(T[4], T[5], AF.Sin, R, W, scale=2 * PI)
        self.tt(T[4], T[4], T[1], ALU.mult, R, W)
        self.tt(T[3], T[3], T[1], ALU.mult, R, W)
        self.ts(T[5], T[4], -1.0, ALU.add, R, W)
        self.tt(T[8], lr, lr, ALU.mult, R, W)
        self.tt(T[0], li, li, ALU.mult, R, W)
        self.tt(T[8], T[8], T[0], ALU.add, R, W)
        self.S.op("dve", lambda e: e.reciprocal(T[8], T[8]), R, W)
        self.tt(T[0], T[5], lr, ALU.mult, R, W)
        self.tt(T[6], T[3], li, ALU.mult, R, W)
        self.tt(T[6], T[6], T[0], ALU.add, R, W)
        self.tt(T[6], T[6], T[8], ALU.mult, R, W)
        self.tt(T[0], T[3], lr, ALU.mult, R, W)
        self.tt(T[7], T[5], li, ALU.mult, R, W)
        self.tt(T[7], T[0], T[7], ALU.subtract, R, W)
        self.tt(T[7], T[7], T[8], ALU.mult, R, W)
        return T[1], T[2], T[6], T[7]

    def s5_setup(self, l):
        S, D = self.S, self.D
        s5f, s5b, cst = self.s5f, self.s5b, self.cst
        F = self.s5F
        stt_ = self.tmp(12, 1, [128, 8, 3])
        self.s5in = stt_.bufs[0]
        S.dma("sp", stt_.t, D["s5st"].t[l], [], [stt_], stt_)
        T9 = self.tmp(13, 1, [128, 9, 8])
        T = [T9.t[:, i, :] for i in range(9)]
        mag, th, _, _ = self.s5_disc(stt_.t[:, :, 0], stt_.t[:, :, 1], stt_.t[:, :, 2], T, [T9.bufs[0]], False)
        R9 = [T9]
        X = self.tmp(14, 2, [128, 8, 128])
        Y = self.tmp(16, 2, [128, 8, 128])
        Z = self.tmp(18, 2, [128, 8, 128])
        jrow = self.consts.t[:, 0:128]
        for sc in range(8):
            self.ts(X.t[:, sc, :], jrow, th[:, sc:sc + 1], ALU.mult, R9 + [self.consts], [X])
        self.frac2pi(Y.t, X.t, 0.0, Z.t, [X, Y, Z], [Y, Z])
        self.act(F["CS"][:, :, 1, :], Y.t, AF.Sin, [Y], [s5f], scale=2 * PI)
        self.frac2pi(Y.t, X.t, 0.5 * PI, Z.t, [X, Y, Z], [Y, Z])
        self.act(F["CS"][:, :, 0, :], Y.t, AF.Sin, [Y], [s5f], scale=2 * PI)
        self.ts(T[3], th, 128.0, ALU.mult, R9, R9)
        self.frac2pi(T[4], T[3], 0.0, T[5], R9, R9)
        self.act(F["E128"][:, :, 1], T[4], AF.Sin, R9, [s5f], scale=2 * PI)
        self.frac2pi(T[4], T[3], 0.5 * PI, T[5], R9, R9)
        self.act(F["E128"][:, :, 0], T[4], AF.Sin, R9, [s5f], scale=2 * PI)
        self.cp(F["rcol"], mag, R9, [s5f], eng="dve")
        S.op("dve", lambda e: e.memset(F["R"], 0.0), [], [s5f])
        ones = self.consts.t[:, 384:511]
        for sc in range(8):
            self.ts(F["R"][:, sc, 1:128], ones, mag[:, sc:sc + 1], ALU.mult, R9 + [self.consts], [s5f])
        S.dma("sp", F["dbg"], D["s5db"].t[l], [], [s5f], s5f)
        for hh in range(2):
            prm = self.tmp(12, 3, [128, 3, 512])
            self.s5in = prm.bufs[0]
            S.dma("sp", prm.t.rearrange("p a (s c) -> p a s c", c=128),
                  D["s5rep"].t[l, :, :, 4 * hh:4 * hh + 4, :].rearrange("a p s c -> p a s c"), [], [prm], prm)
            TT_ = self.tmp(15, 9, [128, 9, 512])
            T = [TT_.t[:, i, :] for i in range(9)]
            TB = TT_.bufs + prm.bufs[1:]
            _, _, zr, zi = self.s5_disc(prm.t[:, 0, :], prm.t[:, 1, :], prm.t[:, 2, :], T, TB, True)
            bx = self.tmp(12, 2, [128, 2, 512])
            S.dma("sp", bx.t.rearrange("p a (s c) -> p a s c", c=128),
                  D["s5bexp"].t[l, :, :, 4 * hh:4 * hh + 4, :].rearrange("a p s c -> p a s c"), [], [bx], bx)
            RR = TB + bx.bufs
            self.tt(T[0], zr, bx.t[:, 0, :], ALU.mult, RR, TB)
            self.tt(T[1], zi, bx.t[:, 1, :], ALU.mult, RR, TB)
            self.tt(F32v(self, "BTre", hh), T[0], T[1], ALU.subtract, RR, [s5b])
            self.tt(T[0], zr, bx.t[:, 1, :], ALU.mult, RR, TB)
            self.tt(T[1], zi, bx.t[:, 0, :], ALU.mult, RR, TB)
            self.tt(F32v(self, "BTim", hh), T[0], T[1], ALU.add, RR, [s5b])
            cx = self.tmp(12, 2, [128, 2, 512])
            S.dma("sp", cx.t.rearrange("p a (s c) -> p a s c", c=128),
                  D["s5cexp"].t[l, :, :, 4 * hh:4 * hh + 4, :].rearrange("a p s c -> p a s c"), [], [cx], cx)
            self.cp(F32v(self, "CTre", hh), cx.t[:, 0, :], [cx], [s5b], eng="dve")
            self.ts(F32v(self, "CTim", hh), cx.t[:, 1, :], -1.0, ALU.mult, [cx], [s5b])
        ident = self.consts.t[:, 128:256]
        for kc in range(2):
            self.ts(self.s5B["Dg"][:, kc, :], ident, F["dbg"][:, kc:kc + 1], ALU.mult, [s5f, self.consts], [s5b])
        S.dma("sp", self.s5f_d.t[l], s5f.t[:], [s5f], [self.s5f_d], s5f)
        S.dma("sp", self.s5b_d.t[l], s5b.t[:], [s5b], [self.s5b_d], s5b)

    def s5_fwd(self, l, s, tb):
        S, u, y = self.S, self.u, self.y
        s5f, s5b = self.s5f, self.s5b
        F, B = self.s5F, self.s5B
        st = self.s5state[l]
        self.s5_calls = getattr(self, "s5_calls", 0) + 1
        S.dma("sp", s5f.t[:], self.s5f_d.t[l], [self.s5f_d], [s5f], s5f)
        S.dma("sp", s5b.t[:], self.s5b_d.t[l], [self.s5b_d], [s5b], s5b)
        us5 = self.tmp(12, 1, [128, 2, NT], BF16)
        for kc in range(2):
            w = self.loadw("win", l * 28 + 16 + kc, 8)
            pp = self.P[kc]
            for k in range(8):
                self.mm(pp.t[:], w.t[:, k, :], u.t[:, k, :], k == 0, k == 7, [w, u], [pp])
            self.cp(us5.t[:, kc, :], pp.t[:], [pp], [us5], eng="act")
        A = self.tmp(13, 1, [128, 4, 128])
        Bt = self.tmp(14, 1, [128, 4, 128])
        bh = [self.tmp(15, 2, [128, 8, 128]), self.tmp(17, 2, [128, 8, 128])]
        xh = [self.tmp(19, 2, [128, 8, 128]), self.tmp(21, 2, [128, 8, 128])]
        xb = [self.tmp(23, 1, [128, 8, 128], BF16), self.tmp(0, 1, [128, 8, 128], BF16)]
        ini = self.tmp(1, 1, [128, 4, 8])
        A2 = self.tmp(2, 1, [128, 4, 128])
        B2 = self.tmp(3, 1, [128, 4, 128])
        ypre = [self.P[4], self.P[5]]
        CS = F["CS"]
        if tb == 0:
            S.op("dve", lambda e: e.memset(st.t[:], 0.0), [], [st])
        for sub in range(4):
            c0 = sub * 128
            for sc in range(8):
                for ri in range(2):
                    pp = self.P[2 * ri + sc // 4]
                    self.mm(pp.t[:, (sc % 4) * 128:(sc % 4 + 1) * 128], B["BT"][:, sc, ri, :],
                            us5.t[:, sc // 4, c0:c0 + 128], True, True, [s5b, us5], [pp])
            for hh in range(2):
                c = CS[:, 4 * hh:4 * hh + 4, 0, :]
                sn = CS[:, 4 * hh:4 * hh + 4, 1, :]
                pre = self.P[hh].t[:].rearrange("p (a b) -> p a b", b=128)
                pim = self.P[2 + hh].t[:].rearrange("p (a b) -> p a b", b=128)
                self.tt(A.t, pre, c, ALU.mult, [self.P[hh], s5f], [A])
                self.tt(Bt.t, pim, sn, ALU.mult, [self.P[2 + hh], s5f], [Bt])
                self.tt(bh[0].t[:, 4 * hh:4 * hh + 4, :], A.t, Bt.t, ALU.add, [A, Bt], [bh[0]])
                self.tt(A.t, pim, c, ALU.mult, [self.P[2 + hh], s5f], [A])
                self.tt(Bt.t, pre, sn, ALU.mult, [self.P[hh], s5f], [Bt])
                self.tt(bh[1].t[:, 4 * hh:4 * hh + 4, :], A.t, Bt.t, ALU.subtract, [A, Bt], [bh[1]])
            if not (tb == 0 and sub == 0):
                i0, i1, i2, i3 = (ini.t[:, i, :] for i in range(4))
                c1, s1 = F["E128"][:, :, 0], F["E128"][:, :, 1]
                self.tt(i0, c1, st.t[:, 0, :], ALU.mult, [s5f, st], [ini])
                self.tt(i1, s1, st.t[:, 1, :], ALU.mult, [s5f, st], [ini])
                self.tt(i0, i0, i1, ALU.subtract, [ini], [ini])
                self.tt(i2, c1, st.t[:, 1, :], ALU.mult, [s5f, st], [ini])
                self.tt(i3, s1, st.t[:, 0, :], ALU.mult, [s5f, st], [ini])
                self.tt(i2, i2, i3, ALU.add, [ini], [ini])
                self.tt(i0, i0, F["rcol"], ALU.mult, [ini, s5f], [ini])
                self.tt(i2, i2, F["rcol"], ALU.mult, [ini, s5f], [ini])
                self.tt(bh[0].t[:, :, 0], bh[0].t[:, :, 0], i0, ALU.add, [bh[0], ini], [bh[0]])
                self.tt(bh[1].t[:, :, 0], bh[1].t[:, :, 0], i2, ALU.add, [bh[1], ini], [bh[1]])
            Rf = F["R"].rearrange("p a b -> p (a b)")
            for ri in range(2):
                S.op("dve", lambda e, ri=ri: e.tensor_tensor_scan(
                    out=xh[ri].t.rearrange("p a b -> p (a b)"), data0=Rf,
                    data1=bh[ri].t.rearrange("p a b -> p (a b)"), initial=0.0, op0=ALU.mult, op1=ALU.add),
                    [bh[ri], s5f], [xh[ri]])
                self.cp(st.t[:, ri, :], xh[ri].t[:, :, 127], [xh[ri]], [st], eng="dve")
            for hh in range(2):
                c = CS[:, 4 * hh:4 * hh + 4, 0, :]
                sn = CS[:, 4 * hh:4 * hh + 4, 1, :]
                hs = slice(4 * hh, 4 * hh + 4)
                re_ = "dve" if self.s5_calls == 1 else "pool"
                self.tt(A2.t, xh[0].t[:, hs, :], c, ALU.mult, [xh[0], s5f], [A2], eng=re_)
                self.tt(B2.t, xh[1].t[:, hs, :], sn, ALU.mult, [xh[1], s5f], [B2], eng=re_)
                self.tt(xb[0].t[:, hs, :], A2.t, B2.t, ALU.subtract, [A2, B2], [xb[0]], eng=re_)
                self.tt(A2.t, xh[1].t[:, hs, :], c, ALU.mult, [xh[1], s5f], [A2], eng=re_)
                self.tt(B2.t, xh[0].t[:, hs, :], sn, ALU.mult, [xh[0], s5f], [B2], eng=re_)
                self.tt(xb[1].t[:, hs, :], A2.t, B2.t, ALU.add, [A2, B2], [xb[1]], eng=re_)
            for kc in range(2):
                pp = ypre[kc]
                o = pp.t[:, c0:c0 + 128]
                n = 0
                for sc in range(4 * kc, 4 * kc + 4):
                    for ri in range(2):
                        self.mm(o, B["CT"][:, sc, ri, :], xb[ri].t[:, sc, :], n == 0, False, [s5b, xb[ri]], [pp])
                        n += 1
                self.mm(o, B["Dg"][:, kc, :], us5.t[:, kc, c0:c0 + 128], False, True, [s5b, us5], [pp])
        yg = self.tmp(13, 2, [128, 2, NT])
        t1 = self.tmp(15, 2, [128, 2, NT])
        ygb = self.tmp(17, 1, [128, 2, NT], BF16)
        for kc in range(2):
            self.cp(yg.t[:, kc, :], ypre[kc].t[:], [ypre[kc]], [yg], eng="act")
        self.act(t1.t, yg.t, AF.Square, [yg], [t1])
        self.ts(t1.t, t1.t, 0.044715, ALU.mult, [t1], [t1], s2=1.0, op1=ALU.add)
        self.tt(t1.t, t1.t, yg.t, ALU.mult, [t1, yg], [t1])
        self.act(t1.t, t1.t, AF.Sigmoid, [t1], [t1], scale=1.5957691216)
        self.tt(yg.t, yg.t, t1.t, ALU.mult, [t1, yg], [yg])
        self.cp(ygb.t, yg.t, [yg], [ygb], eng="dve" if self.s5_calls == 1 else "pool")
        for oc in range(2):
            w = self.loadw("wglu", l * 2 + oc, 2)
            pp = self.P[6 + oc]
            for k in range(2):
                self.mm(pp.t[:], w.t[:, k, :], ygb.t[:, k, :], k == 0, k == 1, [w, ygb], [pp])
            self.act(t1.t[:, oc, :], pp.t[:], AF.Sigmoid, [pp, s5f], [t1], bias=F["dbg"][:, 2 + oc:3 + oc])
            self.tt(y.t[:, 2, oc, :], yg.t[:, oc, :], t1.t[:, oc, :], ALU.mult, [yg, t1], [y])


    def moba_fwd(self, l, s, tb):
        S, u, y, D = self.S, self.u, self.y, self.D
        t0 = tb * NT
        kc_, vc_, km = self.kcache[l], self.vcache[l], self.kmean[l]
        pc, consts = self.pc, self.consts
        cosT = self.tmp(0, 1, [128, NT])
        sinT = self.tmp(1, 1, [128, NT])
        posi = self.tmp(2, 1, [128, NT])
        ang = self.tmp(3, 1, [128, NT])
        fr = self.tmp(4, 1, [128, NT])
        fb = self.tmp(5, 1, [128, NT])
        qf = [self.tmp(6, 1, [128, NT]), self.tmp(7, 1, [128, NT])]
        kf = [self.tmp(8, 1, [128, NT]), self.tmp(9, 1, [128, NT])]
        t1 = self.tmp(10, 1, [128, NT])
        t2 = self.tmp(12, 1, [128, NT])
        qb_ = self.tmp(13, 1, [128, 2, NT], BF16)
        sm = self.tmp(14, 1, [128, 512])
        gm = sm.t[:, 0:32].rearrange("p (a b) -> p a b", b=8)
        mx = sm.t[:, 32:64].rearrange("p (a b) -> p a b", b=8)
        mnegb = sm.t[:, 64:128].bitcast(BF16).rearrange("p (a b) -> p a b", b=32)
        et = [self.tmp(15, 1, [128, 1024], BF16)]
        ets = [et[0].t[:, 0:512].rearrange("p (a b) -> p a b", b=256), et[0].t[:, 512:1024].rearrange("p (a b) -> p a b", b=256)]
        rden = self.tmp(16, 1, [128, 2, 256])
        S.dma("sp", posi.t.bitcast(I32), D["posr"].t[s, :, t0:t0 + NT], [], [posi], posi)
        self.cp(ang.t, posi.t.bitcast(I32), [posi], [ang], eng="dve")
        self.ts(ang.t, ang.t, pc.t[:, 0:1], ALU.mult, [ang, pc], [ang])
        self.frac2pi(fr.t, ang.t, 0.5 * PI, fb.t, [ang, fr, fb], [fr, fb])
        self.act(cosT.t, fr.t, AF.Sin, [fr], [cosT], scale=2 * PI)
        self.frac2pi(fr.t, ang.t, 0.0, fb.t, [ang, fr, fb], [fr, fb])
        self.act(sinT.t, fr.t, AF.Sin, [fr, pc], [sinT], scale=pc.t[:, 2:3])
        for c in range(2):
            for (dst, base) in ((qf[c], 18), (kf[c], 20)):
                w1 = self.loadw("win", l * 28 + base + c, 8)
                w2 = self.loadw("win", l * 28 + base + 6 + c, 8)
                p1, p2 = self.P[0], self.P[1]
                for k in range(8):
                    self.mm(p1.t[:], w1.t[:, k, :], u.t[:, k, :], k == 0, k == 7, [w1, u], [p1])
                for k in range(8):
                    self.mm(p2.t[:], w2.t[:, k, :], u.t[:, k, :], k == 0, k == 7, [w2, u], [p2])
                self.tt(t1.t, p1.t[:], cosT.t, ALU.mult, [p1, cosT], [t1])
                self.tt(t2.t, p2.t[:], sinT.t, ALU.mult, [p2, sinT], [t2])
                self.tt(dst.t, t1.t, t2.t, ALU.add, [t1, t2], [dst], eng="pool")
            self.cp(qb_.t[:, c, :], qf[c].t, [qf[c]], [qb_], eng="pool")
            self.cp(kc_.t[:, c, t0:t0 + NT], kf[c].t, [kf[c]], [kc_], eng="pool")
            S.op("dve", lambda e, c=c: e.tensor_reduce(out=km.t[:, c, 2 * tb:2 * tb + 2],
                                                      in_=kf[c].t.rearrange("p (a b) -> p a b", b=256),
                                                      axis=AX.X, op=ALU.add), [kf[c]], [km])
        self.ts(km.t[:, :, 2 * tb:2 * tb + 2], km.t[:, :, 2 * tb:2 * tb + 2], 1.0 / 256, ALU.mult, [km], [km])
        wv = [self.loadw("win", l * 28 + 22 + i, 8) for i in range(2)]
        for tt_ in range(4):
            pv = self.P[2 + tt_ % 2]
            for i in range(2):
                for k in range(8):
                    self.mm(pv.t[:, i * 128:(i + 1) * 128], u.t[:, k, tt_ * 128:(tt_ + 1) * 128], wv[i].t[:, k, :],
                            k == 0, k == 7, [wv[i], u], [pv])
            self.cp(vc_.t[:, tb * 4 + tt_, :], pv.t[:, 0:256], [pv], [vc_], eng="act")
        if l == 0 and tb == self.dbg.get("ddtb", 0):
            self.dd("cosT", cosT.t, [cosT], [128, NT])
            self.dd("sinT", sinT.t, [sinT], [128, NT])
            self.dd("qf0", qf[0].t, [qf[0]], [128, NT])
            self.dd("kf1", kf[1].t, [kf[1]], [128, NT])
            self.dd("km", km.t[:], [km], [128, 2, 8])
            self.dd("vc", vc_.t[:, tb * 4, :], [vc_], [128, 256], BF16)
        if tb > 0 or True:
            for qt in range(4):
                qblk = 2 * tb + qt // 2
                if qblk == 0:
                    continue
                pg = self.P[6]
                for h in range(4):
                    c, off = h // 2, (h % 2) * 64
                    self.mm(pg.t[:, h * 8:h * 8 + 8], qf[c].t[off:off + 64, qt * 128:(qt + 1) * 128],
                            km.t[off:off + 64, c, :], True, True, [qf[c], km], [pg])
                vm = consts.t[:, 256 + qblk * 8:256 + qblk * 8 + 8].unsqueeze(1).broadcast_to([128, 4, 8])
                self.tt(gm, pg.t[:, 0:32].rearrange("p (a b) -> p a b", b=8), vm, ALU.add, [pg, consts], [sm])
                for h in range(4):
                    S.op("dve", lambda e, h=h: e.max(out=mx[:, h, :], in_=gm[:, h, :]), [sm], [sm])
                for h in range(4):
                    self.ts(gm[:, h, :], gm[:, h, :], mx[:, h, 2:3], ALU.is_ge, [sm], [sm], s2=30000.0, op1=ALU.mult)
                self.ts(mnegb[:, qt, :], sm.t[:, 0:32], -30000.0, ALU.add, [sm], [sm])
                if l == 0 and tb == self.dbg.get("ddtb", 0) and qt == 3:
                    self.dd("sm", sm.t[:, 0:128], [sm], [128, 128])
        it = 0
        for c in range(2):
            for j in range(2):
                qblk = 2 * tb + j
                nkt = 2 * (qblk + 1)
                pacc, pden = self.P[2 + 2 * (it % 2)], self.P[3 + 2 * (it % 2)]
                it += 1
                qs = slice(j * 256, (j + 1) * 256)
                for kt in range(nkt):
                    n = kt // 2
                    ps_ = self.P[kt % 2]
                    e_ = ets[kt % 2]
                    for hh in range(2):
                        h, off = 2 * c + hh, hh * 64
                        o = ps_.t[:, hh * 256:(hh + 1) * 256]
                        self.mm(o, kc_.t[off:off + 64, c, kt * 128:(kt + 1) * 128], qb_.t[off:off + 64, c, qs],
                                True, False, [kc_, qb_], [ps_])
                        if n < qblk:
                            for q2 in range(2):
                                qt = 2 * j + q2
                                lh = mnegb[:, qt, h * 8 + n:h * 8 + n + 1].broadcast_to([128, 128])
                                self.mm(ps_.t[:, hh * 256 + q2 * 128:hh * 256 + (q2 + 1) * 128], lh, self.ident_bf.t[:],
                                        False, True, [sm, self.ident_bf], [ps_])
                        else:
                            self.mm(o, self.ident_bf.t[:], self.cm.t[:, kt % 2, :], False, True,
                                    [self.ident_bf, self.cm], [ps_])
                    if l == 0 and tb == self.dbg.get("ddtb", 0) and c == 0 and j == 0 and kt == 0 and "ps" in self.dbg.get("dd", ()):
                        dbgt = self.tmp(17, 1, [128, 512])
                        self.cp(dbgt.t, ps_.t[:], [ps_], [dbgt], eng="act")
                        self.dd("ps", dbgt.t, [dbgt], [128, 512])
                    self.act(e_, ps_.t[:].rearrange("p (a b) -> p a b", b=256), AF.Exp, [ps_], [et[0]], scale=0.125)
                    if l == 0 and tb == self.dbg.get("ddtb", 0) and c == 0 and j == 0 and kt == 0:
                        self.dd("et", et[0].t[:, 0:512], [et[0]], [128, 512], BF16)
                        self.dd("cm", self.cm.t[:], [self.cm], [128, 2, 256], BF16)
                    e2 = et[0].t[:, (kt % 2) * 512:(kt % 2 + 1) * 512]
                    self.mm(pacc.t[:], vc_.t[:, kt, c * 128:(c + 1) * 128], e2,
                            kt == 0, kt == nkt - 1, [vc_, et[0]], [pacc])
                    self.mm(pden.t[:], self.ones_bf.t[:], e2,
                            kt == 0, kt == nkt - 1, [self.ones_bf, et[0]], [pden])
                S.op("dve", lambda e, pden=pden: e.reciprocal(rden.t.rearrange("p a b -> p (a b)"), pden.t[:]), [pden], [rden])
                if l == 0 and tb == self.dbg.get("ddtb", 0):
                    self.dd("rden%d%d" % (c, j), rden.t, [rden], [128, 2, 256])
                for hh in range(2):
                    off = hh * 64
                    self.tt(y.t[off:off + 64, 3, c, qs], pacc.t[off:off + 64, hh * 256:(hh + 1) * 256],
                            rden.t[off:off + 64, hh, :], ALU.mult, [pacc, rden], [y])

    def tokproj_small(self, l):
        u, pp = self.u, self.P[7]
        for c in range(8):
            for k in range(8):
                self.mm(pp.t[0:64, c * 16:(c + 1) * 16], u.t[:, k, c * 64:(c + 1) * 64], self.wsm.t[:, l, k, :],
                        k == 0, k == 7, [u, self.wsm], [pp])
        self.sp = self.tmp(11, 1, [128, 512])
        self.cp(self.sp.t[0:64, 0:128], pp.t[0:64, 0:128], [pp], [self.sp], eng="act")
        return self.sp.t[0:64, 0:128].rearrange("p (c n) -> p c n", n=16)

    def conv_silu(self, l, tiles, nch, tail, cw, dst, tb):
        S, u = self.S, self.u
        xc = self.tmp(0, 7, [128, nch, NT + 3])
        acc = self.tmp(9, 1, [128, NT])
        if tb == 0:
            S.op("dve", lambda e: e.memset(tail.t[:], 0.0), [], [tail])
        self.cp(xc.t[:, :, 0:3], tail.t[:], [tail], [xc], eng="dve")
        for c in range(nch):
            w = self.loadw("win", l * 28 + tiles + c, 8)
            pp = self.P[c % 2]
            for k in range(8):
                self.mm(pp.t[:], w.t[:, k, :], u.t[:, k, :], k == 0, k == 7, [w, u], [pp])
            self.cp(xc.t[:, c, 3:NT + 3], pp.t[:], [pp], [xc], eng="act")
        self.cp(tail.t[:], xc.t[:, :, NT:NT + 3], [xc], [tail], eng="dve")
        for c in range(nch):
            self.ts(acc.t, xc.t[:, c, 0:NT], cw[:, c * 4:c * 4 + 1], ALU.mult, [xc, self.cwb], [acc])
            for j in range(1, 4):
                self.stt(acc.t, xc.t[:, c, j:NT + j], cw[:, c * 4 + j:c * 4 + j + 1], acc.t, ALU.mult, ALU.add,
                         [xc, acc, self.cwb], [acc])
            self.act(dst.t[:, c, :], acc.t, AF.Silu, [acc], [dst])

    def mlstm_fwd(self, l, s, tb, sp):
        S, u, y = self.S, self.u, self.y
        consts, c2, cst = self.consts, self.consts2, self.cst
        ident = consts.t[:, 128:256]
        ones = consts.t[:, 384:512]
        TRI = c2.t[0:64, 0:64]
        CMASK = c2.t[0:64, 64:128]
        rows = self.mlrow.t[0:64, l, :]
        Cx, mrep = self.mlC[l], self.mlm[l]
        qk = self.tmp(12, 4, [128, 4, NT])
        self.conv_silu(l, 8, 4, self.mltail[l], self.cwb.t[:, l, 24:40], qk, tb)
        self.ts(qk.t[:, 2:4, :], qk.t[:, 2:4, :], 0.125, ALU.mult, [qk], [qk])
        ms = self.dbg.get("mlstop", 99)
        if ms <= 1:
            return
        kz = self.tmp(4, 4, [128, 4, NT])
        S.op("dve", lambda e: e.memset(kz.t, 0.0), [], [kz])
        for h in range(4):
            pr, off = h // 2, (h % 2) * 64
            self.cp(kz.t[off:off + 64, h, :], qk.t[off:off + 64, 2 + pr, :], [qk], [kz], eng="dve")
        if tb == 0:
            S.op("dve", lambda e: e.memset(Cx.t[:], 0.0), [], [Cx])
            S.op("dve", lambda e: e.memset(mrep.t[:], 0.0), [], [mrep])
        A = self.tmp(16, 1, [128, 512])
        R_ = [A, self.sp]
        v3 = lambda lo: A.t[0:64, lo:lo + 32].rearrange("p (c h) -> p c h", h=4)
        li, lf, b_, ak, tx = v3(0), v3(32), v3(64), v3(96), v3(128)
        grep = A.t[:, 160:192].rearrange("p (c h) -> p c h", h=4)
        mkrep = A.t[:, 192:224].rearrange("p (c h) -> p c h", h=4)
        Mall = A.t[:, 224:260].rearrange("p (c h) -> p c h", h=4)
        scall = A.t[:, 260:292].rearrange("p (c h) -> p c h", h=4)
        kws = v3(292)
        mk32 = A.t[0:32, 324:325]
        dg32 = A.t[0:32, 328:360]
        ib = rows[:, 0:4].unsqueeze(1).broadcast_to([64, 8, 4])
        fb = rows[:, 4:8].unsqueeze(1).broadcast_to([64, 8, 4])
        self.tt(li, sp[:, :, 8:12], ib, ALU.add, R_ + [self.mlrow], [A])
        self.tt(tx, sp[:, :, 12:16], fb, ALU.add, R_ + [self.mlrow], [A])
        self.act(tx, tx, AF.Exp, [A], [A], scale=-1.0)
        self.act(tx, tx, AF.Ln, [A, cst], [A], bias=cst.t[0:64, 3:4])
        self.ts(lf, tx, -1.0, ALU.mult, [A], [A])
        p7 = self.P[7]
        lf2 = A.t[0:64, 32:64]
        self.mm(p7.t[0:64, 0:32], TRI, lf2, True, True, [A, c2], [p7])
        self.cp(A.t[0:64, 64:96], p7.t[0:64, 0:32], [p7], [A], eng="act")
        self.mm(p7.t[:, 32:64], ones[0:64, :], lf2, True, True, [A, consts], [p7])
        self.cp(A.t[:, 160:192], p7.t[:, 32:64], [p7], [A], eng="act")
        self.tt(ak, grep[0:64], b_, ALU.subtract, [A], [A])
        self.tt(ak, ak, li, ALU.add, [A], [A])
        self.mm(p7.t[0:32, 64:128], A.t[0:64, 96:128], ident[0:64, 0:64], True, True, [A, consts], [p7])
        S.op("dve", lambda e: e.tensor_reduce(out=mk32, in_=p7.t[0:32, 64:128], axis=AX.X, op=ALU.max), [p7], [A])
        self.ts(dg32, ident[0:32, 0:32], mk32, ALU.mult, [A, consts], [A])
        self.mm(p7.t[:, 128:160], ones[0:32, :], dg32, True, True, [A, consts], [p7])
        self.cp(A.t[:, 192:224], p7.t[:, 128:160], [p7], [A], eng="act")
        self.cp(Mall[:, 0, :], mrep.t[:], [mrep], [A], eng="dve")
        t4 = A.t[:, 364:368]
        for c in range(8):
            self.tt(t4, grep[:, c, :], Mall[:, c, :], ALU.add, [A], [A])
            self.tt(Mall[:, c + 1, :], t4, mkrep[:, c, :], ALU.max, [A], [A])
        self.cp(mrep.t[:], Mall[:, 8, :], [A], [mrep], eng="dve")
        self.tt(scall, grep, Mall[:, 0:8, :], ALU.add, [A], [A])
        self.tt(scall, scall, Mall[:, 1:9, :], ALU.subtract, [A], [A])
        self.act(scall, scall, AF.Exp, [A], [A])
        self.tt(kws, ak, Mall[0:64, 1:9, :], ALU.subtract, [A], [A])
        self.act(kws, kws, AF.Exp, [A], [A])
        if ms <= 2:
            return
        vx = self.tmp(17, 1, [128, 512])
        vext = vx.t[0:64, 0:264].rearrange("p (h e) -> p h e", e=66)
        S.op("dve", lambda e: e.memset(vx.t[0:64, 0:264], 1.0), [], [vx])
        osg = self.tmp(18, 1, [128, 512])
        Dm = self.tmp(19, 1, [128, 512])
        LR = self.tmp(20, 1, [128, 512])
        sq_ = self.tmp(21, 1, [128, 512])
        sT = self.tmp(22, 1, [128, 512])
        ne = self.tmp(23, 1, [128, 512])
        tq = self.tmp(0, 1, [128, 512])
        kt_ = self.tmp(1, 1, [128, 512])
        hh_ = self.tmp(2, 1, [128, 512])
        B = self.tmp(3, 1, [128, 512])
        v4 = lambda T, lo=0: T.t[0:64, lo:lo + 256].rearrange("p (h e) -> p h e", e=64)
        wv = [self.loadw("win", l * 28 + 12 + i, 8) for i in range(4)]
        TL = [(vx, osg, ne, kt_), tuple(self.xs)]
        S.op("dve", lambda e: e.memset(self.xs[0].t[0:64, 0:264], 1.0), [], [self.xs[0]])

        def prepM(c):
            cs = slice(c * 64, (c + 1) * 64)
            vx, osg, ne, kt_ = TL[c % 2]
            vext = vx.t[0:64, 0:264].rearrange("p (h e) -> p h e", e=66)
            Bv = kt_.t[0:64, 256:320]
            mloc, mint, mt, wint, e2, qn, den = (Bv[:, 4 * i:4 * i + 4] for i in range(7))
            ss = Bv[:, 32:36]
            B = kt_
            p0 = self.P[0]
            for i in range(4):
                for k in range(8):
                    self.mm(p0.t[0:64, i * 128:(i + 1) * 128], u.t[:, k, cs], wv[i].t[:, k, :], k == 0, k == 7,
                            [u, wv[i]], [p0])
            self.cp(vext[:, :, 0:64], p0.t[0:64, 0:256].rearrange("p (h e) -> p h e", e=64), [p0], [vx], eng="act")
            self.act(osg.t[0:64, 0:256], p0.t[0:64, 256:512], AF.Sigmoid, [p0], [osg])
            lft = LR.t[0:64, 0:256].rearrange("p (h e) -> p h e", e=64)
            rm = LR.t[0:64, 256:512].rearrange("p (h e) -> p h e", e=64)
            tri_b = TRI.unsqueeze(1).broadcast_to([64, 4, 64])
            id_b = ident[0:64, 0:64].unsqueeze(1).broadcast_to([64, 4, 64])
            self.tt(lft, tri_b, lf[:, c, :].unsqueeze(2).broadcast_to([64, 4, 64]), ALU.mult, [A, c2], [LR])
            self.tt(rm, id_b, li[:, c, :].unsqueeze(2).broadcast_to([64, 4, 64]), ALU.mult, [A, consts], [LR])
            self.tt(rm, rm, lft, ALU.subtract, [LR], [LR])
            p1 = self.P[1]
            for h in range(4):
                o = p1.t[0:64, h * 64:(h + 1) * 64]
                self.mm(o, lft[:, h, :], ones[0:64, 0:64], True, False, [LR, consts], [p1])
                self.mm(o, ones[0:64, 0:64], rm[:, h, :], False, True, [LR, consts], [p1])
            dmv = v4(Dm)
            self.tt(dmv, p1.t[0:64, 0:256].rearrange("p (h e) -> p h e", e=64),
                    CMASK.unsqueeze(1).broadcast_to([64, 4, 64]), ALU.add, [p1, c2], [Dm])
            S.op("dve", lambda e, dmv=dmv, mloc=mloc: e.tensor_reduce(out=mloc, in_=dmv, axis=AX.X, op=ALU.max), [Dm], [B])
            self.tt(mint, b_[:, c, :], Mall[0:64, c, :], ALU.add, [A], [B])
            self.tt(mt, mint, mloc, ALU.max, [B], [B])
            self.tt(wint, mint, mt, ALU.subtract, [B], [B])
            self.act(wint, wint, AF.Exp, [B], [B])
            self.act(e2, mt, AF.Exp, [B], [B], scale=-1.0)
            p2 = self.P[2]
            for h in range(4):
                o = p2.t[0:64, h * 64:(h + 1) * 64]
                self.mm(o, ones[0:64, 0:64], lft[:, h, :], True, False, [LR, consts], [p2])
                self.mm(o, rm[:, h, :], ones[0:64, 0:64], False, True, [LR, consts], [p2])
            etv = v4(sq_)
            self.tt(etv, p2.t[0:64, 0:256].rearrange("p (h e) -> p h e", e=64),
                    c2.t[0:64, 320:384].unsqueeze(1).broadcast_to([64, 4, 64]), ALU.add, [p2, c2], [sq_])
            self.act(etv, etv, AF.Exp, [sq_], [sq_])
            p3 = self.P[3]
            for h in range(4):
                self.mm(p3.t[0:64, h * 64:(h + 1) * 64], kz.t[:, h, cs], qk.t[:, h // 2, cs], True, True, [kz, qk], [p3])
            self.tt(v4(sT), p3.t[0:64, 0:256].rearrange("p (h e) -> p h e", e=64), etv, ALU.mult, [p3, sq_], [sT])
            stv = v4(sT)
            p4 = self.P[4]
            for h in range(4):
                self.mm(p4.t[0:64, h * 66:(h + 1) * 66], stv[:, h, :], vext[:, h, :], True, True, [sT, vx], [p4])
            nev = ne.t[0:64, 0:264].rearrange("p (h e) -> p h e", e=66)
            self.tt(nev, p4.t[0:64, 0:264].rearrange("p (h e) -> p h e", e=66),
                    e2.unsqueeze(2).broadcast_to([64, 4, 66]), ALU.mult, [p4, B], [ne])
            p6 = self.P[6]
            for pr in range(2):
                self.mm(p6.t[0:64, pr * 128:(pr + 1) * 128], qk.t[:, 2 + pr, cs], ident, True, True, [qk, consts], [p6])
            kwv = v4(kt_)
            self.tt(kwv, p6.t[0:64, 0:256].rearrange("p (h e) -> p h e", e=64),
                    kws[:, c, :].unsqueeze(2).broadcast_to([64, 4, 64]), ALU.mult, [p6, A], [kt_])

        def recM(c):
            cs = slice(c * 64, (c + 1) * 64)
            vx, osg, ne, kt_ = TL[c % 2]
            vext = vx.t[0:64, 0:264].rearrange("p (h e) -> p h e", e=66)
            Bv = kt_.t[0:64, 256:320]
            mloc, mint, mt, wint, e2, qn, den = (Bv[:, 4 * i:4 * i + 4] for i in range(7))
            ss = Bv[:, 32:36]
            B = kt_
            p5 = self.P[5]
            for h in range(4):
                self.mm(p5.t[0:64, h * 66:(h + 1) * 66], qk.t[:, h // 2, cs], Cx.t[:, h, :], True, True, [qk, Cx], [p5])
            nev = ne.t[0:64, 0:264].rearrange("p (h e) -> p h e", e=66)
            tqv = tq.t[0:64, 0:264].rearrange("p (h e) -> p h e", e=66)
            self.tt(tqv, p5.t[0:64, 0:264].rearrange("p (h e) -> p h e", e=66),
                    wint.unsqueeze(2).broadcast_to([64, 4, 66]), ALU.mult, [p5, B], [tq])
            self.tt(nev, nev, tqv, ALU.add, [tq, ne], [ne])
            self.act(den, nev[:, :, 64], AF.Abs, [ne], [B])
            self.tt(den, den, e2, ALU.max, [B], [B])
            S.op("dve", lambda e, den=den: e.reciprocal(den, den), [B], [B])
            hv = v4(hh_)
            self.tt(hv, nev[:, :, 0:64], den.unsqueeze(2).broadcast_to([64, 4, 64]), ALU.mult, [ne, B], [hh_])
            h2 = v4(hh_, 256)
            self.tt(h2, hv, hv, ALU.mult, [hh_], [hh_])
            S.op("dve", lambda e, h2=h2, ss=ss: e.tensor_reduce(out=ss, in_=h2, axis=AX.X, op=ALU.add), [hh_], [B])
            self.act(ss, ss, AF.Sqrt, [B, cst], [B], scale=1.0 / 64, bias=cst.t[0:64, 0:1])
            S.op("dve", lambda e, ss=ss: e.reciprocal(ss, ss), [B], [B])
            self.tt(hv, hv, ss.unsqueeze(2).broadcast_to([64, 4, 64]), ALU.mult, [hh_, B], [hh_])
            self.tt(hh_.t[0:64, 0:256], hh_.t[0:64, 0:256], rows[:, 8:264], ALU.mult, [hh_, self.mlrow], [hh_])
            self.tt(hh_.t[0:64, 0:256], hh_.t[0:64, 0:256], osg.t[0:64, 0:256], ALU.mult, [hh_, osg], [hh_])
            for kc in range(2):
                self.mm(p5.t[:, 264 + kc * 64:264 + (kc + 1) * 64], hh_.t[0:64, kc * 128:(kc + 1) * 128],
                        ident[0:64, 0:64], True, True, [hh_, consts], [p5])
                self.cp(y.t[:, 1, kc, cs], p5.t[:, 264 + kc * 64:264 + (kc + 1) * 64], [p5], [y], eng="act")
            p7b = self.P[7]
            for h in range(4):
                pr, off = h // 2, (h % 2) * 64
                o = p7b.t[:, h * 66:(h + 1) * 66]
                self.mm(o, kt_.t[0:64, pr * 128:(pr + 1) * 128], vext[:, h, :], True, True, [kt_, vx], [p7b])
                self.stt(Cx.t[off:off + 64, h, :], Cx.t[off:off + 64, h, :], scall[off:off + 64, c, h:h + 1],
                         p7b.t[off:off + 64, h * 66:(h + 1) * 66], ALU.mult, ALU.add, [Cx, A, p7b], [Cx])

        S.replay(self.record(prepM, 0))
        for c in range(8):
            B_ = self.record(recM, c)
            A_ = self.record(prepM, c + 1) if c < 7 else []
            S.replay(self.merge2(A_, B_))


    def gdn_fwd(self, l, s, tb, sp):
        S, u, y = self.S, self.u, self.y
        consts, c2, cst = self.consts, self.consts2, self.cst
        ident = consts.t[:, 128:256]
        id64 = ident[0:64, 0:64]
        ones = consts.t[:, 384:512]
        on64 = ones[0:64, 0:64]
        neg64 = self.negones.t[0:64, 0:64]
        TRI = c2.t[0:64, 0:64]
        SLADD = self.gmask.t[0:64, 0:64]
        SUADD = self.gmask.t[0:64, 64:128]
        CMT = c2.t[0:64, 320:384]
        BLK = self.gmask.t[:, 128:256]
        rows = self.gdrow.t[0:64, l, :]
        Sz = self.gdS[l]
        b3 = lambda ap: ap.unsqueeze(1).broadcast_to([64, 4, 64])
        v4 = lambda T, lo=0: T.t[0:64, lo:lo + 256].rearrange("p (h e) -> p h e", e=64)
        qkv = self.tmp(12, 6, [128, 6, NT])
        self.conv_silu(l, 0, 6, self.gdtail[l], self.cwb.t[:, l, 0:24], qkv, tb)
        if tb == 0:
            S.op("dve", lambda e: e.memset(Sz.t[:], 0.0), [], [Sz])
        sq = self.tmp(9, 1, [128, NT])
        rs = self.tmp(8, 1, [128, NT])
        for c4 in range(4):
            pp = self.P[c4 % 2]
            self.tt(sq.t, qkv.t[:, c4, :], qkv.t[:, c4, :], ALU.mult, [qkv], [sq])
            self.mm(pp.t[:], BLK, sq.t, True, True, [sq, self.gmask], [pp])
            self.act(rs.t, pp.t[:], AF.Sqrt, [pp, cst], [rs], bias=cst.t[:, 0:1])
            S.op("dve", lambda e: e.reciprocal(rs.t, rs.t), [rs], [rs])
            if c4 < 2:
                self.stt(qkv.t[:, c4, :], qkv.t[:, c4, :], 0.125, rs.t, ALU.mult, ALU.mult, [qkv, rs], [qkv])
            else:
                self.tt(qkv.t[:, c4, :], qkv.t[:, c4, :], rs.t, ALU.mult, [qkv, rs], [qkv])
        kz = self.tmp(4, 4, [128, 4, NT])
        S.op("dve", lambda e: e.memset(kz.t, 0.0), [], [kz])
        for h in range(4):
            pr, off = h // 2, (h % 2) * 64
            self.cp(kz.t[off:off + 64, h, :], qkv.t[off:off + 64, 2 + pr, :], [qkv], [kz], eng="dve")
        A = self.tmp(18, 1, [128, 512])
        v3 = lambda lo: A.t[0:64, lo:lo + 32].rearrange("p (c h) -> p c h", h=4)
        beta, g_, gc, egc, ekd, tx, bneg, begc = v3(0), v3(32), v3(64), v3(96), v3(128), v3(160), v3(192), v3(224)
        gLrep = A.t[:, 256:288].rearrange("p (c h) -> p c h", h=4)
        cdrep = A.t[:, 288:320].rearrange("p (c h) -> p c h", h=4)
        ea = A.t[0:64, 320:324]
        R_ = [A, self.sp]
        self.act(beta, sp[:, :, 0:4], AF.Sigmoid, R_, [A])
        self.tt(tx, sp[:, :, 4:8], rows[:, 4:8].unsqueeze(1).broadcast_to([64, 8, 4]), ALU.add, R_ + [self.gdrow], [A])
        self.act(tx, tx, AF.Exp, [A], [A])
        self.act(tx, tx, AF.Ln, [A, cst], [A], bias=cst.t[0:64, 3:4])
        self.act(ea, rows[:, 0:4], AF.Exp, [self.gdrow], [A])
        self.tt(g_, tx, ea.unsqueeze(1).broadcast_to([64, 8, 4]), ALU.mult, [A], [A])
        self.ts(g_, g_, -1.0, ALU.mult, [A], [A])
        p7 = self.P[7]
        g2 = A.t[0:64, 32:64]
        self.mm(p7.t[0:64, 0:32], TRI, g2, True, True, [A, c2], [p7])
        self.cp(A.t[0:64, 64:96], p7.t[0:64, 0:32], [p7], [A], eng="act")
        self.mm(p7.t[:, 32:64], ones[0:64, :], g2, True, True, [A, consts], [p7])
        self.cp(A.t[:, 256:288], p7.t[:, 32:64], [p7], [A], eng="act")
        self.act(egc, gc, AF.Exp, [A], [A])
        self.tt(ekd, gLrep[0:64], gc, ALU.subtract, [A], [A])
        self.act(ekd, ekd, AF.Exp, [A], [A])
        self.act(cdrep, gLrep, AF.Exp, [A], [A])
        self.ts(bneg, beta, -1.0, ALU.mult, [A], [A])
        self.tt(begc, beta, egc, ALU.mult, [A], [A])
        MM_ = self.tmp(19, 1, [128, 512], BF16)
        Xb = self.tmp(8, 1, [128, 512], BF16)
        Xbv = Xb.t[0:64, 0:512].rearrange("p (h e) -> p h e", e=128)
        X = self.tmp(20, 1, [128, 512])
        DC = self.tmp(21, 1, [128, 512])
        QB = self.tmp(22, 1, [128, 512])
        GT = self.tmp(23, 1, [128, 512])
        VK = self.tmp(0, 1, [128, 512])
        XT = self.tmp(1, 1, [128, 512])
        VN = self.tmp(2, 1, [128, 512])
        ZS = self.tmp(3, 1, [128, 512])
        M2 = self.tmp(10, 1, [128, 512], BF16)
        wz = [self.loadw("win", l * 28 + 6 + i, 8) for i in range(2)]
        Xs = [X, self.xs[0]]
        QBs = [QB, self.xs[1]]
        KZs = [self.xs[2], self.xs[3]]

        def prepN(c):
            cs = slice(c * 64, (c + 1) * 64)
            X, QB, KZ = Xs[c % 2], QBs[c % 2], KZs[c % 2]
            Xv = X.t[0:64, 0:512].rearrange("p (h e) -> p h e", e=128)
            p0 = self.P[0]
            for i in range(2):
                for k in range(8):
                    self.mm(p0.t[0:64, i * 128:(i + 1) * 128], u.t[:, k, cs], wz[i].t[:, k, :], k == 0, k == 7,
                            [u, wz[i]], [p0])
            self.act(KZ.t[0:64, 256:512], p0.t[0:64, 0:256], AF.Silu, [p0], [KZ])
            for pr in range(2):
                self.mm(p0.t[0:64, 256 + pr * 128:256 + (pr + 1) * 128], qkv.t[:, 4 + pr, cs], ident, True, True,
                        [qkv, consts], [p0])
            vtok = v4(VK)
            self.cp(vtok, p0.t[0:64, 256:512].rearrange("p (h e) -> p h e", e=64), [p0], [VK], eng="act")
            p1 = self.P[4]
            for pr in range(2):
                self.mm(p1.t[0:64, pr * 128:(pr + 1) * 128], qkv.t[:, 2 + pr, cs], ident, True, True, [qkv, consts], [p1])
            ktok = v4(VK, 256)
            self.cp(ktok, p1.t[0:64, 0:256].rearrange("p (h e) -> p h e", e=64), [p1], [VK], eng="act")
            for h in range(4):
                uo, wo = (64, 0) if h % 2 == 0 else (0, 64)
                self.ts(Xv[:, h, uo:uo + 64], vtok[:, h, :], beta[:, c, h:h + 1], ALU.mult, [VK, A], [X])
                self.ts(Xv[:, h, wo:wo + 64], ktok[:, h, :], begc[:, c, h:h + 1], ALU.mult, [VK, A], [X])
            kd = v4(KZ)
            self.tt(kd, ktok, ekd[:, c, :].unsqueeze(2).broadcast_to([64, 4, 64]), ALU.mult, [VK, A], [KZ])
            gt = v4(GT)
            self.tt(gt, b3(TRI), g_[:, c, :].unsqueeze(2).broadcast_to([64, 4, 64]), ALU.mult, [A, c2], [GT])
            p2, p3 = self.P[2], self.P[3]
            for h in range(4):
                o = p2.t[0:64, h * 64:(h + 1) * 64]
                self.mm(o, gt[:, h, :], on64, True, False, [GT, consts], [p2])
                self.mm(o, neg64, gt[:, h, :], False, True, [GT, self.negones], [p2])
                o = p2.t[0:64, 256 + h * 64:256 + (h + 1) * 64]
                self.mm(o, on64, gt[:, h, :], True, False, [GT, consts], [p2])
                self.mm(o, gt[:, h, :], neg64, False, True, [GT, self.negones], [p2])
            dS, dT, dQ = v4(DC), v4(DC, 256), v4(QB)
            pD = p2.t[0:64, 0:256].rearrange("p (h e) -> p h e", e=64)
            pDT = p2.t[0:64, 256:512].rearrange("p (h e) -> p h e", e=64)
            self.tt(dS, pD, b3(SLADD), ALU.add, [p2, self.gmask], [DC])
            self.tt(dT, pDT, b3(SUADD), ALU.add, [p2, self.gmask], [DC])
            self.tt(dQ, pDT, b3(CMT), ALU.add, [p2, c2], [QB])
            self.act(DC.t[0:64, 0:512], DC.t[0:64, 0:512], AF.Exp, [DC], [DC])
            self.act(dQ, dQ, AF.Exp, [QB], [QB])
            dgb = v4(GT, 256)
            self.tt(dgb, b3(id64), bneg[:, c, :].unsqueeze(2).broadcast_to([64, 4, 64]), ALU.mult, [A, consts], [GT])
            for h in range(4):
                self.mm(p3.t[0:64, h * 64:(h + 1) * 64], qkv.t[:, 2 + h // 2, cs], kz.t[:, h, cs], True, True, [qkv, kz], [p3])
                self.mm(p3.t[0:64, 256 + h * 64:256 + (h + 1) * 64], on64, dgb[:, h, :], True, True, [GT, consts], [p3])
            pKK = p3.t[0:64, 0:256].rearrange("p (h e) -> p h e", e=64)
            pBf = p3.t[0:64, 256:512].rearrange("p (h e) -> p h e", e=64)
            Mk, MkT = v4(MM_), v4(MM_, 256)
            self.tt(Mk, pKK, dS, ALU.mult, [p3, DC], [MM_])
            self.tt(Mk, Mk, bneg[:, c, :].unsqueeze(2).broadcast_to([64, 4, 64]), ALU.mult, [MM_, A], [MM_])
            self.tt(MkT, pKK, dT, ALU.mult, [p3, DC], [MM_])
            self.tt(MkT, MkT, pBf, ALU.mult, [MM_, p3], [MM_])
            p4 = self.P[4]
            for h in range(4):
                self.mm(p4.t[0:64, h * 64:(h + 1) * 64], kz.t[:, h, cs], qkv.t[:, h // 2, cs], True, True, [qkv, kz], [p4])
            self.tt(dQ, p4.t[0:64, 0:256].rearrange("p (h e) -> p h e", e=64), dQ, ALU.mult, [p4, QB], [QB])
            cur, nxt = MM_, M2
            for step in range(6):
                cM, cMT = v4(cur), v4(cur, 256)
                p5 = self.P[5]
                self.cp(Xb.t[0:64, 0:512], X.t[0:64, 0:512], [X], [Xb], eng="act")
                for h in range(4):
                    self.mm(p5.t[0:64, h * 128:(h + 1) * 128], cMT[:, h, :], Xbv[:, h, :], True, True, [cur, Xb], [p5])
                if step < 5:
                    p6 = self.P[6]
                    for h in range(4):
                        self.mm(p6.t[0:64, h * 64:(h + 1) * 64], cMT[:, h, :], cM[:, h, :], True, True, [cur], [p6])
                        self.mm(p6.t[0:64, 256 + h * 64:256 + (h + 1) * 64], cM[:, h, :], cMT[:, h, :], True, True, [cur], [p6])
                    self.cp(nxt.t[0:64, 0:512], p6.t[0:64, 0:512], [p6], [nxt], eng="act")
                self.tt(X.t[0:64, 0:512], X.t[0:64, 0:512], p5.t[0:64, 0:512], ALU.add, [X, p5], [X])
                cur, nxt = nxt, cur

        def rec(c):
            cs = slice(c * 64, (c + 1) * 64)
            X, QB, KZ = Xs[c % 2], QBs[c % 2], KZs[c % 2]
            Xv = X.t[0:64, 0:512].rearrange("p (h e) -> p h e", e=128)
            dQ = v4(QB)
            p5 = self.P[1]
            for h in range(4):
                self.mm(p5.t[:, h * 64:(h + 1) * 64], Xv[:, h, :], id64, True, True, [X, consts], [p5])
            xt = XT.t[:, 0:256].rearrange("p (h e) -> p h e", e=64)
            self.cp(XT.t[:, 0:256], p5.t[:, 0:256], [p5], [XT], eng="act")
            p6 = self.P[7]
            for h in range(4):
                self.mm(p6.t[0:64, h * 64:(h + 1) * 64], xt[:, h, :], Sz.t[:, h, :], True, True, [XT, Sz], [p6])
                self.mm(p6.t[0:64, 256 + h * 64:256 + (h + 1) * 64], qkv.t[:, h // 2, cs], Sz.t[:, h, :], True, True,
                        [qkv, Sz], [p6])
            vn = v4(VN)
            for h in range(4):
                uo = 64 if h % 2 == 0 else 0
                self.tt(vn[:, h, :], Xv[:, h, uo:uo + 64], p6.t[0:64, h * 64:(h + 1) * 64], ALU.subtract, [X, p6], [VN])
            oq = v4(VN, 256)
            self.tt(oq, p6.t[0:64, 256:512].rearrange("p (h e) -> p h e", e=64),
                    egc[:, c, :].unsqueeze(2).broadcast_to([64, 4, 64]), ALU.mult, [p6, A], [VN])
            p7 = self.P[7]
            for h in range(4):
                self.mm(p7.t[0:64, h * 64:(h + 1) * 64], dQ[:, h, :], vn[:, h, :], True, True, [QB, VN], [p7])
            self.tt(oq, oq, p7.t[0:64, 0:256].rearrange("p (h e) -> p h e", e=64), ALU.add, [VN, p7], [VN])
            p1 = self.P[1]
            for h in range(4):
                pr, off = h // 2, (h % 2) * 64
                self.mm(p1.t[:, 256 + h * 64:256 + (h + 1) * 64], KZ.t[0:64, pr * 128:(pr + 1) * 128], vn[:, h, :],
                        True, True, [KZ, VN], [p1])
                self.stt(Sz.t[off:off + 64, h, :], Sz.t[off:off + 64, h, :], cdrep[off:off + 64, c, h:h + 1],
                         p1.t[off:off + 64, 256 + h * 64:256 + (h + 1) * 64], ALU.mult, ALU.add, [Sz, A, p1], [Sz])
            o2 = v4(ZS, 256)
            ss = A.t[0:64, 328:332]
            self.tt(o2, oq, oq, ALU.mult, [VN], [ZS])
            S.op("dve", lambda e, o2=o2, ss=ss: e.tensor_reduce(out=ss, in_=o2, axis=AX.X, op=ALU.add), [ZS], [A])
            self.act(ss, ss, AF.Sqrt, [A, cst], [A], scale=1.0 / 64, bias=cst.t[0:64, 0:1])
            S.op("dve", lambda e, ss=ss: e.reciprocal(ss, ss), [A], [A])
            self.tt(o2, oq, ss.unsqueeze(2).broadcast_to([64, 4, 64]), ALU.mult, [VN, A], [ZS])
            self.tt(o2, o2, b3(rows[:, 8:72]), ALU.mult, [ZS, self.gdrow], [ZS])
            self.tt(ZS.t[0:64, 256:512], ZS.t[0:64, 256:512], KZ.t[0:64, 256:512], ALU.mult, [ZS, KZ], [ZS])
            p0 = self.P[7]
            for kc in range(2):
                self.mm(p0.t[:, 256 + kc * 64:256 + (kc + 1) * 64], ZS.t[0:64, 256 + kc * 128:256 + (kc + 1) * 128], id64, True, True,
                        [ZS, consts], [p0])
                self.cp(y.t[:, 0, kc, cs], p0.t[:, 256 + kc * 64:256 + (kc + 1) * 64], [p0], [y], eng="act")

        def record(fn, c):
            S.rec = []
            fn(c)
            items, S.rec = S.rec, None
            return items

        def merge2(a, b):
            out, i, j, na, nb = [], 0, 0, len(a), len(b)
            while i < na or j < nb:
                if j >= nb or (i < na and i * nb <= j * na):
                    out.append(a[i])
                    i += 1
                else:
                    out.append(b[j])
                    j += 1
            return out

        S.replay(record(prepN, 0))
        for c in range(8):
            B_ = record(rec, c)
            A_ = record(prepN, c + 1) if c < 7 else []
            S.replay(merge2(A_, B_))

    def mixers(self, l, s, tb):
        only = self.dbg.get("only", "")
        sp = self.tokproj_small(l)
        if not only or "gdn" in only:
            self.gdn_fwd(l, s, tb, sp)
        if not only or "ml" in only:
            self.mlstm_fwd(l, s, tb, sp)
        if not only or "s5" in only:
            self.s5_fwd(l, s, tb)
        if not only or "moba" in only:
            self.moba_fwd(l, s, tb)

    def build(self):
        S = self.S
        self.declare()
        self.setup()
        units = self.dbg.get("units", [(s, tb) for s in range(2) for tb in range(SEQ // NT)])
        stop = self.dbg.get("stop", None)
        for (s, tb) in units:
            t0 = tb * NT
            S.dma("sp", self.h.t[:], self.D["xT"].t[s, :, :, t0:t0 + NT], [], [self.h], self.h)
            first = (s, tb) == units[0]
            for l in range(NL):
                self.ffn(l, 0)
                if stop == "ffn1":
                    break
                if first and l == 0:
                    for l2 in range(NL):
                        self.s5_setup(l2)
                self.norm(l * 4 + 1)
                if "yin" in self.dbg:
                    S.dma("sp", self.y.t[:], self.D["yin"].t[s * 4 + tb], [], [self.y], self.y)
                else:
                    self.mixers(l, s, tb)
                if "dumpy" in self.dbg and l == 0:
                    S.dma("sp", self.dumpy.t[s * 4 + tb], self.y.t[:], [self.y], [self.dumpy], self.y)
                if stop == "y":
                    break
                self.merge(l)
                self.ffn(l, 1)
                self.ple(l, s, t0)
            if stop is not None:
                self.dump_h(s * 4 + tb)
            self.final(s, t0)
        S.finish()
        S.emit_all()


def F32v(k, name, hh):
    ri = 0 if name.endswith("re") else 1
    t = k.s5B["BT" if name.startswith("BT") else "CT"]
    return t[:, 4 * hh:4 * hh + 4, ri, :]


def wt(w, K):
    Kd, N = w.shape
    assert Kd == K * 128 and N % 128 == 0
    a = w.reshape(K, 128, N // 128, 128).transpose(2, 1, 0, 3)
    return np.ascontiguousarray(a).reshape(N // 128 * 128, K * 128)


def fm(a, kc):
    T = a.shape[0]
    return np.ascontiguousarray(a.T.reshape(kc, 128, T).transpose(1, 0, 2))


def prep_shared(inp):
    f = lambda n: np.asarray(inp[n], dtype=np.float32)
    sh = {}
    wgu = []
    wdn = []
    for l in range(NL):
        for nm_gu, nm_d in (("ffn1_w_gu", "ffn1_w_down"), ("ffn2_w_gu", "ffn2_w_down")):
            w = f(nm_gu)[l]
            g = wt(w[:, :2816], 8).reshape(22, 128, 1024)
            u = wt(w[:, 2816:], 8).reshape(22, 128, 1024)
            wgu.append(np.stack([g, u], axis=1).reshape(44 * 128, 1024))
            wdn.append(wt(f(nm_d)[l], 22))
    sh["wgu"] = np.concatenate(wgu, 0)
    sh["wdn"] = np.concatenate(wdn, 0)
    swap = np.concatenate([(np.arange(64) + 32) % 64 + 64 * hh for hh in range(4)])
    cols = np.concatenate([np.arange(0, 1024), np.arange(1032, 2056), np.arange(2064, 3088),
                           2320 + swap, 2576 + swap])
    sh["win"] = np.concatenate([wt(f("w_in")[l][:, cols], 8) for l in range(NL)], 0)
    sh["wgate"] = np.concatenate([
        np.stack([wt(f("w_gate")[l, b], 8).reshape(8, 128, 1024) for b in range(4)], 1).reshape(8 * 4 * 128, 1024)
        for l in range(NL)], 0)
    sh["wbr"] = np.concatenate([
        np.stack([wt(f("w_branch")[l, b], 2).reshape(8, 128, 256) for b in range(4)], 1).reshape(8 * 4 * 128, 256)
        for l in range(NL)], 0)
    sh["wout"] = np.concatenate([wt(f("w_out")[l], 8) for l in range(NL)], 0)
    sh["wplg"] = np.concatenate([wt(f("ple_w_gate")[l], 8) for l in range(NL)], 0)
    sh["wplp"] = np.concatenate([wt(f("ple_w_proj")[l], 2) for l in range(NL)], 0)
    gains = np.zeros((9, 1024), np.float32)
    for l in range(NL):
        gains[l * 4 + 0] = f("ffn1_norm")[l]
        gains[l * 4 + 1] = f("mix_norm")[l]
        gains[l * 4 + 2] = f("ffn2_norm")[l]
        gains[l * 4 + 3] = f("ple_norm")[l]
    gains[8] = f("final_norm")
    sh["gains"] = np.ascontiguousarray(gains.reshape(9, 8, 128).transpose(2, 0, 1))
    sh["wglu"] = np.concatenate([wt(f("s5_w_glu")[l], 2) for l in range(NL)], 0)
    consts = np.zeros((128, 512), np.float32)
    consts[:, 0:128] = np.arange(128, dtype=np.float32)[None, :]
    consts[:, 128:256] = np.eye(128, dtype=np.float32)
    consts[:, 384:512] = 1.0
    for qb in range(8):
        for n in range(8):
            consts[:, 256 + qb * 8 + n] = 0.0 if n < qb else -1e9
    pc = np.zeros((128, 8), np.float32)
    invf = (10000.0 ** (-np.arange(0, 64, 2, dtype=np.float32) / 64)).astype(np.float32)
    for p_ in range(128):
        pc[p_, 0] = invf[p_ % 32]
        pc[p_, 1] = -1.0 if (p_ % 64) < 32 else 1.0
        pc[p_, 2] = pc[p_, 1] * 2 * np.pi
    sh["pc"] = pc
    c2 = np.zeros((128, 512), np.float32)
    ii = np.arange(64)
    c2[0:64, 0:64] = (ii[:, None] <= ii[None, :]).astype(np.float32)
    c2[0:64, 64:128] = np.where(ii[None, :] <= ii[:, None], 0.0, -60000.0)
    c2[0:64, 128:192] = (ii[None, :] < ii[:, None]).astype(np.float32)
    c2[0:64, 192:256] = (ii[None, :] <= ii[:, None]).astype(np.float32)
    c2[0:64, 256:320] = (ii[:, None] < ii[None, :]).astype(np.float32)
    c2[0:64, 320:384] = np.where(ii[:, None] <= ii[None, :], 0.0, -60000.0)
    sh["consts2"] = c2
    gmk = np.zeros((128, 256), np.float32)
    gmk[0:64, 0:64] = np.where(ii[None, :] < ii[:, None], 0.0, -60000.0)
    gmk[0:64, 64:128] = np.where(ii[:, None] < ii[None, :], 0.0, -60000.0)
    pp_ = np.arange(128)
    gmk[:, 128:256] = (pp_[:, None] // 64 == pp_[None, :] // 64).astype(np.float32)
    sh["gmaskf"] = gmk
    win = f("w_in")
    wsm = np.zeros((128, NL, 8, 16), np.float32)
    cwf = np.zeros((128, NL, 40), np.float32)
    mlrow = np.zeros((128, NL, 264), np.float32)
    gdrow = np.zeros((128, NL, 72), np.float32)
    for l in range(NL):
        small = np.concatenate([win[l][:, 1024:1032], win[l][:, 2056:2064]], 1)
        wsm[:, l] = small.reshape(8, 128, 16).transpose(1, 0, 2)
        gc = f("gdn_conv")[l]
        mc = f("mlstm_conv")[l]
        cwf[:, l, 0:24] = gc.T.reshape(6, 128, 4).transpose(1, 0, 2).reshape(128, 24)
        cwf[:, l, 24:40] = mc.T.reshape(4, 128, 4).transpose(1, 0, 2).reshape(128, 16)
        mlrow[:, l, 0:4] = f("mlstm_i_bias")[l][None, :]
        mlrow[:, l, 4:8] = f("mlstm_f_bias")[l][None, :]
        mlrow[:, l, 8:264] = f("mlstm_norm")[l][None, :]
        gdrow[:, l, 0:4] = f("gdn_a_log")[l][None, :]
        gdrow[:, l, 4:8] = f("gdn_dt_bias")[l][None, :]
        gdrow[:, l, 8:72] = f("gdn_norm")[l][None, :]
    sh["wsmf"], sh["cwf"], sh["mlrowf"], sh["gdrowf"] = wsm, cwf, mlrow, gdrow
    cmf = np.zeros((128, 2, 256), np.float32)
    for a in range(2):
        kl = a * 128 + np.arange(128)[:, None]
        cmf[:, a, :] = np.where(kl > np.arange(256)[None, :], -30000.0, 0.0)
    sh["cmf"] = cmf
    sh["consts"] = consts
    lre, lim, ldt = f("s5_lambda_re"), f("s5_lambda_im"), f("s5_log_dt")
    s5st = np.zeros((NL, 128, 8, 3), np.float32)
    s5rep = np.zeros((NL, 3, 128, 8, 128), np.float32)
    s5bexp = np.zeros((NL, 2, 128, 8, 128), np.float32)
    s5cexp = np.zeros((NL, 2, 128, 8, 128), np.float32)
    bre, bim, cre, cim = f("s5_b_re"), f("s5_b_im"), f("s5_c_re"), f("s5_c_im")
    for l in range(NL):
        for sc in range(8):
            for half in range(2):
                g = 2 * sc + half
                ps = slice(half * 64, half * 64 + 64)
                s5st[l, ps, sc, 0] = lre[l, g]
                s5st[l, ps, sc, 1] = lim[l, g]
                s5st[l, ps, sc, 2] = ldt[l, g]
                s5rep[l, 0, :, sc, ps] = lre[l, g][None, :]
                s5rep[l, 1, :, sc, ps] = lim[l, g][None, :]
                s5rep[l, 2, :, sc, ps] = ldt[l, g]
                r0 = (sc % 4) * 32 + half * 16
                s5bexp[l, 0, r0:r0 + 16, sc, ps] = bre[l, g].T
                s5bexp[l, 1, r0:r0 + 16, sc, ps] = bim[l, g].T
                s5cexp[l, 0, ps, sc, r0:r0 + 16] = cre[l, g].T
                s5cexp[l, 1, ps, sc, r0:r0 + 16] = cim[l, g].T
    sh["s5st"], sh["s5rep"], sh["s5bexp"], sh["s5cexp"] = s5st, s5rep, s5bexp, s5cexp
    s5db = np.zeros((NL, 128, 4), np.float32)
    for l in range(NL):
        s5db[l, :, 0:2] = f("s5_d")[l].reshape(2, 128).T
        s5db[l, :, 2:4] = f("s5_b_glu")[l].reshape(2, 128).T
    sh["s5db"] = s5db
    return sh


def prep_core(inp, c):
    x = np.asarray(inp["x"], dtype=np.float32)
    p = np.asarray(inp["p"], dtype=np.float32)
    m = {}
    m["xT"] = np.stack([fm(x[2 * c + s], 8) for s in range(2)], 0)
    m["pT"] = np.stack([np.stack([fm(p[l, 2 * c + s], 2) for s in range(2)], 0) for l in range(NL)], 0)
    pos = np.asarray(inp["positions"]).astype(np.int32)
    m["posr"] = np.ascontiguousarray(np.broadcast_to(pos[2 * c:2 * c + 2, None, :], (2, 128, SEQ)))
    return m


def build_nc(dbg=None):
    nc = bass.Bass("TRN2", target_bir_lowering=False)
    with ExitStack() as st:
        k = Kern(nc, st, dbg)
        k.build()
    return nc


def kernel(**inputs):
    sh = prep_shared(inputs)
    nc = build_nc()
    in_maps = []
    for c in range(8):
        m = dict(sh)
        m.update(prep_core(inputs, c))
        in_maps.append(m)
    res = run_bass_kernel_spmd(nc, in_maps, core_ids=list(range(8)))
    out = np.zeros((16, SEQ, 1024), np.float32)
    for c in range(8):
        o = res.results[c]["out"]
        for s in range(2):
            out[2 * c + s] = o[s].transpose(2, 1, 0).reshape(SEQ, 1024)
    return out
```
